# Optimizing a Trainium2 kernel written in Bass

```python
import jax
import jax.numpy as jnp
from jax import lax
import numpy as np

D_MODEL = 1024
BATCH = 8
SEQ = 2048
DEPTH = 1

GRID_W = 64
CTX_LEN = 256

NA_HEADS = 8
NA_HEAD_DIM = 64
NA_WIDTH = NA_HEADS * NA_HEAD_DIM
NA_KH = 8
NA_KW = 16
NA_QB_W = 16
NA_KB_W = NA_QB_W + NA_KW
NA_NCB = GRID_W // NA_QB_W

HG_HEADS = 4
HG_DK = 128
HG_WIDTH = HG_HEADS * HG_DK
HG_CHUNK = 64

MIX_WIDTH = NA_WIDTH + HG_WIDTH
IN_COLS = 3 * NA_WIDTH + 5 * HG_WIDTH

PEER_HEADS = 8
PEER_NKEYS = 128
PEER_EXPERTS = PEER_NKEYS * PEER_NKEYS
PEER_TOPK = 16
PEER_DQ = 256
PEER_BLOCK = 128

N_MOD = 6
EPS = 1e-6

kernel_name = "hybrid_natten_hgrn2_peer_dit_layer"


def rms_norm(x, g):
    xf = x.astype(jnp.float32)
    y = xf * lax.rsqrt(jnp.mean(xf * xf, axis=-1, keepdims=True) + EPS)
    return (y * g.astype(jnp.float32)).astype(x.dtype)


def modulate(h, shift, scale):
    return h * (1 + scale) + shift


def neighbourhood_attention(q, k, v, k_ctx, v_ctx, rpb):
    B, S, H, Dh = q.shape
    rows = S // GRID_W
    kh = min(NA_KH, rows)
    scale = Dh ** -0.5
    qg = q.reshape(B, rows, GRID_W, H, Dh)
    kg = k.reshape(B, rows, GRID_W, H, Dh)
    vg = v.reshape(B, rows, GRID_W, H, Dh)
    qcol = np.arange(GRID_W).reshape(NA_NCB, NA_QB_W)
    kc0 = np.clip(qcol[:, 0] - NA_KW // 2, 0, GRID_W - NA_KB_W)
    kcol = kc0[:, None] + np.arange(NA_KB_W)[None, :]
    cstart = np.clip(qcol - NA_KW // 2, 0, GRID_W - NA_KW)
    col_ok = (kcol[:, None, :] >= cstart[..., None]) & (kcol[:, None, :] < cstart[..., None] + NA_KW)
    dc_idx = np.clip(kcol[:, None, :] - qcol[..., None] + NA_KW - 1, 0, 2 * NA_KW - 2)
    n_win = kh * NA_KB_W

    def row_block(r):
        rs = jnp.clip(r - kh // 2, 0, rows - kh)
        q_r = lax.dynamic_index_in_dim(qg, r, axis=1, keepdims=False).reshape(B, NA_NCB, NA_QB_W, H, Dh)
        k_rows = lax.dynamic_slice_in_dim(kg, rs, kh, axis=1)
        v_rows = lax.dynamic_slice_in_dim(vg, rs, kh, axis=1)
        kb = k_rows[:, :, kcol]
        vb = v_rows[:, :, kcol]
        dr_idx = rs + jnp.arange(kh) - r + NA_KH - 1
        bias = rpb[:, dr_idx[None, None, :, None], dc_idx[:, :, None, :]]
        s_win = jnp.einsum('bcqhd,bkcjhd->bhcqkj', q_r, kb).astype(jnp.float32) * scale
        s_win = jnp.where(col_ok[None, None, :, :, None, :], s_win + bias.astype(jnp.float32)[None], -jnp.inf)
        s_ctx = jnp.einsum('bcqhd,blhd->bhcql', q_r, k_ctx).astype(jnp.float32) * scale
        p = jax.nn.softmax(jnp.concatenate([s_win.reshape(B, H, NA_NCB, NA_QB_W, n_win), s_ctx], axis=-1), axis=-1)
        p = p.astype(v.dtype)
        p_win = p[..., :n_win].reshape(B, H, NA_NCB, NA_QB_W, kh, NA_KB_W)
        o = (jnp.einsum('bhcqkj,bkcjhd->bcqhd', p_win, vb)
             + jnp.einsum('bhcql,blhd->bcqhd', p[..., n_win:], v_ctx))
        return o.reshape(B, GRID_W, H, Dh)

    out = lax.map(row_block, jnp.arange(rows))
    return jnp.moveaxis(out, 0, 1).reshape(B, S, H * Dh)


def context_attention(q, k, v):
    B, L, H, Dh = q.shape
    s = jnp.einsum('blhd,bmhd->bhlm', q, k).astype(jnp.float32) * Dh ** -0.5
    p = jax.nn.softmax(s, axis=-1).astype(v.dtype)
    return jnp.einsum('bhlm,bmhd->blhd', p, v).reshape(B, L, H * Dh)


def _na_heads(t):
    B, T, _ = t.shape
    return t.reshape(B, T, NA_HEADS, NA_HEAD_DIM)


def _hg_heads(t):
    B, T, _ = t.shape
    return t.reshape(B, T, HG_HEADS, HG_DK).transpose(0, 2, 1, 3)


def _forget(z, lb):
    lb = lb.reshape(HG_HEADS, 1, HG_DK)
    f = lb + (1 - lb) * jax.nn.sigmoid(z.astype(jnp.float32))
    return jnp.log(f), 1 - f


def _time_flip(t, rev):
    return jnp.flip(t, axis=2) if rev else t


def gla_chunked(q, k, v, log_f, s0, with_output):
    B, H, T, _ = q.shape
    n_chunks = T // HG_CHUNK

    def chunks(t):
        return t.astype(jnp.float32).reshape(B, H, n_chunks, HG_CHUNK, t.shape[-1]).transpose(2, 0, 1, 3, 4)

    kc, vc, lfc = chunks(k), chunks(v), chunks(log_f)

    def update(S, k_n, v_n, b):
        b_last = b[:, :, -1:, :]
        k_dec = k_n * jnp.exp(b_last - b)
        return jnp.exp(b_last[:, :, 0, :])[..., None] * S + jnp.einsum('bhsd,bhsv->bhdv', k_dec, v_n)

    if not with_output:
        def state_step(S, xs):
            k_n, v_n, lf_n = xs
            return update(S, k_n, v_n, jnp.cumsum(lf_n, axis=2)), None
        s_fin, _ = lax.scan(state_step, s0, (kc, vc, lfc))
        return None, s_fin

    qc = chunks(q)
    causal = jnp.tril(jnp.ones((HG_CHUNK, HG_CHUNK), dtype=bool))

    def step(S, xs):
        q_n, k_n, v_n, lf_n = xs
        b = jnp.cumsum(lf_n, axis=2)
        pair = b[:, :, :, None, :] - b[:, :, None, :, :]
        decay = jnp.exp(jnp.where(causal[:, :, None], pair, -jnp.inf))
        A = jnp.einsum('bhtd,bhsd,bhtsd->bhts', q_n, k_n, decay)
        o = jnp.einsum('bhts,bhsv->bhtv', A, v_n) + jnp.einsum('bhtd,bhdv->bhtv', q_n * jnp.exp(b), S)
        return update(S, k_n, v_n, b), o

    s_fin, o = lax.scan(step, s0, (qc, kc, vc, lfc))
    o = o.transpose(1, 2, 0, 3, 4).reshape(B, H, T, -1)
    return o, s_fin


def _gated_out(o, g, norm_g):
    B, H, T, Dv = o.shape
    o = o * lax.rsqrt(jnp.mean(o * o, axis=-1, keepdims=True) + EPS) * norm_g.astype(jnp.float32).reshape(H, 1, Dv)
    o = o * jax.nn.silu(_hg_heads(g).astype(jnp.float32))
    return o.transpose(0, 2, 1, 3).reshape(B, T, H * Dv).astype(g.dtype)


def hgrn2_mixer(px, pc, lb, norm_g, need_ctx):
    qx, vx = jax.nn.silu(_hg_heads(px[0])), _hg_heads(px[3])
    qc, vc = jax.nn.silu(_hg_heads(pc[0])), _hg_heads(pc[3])
    s0 = jnp.zeros((qx.shape[0], HG_HEADS, HG_DK, HG_DK), jnp.float32)
    outs_x, outs_c = [], []
    for d in range(2):
        rev = d == 1
        lf_c, k_c = _forget(_hg_heads(pc[1 + d]), lb[d])
        lf_x, k_x = _forget(_hg_heads(px[1 + d]), lb[d])
        o_c, s_c = gla_chunked(_time_flip(qc, rev), _time_flip(k_c, rev), _time_flip(vc, rev),
                               _time_flip(lf_c, rev), s0, need_ctx)
        o_x, _ = gla_chunked(_time_flip(qx, rev), _time_flip(k_x, rev), _time_flip(vx, rev),
                             _time_flip(lf_x, rev), s_c, True)
        outs_x.append(_time_flip(o_x, rev))
        if need_ctx:
            outs_c.append(_time_flip(o_c, rev))
    y_x = _gated_out(outs_x[0] + outs_x[1], px[4], norm_g)
    y_c = _gated_out(outs_c[0] + outs_c[1], pc[4], norm_g) if need_ctx else None
    return y_x, y_c


def peer_ffn(h, wq, sub_keys, u, v):
    B, T, D = h.shape
    tok = h.reshape(-1, PEER_BLOCK, D)

    def block(t):
        q = (t @ wq).reshape(PEER_BLOCK, PEER_HEADS, 2, PEER_DQ // 2)
        s = jnp.einsum('tpcd,pckd->tpck', q, sub_keys).astype(jnp.float32)
        s1, i1 = lax.top_k(s[:, :, 0], PEER_TOPK)
        s2, i2 = lax.top_k(s[:, :, 1], PEER_TOPK)
        cand = (s1[..., :, None] + s2[..., None, :]).reshape(PEER_BLOCK, PEER_HEADS, PEER_TOPK * PEER_TOPK)
        cidx = (i1[..., :, None] * PEER_NKEYS + i2[..., None, :]).reshape(PEER_BLOCK, PEER_HEADS, PEER_TOPK * PEER_TOPK)
        top, pos = lax.top_k(cand, PEER_TOPK)
        idx = jnp.take_along_axis(cidx, pos, axis=-1)
        g = jax.nn.softmax(top, axis=-1)
        a = jax.nn.gelu(jnp.einsum('tpkd,td->tpk', u[idx], t).astype(jnp.float32), approximate=False)
        return jnp.einsum('tpk,tpkd->td', (g * a).astype(t.dtype), v[idx])

    return lax.map(block, tok).reshape(B, T, D)


def setup_inputs(seed: int = 0) -> dict:
    key = jax.random.key(seed)
    ks = jax.random.split(key, 18)

    def nrm(k, shape, scale):
        return jax.random.normal(k, shape, jnp.float32) * scale

    return {
        'x': nrm(ks[0], (BATCH, SEQ, D_MODEL), 1.0),
        'c': nrm(ks[1], (BATCH, D_MODEL), 1.0),
        'ctx': nrm(ks[2], (BATCH, CTX_LEN, D_MODEL), 1.0),
        'c_ctx': nrm(ks[3], (D_MODEL,), 1.0),
        'w_mod': nrm(ks[4], (DEPTH, D_MODEL, N_MOD * D_MODEL), 0.5 * D_MODEL ** -0.5),
        'b_mod': nrm(ks[5], (DEPTH, N_MOD * D_MODEL), 0.01),
        'norm1': 1.0 + nrm(ks[6], (DEPTH, D_MODEL), 0.05),
        'norm2': 1.0 + nrm(ks[7], (DEPTH, D_MODEL), 0.05),
        'w_in': nrm(ks[8], (DEPTH, D_MODEL, IN_COLS), D_MODEL ** -0.5),
        'w_out': nrm(ks[9], (DEPTH, MIX_WIDTH, D_MODEL), MIX_WIDTH ** -0.5),
        'na_rpb': nrm(ks[10], (DEPTH, NA_HEADS, 2 * NA_KH - 1, 2 * NA_KW - 1), 0.2),
        'hg_lb': nrm(ks[11], (DEPTH + 1, 2, HG_WIDTH), 1.0),
        'hg_norm': 1.0 + nrm(ks[12], (DEPTH, HG_WIDTH), 0.05),
        'peer_wq': nrm(ks[13], (DEPTH, D_MODEL, PEER_HEADS * PEER_DQ), D_MODEL ** -0.5),
        'peer_keys': nrm(ks[14], (DEPTH, PEER_HEADS, 2, PEER_NKEYS, PEER_DQ // 2), (PEER_DQ // 2) ** -0.5),
        'peer_u': nrm(ks[15], (DEPTH, PEER_EXPERTS, D_MODEL), D_MODEL ** -0.5),
        'peer_v': nrm(ks[16], (DEPTH, PEER_EXPERTS, D_MODEL), PEER_HEADS ** -0.5),
        'norm_f': 1.0 + nrm(ks[17], (D_MODEL,), 0.05),
    }


def reference(x, c, ctx, c_ctx, w_mod, b_mod, norm1, norm2, w_in, w_out, na_rpb, hg_lb, hg_norm,
              peer_wq, peer_keys, peer_u, peer_v, norm_f):
    lb_all = jnp.cumsum(jax.nn.softmax(hg_lb.astype(jnp.float32), axis=0), axis=0)
    splits = [NA_WIDTH, 2 * NA_WIDTH, 3 * NA_WIDTH] + [3 * NA_WIDTH + j * HG_WIDTH for j in range(1, 5)]
    for l in range(DEPTH):
        need_ctx = l < DEPTH - 1
        mod_x = (jax.nn.silu(c) @ w_mod[l] + b_mod[l])[:, None, :]
        mod_c = jax.nn.silu(c_ctx) @ w_mod[l] + b_mod[l]
        sh1, sc1, gt1, sh2, sc2, gt2 = jnp.split(mod_x, N_MOD, axis=-1)
        csh1, csc1, cgt1, csh2, csc2, cgt2 = jnp.split(mod_c, N_MOD, axis=-1)

        px = jnp.split(modulate(rms_norm(x, norm1[l]), sh1, sc1) @ w_in[l], splits, axis=-1)
        pc = jnp.split(modulate(rms_norm(ctx, norm1[l]), csh1, csc1) @ w_in[l], splits, axis=-1)
        k_ctx, v_ctx = _na_heads(pc[1]), _na_heads(pc[2])
        na_x = neighbourhood_attention(_na_heads(px[0]), _na_heads(px[1]), _na_heads(px[2]),
                                       k_ctx, v_ctx, na_rpb[l])
        hg_x, hg_c = hgrn2_mixer(px[3:], pc[3:], lb_all[l], hg_norm[l], need_ctx)
        x = x + gt1 * (jnp.concatenate([na_x, hg_x], axis=-1) @ w_out[l])
        if need_ctx:
            na_c = context_attention(_na_heads(pc[0]), k_ctx, v_ctx)
            ctx = ctx + cgt1 * (jnp.concatenate([na_c, hg_c], axis=-1) @ w_out[l])
            ctx = ctx + cgt2 * peer_ffn(modulate(rms_norm(ctx, norm2[l]), csh2, csc2),
                                        peer_wq[l], peer_keys[l], peer_u[l], peer_v[l])

        x = x + gt2 * peer_ffn(modulate(rms_norm(x, norm2[l]), sh2, sc2),
                               peer_wq[l], peer_keys[l], peer_u[l], peer_v[l])
    return rms_norm(x, norm_f)
```

```python
import contextlib
import numpy as np
import concourse.bass as bass
import concourse.mybir as mybir
from concourse.bass_utils import run_bass_kernel_spmd

F32 = mybir.dt.float32
BF16 = mybir.dt.bfloat16
I32 = mybir.dt.int32
U32 = mybir.dt.uint32
ALU = mybir.AluOpType
AF = mybir.ActivationFunctionType
AX = mybir.AxisListType

D = 1024
SEQ = 2048
CTX = 256
NT = 16
NTT = 18
TOK = SEQ + CTX
NCH = TOK // 64
EPS = 1e-6
MASKV = -30000.0
NEG = -1.0e30


class Sched:
    COMPUTE = ("pe", "dve", "act", "pool")

    def __init__(self, nc, n_dsem=None):
        self.nc = nc
        self.engs = {"pe": nc.tensor, "dve": nc.vector, "act": nc.scalar,
                     "pool": nc.gpsimd, "sp": nc.sync}
        self.n_dsem = n_dsem or {"sp": 8, "act": 4, "pool": 16}
        self.es = contextlib.ExitStack()
        self.csem = {e: self.es.enter_context(nc.semaphore("cs_" + e)) for e in self.COMPUTE}
        self.dsem = {q: [self.es.enter_context(nc.semaphore(f"ds_{q}{j}")) for j in range(n)]
                     for q, n in self.n_dsem.items()}
        self.ccount = {e: 0 for e in self.COMPUTE}
        self.dcount = {q: 0 for q in self.n_dsem}
        self.clock = {e: {} for e in self.engs}
        self.bar_sig = 0
        self.bar_clock = {}
        self.bar_tile = None
        self.ops = []
        self.last_writer = {}
        self.readers = {}
        self.total_ops = 0

    def op(self, eng, fn, reads=(), writes=(), dma=False):
        deps = set()
        for r in reads:
            w = self.last_writer.get(r)
            if w is not None:
                deps.add(w)
        for w_ in writes:
            w = self.last_writer.get(w_)
            if w is not None:
                deps.add(w)
            for rd in self.readers.get(w_, ()):
                deps.add(rd)
        i = len(self.ops)
        deps.discard(i)
        self.ops.append(dict(eng=eng, fn=fn, deps=deps, dma=dma))
        for r in reads:
            self.readers.setdefault(r, []).append(i)
        for w_ in writes:
            self.last_writer[w_] = i
            self.readers[w_] = []
        return i

    def dma(self, q, fn, reads=(), writes=()):
        return self.op(q, fn, reads, writes, dma=True)

    def _wait(self, E, key, sem, val):
        ck = self.clock[E]
        if ck.get(key, 0) < val:
            self.engs[E].wait_ge(sem, val)
            ck[key] = val

    def _merge(self, E, clk):
        ck = self.clock[E]
        for k, v in clk.items():
            if ck.get(k, 0) < v:
                ck[k] = v

    def flush(self, barrier=True):
        ops = self.ops
        need_sig = [False] * len(ops)
        for i, o in enumerate(ops):
            for d in o["deps"]:
                od = ops[d]
                if od["dma"]:
                    continue
                if od["eng"] == "pe" and o["eng"] == "pe" and not o["dma"]:
                    continue
                need_sig[d] = True
        if barrier:
            last = {}
            for i, o in enumerate(ops):
                if not o["dma"]:
                    last[o["eng"]] = i
            for e, i in last.items():
                need_sig[i] = True
        for i, o in enumerate(ops):
            E = o["eng"]
            eng = self.engs[E]
            if self.bar_sig:
                self._wait(E, ("c", "dve"), self.csem["dve"], self.bar_sig)
                self._merge(E, self.bar_clock)
            for d in sorted(o["deps"]):
                od = ops[d]
                if od["dma"]:
                    q = od["eng"]
                    self._wait(E, ("d", q, od["dsem_idx"]), self.dsem[q][od["dsem_idx"]], od["dval"])
                else:
                    F = od["eng"]
                    if F == "pe" and E == "pe" and not o["dma"]:
                        continue
                    self._wait(E, ("c", F), self.csem[F], od["sig"])
                self._merge(E, od["clk"])
            if o["dma"]:
                n = self.n_dsem[E]
                j = self.dcount[E] % n
                prev = self.dcount[E] // n
                if prev > 0:
                    self._wait(E, ("d", E, j), self.dsem[E][j], 16 * prev)
                ins = o["fn"](eng)
                ins.then_inc(self.dsem[E][j], 16)
                o["dsem_idx"] = j
                o["dval"] = 16 * (prev + 1)
                self.dcount[E] += 1
                o["clk"] = dict(self.clock[E])
            else:
                ins = o["fn"](eng)
                if need_sig[i]:
                    self.ccount[E] += 1
                    ins.then_inc(self.csem[E], 1)
                    o["sig"] = self.ccount[E]
                else:
                    o["sig"] = None
                o["clk"] = dict(self.clock[E])
            o["fn"] = None
        self.total_ops += len(ops)
        if barrier:
            self._barrier()
        self.ops = []
        self.last_writer = {}
        self.readers = {}

    def _wait_all(self, E):
        for q, n in self.n_dsem.items():
            for j in range(n):
                uses = (self.dcount[q] - j + n - 1) // n if self.dcount[q] > j else 0
                if uses > 0:
                    self._wait(E, ("d", q, j), self.dsem[q][j], 16 * uses)
        for e in self.COMPUTE:
            if self.ccount[e] > 0:
                self._wait(E, ("c", e), self.csem[e], self.ccount[e])

    def _barrier(self):
        self._wait_all("dve")
        ins = self.engs["dve"].memset(self.bar_tile, 0.0)
        self.ccount["dve"] += 1
        ins.then_inc(self.csem["dve"], 1)
        self.bar_sig = self.ccount["dve"]
        self.bar_clock = dict(self.clock["dve"])

    def finish(self, eng="sp"):
        self.flush(barrier=True)
        self._wait_all(eng)
        self.es.close()


NA_VARIANTS = [(-2, True), (-1, False), (0, False), (1, False), (2, True),
               (-3, False), (-2, False), (2, False), (3, False)]


def na_chunks(i):
    if i in (0, 1, 14, 15):
        cs = range(0, 4) if i < 2 else range(12, 16)
        out = []
        for c in cs:
            d = c - i
            t = {(-3): 5, (-2): 6, (-1): 1, 0: 2, 1: 3, 2: 7, 3: 8}[d]
            out.append((c, t))
        return out
    return [(i + d, d + 2) for d in range(-2, 3)]


def build_bias_index():
    idx = np.full((128, 9, 128), 15 * 31, dtype=np.int64)
    for t, (d, partial) in enumerate(NA_VARIANTS):
        for j in range(2):
            for jq in range(2):
                dr = 2 * d + j - jq
                if abs(dr) > 7:
                    continue
                if partial:
                    if d == -2 and not (j >= jq):
                        continue
                    if d == 2 and not (j == 0 and jq == 1):
                        continue
                for cq in range(64):
                    cstart = min(max(cq - 8, 0), 48)
                    for ck in range(cstart, cstart + 16):
                        idx[j * 64 + ck, t, jq * 64 + cq] = (dr + 7) * 31 + (ck - cq + 15)
    return idx


_BIAS_IDX = None


class _Stop(Exception):
    pass


def build(debug=False, stop=None):
    nc = bass.Bass("TRN2", target_bir_lowering=False)
    try:
        return _build(nc, debug, stop)
    except _Stop:
        return nc


def _build(nc, debug, stop):

    def din(name, shape, dt=F32):
        return nc.dram_tensor(name, shape, dt, kind="ExternalInput").ap()

    x = din("x", [SEQ, D])
    ctx = din("ctx", [CTX, D])
    ccol = din("ccol", [128, 16])
    w_mod = din("w_mod", [D, 6 * D])
    b_mod = din("b_mod", [1, 6 * D])
    n1c = din("n1c", [128, 8])
    n2c = din("n2c", [128, 8])
    n2b = din("n2b", [128, D])
    nfb = din("nfb", [128, D])
    w_in = din("w_in", [D, 4096])
    w_out = din("w_out", [D, D])
    wq = din("wq", [D, 2048])
    keysT = din("keysT", [128, 2048])
    u_t = din("u", [16384, D])
    v_t = din("v", [16384, D])
    lbraw = din("lbraw", [128, 16])
    hgn = din("hgn", [128, 512])
    biasT = din("biasT", [128, 8 * 9 * 128])
    cst = din("cst", [128, 528])
    rmask_d = din("rmask", [128, TOK])
    out = nc.dram_tensor("out", [SEQ, D], F32, kind="ExternalOutput").ap()
    scr_bc = nc.dram_tensor("scr_bc", [4, 128, D], F32, kind="Internal").ap()
    uv_bf = nc.dram_tensor("uv_bf", [16384, 2 * D], BF16, kind="Internal").ap()
    dbg = {}
    if debug:
        dbg["hT"] = nc.dram_tensor("d_hT", [128, 8 * TOK], BF16, kind="ExternalOutput").ap()
        dbg["mixT"] = nc.dram_tensor("d_mixT", [128, 8 * SEQ], BF16, kind="ExternalOutput").ap()
        dbg["x1"] = nc.dram_tensor("d_x1", [128, NT * D], F32, kind="ExternalOutput").ap()
        dbg["mc"] = nc.dram_tensor("d_mc", [128, 48], F32, kind="ExternalOutput").ap()
        dbg["eidx"] = nc.dram_tensor("d_eidx", [128, NT * 128], I32, kind="ExternalOutput").ap()
        dbg["gw"] = nc.dram_tensor("d_gw", [128, NT * 128], F32, kind="ExternalOutput").ap()

    es = contextlib.ExitStack()

    def sb(name, shape, dt=F32, stack=None):
        return (stack or es).enter_context(nc.sbuf_tensor(name, shape, dt))

    bar = sb("bar", [128, 1])
    cA = sb("cA", [128, 528])
    identb = sb("identb", [128, 128], BF16)
    mc = sb("mc", [128, 48])
    lb = sb("lb", [128, 8])
    oml = sb("oml", [128, 8])
    ps = [es.enter_context(nc.psum_tensor(f"ps{j}", [128, 512], F32)) for j in range(6)]
    pT = [es.enter_context(nc.psum_tensor(f"pT{j}", [128, 1024], BF16)) for j in range(2)]

    S = Sched(nc)
    S.bar_tile = bar[:]

    def phase_end(name):
        S.flush()
        if stop == name:
            S.finish()
            raise _Stop()

    IDENT = cA[:, 0:128]
    TRIF = cA[:, 128:256]
    TRIB = cA[:, 256:384]
    ONES = cA[:, 384:512]
    IOTA16 = cA[:, 512:528]

    with contextlib.ExitStack() as p0:
        cc = sb("cc", [128, 16], stack=p0)
        scl = sb("scl", [128, 8, 33], stack=p0)
        wm = [sb(f"wm{j}", [128, 8, 512], stack=p0) for j in range(2)]
        bm = sb("bm", [33, 6 * D], stack=p0)
        modrow = sb("modrow", [33, 6 * D], stack=p0)
        mcol = sb("mcol", [128, 48], stack=p0)
        n1 = sb("n1", [128, 8], stack=p0)
        n2 = sb("n2", [128, 8], stack=p0)
        n2bt = sb("n2bt", [128, D], stack=p0)
        bct = [sb(f"bct{j}", [128, D], stack=p0) for j in range(2)]
        lbr = sb("lbr", [128, 16], stack=p0)

        S.dma("sp", lambda e: e.dma_start(out=cA[:], in_=cst), writes=["cA"])
        S.dma("sp", lambda e: e.dma_start(out=cc[:], in_=ccol), writes=["cc"])
        S.dma("sp", lambda e: e.dma_start(out=n1[:], in_=n1c), writes=["n1"])
        S.dma("sp", lambda e: e.dma_start(out=n2[:], in_=n2c), writes=["n2"])
        S.dma("sp", lambda e: e.dma_start(out=lbr[:], in_=lbraw), writes=["lbr"])
        S.dma("sp", lambda e: e.dma_start(out=n2bt[:], in_=n2b), writes=["n2bt"])
        S.op("dve", lambda e: e.memset(bm[:], 0.0), writes=["bm"])
        S.dma("sp", lambda e: e.dma_start(out=bm[0:1, :], in_=b_mod), reads=["bm"], writes=["bm0"])
        S.dma("sp", lambda e: e.dma_start(out=bm[32:33, :], in_=b_mod), reads=["bm"], writes=["bm32"])
        S.op("dve", lambda e: e.tensor_copy(out=identb[:], in_=IDENT), reads=["cA"], writes=["identb"])
        S.op("dve", lambda e: e.memset(scl[:], 0.0), writes=["scl"])
        ccv = cc[:].rearrange("p (k t) -> p k t", t=2)
        S.op("act", lambda e: e.activation(out=scl[:, :, 0:1], in_=ccv[:, :, 0:1], func=AF.Silu),
             reads=["cc", "scl"], writes=["scl"])
        S.op("act", lambda e: e.activation(out=scl[:, :, 32:33], in_=ccv[:, :, 1:2], func=AF.Silu),
             reads=["cc", "scl"], writes=["scl"])
        S.op("dve", lambda e: e.tensor_tensor(out=lb[:], in0=lbr[:, 0:8], in1=lbr[:, 8:16], op=ALU.subtract),
             reads=["lbr"], writes=["lb"])
        S.op("act", lambda e: e.activation(out=lb[:], in_=lb[:], func=AF.Sigmoid), reads=["lb"], writes=["lb"])
        S.op("dve", lambda e: e.tensor_scalar(out=oml[:], in0=lb[:], scalar1=-1.0, scalar2=1.0,
                                              op0=ALU.mult, op1=ALU.add), reads=["lb"], writes=["oml"])
        for n in range(12):
            wb = wm[n % 2]
            S.dma("sp" if n % 2 == 0 else "act",
                  lambda e, n=n, wb=wb: e.dma_start(
                      out=wb[:], in_=w_mod[:, n * 512:(n + 1) * 512].rearrange("(k p) n -> p k n", p=128)),
                  writes=[("wm", n % 2)])
            pb = ps[n % 2]
            for k in range(8):
                S.op("pe", lambda e, k=k, wb=wb, pb=pb: e.matmul(pb[0:33, :], scl[:, k, :], wb[:, k, :],
                                                                start=(k == 0), stop=(k == 7)),
                     reads=["scl", ("wm", n % 2)], writes=[("ps", n % 2)])
            S.op("dve", lambda e, n=n, pb=pb: e.tensor_tensor(out=modrow[:, n * 512:(n + 1) * 512], in0=pb[0:33, :],
                                                             in1=bm[:, n * 512:(n + 1) * 512], op=ALU.add),
                 reads=[("ps", n % 2), "bm", "bm0", "bm32"], writes=[("modrow", n)])
        mr_all = [("modrow", n) for n in range(12)]
        col_specs = [(0, 0), (0, 1), (0, 3), (0, 4), (32, 0), (32, 1)]
        for si, (r, vi) in enumerate(col_specs):
            for k in range(8):
                c0 = 2 * (si * 8 + k)
                S.op("pe", lambda e, r=r, vi=vi, k=k, c0=c0: e.matmul(
                    ps[2][:, c0:c0 + 2], modrow[r:r + 1, vi * D + k * 128: vi * D + (k + 1) * 128],
                    cA[r:r + 1, 384:386], start=True, stop=True),
                    reads=mr_all + ["cA"], writes=[("ps", 2)])
        S.op("dve", lambda e: e.tensor_copy(out=mcol[:].unsqueeze(2), in_=ps[2][:, 0:96].rearrange("p (c two) -> p c two", two=2)[:, :, 0:1]), reads=[("ps", 2)], writes=["mcol"])
        S.op("dve", lambda e: e.scalar_tensor_tensor(out=mc[:, 0:8], in0=mcol[:, 8:16], scalar=1.0, in1=n1[:],
                                                     op0=ALU.add, op1=ALU.mult), reads=["mcol", "n1"], writes=["mc0"])
        S.op("dve", lambda e: e.tensor_copy(out=mc[:, 8:16], in_=mcol[:, 0:8]), reads=["mcol"], writes=["mc1"])
        S.op("dve", lambda e: e.scalar_tensor_tensor(out=mc[:, 16:24], in0=mcol[:, 40:48], scalar=1.0, in1=n1[:],
                                                     op0=ALU.add, op1=ALU.mult), reads=["mcol", "n1"], writes=["mc2"])
        S.op("dve", lambda e: e.tensor_copy(out=mc[:, 24:32], in_=mcol[:, 32:40]), reads=["mcol"], writes=["mc3"])
        S.op("dve", lambda e: e.scalar_tensor_tensor(out=mc[:, 32:40], in0=mcol[:, 24:32], scalar=1.0, in1=n2[:],
                                                     op0=ALU.add, op1=ALU.mult), reads=["mcol", "n2"], writes=["mc4"])
        S.op("dve", lambda e: e.tensor_copy(out=mc[:, 40:48], in_=mcol[:, 16:24]), reads=["mcol"], writes=["mc5"])
        for j, (vi, kind) in enumerate([(2, "copy"), (5, "copy"), (4, "g2"), (3, "copy")]):
            bt_ = bct[j % 2]
            for hf in range(2):
                pb = ps[3 + hf]
                S.op("pe", lambda e, vi=vi, hf=hf, pb=pb: e.matmul(
                    pb[:, :], cA[0:1, 384:512], modrow[0:1, vi * D + hf * 512: vi * D + (hf + 1) * 512],
                    start=True, stop=True), reads=mr_all + ["cA"], writes=[("ps", 3 + hf)])
                if kind == "copy":
                    S.op("dve", lambda e, bt_=bt_, hf=hf, pb=pb: e.tensor_copy(out=bt_[:, hf * 512:(hf + 1) * 512], in_=pb[:, :]),
                         reads=[("ps", 3 + hf)], writes=[("bct", j % 2, hf)])
                else:
                    S.op("dve", lambda e, bt_=bt_, hf=hf, pb=pb: e.scalar_tensor_tensor(
                        out=bt_[:, hf * 512:(hf + 1) * 512], in0=pb[:, :], scalar=1.0,
                        in1=n2bt[:, hf * 512:(hf + 1) * 512], op0=ALU.add, op1=ALU.mult),
                        reads=[("ps", 3 + hf), "n2bt"], writes=[("bct", j % 2, hf)])
            S.dma("sp", lambda e, j=j, bt_=bt_: e.dma_start(out=scr_bc[j], in_=bt_[:]),
                  reads=[("bct", j % 2, 0), ("bct", j % 2, 1)], writes=[("scr", j)])
        if debug:
            S.dma("sp", lambda e: e.dma_start(out=dbg["mc"], in_=mc[:]), reads=[f"mc{j}" for j in range(6)])
        phase_end("p0")

    R = sb("R", [128, NT * D])
    x1 = R[:].rearrange("p (t d) -> p t d", d=D)
    hT = R[:, 0:9216].bitcast(BF16).rearrange("p (k t) -> p k t", k=8)
    vtok = R[:, 9216:13824].bitcast(BF16).rearrange("p (t d) -> p t d", d=512)
    with contextlib.ExitStack() as pm:
        mixT = sb("mixT", [128, 8, SEQ], BF16, stack=pm)

        with contextlib.ExitStack() as p1:
            xt = [sb(f"xt{j}", [128, D], stack=p1) for j in range(2)]
            xs = [sb(f"xs{j}", [128, D], BF16, stack=p1) for j in range(2)]
            junk = sb("junk1", [128, D], BF16, stack=p1)
            st = sb("st1", [128, 3 * NTT], stack=p1)
            for T in range(NTT):
                b = T % 2
                src = x[T * 128:(T + 1) * 128, :] if T < NT else ctx[(T - NT) * 128:(T - NT + 1) * 128, :]
                S.dma("sp" if b == 0 else "act", lambda e, b=b, src=src: e.dma_start(out=xt[b][:], in_=src),
                      writes=[("xt", b)])
                S.op("act", lambda e, b=b, T=T: e.activation(out=junk[:], in_=xt[b][:], func=AF.Square,
                                                             accum_out=st[:, T:T + 1]),
                     reads=[("xt", b)], writes=["junk", ("ssq", T)])
                S.op("dve", lambda e, T=T: e.tensor_scalar(out=st[:, NTT + T:NTT + T + 1], in0=st[:, T:T + 1],
                                                           scalar1=1.0 / D, scalar2=EPS, op0=ALU.mult, op1=ALU.add),
                     reads=[("ssq", T)], writes=[("ms", T)])
                S.op("act", lambda e, T=T: e.activation(out=st[:, NTT + T:NTT + T + 1], in_=st[:, NTT + T:NTT + T + 1],
                                                        func=AF.Sqrt), reads=[("ms", T)], writes=[("ms", T)])
                S.op("dve", lambda e, T=T: e.reciprocal(out=st[:, 2 * NTT + T:2 * NTT + T + 1],
                                                        in_=st[:, NTT + T:NTT + T + 1]),
                     reads=[("ms", T)], writes=[("rstd", T)])
                S.op("act", lambda e, b=b, T=T: e.activation(out=xs[b][:], in_=xt[b][:], func=AF.Copy,
                                                             scale=st[:, 2 * NTT + T:2 * NTT + T + 1]),
                     reads=[("xt", b), ("rstd", T)], writes=[("xs", b)])
                for k in range(8):
                    S.op("pe", lambda e, b=b, k=k: e.transpose(pT[b][:, k * 128:(k + 1) * 128],
                                                               xs[b][:, k * 128:(k + 1) * 128], identb[:]),
                         reads=[("xs", b), "identb"], writes=[("pT", b)])
                go, so = (0, 8) if T < NT else (16, 24)
                for k in range(8):
                    S.op("dve", lambda e, b=b, k=k, T=T, go=go, so=so: e.tensor_scalar(
                        out=hT[:, k, T * 128:(T + 1) * 128], in0=pT[b][:, k * 128:(k + 1) * 128],
                        scalar1=mc[:, go + k:go + k + 1], scalar2=mc[:, so + k:so + k + 1],
                        op0=ALU.mult, op1=ALU.add),
                        reads=[("pT", b)], writes=[("hT", T)])
            if debug:
                S.dma("sp", lambda e: e.dma_start(out=dbg["hT"], in_=R[:, 0:9216].bitcast(BF16)),
                      reads=[("hT", T) for T in range(NTT)])
            phase_end("p1")
        hT_all = [("hT", T) for T in range(NTT)]

        def load_w(tile_ap, dram_w, col0, ncols, key):
            for k0 in range(0, 8, 4):
                S.dma("pool", lambda e, k0=k0: e.dma_start(
                    out=tile_ap[:, k0:k0 + 4, :],
                    in_=dram_w[k0 * 128:(k0 + 4) * 128, col0:col0 + ncols].rearrange("(k p) n -> p k n", p=128)),
                    writes=[(key, k0)])
            return [(key, 0), (key, 4)]

        with contextlib.ExitStack() as p2:
            qT = sb("qT", [128, 4, SEQ], BF16, stack=p2)
            kT = sb("kT", [128, 4, TOK], BF16, stack=p2)
            vaug = sb("vaug", [128, NTT, 8, 65], BF16, stack=p2)
            p2a = contextlib.ExitStack()
            wna = sb("wna", [128, 8, 1536], BF16, stack=p2a)
            wk = load_w(wna, w_in, 0, 1536, "wna")
            S.op("pool", lambda e: e.memset(vaug[:, :, :, 64:65], 1.0), writes=["vones"])
            cnt = 0
            for which, dst, ntok, cbase in (("q", qT, SEQ, 0), ("k", kT, TOK, 512)):
                for hp in range(4):
                    for t0 in range(0, ntok, 512):
                        tw = min(512, ntok - t0)
                        pb = cnt % 4
                        for k in range(8):
                            S.op("pe", lambda e, k=k, hp=hp, t0=t0, tw=tw, pb=pb, cbase=cbase: e.matmul(
                                ps[pb][:, 0:tw], wna[:, k, cbase + hp * 128: cbase + (hp + 1) * 128],
                                hT[:, k, t0:t0 + tw], start=(k == 0), stop=(k == 7)),
                                reads=wk + hT_all, writes=[("ps", pb)])
                        eng = "act" if cnt % 2 == 0 else "dve"
                        if eng == "act":
                            S.op("act", lambda e, dst=dst, hp=hp, t0=t0, tw=tw, pb=pb: e.activation(
                                out=dst[:, hp, t0:t0 + tw], in_=ps[pb][:, 0:tw], func=AF.Copy),
                                reads=[("ps", pb)], writes=[(which, hp, t0)])
                        else:
                            S.op("dve", lambda e, dst=dst, hp=hp, t0=t0, tw=tw, pb=pb: e.tensor_copy(
                                out=dst[:, hp, t0:t0 + tw], in_=ps[pb][:, 0:tw]),
                                reads=[("ps", pb)], writes=[(which, hp, t0)])
                        cnt += 1
            for T in range(NTT):
                pb = cnt % 4
                for k in range(8):
                    S.op("pe", lambda e, k=k, T=T, pb=pb: e.matmul(
                        ps[pb][:, :], hT[:, k, T * 128:(T + 1) * 128], wna[:, k, 1024:1536],
                        start=(k == 0), stop=(k == 7)), reads=wk + hT_all, writes=[("ps", pb)])
                if cnt % 2 == 0:
                    S.op("act", lambda e, T=T, pb=pb: e.activation(
                        out=vaug[:, T, :, 0:64], in_=ps[pb][:, :].rearrange("p (h d) -> p h d", d=64), func=AF.Copy),
                        reads=[("ps", pb)], writes=[("v", T)])
                else:
                    S.op("dve", lambda e, T=T, pb=pb: e.tensor_copy(
                        out=vaug[:, T, :, 0:64], in_=ps[pb][:, :].rearrange("p (h d) -> p h d", d=64)),
                        reads=[("ps", pb)], writes=[("v", T)])
                cnt += 1
            phase_end("p2a")
            p2a.close()
            bt = sb("bt", [128, 8, 9, 128], stack=p2)
            Ssb = [sb(f"Ssb{j}", [128, 640], stack=p2) for j in range(2)]
            Pb = [sb(f"Pb{j}", [128, 896], BF16, stack=p2) for j in range(2)]
            rden = sb("rden", [128, 16], stack=p2)
            natok = [sb(f"natok{j}", [128, 512], BF16, stack=p2) for j in range(2)]
            S.dma("sp", lambda e: e.dma_start(out=bt[:].rearrange("p h t q -> p (h t q)"), in_=biasT), writes=["bt"])

            qk_all_r = []
            it = 0
            for i in range(NT):
                chunks = na_chunks(i)
                nw = len(chunks)
                nb = i % 2
                for h in range(8):
                    hp, po = h // 2, (h % 2) * 64
                    sbuf_i = it % 2
                    b0, b1 = ps[2 * sbuf_i], ps[2 * sbuf_i + 1]

                    def sloc(j):
                        return (b0, j * 128) if j < 4 else (b1, (j - 4) * 128)
                    for j, (c, t) in enumerate(chunks):
                        bk, co = sloc(j)
                        S.op("pe", lambda e, bk=bk, co=co, c=c, hp=hp, po=po, i=i: e.matmul(
                            bk[:, co:co + 128], kT[po:po + 64, hp, c * 128:(c + 1) * 128],
                            qT[po:po + 64, hp, i * 128:(i + 1) * 128], start=True, stop=True),
                            reads=[], writes=[("psS", sbuf_i, j // 4)])
                    for cc_ in range(2):
                        S.op("pe", lambda e, cc_=cc_, hp=hp, po=po, i=i, b1=b1: e.matmul(
                            b1[:, 128 + cc_ * 128: 256 + cc_ * 128],
                            kT[po:po + 64, hp, SEQ + cc_ * 128: SEQ + (cc_ + 1) * 128],
                            qT[po:po + 64, hp, i * 128:(i + 1) * 128], start=True, stop=True),
                            reads=[], writes=[("psS", sbuf_i, 1)])
                    for j, (c, t) in enumerate(chunks):
                        bk, co = sloc(j)
                        S.op("dve", lambda e, bk=bk, co=co, j=j, t=t, h=h, sbuf_i=sbuf_i: e.scalar_tensor_tensor(
                            out=Ssb[sbuf_i][:, j * 128:(j + 1) * 128], in0=bk[:, co:co + 128], scalar=0.125,
                            in1=bt[:, h, t, :], op0=ALU.mult, op1=ALU.add),
                            reads=[("psS", sbuf_i, j // 4), "bt"], writes=[("Ssb", sbuf_i)])
                    S.op("act", lambda e, nw=nw, sbuf_i=sbuf_i: e.activation(
                        out=Pb[sbuf_i][:, 0:nw * 128], in_=Ssb[sbuf_i][:, 0:nw * 128], func=AF.Exp),
                        reads=[("Ssb", sbuf_i)], writes=[("Pw", sbuf_i)])
                    S.op("act", lambda e, sbuf_i=sbuf_i, b1=b1: e.activation(
                        out=Pb[sbuf_i][:, 640:896], in_=b1[:, 128:384], func=AF.Exp, scale=0.125),
                        reads=[("psS", sbuf_i, 1)], writes=[("Pc", sbuf_i)])
                    ob = ps[4 + h // 4]
                    oc = (h % 4) * 128
                    nmm = nw + 2
                    for j, (c, t) in enumerate(chunks):
                        S.op("pe", lambda e, j=j, c=c, h=h, ob=ob, oc=oc, sbuf_i=sbuf_i, nmm=nmm: e.matmul(
                            ob[:, oc:oc + 65], Pb[sbuf_i][:, j * 128:(j + 1) * 128], vaug[:, c, h, :],
                            start=(j == 0), stop=False),
                            reads=[("Pw", sbuf_i)], writes=[("psO", h)])
                    for cc_ in range(2):
                        S.op("pe", lambda e, cc_=cc_, h=h, ob=ob, oc=oc, sbuf_i=sbuf_i: e.matmul(
                            ob[:, oc:oc + 65], Pb[sbuf_i][:, 640 + cc_ * 128: 768 + cc_ * 128], vaug[:, NT + cc_, h, :],
                            start=False, stop=(cc_ == 1)),
                            reads=[("Pc", sbuf_i)], writes=[("psO", h)])
                    S.op("dve", lambda e, h=h, ob=ob, oc=oc: e.reciprocal(out=rden[:, h:h + 1], in_=ob[:, oc + 64:oc + 65]),
                         reads=[("psO", h)], writes=[("rden", h)])
                    S.op("dve", lambda e, h=h, ob=ob, oc=oc, nb=nb: e.tensor_scalar(
                        out=natok[nb][:, h * 64:(h + 1) * 64], in0=ob[:, oc:oc + 64], scalar1=rden[:, h:h + 1],
                        scalar2=None, op0=ALU.mult),
                        reads=[("psO", h), ("rden", h)], writes=[("natok", nb)])
                    it += 1
                for j in range(4):
                    S.op("pe", lambda e, j=j, nb=nb: e.transpose(pT[nb][:, j * 128:(j + 1) * 128],
                                                                natok[nb][:, j * 128:(j + 1) * 128], identb[:]),
                         reads=[("natok", nb)], writes=[("pT", nb)])
                S.op("act", lambda e, i=i, nb=nb: e.activation(
                    out=mixT[:, 0:4, i * 128:(i + 1) * 128],
                    in_=pT[nb][:, 0:512].rearrange("p (j t) -> p j t", t=128), func=AF.Copy),
                    reads=[("pT", nb)], writes=[("mixna", i)])
            phase_end("p2")

        with contextlib.ExitStack() as p3:
            oacc = sb("oacc", [128, NT, 512], stack=p3)
            with contextlib.ExitStack() as p3a:
                wv = sb("wv", [128, 8, 512], BF16, stack=p3a)
                wk = load_w(wv, w_in, 3 * 512 + 3 * 512, 512, "wv")
                for T in range(NTT):
                    pb = T % 4
                    for k in range(8):
                        S.op("pe", lambda e, k=k, T=T, pb=pb: e.matmul(
                            ps[pb][:, :], hT[:, k, T * 128:(T + 1) * 128], wv[:, k, :],
                            start=(k == 0), stop=(k == 7)), reads=wk, writes=[("ps", pb)])
                    if T % 2 == 0:
                        S.op("act", lambda e, T=T, pb=pb: e.activation(out=vtok[:, T, :], in_=ps[pb][:, :], func=AF.Copy),
                             reads=[("ps", pb)], writes=[("vtok", T)])
                    else:
                        S.op("dve", lambda e, T=T, pb=pb: e.tensor_copy(out=vtok[:, T, :], in_=ps[pb][:, :]),
                             reads=[("ps", pb)], writes=[("vtok", T)])
                phase_end("p3a")
            with contextlib.ExitStack() as p3b:
                rmask = sb("rmask_sb", [128, TOK], stack=p3b)
                A_ = sb("hgA", [128, TOK], stack=p3b)
                B_ = sb("hgB", [128, TOK], stack=p3b)
                C_ = sb("hgC", [128, TOK], stack=p3b)
                qsb = sb("hgqs", [128, 512], stack=p3b)
                Qt = sb("hgQt", [128, SEQ], BF16, stack=p3b)
                Qs = sb("hgQs", [128, SEQ], BF16, stack=p3b)
                Ks = sb("hgKs", [128, TOK], BF16, stack=p3b)
                Kf = sb("hgKf", [128, TOK], BF16, stack=p3b)
                Ktok = sb("hgKtok", [128, NTT, 128], BF16, stack=p3b)
                wqf = sb("hgwqf", [128, 8, 256], BF16, stack=p3b)
                sc_ = sb("hgsc", [128, 5, NCH], stack=p3b)
                Sst = sb("hgS", [128, 128], stack=p3b)
                Sdec = sb("hgSdec", [128, 128], stack=p3b)
                Smb = [sb(f"hgSmb{j}", [128, 128], BF16, stack=p3b) for j in range(2)]
                Qe = sb("hgQe", [128, SEQ], BF16, stack=p3b)
                Qo = sb("hgQo", [128, SEQ], BF16, stack=p3b)
                ATs = sb("hgATs", [128, 128], BF16, stack=p3b)

                S.dma("sp", lambda e: e.dma_start(out=rmask[:], in_=rmask_d), writes=["rmask"])

                def v3(t, n=TOK):
                    return t[:, 0:n].rearrange("p (t s) -> p t s", s=64)

                for dr in range(2):
                    for hh in range(4):
                        dh = dr * 4 + hh
                        S.dma("pool", lambda e, hh=hh: e.dma_start(
                            out=wqf[:, :, 0:128],
                            in_=w_in[:, 1536 + hh * 128:1536 + (hh + 1) * 128].rearrange("(k p) n -> p k n", p=128)),
                            writes=["wq_h"])
                        S.dma("pool", lambda e, hh=hh, dr=dr: e.dma_start(
                            out=wqf[:, :, 128:256],
                            in_=w_in[:, 2048 + dr * 512 + hh * 128:2048 + dr * 512 + (hh + 1) * 128].rearrange(
                                "(k p) n -> p k n", p=128)), writes=["wf_h"])
                        for ci, t0 in enumerate(range(0, TOK, 512)):
                            tw = min(512, TOK - t0)
                            pb = ci % 4
                            for k in range(8):
                                S.op("pe", lambda e, k=k, t0=t0, tw=tw, pb=pb: e.matmul(
                                    ps[pb][:, 0:tw], wqf[:, k, 128:256], hT[:, k, t0:t0 + tw],
                                    start=(k == 0), stop=(k == 7)), reads=["wf_h"], writes=[("ps", pb)])
                            S.op("act", lambda e, t0=t0, tw=tw, pb=pb: e.activation(
                                out=A_[:, t0:t0 + tw], in_=ps[pb][:, 0:tw], func=AF.Sigmoid),
                                reads=[("ps", pb)], writes=["A"])
                        S.op("dve", lambda e, dh=dh: e.tensor_scalar(out=A_[:], in0=A_[:], scalar1=oml[:, dh:dh + 1],
                                                                     scalar2=lb[:, dh:dh + 1], op0=ALU.mult, op1=ALU.add),
                             reads=["A"], writes=["A"])
                        S.op("act", lambda e: e.activation(out=B_[:], in_=A_[:], func=AF.Ln), reads=["A"], writes=["B"])
                        S.op("dve", lambda e: e.tensor_scalar(out=A_[:], in0=A_[:], scalar1=-1.0, scalar2=1.0,
                                                              op0=ALU.mult, op1=ALU.add), reads=["A", "B"], writes=["A"])
                        S.op("dve", lambda e: e.tensor_tensor_scan(out=C_[:], data0=rmask[:], data1=B_[:], initial=0.0,
                                                                   op0=ALU.mult, op1=ALU.add),
                             reads=["B", "rmask"], writes=["C"])
                        if dr == 0:
                            gbuf, gkey = C_, "C"
                            refpos = 31
                        else:
                            S.op("dve", lambda e: e.tensor_tensor(out=B_[:], in0=B_[:], in1=C_[:], op=ALU.subtract),
                                 reads=["B", "C"], writes=["B"])
                            S.op("dve", lambda e: e.tensor_tensor(
                                out=v3(B_), in0=v3(B_), in1=v3(C_)[:, :, 63:64].to_broadcast([128, NCH, 64]), op=ALU.add),
                                reads=["B", "C"], writes=["B"])
                            gbuf, gkey = B_, "B"
                            refpos = 32
                        endpos = 63 if dr == 0 else 0
                        S.op("dve", lambda e, gbuf=gbuf, refpos=refpos: e.tensor_copy(
                            out=sc_[:, 0, :].unsqueeze(2), in_=v3(gbuf)[:, :, refpos:refpos + 1]), reads=[gkey], writes=["sc0"])
                        S.op("dve", lambda e, gbuf=gbuf, endpos=endpos: e.tensor_copy(
                            out=sc_[:, 1, :].unsqueeze(2), in_=v3(gbuf)[:, :, endpos:endpos + 1]), reads=[gkey], writes=["sc1"])
                        S.op("act", lambda e: e.activation(out=sc_[:, 2:4, :], in_=sc_[:, 0:2, :], func=AF.Exp),
                             reads=["sc0", "sc1"], writes=["sc23"])
                        S.op("dve", lambda e: e.tensor_tensor(out=sc_[:, 4, :], in0=sc_[:, 1, :], in1=sc_[:, 0, :],
                                                              op=ALU.subtract), reads=["sc0", "sc1"], writes=["sc4"])
                        S.op("act", lambda e: e.activation(out=sc_[:, 4, :], in_=sc_[:, 4, :], func=AF.Exp),
                             reads=["sc4"], writes=["sc4"])
                        obuf, okey = (B_, "B") if dr == 0 else (C_, "C")
                        S.op("dve", lambda e, gbuf=gbuf: e.tensor_tensor(
                            out=v3(gbuf), in0=v3(gbuf), in1=sc_[:, 0, :].unsqueeze(2).to_broadcast([128, NCH, 64]),
                            op=ALU.subtract), reads=[gkey, "sc0", "sc1"], writes=[gkey])
                        S.op("act", lambda e, gbuf=gbuf, obuf=obuf: e.activation(out=obuf[:, 0:SEQ], in_=gbuf[:, 0:SEQ], func=AF.Exp),
                             reads=[gkey, okey], writes=[okey])
                        S.op("act", lambda e, gbuf=gbuf: e.activation(out=gbuf[:], in_=gbuf[:], func=AF.Exp, scale=-1.0),
                             reads=[gkey, okey], writes=[gkey])
                        S.op("dve", lambda e, gbuf=gbuf: e.tensor_tensor(out=Kf[:], in0=A_[:], in1=gbuf[:], op=ALU.mult),
                             reads=["A", gkey], writes=["Kf"])
                        hk = 1 if dr == 0 else 0
                        hq = 1 - hk
                        S.op("pool", lambda e: e.tensor_copy(out=Ks[:], in_=Kf[:]), reads=["Kf"], writes=["Ks"])
                        S.op("pool", lambda e, hk=hk: e.memset(
                            Ks[:].rearrange("p (t two s) -> p t two s", two=2, s=32)[:, :, hk, :], 0.0),
                            reads=["Ks"], writes=["Ks"])
                        for ci, t0 in enumerate(range(0, SEQ, 512)):
                            pb = ci % 4
                            for k in range(8):
                                S.op("pe", lambda e, k=k, t0=t0, pb=pb: e.matmul(
                                    ps[pb][:, :], wqf[:, k, 0:128], hT[:, k, t0:t0 + 512],
                                    start=(k == 0), stop=(k == 7)), reads=["wq_h"], writes=[("ps", pb)])
                            S.op("act", lambda e, pb=pb: e.activation(out=qsb[:], in_=ps[pb][:, :], func=AF.Silu),
                                 reads=[("ps", pb)], writes=["qsb"])
                            S.op("dve", lambda e, t0=t0, obuf=obuf: e.tensor_tensor(out=Qt[:, t0:t0 + 512], in0=qsb[:],
                                                                                    in1=obuf[:, t0:t0 + 512], op=ALU.mult),
                                 reads=["qsb", okey], writes=["Qt"])
                        S.op("pool", lambda e: e.tensor_copy(out=Qs[:], in_=Qt[:]), reads=["Qt"], writes=["Qs"])
                        S.op("pool", lambda e, hq=hq: e.memset(
                            Qs[:].rearrange("p (t two s) -> p t two s", two=2, s=32)[:, :, hq, :], 0.0),
                            reads=["Qs"], writes=["Qs"])
                        S.op("pool", lambda e: e.tensor_copy(out=Qe[:], in_=Qt[:]), reads=["Qt"], writes=["Qe"])
                        S.op("pool", lambda e: e.memset(
                            Qe[:].rearrange("p (t two s) -> p t two s", two=2, s=64)[:, :, 1, :], 0.0),
                            reads=["Qe"], writes=["Qe"])
                        S.op("pool", lambda e: e.tensor_copy(out=Qo[:], in_=Qt[:]), reads=["Qt"], writes=["Qo"])
                        S.op("pool", lambda e: e.memset(
                            Qo[:].rearrange("p (t two s) -> p t two s", two=2, s=64)[:, :, 0, :], 0.0),
                            reads=["Qo"], writes=["Qo"])
                        for T in range(NTT):
                            nb = T % 2
                            S.op("pe", lambda e, T=T, nb=nb: e.transpose(pT[nb][:, 0:128], Kf[:, T * 128:(T + 1) * 128], identb[:]),
                                 reads=["Kf"], writes=[("pT", nb)])
                            S.op("act", lambda e, T=T, nb=nb: e.activation(out=Ktok[:, T, :], in_=pT[nb][:, 0:128], func=AF.Copy),
                                 reads=[("pT", nb)], writes=[("Ktok", T)])
                        S.op("pool", lambda e, hk=hk: e.memset(
                            Kf[:].rearrange("p (t two s) -> p t two s", two=2, s=32)[:, :, 1 - hk, :], 0.0),
                            reads=["Kf"], writes=["Kf"])
                        S.op("dve", lambda e: e.memset(Sst[:], 0.0), writes=["S"])
                        order = [16, 17] + list(range(NT)) if dr == 0 else [17, 16] + list(range(NT - 1, -1, -1))
                        tri = TRIF if dr == 0 else TRIB
                        for T in order:
                            lat = T < NT
                            cs = [2 * T, 2 * T + 1] if dr == 0 else [2 * T + 1, 2 * T]
                            if lat:
                                S.op("pe", lambda e, T=T: e.matmul(ps[4][:, 0:128], Ks[:, T * 128:(T + 1) * 128],
                                                                   Qt[:, T * 128:(T + 1) * 128], start=True, stop=False),
                                     reads=["Ks", "Qt"], writes=[("ps", 4)])
                                S.op("pe", lambda e, T=T: e.matmul(ps[4][:, 0:128], Kf[:, T * 128:(T + 1) * 128],
                                                                   Qs[:, T * 128:(T + 1) * 128], start=False, stop=True),
                                     reads=["Kf", "Qs"], writes=[("ps", 4)])
                                S.op("dve", lambda e, tri=tri: e.tensor_tensor(out=ATs[:], in0=ps[4][:, 0:128], in1=tri, op=ALU.mult),
                                     reads=[("ps", 4), "cA"], writes=["ATs"])
                                S.op("pe", lambda e, T=T, hh=hh: e.matmul(ps[5][:, 0:128], ATs[:], vtok[:, T, hh * 128:(hh + 1) * 128],
                                                                          start=True, stop=False),
                                     reads=["ATs"], writes=[("ps", 5)])
                            for ci, c in enumerate(cs):
                                par = c % 2
                                if lat:
                                    S.op("dve", lambda e, c=c, ci=ci: e.tensor_scalar(
                                        out=Smb[ci][:], in0=Sst[:], scalar1=sc_[:, 2, c:c + 1], scalar2=None, op0=ALU.mult),
                                        reads=["S", "sc23"], writes=[("Smb", ci)])
                                    Qz, qzk = (Qe, "Qe") if par == 0 else (Qo, "Qo")
                                    S.op("pe", lambda e, T=T, ci=ci, Qz=Qz: e.matmul(
                                        ps[5][:, 0:128], Qz[:, T * 128:(T + 1) * 128], Smb[ci][:], start=False, stop=(ci == 1)),
                                        reads=[("Smb", ci), qzk], writes=[("ps", 5)])
                                S.op("pe", lambda e, T=T, hh=hh, par=par: e.matmul(
                                    ps[3][:, 0:128], Ktok[par * 64:(par + 1) * 64, T, :],
                                    vtok[par * 64:(par + 1) * 64, T, hh * 128:(hh + 1) * 128], start=True, stop=True),
                                    reads=[("Ktok", T)], writes=[("ps", 3)])
                                S.op("dve", lambda e, c=c: e.tensor_scalar(out=Sdec[:], in0=Sst[:], scalar1=sc_[:, 3, c:c + 1],
                                                                           scalar2=None, op0=ALU.mult),
                                     reads=["S", "sc23"], writes=["Sdec"])
                                S.op("dve", lambda e, c=c: e.scalar_tensor_tensor(
                                    out=Sst[:], in0=ps[3][:, 0:128], scalar=sc_[:, 4, c:c + 1], in1=Sdec[:],
                                    op0=ALU.mult, op1=ALU.add), reads=[("ps", 3), "Sdec", "sc4"], writes=["S"])
                            if lat:
                                if dr == 0:
                                    S.op("act", lambda e, T=T, hh=hh: e.activation(
                                        out=oacc[:, T, hh * 128:(hh + 1) * 128], in_=ps[5][:, 0:128], func=AF.Copy),
                                        reads=[("ps", 5)], writes=[("oacc", T, hh)])
                                else:
                                    S.op("dve", lambda e, T=T, hh=hh: e.tensor_tensor(
                                        out=oacc[:, T, hh * 128:(hh + 1) * 128], in0=ps[5][:, 0:128],
                                        in1=oacc[:, T, hh * 128:(hh + 1) * 128], op=ALU.add),
                                        reads=[("ps", 5), ("oacc", T, hh)], writes=[("oacc", T, hh)])
                phase_end("p3b")
            with contextlib.ExitStack() as p3c:
                wg = sb("wg", [128, 8, 512], BF16, stack=p3c)
                hgnb = sb("hgnb", [128, 512], stack=p3c)
                sg = [sb(f"sg{j}", [128, 512], stack=p3c) for j in range(2)]
                yb = [sb(f"yb{j}", [128, 512], stack=p3c) for j in range(2)]
                yt = [sb(f"yt{j}", [128, 512], BF16, stack=p3c) for j in range(2)]
                jk = sb("jk3", [128, 128], BF16, stack=p3c)
                st3 = sb("st3", [128, NT, 8], stack=p3c)
                wk = load_w(wg, w_in, 1536 + 4 * 512, 512, "wg")
                S.dma("sp", lambda e: e.dma_start(out=hgnb[:], in_=hgn), writes=["hgnb"])
                for T in range(NT):
                    b = T % 2
                    for k in range(8):
                        S.op("pe", lambda e, k=k, T=T, b=b: e.matmul(
                            ps[b][:, :], hT[:, k, T * 128:(T + 1) * 128], wg[:, k, :],
                            start=(k == 0), stop=(k == 7)), reads=wk, writes=[("ps", b)])
                    S.op("act", lambda e, b=b: e.activation(out=sg[b][:], in_=ps[b][:, :], func=AF.Silu),
                         reads=[("ps", b)], writes=[("sg", b)])
                    for hh in range(4):
                        S.op("act", lambda e, T=T, hh=hh: e.activation(
                            out=jk[:], in_=oacc[:, T, hh * 128:(hh + 1) * 128], func=AF.Square,
                            accum_out=st3[:, T, hh:hh + 1]), reads=[], writes=["jk3", ("ss3", T)])
                    S.op("dve", lambda e, T=T: e.tensor_scalar(out=st3[:, T, 4:8], in0=st3[:, T, 0:4], scalar1=1.0 / 128,
                                                               scalar2=EPS, op0=ALU.mult, op1=ALU.add),
                         reads=[("ss3", T)], writes=[("ms3", T)])
                    S.op("act", lambda e, T=T: e.activation(out=st3[:, T, 4:8], in_=st3[:, T, 4:8], func=AF.Sqrt),
                         reads=[("ms3", T)], writes=[("ms3", T)])
                    S.op("dve", lambda e, T=T: e.reciprocal(out=st3[:, T, 0:4], in_=st3[:, T, 4:8]),
                         reads=[("ms3", T)], writes=[("rs3", T)])
                    S.op("dve", lambda e, T=T, b=b: e.tensor_tensor(
                        out=yb[b][:].rearrange("p (h d) -> p h d", d=128),
                        in0=oacc[:, T, :].rearrange("p (h d) -> p h d", d=128),
                        in1=st3[:, T, 0:4].unsqueeze(2).to_broadcast([128, 4, 128]), op=ALU.mult),
                        reads=[("rs3", T)], writes=[("yb", b)])
                    S.op("dve", lambda e, b=b: e.tensor_tensor(out=yb[b][:], in0=yb[b][:], in1=hgnb[:], op=ALU.mult),
                         reads=[("yb", b), "hgnb"], writes=[("yb", b)])
                    S.op("dve", lambda e, b=b: e.tensor_tensor(out=yt[b][:], in0=yb[b][:], in1=sg[b][:], op=ALU.mult),
                         reads=[("yb", b), ("sg", b)], writes=[("yt", b)])
                    for j in range(4):
                        S.op("pe", lambda e, j=j, b=b: e.transpose(pT[b][:, j * 128:(j + 1) * 128],
                                                                   yt[b][:, j * 128:(j + 1) * 128], identb[:]),
                             reads=[("yt", b)], writes=[("pT", b)])
                    S.op("act", lambda e, T=T, b=b: e.activation(
                        out=mixT[:, 4:8, T * 128:(T + 1) * 128],
                        in_=pT[b][:, 0:512].rearrange("p (j t) -> p j t", t=128), func=AF.Copy),
                        reads=[("pT", b)], writes=[("mixhg", T)])
                if debug:
                    S.dma("sp", lambda e: e.dma_start(out=dbg["mixT"], in_=mixT[:].rearrange("p k t -> p (k t)")),
                          reads=[("mixhg", T) for T in range(NT)])
                phase_end("p3")

        with contextlib.ExitStack() as p4:
            wo32 = sb("wo32", [128, 8, D], stack=p4)
            wob = sb("wob", [128, 8, D], BF16, stack=p4)
            g1b = sb("g1b", [128, D], stack=p4)
            S.dma("sp", lambda e: e.dma_start(out=g1b[:], in_=scr_bc[0]), writes=["g1b"])
            for k in range(8):
                S.dma("sp" if k % 2 == 0 else "act", lambda e, k=k: e.dma_start(
                    out=wo32[:, k, :], in_=w_out[k * 128:(k + 1) * 128, :]), writes=[("wo32", k)])
                S.op("dve" if k % 2 == 0 else "pool", lambda e, k=k: e.tensor_tensor(
                    out=wob[:, k, :], in0=wo32[:, k, :], in1=g1b[:], op=ALU.mult),
                    reads=[("wo32", k), "g1b"], writes=[("wob", k)])
            wob_all = [("wob", k) for k in range(8)]
            for T in range(NT):
                S.dma("sp" if T % 2 == 0 else "act", lambda e, T=T: e.dma_start(
                    out=x1[:, T, :], in_=x[T * 128:(T + 1) * 128, :]), writes=[("x1", T)])
                for hf in range(2):
                    pb = (2 * T + hf) % 4
                    for k in range(8):
                        S.op("pe", lambda e, k=k, T=T, hf=hf, pb=pb: e.matmul(
                            ps[pb][:, :], mixT[:, k, T * 128:(T + 1) * 128], wob[:, k, hf * 512:(hf + 1) * 512],
                            start=(k == 0), stop=(k == 7)), reads=wob_all, writes=[("ps", pb)])
                    S.op("dve", lambda e, T=T, hf=hf, pb=pb: e.tensor_tensor(
                        out=x1[:, T, hf * 512:(hf + 1) * 512], in0=ps[pb][:, :], in1=x1[:, T, hf * 512:(hf + 1) * 512],
                        op=ALU.add), reads=[("ps", pb), ("x1", T)], writes=[("x1", T)])
            if debug:
                S.dma("sp", lambda e: e.dma_start(out=dbg["x1"], in_=R[:]),
                      reads=[("x1", T) for T in range(NT)])
            phase_end("p4")

    eidx = sb("eidx", [128, NT, 128], I32)
    gw = sb("gw", [128, NT, 128])
    rs2 = sb("rs2", [128, NT])
    with contextlib.ExitStack() as p5:
        wqb = sb("wqb", [128, 8, 2048], BF16, stack=p5)
        kTb = sb("kTb", [128, 16, 128], BF16, stack=p5)
        junk = sb("junk5", [128, D], BF16, stack=p5)
        xs = [sb(f"xs5{j}", [128, D], BF16, stack=p5) for j in range(2)]
        h2T = [sb(f"h2T{j}", [128, 8, 128], BF16, stack=p5) for j in range(2)]
        qTp = [sb(f"qTp{j}", [128, 16, 128], BF16, stack=p5) for j in range(2)]
        ssb = sb("ssb", [128, 16, 128], stack=p5)
        s2 = sb("s2", [128, 16, 128], stack=p5)
        top = sb("top", [128, 16, 16], stack=p5)
        itop = sb("itop", [128, 16, 16], U32, stack=p5)
        itf = sb("itf", [128, 16, 16], stack=p5)
        cand = sb("cand", [128, 8, 256], stack=p5)
        cand2 = sb("cand2", [128, 8, 256], stack=p5)
        ctop = sb("ctop", [128, 8, 16], stack=p5)
        cpos = sb("cpos", [128, 8, 16], U32, stack=p5)
        paf = sb("paf", [128, 128], stack=p5)
        pai = sb("pai", [128, 128], I32, stack=p5)
        pbf = sb("pbf", [128, 128], stack=p5)
        oh = sb("oh", [128, 128, 16], stack=p5)
        selA = sb("selA", [128, 128], stack=p5)
        selB = sb("selB", [128, 128], stack=p5)
        ef = sb("ef", [128, 128], stack=p5)
        ee = sb("ee", [128, 8, 16], stack=p5)
        zz = sb("zz", [128, 16], stack=p5)
        st5 = sb("st5", [128, 2 * NT], stack=p5)

        stg = [sb(f"stg{j}", [128, 4, D], BF16, stack=p5) for j in range(2)]
        conv_steps = [(tab, c) for tab in range(2) for c in range(32)]

        def emit_conv(si):
            tab, c = conv_steps[si]
            src = (u_t, v_t)[tab].rearrange("(p j c) d -> c p j d", p=128, j=4, c=32)[c]
            dst = uv_bf.rearrange("(p j c) d -> c p j d", p=128, j=4, c=32)[c][:, :, tab * D:(tab + 1) * D]
            b = si % 2
            S.dma("pool", lambda e: e.dma_start(out=stg[b][:], in_=src), writes=[("stg", b)])
            S.dma("sp", lambda e: e.dma_start(out=dst, in_=stg[b][:]), reads=[("stg", b)], writes=[("tab", tab, c)])

        wkq = load_w(wqb[:, :, 0:1024], wq, 0, 1024, "wqa") + load_w(wqb[:, :, 1024:2048], wq, 1024, 1024, "wqb")
        S.dma("pool", lambda e: e.dma_start(out=kTb[:].rearrange("p c k -> p (c k)"), in_=keysT), writes=["kTb"])
        for T in range(NT):
            b = T % 2
            for q_ in range(4):
                emit_conv(4 * T + q_)
            S.op("act", lambda e, T=T: e.activation(out=junk[:], in_=x1[:, T, :], func=AF.Square,
                                                    accum_out=st5[:, T:T + 1]), reads=[], writes=["junk5", ("ssq5", T)])
            S.op("dve", lambda e, T=T: e.tensor_scalar(out=st5[:, NT + T:NT + T + 1], in0=st5[:, T:T + 1],
                                                       scalar1=1.0 / D, scalar2=EPS, op0=ALU.mult, op1=ALU.add),
                 reads=[("ssq5", T)], writes=[("ms5", T)])
            S.op("act", lambda e, T=T: e.activation(out=st5[:, NT + T:NT + T + 1], in_=st5[:, NT + T:NT + T + 1],
                                                    func=AF.Sqrt), reads=[("ms5", T)], writes=[("ms5", T)])
            S.op("dve", lambda e, T=T: e.reciprocal(out=rs2[:, T:T + 1], in_=st5[:, NT + T:NT + T + 1]),
                 reads=[("ms5", T)], writes=[("rs2", T)])
            S.op("act", lambda e, b=b, T=T: e.activation(out=xs[b][:], in_=x1[:, T, :], func=AF.Copy,
                                                         scale=rs2[:, T:T + 1]), reads=[("rs2", T)], writes=[("xs5", b)])
            for k in range(8):
                S.op("pe", lambda e, b=b, k=k: e.transpose(pT[b][:, k * 128:(k + 1) * 128],
                                                           xs[b][:, k * 128:(k + 1) * 128], identb[:]),
                     reads=[("xs5", b)], writes=[("pT", b)])
            for k in range(8):
                S.op("dve" if k % 2 == 0 else "act", (lambda e, b=b, k=k: e.tensor_scalar(
                    out=h2T[b][:, k, :], in0=pT[b][:, k * 128:(k + 1) * 128],
                    scalar1=mc[:, 32 + k:33 + k], scalar2=mc[:, 40 + k:41 + k], op0=ALU.mult, op1=ALU.add))
                    if k % 2 == 0 else (lambda e, b=b, k=k: e.activation(
                        out=h2T[b][:, k, :], in_=pT[b][:, k * 128:(k + 1) * 128], func=AF.Identity,
                        scale=mc[:, 32 + k:33 + k], bias=mc[:, 40 + k:41 + k])),
                    reads=[("pT", b)], writes=[("h2T", b)])
            for g4 in range(4):
                pb = g4
                for j in range(4):
                    pc = g4 * 4 + j
                    for k in range(8):
                        S.op("pe", lambda e, b=b, k=k, pc=pc, j=j, pb=pb: e.matmul(
                            ps[pb][:, j * 128:(j + 1) * 128], wqb[:, k, pc * 128:(pc + 1) * 128], h2T[b][:, k, :],
                            start=(k == 0), stop=(k == 7)), reads=wkq + [("h2T", b)], writes=[("ps", pb)])
                if g4 % 2 == 0:
                    S.op("act", lambda e, b=b, g4=g4, pb=pb: e.activation(
                        out=qTp[b][:, g4 * 4:(g4 + 1) * 4, :], in_=ps[pb][:, :].rearrange("p (j t) -> p j t", t=128),
                        func=AF.Copy), reads=[("ps", pb)], writes=[("qTp", b, g4)])
                else:
                    S.op("dve", lambda e, b=b, g4=g4, pb=pb: e.tensor_copy(
                        out=qTp[b][:, g4 * 4:(g4 + 1) * 4, :], in_=ps[pb][:, :].rearrange("p (j t) -> p j t", t=128)),
                        reads=[("ps", pb)], writes=[("qTp", b, g4)])
            for g4 in range(4):
                pb = 4 + (g4 % 2)
                for j in range(4):
                    pc = g4 * 4 + j
                    S.op("pe", lambda e, b=b, pc=pc, j=j, pb=pb: e.matmul(
                        ps[pb][:, j * 128:(j + 1) * 128], qTp[b][:, pc, :], kTb[:, pc, :], start=True, stop=True),
                        reads=[("qTp", b, g4), "kTb"], writes=[("ps", pb)])
                S.op("act", lambda e, g4=g4, pb=pb: e.activation(
                    out=ssb[:, g4 * 4:(g4 + 1) * 4, :], in_=ps[pb][:, :].rearrange("p (j t) -> p j t", t=128),
                    func=AF.Copy), reads=[("ps", pb)], writes=[("ssb", g4)])
            for pc in range(16):
                S.op("dve", lambda e, pc=pc: e.max(out=top[:, pc, 0:8], in_=ssb[:, pc, :]),
                     reads=[("ssb", pc // 4)], writes=[("top", pc, 0)])
            for pc in range(16):
                S.op("dve", lambda e, pc=pc: e.match_replace(out=s2[:, pc, :], in_to_replace=top[:, pc, 0:8],
                                                             in_values=ssb[:, pc, :], imm_value=NEG),
                     reads=[("ssb", pc // 4), ("top", pc, 0)], writes=[("s2", pc)])
            for pc in range(16):
                S.op("dve", lambda e, pc=pc: e.max(out=top[:, pc, 8:16], in_=s2[:, pc, :]),
                     reads=[("s2", pc)], writes=[("top", pc, 1)])
            for pc in range(16):
                S.op("dve", lambda e, pc=pc: e.max_index(out=itop[:, pc, 0:8], in_max=top[:, pc, 0:8], in_values=ssb[:, pc, :]),
                     reads=[("ssb", pc // 4), ("top", pc, 0)], writes=[("itop", pc, 0)])
            for pc in range(16):
                S.op("dve", lambda e, pc=pc: e.max_index(out=itop[:, pc, 8:16], in_max=top[:, pc, 8:16], in_values=ssb[:, pc, :]),
                     reads=[("ssb", pc // 4), ("top", pc, 1)], writes=[("itop", pc, 1)])
            tops = [("top", pc, j) for pc in range(16) for j in range(2)]
            itops = [("itop", pc, j) for pc in range(16) for j in range(2)]
            S.op("dve", lambda e: e.tensor_copy(out=itf[:], in_=itop[:]), reads=itops, writes=["itf"])
            topv = top[:].rearrange("p (h c) a -> p h c a", c=2)
            S.op("dve", lambda e: e.tensor_tensor(
                out=cand[:].rearrange("p h (a b) -> p h a b", b=16),
                in0=topv[:, :, 0, :].unsqueeze(3).to_broadcast([128, 8, 16, 16]),
                in1=topv[:, :, 1, :].unsqueeze(2).to_broadcast([128, 8, 16, 16]), op=ALU.add),
                reads=tops, writes=["cand"])
            for p in range(8):
                S.op("dve", lambda e, p=p: e.max(out=ctop[:, p, 0:8], in_=cand[:, p, :]), reads=["cand"], writes=[("ctop", p, 0)])
            for p in range(8):
                S.op("dve", lambda e, p=p: e.match_replace(out=cand2[:, p, :], in_to_replace=ctop[:, p, 0:8],
                                                           in_values=cand[:, p, :], imm_value=NEG),
                     reads=["cand", ("ctop", p, 0)], writes=[("cand2", p)])
            for p in range(8):
                S.op("dve", lambda e, p=p: e.max(out=ctop[:, p, 8:16], in_=cand2[:, p, :]), reads=[("cand2", p)], writes=[("ctop", p, 1)])
            for p in range(8):
                S.op("dve", lambda e, p=p: e.max_index(out=cpos[:, p, 0:8], in_max=ctop[:, p, 0:8], in_values=cand[:, p, :]),
                     reads=["cand", ("ctop", p, 0)], writes=[("cpos", p, 0)])
            for p in range(8):
                S.op("dve", lambda e, p=p: e.max_index(out=cpos[:, p, 8:16], in_max=ctop[:, p, 8:16], in_values=cand[:, p, :]),
                     reads=["cand", ("ctop", p, 1)], writes=[("cpos", p, 1)])
            ctops = [("ctop", p, j) for p in range(8) for j in range(2)]
            cposs = [("cpos", p, j) for p in range(8) for j in range(2)]
            cposf = cpos[:].rearrange("p h j -> p (h j)")
            S.op("dve", lambda e: e.tensor_copy(out=selA[:], in_=cposf), reads=cposs + [("sel", 0)], writes=["posf"])
            S.op("dve", lambda e: e.tensor_scalar(out=pbf[:], in0=selA[:], scalar1=0.0625, scalar2=None, op0=ALU.mult),
                 reads=["posf"], writes=["pbf"])
            S.op("dve", lambda e: e.tensor_copy(out=pai[:], in_=pbf[:]), reads=["pbf"], writes=["pai"])
            S.op("dve", lambda e: e.tensor_copy(out=paf[:], in_=pai[:]), reads=["pai"], writes=["paf"])
            S.op("dve", lambda e: e.scalar_tensor_tensor(out=pbf[:], in0=paf[:], scalar=16.0, in1=selA[:],
                                                         op0=ALU.mult, op1=ALU.is_gt), reads=["paf", "posf", "pai"], writes=["pbf"])
            S.op("dve", lambda e: e.tensor_tensor(out=paf[:], in0=paf[:], in1=pbf[:], op=ALU.subtract),
                 reads=["paf", "pbf"], writes=["paf"])
            S.op("dve", lambda e: e.scalar_tensor_tensor(out=pbf[:], in0=paf[:], scalar=-16.0, in1=selA[:],
                                                         op0=ALU.mult, op1=ALU.add), reads=["paf", "posf"], writes=["pbf"])
            itv = itf[:].rearrange("p (h c) a -> p h c a", c=2)
            for which, pf, sel in ((0, paf, selA), (1, pbf, selB)):
                S.op("dve", lambda e, pf=pf: e.tensor_tensor(
                    out=oh[:], in0=pf[:].unsqueeze(2).to_broadcast([128, 128, 16]),
                    in1=IOTA16.unsqueeze(1).to_broadcast([128, 128, 16]), op=ALU.is_equal),
                    reads=["paf", "pbf", "cA", "oh"], writes=["oh"])
                S.op("dve", lambda e, which=which: e.tensor_tensor(
                    out=oh[:].rearrange("p (h j) a -> p h j a", j=16),
                    in0=oh[:].rearrange("p (h j) a -> p h j a", j=16),
                    in1=itv[:, :, which, :].unsqueeze(2).to_broadcast([128, 8, 16, 16]), op=ALU.mult),
                    reads=["oh", "itf"], writes=["oh"])
                S.op("dve", lambda e, sel=sel: e.tensor_reduce(out=sel[:], in_=oh[:], axis=AX.X, op=ALU.add),
                     reads=["oh", "posf"], writes=[("sel", which)])
            S.op("dve", lambda e: e.scalar_tensor_tensor(out=ef[:], in0=selA[:], scalar=128.0, in1=selB[:],
                                                         op0=ALU.mult, op1=ALU.add),
                 reads=[("sel", 0), ("sel", 1)], writes=["ef"])
            S.op("dve", lambda e, T=T: e.tensor_copy(out=eidx[:, T, :], in_=ef[:]), reads=["ef"], writes=[("eidx", T)])
            S.op("dve", lambda e: e.tensor_tensor(out=ee[:], in0=ctop[:], in1=ctop[:, :, 0:1].to_broadcast([128, 8, 16]),
                                                  op=ALU.subtract), reads=ctops, writes=["ee"])
            S.op("act", lambda e: e.activation(out=ee[:], in_=ee[:], func=AF.Exp), reads=["ee"], writes=["ee"])
            S.op("dve", lambda e: e.tensor_reduce(out=zz[:, 0:8], in_=ee[:], axis=AX.X, op=ALU.add),
                 reads=["ee"], writes=["zz"])
            S.op("dve", lambda e: e.reciprocal(out=zz[:, 8:16], in_=zz[:, 0:8]), reads=["zz"], writes=["zz2"])
            S.op("dve", lambda e, T=T: e.tensor_tensor(
                out=gw[:, T, :].rearrange("p (h j) -> p h j", j=16), in0=ee[:],
                in1=zz[:, 8:16].unsqueeze(2).to_broadcast([128, 8, 16]), op=ALU.mult),
                reads=["ee", "zz2"], writes=[("gw", T)])
        if debug:
            S.dma("sp", lambda e: e.dma_start(out=dbg["eidx"], in_=eidx[:].rearrange("p t s -> p (t s)")),
                  reads=[("eidx", T) for T in range(NT)])
            S.dma("sp", lambda e: e.dma_start(out=dbg["gw"], in_=gw[:].rearrange("p t s -> p (t s)")),
                  reads=[("gw", T) for T in range(NT)])
        phase_end("p5a")

    with contextlib.ExitStack() as p6:
        NB = 12
        ring = [sb(f"ring{j}", [128, 2 * D], BF16, stack=p6) for j in range(NB)]
        NDG = 6
        dg = [sb(f"dg{j}", [128, 128], BF16, stack=p6) for j in range(NDG)]
        bc = sb("bc5", [128, 4, D], stack=p6)
        h2 = [sb(f"h2_{j}", [128, D], stack=p6) for j in range(2)]
        junk = sb("junk6", [128, D], BF16, stack=p6)
        accs = sb("accs", [128, D], stack=p6)
        aa = [sb(f"aa{j}", [128, 128], stack=p6) for j in range(2)]
        gl = [sb(f"gl{j}", [128, 128], stack=p6) for j in range(2)]
        ww = [sb(f"ww{j}", [128, 128], stack=p6) for j in range(2)]
        st6 = sb("st6", [128, 2 * NT], stack=p6)
        S.dma("sp", lambda e: e.dma_start(out=bc[:, 0, :], in_=scr_bc[1]), writes=[("bc", 0)])
        S.dma("sp", lambda e: e.dma_start(out=bc[:, 1, :], in_=scr_bc[2]), writes=[("bc", 1)])
        S.dma("sp", lambda e: e.dma_start(out=bc[:, 2, :], in_=scr_bc[3]), writes=[("bc", 2)])
        S.dma("sp", lambda e: e.dma_start(out=bc[:, 3, :], in_=nfb), writes=[("bc", 3)])
        gi = gd = 0
        for T in range(NT):
            pu = T % 2
            S.op("dve", lambda e, T=T, pu=pu: e.scalar_tensor_tensor(out=h2[pu][:], in0=x1[:, T, :], scalar=rs2[:, T:T + 1],
                                                                     in1=bc[:, 1, :], op0=ALU.mult, op1=ALU.mult),
                 reads=[("bc", 1)], writes=[("h2", pu)])
            S.op("dve", lambda e, pu=pu: e.tensor_tensor(out=h2[pu][:], in0=h2[pu][:], in1=bc[:, 2, :], op=ALU.add),
                 reads=[("h2", pu), ("bc", 2)], writes=[("h2", pu)])
            S.op("dve", lambda e, pu=pu: e.memset(aa[pu][:], 0.0), writes=[("aa", pu)])
            for s_ in range(128):
                r = gi % NB
                gi += 1
                S.dma("pool", lambda e, T=T, s_=s_, r=r: e.indirect_dma_start(
                    out=ring[r][:], out_offset=None, in_=uv_bf,
                    in_offset=bass.IndirectOffsetOnAxis(ap=eidx[:, T, s_:s_ + 1], axis=0)),
                    reads=[], writes=[("ring", r)])
                S.op("dve", lambda e, s_=s_, r=r, pu=pu: e.scalar_tensor_tensor(
                    out=junk[:], in0=ring[r][:, 0:D], scalar=1.0, in1=h2[pu][:], op0=ALU.mult, op1=ALU.mult,
                    accum_out=aa[pu][:, s_:s_ + 1]), reads=[("ring", r), ("h2", pu), ("aa", pu)],
                    writes=["junk6", ("aas", pu, s_)])
                S.op("act", lambda e, s_=s_, pu=pu: e.activation(out=gl[pu][:, s_:s_ + 1], in_=aa[pu][:, s_:s_ + 1], func=AF.Gelu),
                     reads=[("aas", pu, s_)], writes=[("gl", pu, s_)])
                S.op("act", lambda e, T=T, s_=s_, pu=pu: e.activation(out=ww[pu][:, s_:s_ + 1], in_=gl[pu][:, s_:s_ + 1],
                                                                     func=AF.Copy, scale=gw[:, T, s_:s_ + 1]),
                     reads=[("gl", pu, s_)], writes=[("ww", pu, s_)])
                dj = gd % NDG
                gd += 1
                S.op("act", lambda e, s_=s_, dj=dj, pu=pu: e.activation(
                    out=dg[dj][:], in_=identb[:], func=AF.Copy, scale=ww[pu][:, s_:s_ + 1]),
                    reads=[("ww", pu, s_)], writes=[("dg", dj)])
                for hf in range(2):
                    S.op("pe", lambda e, s_=s_, dj=dj, r=r, pu=pu, hf=hf: e.matmul(
                        ps[2 * pu + hf][:, :], dg[dj][:], ring[r][:, D + hf * 512: D + (hf + 1) * 512],
                        start=(s_ == 0), stop=(s_ == 127)),
                        reads=[("dg", dj), ("ring", r)], writes=[("accP", pu, hf)])
            for hf in range(2):
                S.op("dve", lambda e, pu=pu, hf=hf: e.tensor_tensor(
                    out=accs[:, hf * 512:(hf + 1) * 512], in0=ps[2 * pu + hf][:, :], in1=bc[:, 0, hf * 512:(hf + 1) * 512],
                    op=ALU.mult), reads=[("accP", pu, hf), ("bc", 0)], writes=[("accs", hf)])
            S.op("dve", lambda e, T=T: e.tensor_tensor(out=accs[:], in0=accs[:], in1=x1[:, T, :], op=ALU.add),
                 reads=[("accs", 0), ("accs", 1)], writes=["accsum"])
            S.op("act", lambda e, T=T: e.activation(out=junk[:], in_=accs[:], func=AF.Square, accum_out=st6[:, T:T + 1]),
                 reads=["accsum"], writes=["junk6", ("ssq6", T)])
            S.op("dve", lambda e, T=T: e.tensor_scalar(out=st6[:, NT + T:NT + T + 1], in0=st6[:, T:T + 1],
                                                       scalar1=1.0 / D, scalar2=EPS, op0=ALU.mult, op1=ALU.add),
                 reads=[("ssq6", T)], writes=[("ms6", T)])
            S.op("act", lambda e, T=T: e.activation(out=st6[:, NT + T:NT + T + 1], in_=st6[:, NT + T:NT + T + 1],
                                                    func=AF.Sqrt), reads=[("ms6", T)], writes=[("ms6", T)])
            S.op("dve", lambda e, T=T: e.reciprocal(out=st6[:, T:T + 1], in_=st6[:, NT + T:NT + T + 1]),
                 reads=[("ms6", T)], writes=[("rs6", T)])
            S.op("dve", lambda e, T=T: e.scalar_tensor_tensor(out=x1[:, T, :], in0=accs[:], scalar=st6[:, T:T + 1],
                                                              in1=bc[:, 3, :], op0=ALU.mult, op1=ALU.mult),
                 reads=["accsum", ("rs6", T), ("bc", 3)], writes=[("xo", T), ("accs", 0), ("accs", 1)])
            S.dma("sp", lambda e, T=T: e.dma_start(out=out[T * 128:(T + 1) * 128, :], in_=x1[:, T, :]),
                  reads=[("xo", T)], writes=[("out", T)])
        phase_end("p5b")
    S.finish()
    es.close()
    return nc


def _col(v):
    return np.ascontiguousarray(np.asarray(v, np.float32).reshape(8, 128).T)


def make_inputs(inp):
    global _BIAS_IDX
    f = lambda a: np.ascontiguousarray(np.asarray(a, dtype=np.float32))
    if _BIAS_IDX is None:
        _BIAS_IDX = build_bias_index()
    rpb = f(inp["na_rpb"])[0]
    ext = np.concatenate([rpb.reshape(8, -1), np.full((8, 1), MASKV, np.float32)], axis=1)
    biasT = np.stack([ext[h][_BIAS_IDX] for h in range(8)], axis=1)
    cstm = np.zeros((128, 528), np.float32)
    cstm[:, 0:128] = np.eye(128, dtype=np.float32)
    sidx = np.arange(128)
    blk = (sidx[:, None] // 64) == (sidx[None, :] // 64)
    cstm[:, 128:256] = (blk & (sidx[:, None] <= sidx[None, :])).astype(np.float32)
    cstm[:, 256:384] = (blk & (sidx[:, None] >= sidx[None, :])).astype(np.float32)
    cstm[:, 384:512] = 1.0
    cstm[:, 512:528] = np.arange(16, dtype=np.float32)[None, :]
    rmask = np.ones((128, TOK), np.float32)
    rmask[:, ::64] = 0.0
    c_ctx = f(inp["c_ctx"])
    hg_lb = f(inp["hg_lb"])
    lbraw = np.ascontiguousarray(hg_lb.reshape(2, 2, 4, 128).transpose(3, 0, 1, 2).reshape(128, 16))
    keys = f(inp["peer_keys"])[0]
    keysT = np.ascontiguousarray(keys.transpose(3, 0, 1, 2).reshape(128, 16 * 128))
    shared = dict(
        w_mod=f(inp["w_mod"])[0], b_mod=f(inp["b_mod"])[0].reshape(1, -1),
        n1c=_col(f(inp["norm1"])[0]), n2c=_col(f(inp["norm2"])[0]),
        n2b=np.ascontiguousarray(np.broadcast_to(f(inp["norm2"])[0][None, :], (128, D))),
        nfb=np.ascontiguousarray(np.broadcast_to(f(inp["norm_f"])[None, :], (128, D))),
        w_in=f(inp["w_in"])[0], w_out=f(inp["w_out"])[0], wq=f(inp["peer_wq"])[0],
        keysT=keysT, u=f(inp["peer_u"])[0], v=f(inp["peer_v"])[0], lbraw=lbraw,
        hgn=np.ascontiguousarray(np.broadcast_to(f(inp["hg_norm"])[0][None, :], (128, 512))),
        biasT=np.ascontiguousarray(biasT.reshape(128, -1)), cst=cstm, rmask=rmask,
    )
    xs = f(inp["x"]); cs = f(inp["c"]); ctxs = f(inp["ctx"])
    maps = []
    for b in range(xs.shape[0]):
        cc = np.stack([_col(cs[b]), _col(c_ctx)], axis=2).reshape(128, 16)
        m = dict(shared)
        m.update(x=xs[b], ctx=ctxs[b], ccol=np.ascontiguousarray(cc))
        maps.append(m)
    return maps


def kernel(**inputs):
    maps = make_inputs(inputs)
    nc = build()
    res = run_bass_kernel_spmd(nc, maps, core_ids=list(range(len(maps))))
    return np.stack([np.asarray(r["out"], dtype=np.float32) for r in res.results], axis=0)
```

```python
import contextlib
import numpy as np
import concourse.bass as bass
import concourse.mybir as mybir
from concourse.bass_utils import run_bass_kernel_spmd

F32 = mybir.dt.float32
BF16 = mybir.dt.bfloat16
I32 = mybir.dt.int32
U32 = mybir.dt.uint32
ALU = mybir.AluOpType
AF = mybir.ActivationFunctionType
AX = mybir.AxisListType

D = 1024
SEQ = 2048
CTX = 256
NT = 16
NTT = 18
TOK = SEQ + CTX
NCH = TOK // 64
EPS = 1e-6
MASKV = -30000.0
NEG = -1.0e30


class Sched:
    COMPUTE = ("pe", "dve", "act", "pool")

    def __init__(self, nc, n_dsem=None):
        self.nc = nc
        self.engs = {"pe": nc.tensor, "dve": nc.vector, "act": nc.scalar,
                     "pool": nc.gpsimd, "sp": nc.sync}
        self.n_dsem = n_dsem or {"sp": 8, "act": 4, "pool": 16}
        self.es = contextlib.ExitStack()
        self.csem = {e: self.es.enter_context(nc.semaphore("cs_" + e)) for e in self.COMPUTE}
        self.dsem = {q: [self.es.enter_context(nc.semaphore(f"ds_{q}{j}")) for j in range(n)]
                     for q, n in self.n_dsem.items()}
        self.ccount = {e: 0 for e in self.COMPUTE}
        self.dcount = {q: 0 for q in self.n_dsem}
        self.clock = {e: {} for e in self.engs}
        self.bar_sig = 0
        self.bar_clock = {}
        self.bar_tile = None
        self.ops = []
        self.last_writer = {}
        self.readers = {}
        self.total_ops = 0

    def op(self, eng, fn, reads=(), writes=(), dma=False):
        deps = set()
        for r in reads:
            w = self.last_writer.get(r)
            if w is not None:
                deps.add(w)
        for w_ in writes:
            w = self.last_writer.get(w_)
            if w is not None:
                deps.add(w)
            for rd in self.readers.get(w_, ()):
                deps.add(rd)
        i = len(self.ops)
        deps.discard(i)
        self.ops.append(dict(eng=eng, fn=fn, deps=deps, dma=dma))
        for r in reads:
            self.readers.setdefault(r, []).append(i)
        for w_ in writes:
            self.last_writer[w_] = i
            self.readers[w_] = []
        return i

    def dma(self, q, fn, reads=(), writes=()):
        return self.op(q, fn, reads, writes, dma=True)

    def _wait(self, E, key, sem, val):
        ck = self.clock[E]
        if ck.get(key, 0) < val:
            self.engs[E].wait_ge(sem, val)
            ck[key] = val

    def _merge(self, E, clk):
        ck = self.clock[E]
        for k, v in clk.items():
            if ck.get(k, 0) < v:
                ck[k] = v

    def flush(self, barrier=True):
        ops = self.ops
        need_sig = [False] * len(ops)
        for i, o in enumerate(ops):
            for d in o["deps"]:
                od = ops[d]
                if od["dma"]:
                    continue
                if od["eng"] == "pe" and o["eng"] == "pe" and not o["dma"]:
                    continue
                need_sig[d] = True
        if barrier:
            last = {}
            for i, o in enumerate(ops):
                if not o["dma"]:
                    last[o["eng"]] = i
            for e, i in last.items():
                need_sig[i] = True
        for i, o in enumerate(ops):
            E = o["eng"]
            eng = self.engs[E]
            if self.bar_sig:
                self._wait(E, ("c", "dve"), self.csem["dve"], self.bar_sig)
                self._merge(E, self.bar_clock)
            for d in sorted(o["deps"]):
                od = ops[d]
                if od["dma"]:
                    q = od["eng"]
                    self._wait(E, ("d", q, od["dsem_idx"]), self.dsem[q][od["dsem_idx"]], od["dval"])
                else:
                    F = od["eng"]
                    if F == "pe" and E == "pe" and not o["dma"]:
                        continue
                    self._wait(E, ("c", F), self.csem[F], od["sig"])
                self._merge(E, od["clk"])
            if o["dma"]:
                n = self.n_dsem[E]
                j = self.dcount[E] % n
                prev = self.dcount[E] // n
                if prev > 0:
                    self._wait(E, ("d", E, j), self.dsem[E][j], 16 * prev)
                ins = o["fn"](eng)
                ins.then_inc(self.dsem[E][j], 16)
                o["dsem_idx"] = j
                o["dval"] = 16 * (prev + 1)
                self.dcount[E] += 1
                o["clk"] = dict(self.clock[E])
            else:
                ins = o["fn"](eng)
                if need_sig[i]:
                    self.ccount[E] += 1
                    ins.then_inc(self.csem[E], 1)
                    o["sig"] = self.ccount[E]
                else:
                    o["sig"] = None
                o["clk"] = dict(self.clock[E])
            o["fn"] = None
        self.total_ops += len(ops)
        if barrier:
            self._barrier()
        self.ops = []
        self.last_writer = {}
        self.readers = {}

    def _wait_all(self, E):
        for q, n in self.n_dsem.items():
            for j in range(n):
                uses = (self.dcount[q] - j + n - 1) // n if self.dcount[q] > j else 0
                if uses > 0:
                    self._wait(E, ("d", q, j), self.dsem[q][j], 16 * uses)
        for e in self.COMPUTE:
            if self.ccount[e] > 0:
                self._wait(E, ("c", e), self.csem[e], self.ccount[e])

    def _barrier(self):
        self._wait_all("dve")
        ins = self.engs["dve"].memset(self.bar_tile, 0.0)
        self.ccount["dve"] += 1
        ins.then_inc(self.csem["dve"], 1)
        self.bar_sig = self.ccount["dve"]
        self.bar_clock = dict(self.clock["dve"])

    def finish(self, eng="sp"):
        self.flush(barrier=True)
        self._wait_all(eng)
        self.es.close()


NA_VARIANTS = [(-2, True), (-1, False), (0, False), (1, False), (2, True),
               (-3, False), (-2, False), (2, False), (3, False)]


def na_chunks(i):
    if i in (0, 1, 14, 15):
        cs = range(0, 4) if i < 2 else range(12, 16)
        out = []
        for c in cs:
            d = c - i
            t = {(-3): 5, (-2): 6, (-1): 1, 0: 2, 1: 3, 2: 7, 3: 8}[d]
            out.append((c, t))
        return out
    return [(i + d, d + 2) for d in range(-2, 3)]


def build_bias_index():
    idx = np.full((128, 9, 128), 15 * 31, dtype=np.int64)
    for t, (d, partial) in enumerate(NA_VARIANTS):
        for j in range(2):
            for jq in range(2):
                dr = 2 * d + j - jq
                if abs(dr) > 7:
                    continue
                if partial:
                    if d == -2 and not (j >= jq):
                        continue
                    if d == 2 and not (j == 0 and jq == 1):
                        continue
                for cq in range(64):
                    cstart = min(max(cq - 8, 0), 48)
                    for ck in range(cstart, cstart + 16):
                        idx[j * 64 + ck, t, jq * 64 + cq] = (dr + 7) * 31 + (ck - cq + 15)
    return idx


_BIAS_IDX = None


import os as _os
_NOCONV = bool(_os.environ.get('NOCONV'))


class _Stop(Exception):
    pass


def build(debug=False, stop=None):
    nc = bass.Bass("TRN2", target_bir_lowering=False)
    try:
        return _build(nc, debug, stop)
    except _Stop:
        return nc


def _build(nc, debug, stop):

    def din(name, shape, dt=F32):
        return nc.dram_tensor(name, shape, dt, kind="ExternalInput").ap()

    x = din("x", [SEQ, D])
    ctx = din("ctx", [CTX, D])
    ccol = din("ccol", [128, 16])
    w_mod = din("w_mod", [D, 6 * D])
    b_mod = din("b_mod", [1, 6 * D])
    n1c = din("n1c", [128, 8])
    n2c = din("n2c", [128, 8])
    n2b = din("n2b", [128, D])
    nfb = din("nfb", [128, D])
    w_in = din("w_in", [D, 4096])
    w_out = din("w_out", [D, D])
    wq = din("wq", [D, 2048])
    keysT = din("keysT", [128, 2048])
    u_t = din("u", [16384, D])
    v_t = din("v", [16384, D])
    lbraw = din("lbraw", [128, 16])
    hgn = din("hgn", [128, 512])
    biasT = din("biasT", [128, 8 * 9 * 128])
    cst = din("cst", [128, 528])
    rmask_d = din("rmask", [128, TOK])
    out = nc.dram_tensor("out", [SEQ, D], F32, kind="ExternalOutput").ap()
    scr_bc = nc.dram_tensor("scr_bc", [4, 128, D], F32, kind="Internal").ap()
    uv_bf = nc.dram_tensor("uv_bf", [16384, 2 * D], BF16, kind="Internal").ap()
    dbg = {}
    if debug:
        dbg["hT"] = nc.dram_tensor("d_hT", [128, 8 * TOK], BF16, kind="ExternalOutput").ap()
        dbg["mixT"] = nc.dram_tensor("d_mixT", [128, 8 * SEQ], BF16, kind="ExternalOutput").ap()
        dbg["x1"] = nc.dram_tensor("d_x1", [128, NT * D], F32, kind="ExternalOutput").ap()
        dbg["mc"] = nc.dram_tensor("d_mc", [128, 48], F32, kind="ExternalOutput").ap()
        dbg["eidx"] = nc.dram_tensor("d_eidx", [128, NT * 128], I32, kind="ExternalOutput").ap()
        dbg["gw"] = nc.dram_tensor("d_gw", [128, NT * 128], F32, kind="ExternalOutput").ap()

    es = contextlib.ExitStack()

    def sb(name, shape, dt=F32, stack=None):
        return (stack or es).enter_context(nc.sbuf_tensor(name, shape, dt))

    bar = sb("bar", [128, 1])
    cA = sb("cA", [128, 528])
    identb = sb("identb", [128, 128], BF16)
    mc = sb("mc", [128, 48])
    lb = sb("lb", [128, 8])
    oml = sb("oml", [128, 8])
    ps = [es.enter_context(nc.psum_tensor(f"ps{j}", [128, 512], F32)) for j in range(6)]
    pT = [es.enter_context(nc.psum_tensor(f"pT{j}", [128, 1024], BF16)) for j in range(2)]

    S = Sched(nc)
    S.bar_tile = bar[:]

    def phase_end(name):
        S.flush()
        if stop == name:
            S.finish()
            raise _Stop()

    IDENT = cA[:, 0:128]
    TRIF = cA[:, 128:256]
    TRIB = cA[:, 256:384]
    ONES = cA[:, 384:512]
    IOTA16 = cA[:, 512:528]

    with contextlib.ExitStack() as p0:
        cc = sb("cc", [128, 16], stack=p0)
        scl = sb("scl", [128, 8, 33], stack=p0)
        wm = [sb(f"wm{j}", [128, 8, 512], stack=p0) for j in range(2)]
        bm = sb("bm", [33, 6 * D], stack=p0)
        modrow = sb("modrow", [33, 6 * D], stack=p0)
        mcol = sb("mcol", [128, 48], stack=p0)
        n1 = sb("n1", [128, 8], stack=p0)
        n2 = sb("n2", [128, 8], stack=p0)
        n2bt = sb("n2bt", [128, D], stack=p0)
        bct = [sb(f"bct{j}", [128, D], stack=p0) for j in range(2)]
        lbr = sb("lbr", [128, 16], stack=p0)

        S.dma("sp", lambda e: e.dma_start(out=cA[:], in_=cst), writes=["cA"])
        S.dma("sp", lambda e: e.dma_start(out=cc[:], in_=ccol), writes=["cc"])
        S.dma("sp", lambda e: e.dma_start(out=n1[:], in_=n1c), writes=["n1"])
        S.dma("sp", lambda e: e.dma_start(out=n2[:], in_=n2c), writes=["n2"])
        S.dma("sp", lambda e: e.dma_start(out=lbr[:], in_=lbraw), writes=["lbr"])
        S.dma("sp", lambda e: e.dma_start(out=n2bt[:], in_=n2b), writes=["n2bt"])
        S.op("dve", lambda e: e.memset(bm[:], 0.0), writes=["bm"])
        S.dma("sp", lambda e: e.dma_start(out=bm[0:1, :], in_=b_mod), reads=["bm"], writes=["bm0"])
        S.dma("sp", lambda e: e.dma_start(out=bm[32:33, :], in_=b_mod), reads=["bm"], writes=["bm32"])
        S.op("dve", lambda e: e.tensor_copy(out=identb[:], in_=IDENT), reads=["cA"], writes=["identb"])
        S.op("dve", lambda e: e.memset(scl[:], 0.0), writes=["scl"])
        ccv = cc[:].rearrange("p (k t) -> p k t", t=2)
        S.op("act", lambda e: e.activation(out=scl[:, :, 0:1], in_=ccv[:, :, 0:1], func=AF.Silu),
             reads=["cc", "scl"], writes=["scl"])
        S.op("act", lambda e: e.activation(out=scl[:, :, 32:33], in_=ccv[:, :, 1:2], func=AF.Silu),
             reads=["cc", "scl"], writes=["scl"])
        S.op("dve", lambda e: e.tensor_tensor(out=lb[:], in0=lbr[:, 0:8], in1=lbr[:, 8:16], op=ALU.subtract),
             reads=["lbr"], writes=["lb"])
        S.op("act", lambda e: e.activation(out=lb[:], in_=lb[:], func=AF.Sigmoid), reads=["lb"], writes=["lb"])
        S.op("dve", lambda e: e.tensor_scalar(out=oml[:], in0=lb[:], scalar1=-1.0, scalar2=1.0,
                                              op0=ALU.mult, op1=ALU.add), reads=["lb"], writes=["oml"])
        for n in range(12):
            wb = wm[n % 2]
            S.dma("sp" if n % 2 == 0 else "act",
                  lambda e, n=n, wb=wb: e.dma_start(
                      out=wb[:], in_=w_mod[:, n * 512:(n + 1) * 512].rearrange("(k p) n -> p k n", p=128)),
                  writes=[("wm", n % 2)])
            pb = ps[n % 2]
            for k in range(8):
                S.op("pe", lambda e, k=k, wb=wb, pb=pb: e.matmul(pb[0:33, :], scl[:, k, :], wb[:, k, :],
                                                                start=(k == 0), stop=(k == 7)),
                     reads=["scl", ("wm", n % 2)], writes=[("ps", n % 2)])
            S.op("dve", lambda e, n=n, pb=pb: e.tensor_tensor(out=modrow[:, n * 512:(n + 1) * 512], in0=pb[0:33, :],
                                                             in1=bm[:, n * 512:(n + 1) * 512], op=ALU.add),
                 reads=[("ps", n % 2), "bm", "bm0", "bm32"], writes=[("modrow", n)])
        mr_all = [("modrow", n) for n in range(12)]
        col_specs = [(0, 0), (0, 1), (0, 3), (0, 4), (32, 0), (32, 1)]
        for si, (r, vi) in enumerate(col_specs):
            for k in range(8):
                c0 = 2 * (si * 8 + k)
                S.op("pe", lambda e, r=r, vi=vi, k=k, c0=c0: e.matmul(
                    ps[2][:, c0:c0 + 2], modrow[r:r + 1, vi * D + k * 128: vi * D + (k + 1) * 128],
                    cA[r:r + 1, 384:386], start=True, stop=True),
                    reads=mr_all + ["cA"], writes=[("ps", 2)])
        S.op("dve", lambda e: e.tensor_copy(out=mcol[:].unsqueeze(2), in_=ps[2][:, 0:96].rearrange("p (c two) -> p c two", two=2)[:, :, 0:1]), reads=[("ps", 2)], writes=["mcol"])
        S.op("dve", lambda e: e.scalar_tensor_tensor(out=mc[:, 0:8], in0=mcol[:, 8:16], scalar=1.0, in1=n1[:],
                                                     op0=ALU.add, op1=ALU.mult), reads=["mcol", "n1"], writes=["mc0"])
        S.op("dve", lambda e: e.tensor_copy(out=mc[:, 8:16], in_=mcol[:, 0:8]), reads=["mcol"], writes=["mc1"])
        S.op("dve", lambda e: e.scalar_tensor_tensor(out=mc[:, 16:24], in0=mcol[:, 40:48], scalar=1.0, in1=n1[:],
                                                     op0=ALU.add, op1=ALU.mult), reads=["mcol", "n1"], writes=["mc2"])
        S.op("dve", lambda e: e.tensor_copy(out=mc[:, 24:32], in_=mcol[:, 32:40]), reads=["mcol"], writes=["mc3"])
        S.op("dve", lambda e: e.scalar_tensor_tensor(out=mc[:, 32:40], in0=mcol[:, 24:32], scalar=1.0, in1=n2[:],
                                                     op0=ALU.add, op1=ALU.mult), reads=["mcol", "n2"], writes=["mc4"])
        S.op("dve", lambda e: e.tensor_copy(out=mc[:, 40:48], in_=mcol[:, 16:24]), reads=["mcol"], writes=["mc5"])
        for j, (vi, kind) in enumerate([(2, "copy"), (5, "copy"), (4, "g2"), (3, "copy")]):
            bt_ = bct[j % 2]
            for hf in range(2):
                pb = ps[3 + hf]
                S.op("pe", lambda e, vi=vi, hf=hf, pb=pb: e.matmul(
                    pb[:, :], cA[0:1, 384:512], modrow[0:1, vi * D + hf * 512: vi * D + (hf + 1) * 512],
                    start=True, stop=True), reads=mr_all + ["cA"], writes=[("ps", 3 + hf)])
                if kind == "copy":
                    S.op("dve", lambda e, bt_=bt_, hf=hf, pb=pb: e.tensor_copy(out=bt_[:, hf * 512:(hf + 1) * 512], in_=pb[:, :]),
                         reads=[("ps", 3 + hf)], writes=[("bct", j % 2, hf)])
                else:
                    S.op("dve", lambda e, bt_=bt_, hf=hf, pb=pb: e.scalar_tensor_tensor(
                        out=bt_[:, hf * 512:(hf + 1) * 512], in0=pb[:, :], scalar=1.0,
                        in1=n2bt[:, hf * 512:(hf + 1) * 512], op0=ALU.add, op1=ALU.mult),
                        reads=[("ps", 3 + hf), "n2bt"], writes=[("bct", j % 2, hf)])
            S.dma("sp", lambda e, j=j, bt_=bt_: e.dma_start(out=scr_bc[j], in_=bt_[:]),
                  reads=[("bct", j % 2, 0), ("bct", j % 2, 1)], writes=[("scr", j)])
        if debug:
            S.dma("sp", lambda e: e.dma_start(out=dbg["mc"], in_=mc[:]), reads=[f"mc{j}" for j in range(6)])
        phase_end("p0")

    R = sb("R", [128, NT * D])
    x1 = R[:].rearrange("p (t d) -> p t d", d=D)
    hT = R[:, 0:9216].bitcast(BF16).rearrange("p (k t) -> p k t", k=8)
    vtok = R[:, 9216:13824].bitcast(BF16).rearrange("p (t d) -> p t d", d=512)
    with contextlib.ExitStack() as pm:
        mixT = sb("mixT", [128, 8, SEQ], BF16, stack=pm)

        with contextlib.ExitStack() as p1:
            xt = [sb(f"xt{j}", [128, D], stack=p1) for j in range(2)]
            xs = [sb(f"xs{j}", [128, D], BF16, stack=p1) for j in range(2)]
            junk = sb("junk1", [128, D], BF16, stack=p1)
            st = sb("st1", [128, 3 * NTT], stack=p1)
            for T in range(NTT):
                b = T % 2
                src = x[T * 128:(T + 1) * 128, :] if T < NT else ctx[(T - NT) * 128:(T - NT + 1) * 128, :]
                S.dma("sp" if b == 0 else "act", lambda e, b=b, src=src: e.dma_start(out=xt[b][:], in_=src),
                      writes=[("xt", b)])
                S.op("act", lambda e, b=b, T=T: e.activation(out=junk[:], in_=xt[b][:], func=AF.Square,
                                                             accum_out=st[:, T:T + 1]),
                     reads=[("xt", b)], writes=["junk", ("ssq", T)])
                S.op("dve", lambda e, T=T: e.tensor_scalar(out=st[:, NTT + T:NTT + T + 1], in0=st[:, T:T + 1],
                                                           scalar1=1.0 / D, scalar2=EPS, op0=ALU.mult, op1=ALU.add),
                     reads=[("ssq", T)], writes=[("ms", T)])
                S.op("act", lambda e, T=T: e.activation(out=st[:, NTT + T:NTT + T + 1], in_=st[:, NTT + T:NTT + T + 1],
                                                        func=AF.Sqrt), reads=[("ms", T)], writes=[("ms", T)])
                S.op("dve", lambda e, T=T: e.reciprocal(out=st[:, 2 * NTT + T:2 * NTT + T + 1],
                                                        in_=st[:, NTT + T:NTT + T + 1]),
                     reads=[("ms", T)], writes=[("rstd", T)])
                S.op("act", lambda e, b=b, T=T: e.activation(out=xs[b][:], in_=xt[b][:], func=AF.Copy,
                                                             scale=st[:, 2 * NTT + T:2 * NTT + T + 1]),
                     reads=[("xt", b), ("rstd", T)], writes=[("xs", b)])
                for k in range(8):
                    S.op("pe", lambda e, b=b, k=k: e.transpose(pT[b][:, k * 128:(k + 1) * 128],
                                                               xs[b][:, k * 128:(k + 1) * 128], identb[:]),
                         reads=[("xs", b), "identb"], writes=[("pT", b)])
                go, so = (0, 8) if T < NT else (16, 24)
                for k in range(8):
                    S.op("dve", lambda e, b=b, k=k, T=T, go=go, so=so: e.tensor_scalar(
                        out=hT[:, k, T * 128:(T + 1) * 128], in0=pT[b][:, k * 128:(k + 1) * 128],
                        scalar1=mc[:, go + k:go + k + 1], scalar2=mc[:, so + k:so + k + 1],
                        op0=ALU.mult, op1=ALU.add),
                        reads=[("pT", b)], writes=[("hT", T)])
            if debug:
                S.dma("sp", lambda e: e.dma_start(out=dbg["hT"], in_=R[:, 0:9216].bitcast(BF16)),
                      reads=[("hT", T) for T in range(NTT)])
            phase_end("p1")
        hT_all = [("hT", T) for T in range(NTT)]

        def load_w(tile_ap, dram_w, col0, ncols, key):
            for k0 in range(0, 8, 4):
                S.dma("pool", lambda e, k0=k0: e.dma_start(
                    out=tile_ap[:, k0:k0 + 4, :],
                    in_=dram_w[k0 * 128:(k0 + 4) * 128, col0:col0 + ncols].rearrange("(k p) n -> p k n", p=128)),
                    writes=[(key, k0)])
            return [(key, 0), (key, 4)]

        with contextlib.ExitStack() as p2:
            qT = sb("qT", [128, 4, SEQ], BF16, stack=p2)
            kT = sb("kT", [128, 4, TOK], BF16, stack=p2)
            vaug = sb("vaug", [128, NTT, 8, 65], BF16, stack=p2)
            p2a = contextlib.ExitStack()
            wna = sb("wna", [128, 8, 1536], BF16, stack=p2a)
            wk = load_w(wna, w_in, 0, 1536, "wna")
            S.op("pool", lambda e: e.memset(vaug[:, :, :, 64:65], 1.0), writes=["vones"])
            cnt = 0
            for which, dst, ntok, cbase in (("q", qT, SEQ, 0), ("k", kT, TOK, 512)):
                for hp in range(4):
                    for t0 in range(0, ntok, 512):
                        tw = min(512, ntok - t0)
                        pb = cnt % 4
                        for k in range(8):
                            S.op("pe", lambda e, k=k, hp=hp, t0=t0, tw=tw, pb=pb, cbase=cbase: e.matmul(
                                ps[pb][:, 0:tw], wna[:, k, cbase + hp * 128: cbase + (hp + 1) * 128],
                                hT[:, k, t0:t0 + tw], start=(k == 0), stop=(k == 7)),
                                reads=wk + hT_all, writes=[("ps", pb)])
                        eng = "act" if cnt % 2 == 0 else "dve"
                        if eng == "act":
                            S.op("act", lambda e, dst=dst, hp=hp, t0=t0, tw=tw, pb=pb: e.activation(
                                out=dst[:, hp, t0:t0 + tw], in_=ps[pb][:, 0:tw], func=AF.Copy),
                                reads=[("ps", pb)], writes=[(which, hp, t0)])
                        else:
                            S.op("dve", lambda e, dst=dst, hp=hp, t0=t0, tw=tw, pb=pb: e.tensor_copy(
                                out=dst[:, hp, t0:t0 + tw], in_=ps[pb][:, 0:tw]),
                                reads=[("ps", pb)], writes=[(which, hp, t0)])
                        cnt += 1
            for T in range(NTT):
                pb = cnt % 4
                for k in range(8):
                    S.op("pe", lambda e, k=k, T=T, pb=pb: e.matmul(
                        ps[pb][:, :], hT[:, k, T * 128:(T + 1) * 128], wna[:, k, 1024:1536],
                        start=(k == 0), stop=(k == 7)), reads=wk + hT_all, writes=[("ps", pb)])
                if cnt % 2 == 0:
                    S.op("act", lambda e, T=T, pb=pb: e.activation(
                        out=vaug[:, T, :, 0:64], in_=ps[pb][:, :].rearrange("p (h d) -> p h d", d=64), func=AF.Copy),
                        reads=[("ps", pb)], writes=[("v", T)])
                else:
                    S.op("dve", lambda e, T=T, pb=pb: e.tensor_copy(
                        out=vaug[:, T, :, 0:64], in_=ps[pb][:, :].rearrange("p (h d) -> p h d", d=64)),
                        reads=[("ps", pb)], writes=[("v", T)])
                cnt += 1
            phase_end("p2a")
            p2a.close()
            bt = sb("bt", [128, 8, 9, 128], stack=p2)
            Ssb = [sb(f"Ssb{j}", [128, 640], stack=p2) for j in range(2)]
            Pb = [sb(f"Pb{j}", [128, 896], BF16, stack=p2) for j in range(2)]
            rden = sb("rden", [128, 16], stack=p2)
            natok = [sb(f"natok{j}", [128, 512], BF16, stack=p2) for j in range(2)]
            S.dma("sp", lambda e: e.dma_start(out=bt[:].rearrange("p h t q -> p (h t q)"), in_=biasT), writes=["bt"])

            qk_all_r = []
            it = 0
            for i in range(NT):
                chunks = na_chunks(i)
                nw = len(chunks)
                nb = i % 2
                for h in range(8):
                    hp, po = h // 2, (h % 2) * 64
                    sbuf_i = it % 2
                    b0, b1 = ps[2 * sbuf_i], ps[2 * sbuf_i + 1]

                    def sloc(j):
                        return (b0, j * 128) if j < 4 else (b1, (j - 4) * 128)
                    for j, (c, t) in enumerate(chunks):
                        bk, co = sloc(j)
                        S.op("pe", lambda e, bk=bk, co=co, c=c, hp=hp, po=po, i=i: e.matmul(
                            bk[:, co:co + 128], kT[po:po + 64, hp, c * 128:(c + 1) * 128],
                            qT[po:po + 64, hp, i * 128:(i + 1) * 128], start=True, stop=True),
                            reads=[], writes=[("psS", sbuf_i, j // 4)])
                    for cc_ in range(2):
                        S.op("pe", lambda e, cc_=cc_, hp=hp, po=po, i=i, b1=b1: e.matmul(
                            b1[:, 128 + cc_ * 128: 256 + cc_ * 128],
                            kT[po:po + 64, hp, SEQ + cc_ * 128: SEQ + (cc_ + 1) * 128],
                            qT[po:po + 64, hp, i * 128:(i + 1) * 128], start=True, stop=True),
                            reads=[], writes=[("psS", sbuf_i, 1)])
                    for j, (c, t) in enumerate(chunks):
                        bk, co = sloc(j)
                        S.op("dve", lambda e, bk=bk, co=co, j=j, t=t, h=h, sbuf_i=sbuf_i: e.scalar_tensor_tensor(
                            out=Ssb[sbuf_i][:, j * 128:(j + 1) * 128], in0=bk[:, co:co + 128], scalar=0.125,
                            in1=bt[:, h, t, :], op0=ALU.mult, op1=ALU.add),
                            reads=[("psS", sbuf_i, j // 4), "bt"], writes=[("Ssb", sbuf_i)])
                    S.op("act", lambda e, nw=nw, sbuf_i=sbuf_i: e.activation(
                        out=Pb[sbuf_i][:, 0:nw * 128], in_=Ssb[sbuf_i][:, 0:nw * 128], func=AF.Exp),
                        reads=[("Ssb", sbuf_i)], writes=[("Pw", sbuf_i)])
                    S.op("act", lambda e, sbuf_i=sbuf_i, b1=b1: e.activation(
                        out=Pb[sbuf_i][:, 640:896], in_=b1[:, 128:384], func=AF.Exp, scale=0.125),
                        reads=[("psS", sbuf_i, 1)], writes=[("Pc", sbuf_i)])
                    ob = ps[4 + h // 4]
                    oc = (h % 4) * 128
                    nmm = nw + 2
                    for j, (c, t) in enumerate(chunks):
                        S.op("pe", lambda e, j=j, c=c, h=h, ob=ob, oc=oc, sbuf_i=sbuf_i, nmm=nmm: e.matmul(
                            ob[:, oc:oc + 65], Pb[sbuf_i][:, j * 128:(j + 1) * 128], vaug[:, c, h, :],
                            start=(j == 0), stop=False),
                            reads=[("Pw", sbuf_i)], writes=[("psO", h)])
                    for cc_ in range(2):
                        S.op("pe", lambda e, cc_=cc_, h=h, ob=ob, oc=oc, sbuf_i=sbuf_i: e.matmul(
                            ob[:, oc:oc + 65], Pb[sbuf_i][:, 640 + cc_ * 128: 768 + cc_ * 128], vaug[:, NT + cc_, h, :],
                            start=False, stop=(cc_ == 1)),
                            reads=[("Pc", sbuf_i)], writes=[("psO", h)])
                    S.op("dve", lambda e, h=h, ob=ob, oc=oc: e.reciprocal(out=rden[:, h:h + 1], in_=ob[:, oc + 64:oc + 65]),
                         reads=[("psO", h)], writes=[("rden", h)])
                    S.op("dve", lambda e, h=h, ob=ob, oc=oc, nb=nb: e.tensor_scalar(
                        out=natok[nb][:, h * 64:(h + 1) * 64], in0=ob[:, oc:oc + 64], scalar1=rden[:, h:h + 1],
                        scalar2=None, op0=ALU.mult),
                        reads=[("psO", h), ("rden", h)], writes=[("natok", nb)])
                    it += 1
                for j in range(4):
                    S.op("pe", lambda e, j=j, nb=nb: e.transpose(pT[nb][:, j * 128:(j + 1) * 128],
                                                                natok[nb][:, j * 128:(j + 1) * 128], identb[:]),
                         reads=[("natok", nb)], writes=[("pT", nb)])
                S.op("act", lambda e, i=i, nb=nb: e.activation(
                    out=mixT[:, 0:4, i * 128:(i + 1) * 128],
                    in_=pT[nb][:, 0:512].rearrange("p (j t) -> p j t", t=128), func=AF.Copy),
                    reads=[("pT", nb)], writes=[("mixna", i)])
            phase_end("p2")

        with contextlib.ExitStack() as p3:
            oacc = sb("oacc", [128, NT, 512], stack=p3)
            with contextlib.ExitStack() as p3a:
                wv = sb("wv", [128, 8, 512], BF16, stack=p3a)
                wk = load_w(wv, w_in, 3 * 512 + 3 * 512, 512, "wv")
                for T in range(NTT):
                    pb = T % 4
                    for k in range(8):
                        S.op("pe", lambda e, k=k, T=T, pb=pb: e.matmul(
                            ps[pb][:, :], hT[:, k, T * 128:(T + 1) * 128], wv[:, k, :],
                            start=(k == 0), stop=(k == 7)), reads=wk, writes=[("ps", pb)])
                    if T % 2 == 0:
                        S.op("act", lambda e, T=T, pb=pb: e.activation(out=vtok[:, T, :], in_=ps[pb][:, :], func=AF.Copy),
                             reads=[("ps", pb)], writes=[("vtok", T)])
                    else:
                        S.op("dve", lambda e, T=T, pb=pb: e.tensor_copy(out=vtok[:, T, :], in_=ps[pb][:, :]),
                             reads=[("ps", pb)], writes=[("vtok", T)])
                phase_end("p3a")
            with contextlib.ExitStack() as p3b:
                rmask = sb("rmask_sb", [128, TOK], stack=p3b)
                A_ = sb("hgA", [128, TOK], stack=p3b)
                B_ = sb("hgB", [128, TOK], stack=p3b)
                C_ = sb("hgC", [128, TOK], stack=p3b)
                qsb = sb("hgqs", [128, 512], stack=p3b)
                Qt = sb("hgQt", [128, SEQ], BF16, stack=p3b)
                Qs = sb("hgQs", [128, SEQ], BF16, stack=p3b)
                Ks = sb("hgKs", [128, TOK], BF16, stack=p3b)
                Kf = sb("hgKf", [128, TOK], BF16, stack=p3b)
                Ktok = sb("hgKtok", [128, NTT, 128], BF16, stack=p3b)
                wqf = sb("hgwqf", [128, 8, 256], BF16, stack=p3b)
                sc_ = sb("hgsc", [128, 5, NCH], stack=p3b)
                Sst = [sb(f"hgS{j}", [128, 128], stack=p3b) for j in range(2)]
                Usc = [sb(f"hgUsc{j}", [128, 128], stack=p3b) for j in range(4)]
                Smb = [sb(f"hgSmb{j}", [128, 128], BF16, stack=p3b) for j in range(2)]
                Qe = sb("hgQe", [128, SEQ], BF16, stack=p3b)
                Qo = sb("hgQo", [128, SEQ], BF16, stack=p3b)
                ATs = [sb(f"hgATs{j}", [128, 128], BF16, stack=p3b) for j in range(2)]

                S.dma("sp", lambda e: e.dma_start(out=rmask[:], in_=rmask_d), writes=["rmask"])

                def v3(t, n=TOK):
                    return t[:, 0:n].rearrange("p (t s) -> p t s", s=64)

                for dr in range(2):
                    for hh in range(4):
                        dh = dr * 4 + hh
                        S.dma("pool", lambda e, hh=hh: e.dma_start(
                            out=wqf[:, :, 0:128],
                            in_=w_in[:, 1536 + hh * 128:1536 + (hh + 1) * 128].rearrange("(k p) n -> p k n", p=128)),
                            writes=["wq_h"])
                        S.dma("pool", lambda e, hh=hh, dr=dr: e.dma_start(
                            out=wqf[:, :, 128:256],
                            in_=w_in[:, 2048 + dr * 512 + hh * 128:2048 + dr * 512 + (hh + 1) * 128].rearrange(
                                "(k p) n -> p k n", p=128)), writes=["wf_h"])
                        for ci, t0 in enumerate(range(0, TOK, 512)):
                            tw = min(512, TOK - t0)
                            pb = ci % 4
                            for k in range(8):
                                S.op("pe", lambda e, k=k, t0=t0, tw=tw, pb=pb: e.matmul(
                                    ps[pb][:, 0:tw], wqf[:, k, 128:256], hT[:, k, t0:t0 + tw],
                                    start=(k == 0), stop=(k == 7)), reads=["wf_h"], writes=[("ps", pb)])
                            S.op("act", lambda e, t0=t0, tw=tw, pb=pb: e.activation(
                                out=A_[:, t0:t0 + tw], in_=ps[pb][:, 0:tw], func=AF.Sigmoid),
                                reads=[("ps", pb)], writes=["A"])
                        S.op("dve", lambda e, dh=dh: e.tensor_scalar(out=A_[:], in0=A_[:], scalar1=oml[:, dh:dh + 1],
                                                                     scalar2=lb[:, dh:dh + 1], op0=ALU.mult, op1=ALU.add),
                             reads=["A"], writes=["A"])
                        S.op("act", lambda e: e.activation(out=B_[:], in_=A_[:], func=AF.Ln), reads=["A"], writes=["B"])
                        S.op("dve", lambda e: e.tensor_scalar(out=A_[:], in0=A_[:], scalar1=-1.0, scalar2=1.0,
                                                              op0=ALU.mult, op1=ALU.add), reads=["A", "B"], writes=["A"])
                        S.op("dve", lambda e: e.tensor_tensor_scan(out=C_[:], data0=rmask[:], data1=B_[:], initial=0.0,
                                                                   op0=ALU.mult, op1=ALU.add),
                             reads=["B", "rmask"], writes=["C"])
                        if dr == 0:
                            gbuf, gkey = C_, "C"
                            refpos = 31
                        else:
                            S.op("dve", lambda e: e.tensor_tensor(out=B_[:], in0=B_[:], in1=C_[:], op=ALU.subtract),
                                 reads=["B", "C"], writes=["B"])
                            S.op("dve", lambda e: e.tensor_tensor(
                                out=v3(B_), in0=v3(B_), in1=v3(C_)[:, :, 63:64].to_broadcast([128, NCH, 64]), op=ALU.add),
                                reads=["B", "C"], writes=["B"])
                            gbuf, gkey = B_, "B"
                            refpos = 32
                        endpos = 63 if dr == 0 else 0
                        S.op("dve", lambda e, gbuf=gbuf, refpos=refpos: e.tensor_copy(
                            out=sc_[:, 0, :].unsqueeze(2), in_=v3(gbuf)[:, :, refpos:refpos + 1]), reads=[gkey], writes=["sc0"])
                        S.op("dve", lambda e, gbuf=gbuf, endpos=endpos: e.tensor_copy(
                            out=sc_[:, 1, :].unsqueeze(2), in_=v3(gbuf)[:, :, endpos:endpos + 1]), reads=[gkey], writes=["sc1"])
                        S.op("act", lambda e: e.activation(out=sc_[:, 2:4, :], in_=sc_[:, 0:2, :], func=AF.Exp),
                             reads=["sc0", "sc1"], writes=["sc23"])
                        S.op("dve", lambda e: e.tensor_tensor(out=sc_[:, 4, :], in0=sc_[:, 1, :], in1=sc_[:, 0, :],
                                                              op=ALU.subtract), reads=["sc0", "sc1"], writes=["sc4"])
                        S.op("act", lambda e: e.activation(out=sc_[:, 4, :], in_=sc_[:, 4, :], func=AF.Exp),
                             reads=["sc4"], writes=["sc4"])
                        obuf, okey = (B_, "B") if dr == 0 else (C_, "C")
                        S.op("dve", lambda e, gbuf=gbuf: e.tensor_tensor(
                            out=v3(gbuf), in0=v3(gbuf), in1=sc_[:, 0, :].unsqueeze(2).to_broadcast([128, NCH, 64]),
                            op=ALU.subtract), reads=[gkey, "sc0", "sc1"], writes=[gkey])
                        S.op("act", lambda e, gbuf=gbuf, obuf=obuf: e.activation(out=obuf[:, 0:SEQ], in_=gbuf[:, 0:SEQ], func=AF.Exp),
                             reads=[gkey, okey], writes=[okey])
                        S.op("act", lambda e, gbuf=gbuf: e.activation(out=gbuf[:], in_=gbuf[:], func=AF.Exp, scale=-1.0),
                             reads=[gkey, okey], writes=[gkey])
                        S.op("dve", lambda e, gbuf=gbuf: e.tensor_tensor(out=Kf[:], in0=A_[:], in1=gbuf[:], op=ALU.mult),
                             reads=["A", gkey], writes=["Kf"])
                        hk = 1 if dr == 0 else 0
                        hq = 1 - hk
                        def h32(t):
                            return t[:].rearrange("p (t two s) -> p t two s", two=2, s=32)

                        def h64(t):
                            return t[:].rearrange("p (t two s) -> p t two s", two=2, s=64)
                        if hh == 0:
                            S.op("pool", lambda e: e.memset(Ks[:], 0.0), reads=["Ks"], writes=["Ks"])
                            S.op("pool", lambda e: e.memset(Qs[:], 0.0), reads=["Qs"], writes=["Qs"])
                            if dr == 0:
                                S.op("pool", lambda e: e.memset(Qe[:], 0.0), reads=["Qe"], writes=["Qe"])
                                S.op("pool", lambda e: e.memset(Qo[:], 0.0), reads=["Qo"], writes=["Qo"])
                        S.op("dve", lambda e, hk=hk: e.tensor_copy(out=h32(Ks)[:, :, 1 - hk, :], in_=h32(Kf)[:, :, 1 - hk, :]),
                             reads=["Kf", "Ks"], writes=["Ks"])
                        for ci, t0 in enumerate(range(0, SEQ, 512)):
                            pb = ci % 4
                            for k in range(8):
                                S.op("pe", lambda e, k=k, t0=t0, pb=pb: e.matmul(
                                    ps[pb][:, :], wqf[:, k, 0:128], hT[:, k, t0:t0 + 512],
                                    start=(k == 0), stop=(k == 7)), reads=["wq_h"], writes=[("ps", pb)])
                            S.op("act", lambda e, pb=pb: e.activation(out=qsb[:], in_=ps[pb][:, :], func=AF.Silu),
                                 reads=[("ps", pb)], writes=["qsb"])
                            S.op("dve", lambda e, t0=t0, obuf=obuf: e.tensor_tensor(out=Qt[:, t0:t0 + 512], in0=qsb[:],
                                                                                    in1=obuf[:, t0:t0 + 512], op=ALU.mult),
                                 reads=["qsb", okey], writes=["Qt"])
                        S.op("dve", lambda e, hq=hq: e.tensor_copy(out=h32(Qs)[:, :, 1 - hq, :], in_=h32(Qt)[:, :, 1 - hq, :]),
                             reads=["Qt", "Qs"], writes=["Qs"])
                        S.op("act", lambda e: e.activation(out=h64(Qe)[:, :, 0, :], in_=h64(Qt)[:, :, 0, :], func=AF.Copy),
                             reads=["Qt", "Qe"], writes=["Qe"])
                        S.op("act", lambda e: e.activation(out=h64(Qo)[:, :, 1, :], in_=h64(Qt)[:, :, 1, :], func=AF.Copy),
                             reads=["Qt", "Qo"], writes=["Qo"])
                        for T in range(NTT):
                            nb = T % 2
                            S.op("pe", lambda e, T=T, nb=nb: e.transpose(pT[nb][:, 0:128], Kf[:, T * 128:(T + 1) * 128], identb[:]),
                                 reads=["Kf"], writes=[("pT", nb)])
                            S.op("act", lambda e, T=T, nb=nb: e.activation(out=Ktok[:, T, :], in_=pT[nb][:, 0:128], func=AF.Copy),
                                 reads=[("pT", nb)], writes=[("Ktok", T)])
                        S.op("pool", lambda e, hk=hk: e.memset(
                            Kf[:].rearrange("p (t two s) -> p t two s", two=2, s=32)[:, :, 1 - hk, :], 0.0),
                            reads=["Kf"], writes=["Kf"])
                        order = [16, 17] + list(range(NT)) if dr == 0 else [17, 16] + list(range(NT - 1, -1, -1))
                        tri = TRIF if dr == 0 else TRIB
                        kcnt = [0]
                        S.op("dve", lambda e: e.memset(Sst[0][:], 0.0), reads=[("S", 0)], writes=[("S", 0)])

                        def chunks_of(T):
                            return [2 * T, 2 * T + 1] if dr == 0 else [2 * T + 1, 2 * T]

                        def stage_a(n):
                            T = order[n]
                            q = n % 2
                            for ci, c in enumerate(chunks_of(T)):
                                par = c % 2
                                S.op("pe", lambda e, T=T, par=par, ci=ci, hh=hh: e.matmul(
                                    ps[2 + ci][:, 0:128], Ktok[par * 64:(par + 1) * 64, T, :],
                                    vtok[par * 64:(par + 1) * 64, T, hh * 128:(hh + 1) * 128], start=True, stop=True),
                                    reads=[("Ktok", T)], writes=[("ps", 2 + ci)])
                                S.op("act", lambda e, c=c, ci=ci, q=q: e.activation(
                                    out=Usc[2 * q + ci][:], in_=ps[2 + ci][:, 0:128], func=AF.Copy, scale=sc_[:, 4, c:c + 1]),
                                    reads=[("ps", 2 + ci), "sc4"], writes=[("Usc", 2 * q + ci)])
                            if T < NT:
                                S.op("pe", lambda e, T=T: e.matmul(ps[4][:, 0:128], Ks[:, T * 128:(T + 1) * 128],
                                                                   Qt[:, T * 128:(T + 1) * 128], start=True, stop=False),
                                     reads=["Ks", "Qt"], writes=[("ps", 4)])
                                S.op("pe", lambda e, T=T: e.matmul(ps[4][:, 0:128], Kf[:, T * 128:(T + 1) * 128],
                                                                   Qs[:, T * 128:(T + 1) * 128], start=False, stop=True),
                                     reads=["Kf", "Qs"], writes=[("ps", 4)])

                        def stage_a2(n):
                            T = order[n]
                            q = n % 2
                            if T < NT:
                                S.op("dve", lambda e, q=q, tri=tri: e.tensor_tensor(out=ATs[q][:], in0=ps[4][:, 0:128], in1=tri, op=ALU.mult),
                                     reads=[("ps", 4), "cA"], writes=[("ATs", q)])
                                ob = ps[5] if q == 0 else ps[1]
                                S.op("pe", lambda e, T=T, q=q, ob=ob, hh=hh: e.matmul(ob[:, 0:128], ATs[q][:], vtok[:, T, hh * 128:(hh + 1) * 128],
                                                                               start=True, stop=False),
                                     reads=[("ATs", q)], writes=[("ps", 5 if q == 0 else 1)])

                        def stage_b(n):
                            T = order[n]
                            q = n % 2
                            lat = T < NT
                            ob = ps[5] if q == 0 else ps[1]
                            for ci, c in enumerate(chunks_of(T)):
                                par = c % 2
                                k = kcnt[0]
                                kcnt[0] += 1
                                si, so = k % 2, (k + 1) % 2
                                if lat:
                                    S.op("act", lambda e, c=c, ci=ci, si=si: e.activation(
                                        out=Smb[ci][:], in_=Sst[si][:], func=AF.Copy, scale=sc_[:, 2, c:c + 1]),
                                        reads=[("S", si), "sc23"], writes=[("Smb", ci)])
                                    Qz, qzk = (Qe, "Qe") if par == 0 else (Qo, "Qo")
                                    S.op("pe", lambda e, T=T, ci=ci, Qz=Qz, ob=ob: e.matmul(
                                        ob[:, 0:128], Qz[:, T * 128:(T + 1) * 128], Smb[ci][:], start=False, stop=(ci == 1)),
                                        reads=[("Smb", ci), qzk], writes=[("ps", 5 if q == 0 else 1)])
                                S.op("dve", lambda e, c=c, ci=ci, q=q, si=si, so=so: e.scalar_tensor_tensor(
                                    out=Sst[so][:], in0=Sst[si][:], scalar=sc_[:, 3, c:c + 1], in1=Usc[2 * q + ci][:],
                                    op0=ALU.mult, op1=ALU.add), reads=[("Usc", 2 * q + ci), ("S", si), "sc23"], writes=[("S", so)])
                            if lat:
                                if dr == 0:
                                    S.op("act", lambda e, T=T, ob=ob, hh=hh: e.activation(
                                        out=oacc[:, T, hh * 128:(hh + 1) * 128], in_=ob[:, 0:128], func=AF.Copy),
                                        reads=[("ps", 5 if q == 0 else 1)], writes=[("oacc", T, hh)])
                                else:
                                    S.op("dve", lambda e, T=T, ob=ob, hh=hh: e.tensor_tensor(
                                        out=oacc[:, T, hh * 128:(hh + 1) * 128], in0=ob[:, 0:128],
                                        in1=oacc[:, T, hh * 128:(hh + 1) * 128], op=ALU.add),
                                        reads=[("ps", 5 if q == 0 else 1), ("oacc", T, hh)], writes=[("oacc", T, hh)])

                        stage_a(0)
                        stage_a2(0)
                        for n in range(len(order)):
                            if n + 1 < len(order):
                                stage_a(n + 1)
                            stage_b(n)
                            if n + 1 < len(order):
                                stage_a2(n + 1)
                        if kcnt[0] % 2 == 1:
                            pass
                phase_end("p3b")
            with contextlib.ExitStack() as p3c:
                wg = sb("wg", [128, 8, 512], BF16, stack=p3c)
                hgnb = sb("hgnb", [128, 512], stack=p3c)
                sg = [sb(f"sg{j}", [128, 512], stack=p3c) for j in range(2)]
                yb = [sb(f"yb{j}", [128, 512], stack=p3c) for j in range(2)]
                yt = [sb(f"yt{j}", [128, 512], BF16, stack=p3c) for j in range(2)]
                jk = sb("jk3", [128, 128], BF16, stack=p3c)
                st3 = sb("st3", [128, NT, 8], stack=p3c)
                wk = load_w(wg, w_in, 1536 + 4 * 512, 512, "wg")
                S.dma("sp", lambda e: e.dma_start(out=hgnb[:], in_=hgn), writes=["hgnb"])
                for T in range(NT):
                    b = T % 2
                    for k in range(8):
                        S.op("pe", lambda e, k=k, T=T, b=b: e.matmul(
                            ps[b][:, :], hT[:, k, T * 128:(T + 1) * 128], wg[:, k, :],
                            start=(k == 0), stop=(k == 7)), reads=wk, writes=[("ps", b)])
                    S.op("act", lambda e, b=b: e.activation(out=sg[b][:], in_=ps[b][:, :], func=AF.Silu),
                         reads=[("ps", b)], writes=[("sg", b)])
                    for hh in range(4):
                        S.op("act", lambda e, T=T, hh=hh: e.activation(
                            out=jk[:], in_=oacc[:, T, hh * 128:(hh + 1) * 128], func=AF.Square,
                            accum_out=st3[:, T, hh:hh + 1]), reads=[], writes=["jk3", ("ss3", T)])
                    S.op("dve", lambda e, T=T: e.tensor_scalar(out=st3[:, T, 4:8], in0=st3[:, T, 0:4], scalar1=1.0 / 128,
                                                               scalar2=EPS, op0=ALU.mult, op1=ALU.add),
                         reads=[("ss3", T)], writes=[("ms3", T)])
                    S.op("act", lambda e, T=T: e.activation(out=st3[:, T, 4:8], in_=st3[:, T, 4:8], func=AF.Sqrt),
                         reads=[("ms3", T)], writes=[("ms3", T)])
                    S.op("dve", lambda e, T=T: e.reciprocal(out=st3[:, T, 0:4], in_=st3[:, T, 4:8]),
                         reads=[("ms3", T)], writes=[("rs3", T)])
                    S.op("dve", lambda e, T=T, b=b: e.tensor_tensor(
                        out=yb[b][:].rearrange("p (h d) -> p h d", d=128),
                        in0=oacc[:, T, :].rearrange("p (h d) -> p h d", d=128),
                        in1=st3[:, T, 0:4].unsqueeze(2).to_broadcast([128, 4, 128]), op=ALU.mult),
                        reads=[("rs3", T)], writes=[("yb", b)])
                    S.op("dve", lambda e, b=b: e.tensor_tensor(out=yb[b][:], in0=yb[b][:], in1=hgnb[:], op=ALU.mult),
                         reads=[("yb", b), "hgnb"], writes=[("yb", b)])
                    S.op("dve", lambda e, b=b: e.tensor_tensor(out=yt[b][:], in0=yb[b][:], in1=sg[b][:], op=ALU.mult),
                         reads=[("yb", b), ("sg", b)], writes=[("yt", b)])
                    for j in range(4):
                        S.op("pe", lambda e, j=j, b=b: e.transpose(pT[b][:, j * 128:(j + 1) * 128],
                                                                   yt[b][:, j * 128:(j + 1) * 128], identb[:]),
                             reads=[("yt", b)], writes=[("pT", b)])
                    S.op("act", lambda e, T=T, b=b: e.activation(
                        out=mixT[:, 4:8, T * 128:(T + 1) * 128],
                        in_=pT[b][:, 0:512].rearrange("p (j t) -> p j t", t=128), func=AF.Copy),
                        reads=[("pT", b)], writes=[("mixhg", T)])
                if debug:
                    S.dma("sp", lambda e: e.dma_start(out=dbg["mixT"], in_=mixT[:].rearrange("p k t -> p (k t)")),
                          reads=[("mixhg", T) for T in range(NT)])
                phase_end("p3")

        with contextlib.ExitStack() as p4:
            wo32 = sb("wo32", [128, 8, D], stack=p4)
            wob = sb("wob", [128, 8, D], BF16, stack=p4)
            g1b = sb("g1b", [128, D], stack=p4)
            S.dma("sp", lambda e: e.dma_start(out=g1b[:], in_=scr_bc[0]), writes=["g1b"])
            for k in range(8):
                S.dma("sp" if k % 2 == 0 else "act", lambda e, k=k: e.dma_start(
                    out=wo32[:, k, :], in_=w_out[k * 128:(k + 1) * 128, :]), writes=[("wo32", k)])
                S.op("dve" if k % 2 == 0 else "pool", lambda e, k=k: e.tensor_tensor(
                    out=wob[:, k, :], in0=wo32[:, k, :], in1=g1b[:], op=ALU.mult),
                    reads=[("wo32", k), "g1b"], writes=[("wob", k)])
            wob_all = [("wob", k) for k in range(8)]
            for T in range(NT):
                S.dma("sp" if T % 2 == 0 else "act", lambda e, T=T: e.dma_start(
                    out=x1[:, T, :], in_=x[T * 128:(T + 1) * 128, :]), writes=[("x1", T)])
                for hf in range(2):
                    pb = (2 * T + hf) % 4
                    for k in range(8):
                        S.op("pe", lambda e, k=k, T=T, hf=hf, pb=pb: e.matmul(
                            ps[pb][:, :], mixT[:, k, T * 128:(T + 1) * 128], wob[:, k, hf * 512:(hf + 1) * 512],
                            start=(k == 0), stop=(k == 7)), reads=wob_all, writes=[("ps", pb)])
                    S.op("dve", lambda e, T=T, hf=hf, pb=pb: e.tensor_tensor(
                        out=x1[:, T, hf * 512:(hf + 1) * 512], in0=ps[pb][:, :], in1=x1[:, T, hf * 512:(hf + 1) * 512],
                        op=ALU.add), reads=[("ps", pb), ("x1", T)], writes=[("x1", T)])
            if debug:
                S.dma("sp", lambda e: e.dma_start(out=dbg["x1"], in_=R[:]),
                      reads=[("x1", T) for T in range(NT)])
            phase_end("p4")

    eidx = sb("eidx", [128, NT, 128], I32)
    gw = sb("gw", [128, NT, 128])
    rs2 = sb("rs2", [128, NT])
    with contextlib.ExitStack() as p5:
        wqb = sb("wqb", [128, 8, 2048], BF16, stack=p5)
        kTb = sb("kTb", [128, 16, 128], BF16, stack=p5)
        junk = sb("junk5", [128, D], BF16, stack=p5)
        xs = [sb(f"xs5{j}", [128, D], BF16, stack=p5) for j in range(2)]
        h2T = [sb(f"h2T{j}", [128, 8, 128], BF16, stack=p5) for j in range(2)]
        qTp = [sb(f"qTp{j}", [128, 16, 128], BF16, stack=p5) for j in range(2)]
        ssb = sb("ssb", [128, 16, 128], stack=p5)
        s2 = sb("s2", [128, 16, 128], stack=p5)
        top = sb("top", [128, 16, 16], stack=p5)
        itop = sb("itop", [128, 16, 16], U32, stack=p5)
        itf = sb("itf", [128, 16, 16], stack=p5)
        cand = sb("cand", [128, 8, 256], stack=p5)
        cand2 = sb("cand2", [128, 8, 256], stack=p5)
        ctop = sb("ctop", [128, 8, 16], stack=p5)
        cpos = sb("cpos", [128, 8, 16], U32, stack=p5)
        paf = sb("paf", [128, 128], stack=p5)
        pai = sb("pai", [128, 128], I32, stack=p5)
        pbf = sb("pbf", [128, 128], stack=p5)
        oh = sb("oh", [128, 128, 16], stack=p5)
        selA = sb("selA", [128, 128], stack=p5)
        selB = sb("selB", [128, 128], stack=p5)
        ef = sb("ef", [128, 128], stack=p5)
        ee = sb("ee", [128, 8, 16], stack=p5)
        zz = sb("zz", [128, 16], stack=p5)
        st5 = sb("st5", [128, 2 * NT], stack=p5)

        stg = [sb(f"stg{j}", [128, 4, D], BF16, stack=p5) for j in range(2)]
        conv_steps = [(tab, c) for tab in range(2) for c in range(32)]

        def emit_conv(si):
            tab, c = conv_steps[si]
            src = (u_t, v_t)[tab].rearrange("(p j c) d -> c p j d", p=128, j=4, c=32)[c]
            dst = uv_bf.rearrange("(p j c) d -> c p j d", p=128, j=4, c=32)[c][:, :, tab * D:(tab + 1) * D]
            b = si % 2
            S.dma("pool", lambda e: e.dma_start(out=stg[b][:], in_=src), writes=[("stg", b)])
            S.dma("sp", lambda e: e.dma_start(out=dst, in_=stg[b][:]), reads=[("stg", b)], writes=[("tab", tab, c)])

        wkq = load_w(wqb[:, :, 0:1024], wq, 0, 1024, "wqa") + load_w(wqb[:, :, 1024:2048], wq, 1024, 1024, "wqb")
        S.dma("pool", lambda e: e.dma_start(out=kTb[:].rearrange("p c k -> p (c k)"), in_=keysT), writes=["kTb"])
        for T in range(NT):
            b = T % 2
            if not _NOCONV:
                for q_ in range(4):
                    emit_conv(4 * T + q_)
            S.op("act", lambda e, T=T: e.activation(out=junk[:], in_=x1[:, T, :], func=AF.Square,
                                                    accum_out=st5[:, T:T + 1]), reads=[], writes=["junk5", ("ssq5", T)])
            S.op("dve", lambda e, T=T: e.tensor_scalar(out=st5[:, NT + T:NT + T + 1], in0=st5[:, T:T + 1],
                                                       scalar1=1.0 / D, scalar2=EPS, op0=ALU.mult, op1=ALU.add),
                 reads=[("ssq5", T)], writes=[("ms5", T)])
            S.op("act", lambda e, T=T: e.activation(out=st5[:, NT + T:NT + T + 1], in_=st5[:, NT + T:NT + T + 1],
                                                    func=AF.Sqrt), reads=[("ms5", T)], writes=[("ms5", T)])
            S.op("dve", lambda e, T=T: e.reciprocal(out=rs2[:, T:T + 1], in_=st5[:, NT + T:NT + T + 1]),
                 reads=[("ms5", T)], writes=[("rs2", T)])
            S.op("act", lambda e, b=b, T=T: e.activation(out=xs[b][:], in_=x1[:, T, :], func=AF.Copy,
                                                         scale=rs2[:, T:T + 1]), reads=[("rs2", T)], writes=[("xs5", b)])
            for k in range(8):
                S.op("pe", lambda e, b=b, k=k: e.transpose(pT[b][:, k * 128:(k + 1) * 128],
                                                           xs[b][:, k * 128:(k + 1) * 128], identb[:]),
                     reads=[("xs5", b)], writes=[("pT", b)])
            for k in range(8):
                S.op("dve" if k % 2 == 0 else "act", (lambda e, b=b, k=k: e.tensor_scalar(
                    out=h2T[b][:, k, :], in0=pT[b][:, k * 128:(k + 1) * 128],
                    scalar1=mc[:, 32 + k:33 + k], scalar2=mc[:, 40 + k:41 + k], op0=ALU.mult, op1=ALU.add))
                    if k % 2 == 0 else (lambda e, b=b, k=k: e.activation(
                        out=h2T[b][:, k, :], in_=pT[b][:, k * 128:(k + 1) * 128], func=AF.Identity,
                        scale=mc[:, 32 + k:33 + k], bias=mc[:, 40 + k:41 + k])),
                    reads=[("pT", b)], writes=[("h2T", b)])
            for g4 in range(4):
                pb = g4
                for j in range(4):
                    pc = g4 * 4 + j
                    for k in range(8):
                        S.op("pe", lambda e, b=b, k=k, pc=pc, j=j, pb=pb: e.matmul(
                            ps[pb][:, j * 128:(j + 1) * 128], wqb[:, k, pc * 128:(pc + 1) * 128], h2T[b][:, k, :],
                            start=(k == 0), stop=(k == 7)), reads=wkq + [("h2T", b)], writes=[("ps", pb)])
                if g4 % 2 == 0:
                    S.op("act", lambda e, b=b, g4=g4, pb=pb: e.activation(
                        out=qTp[b][:, g4 * 4:(g4 + 1) * 4, :], in_=ps[pb][:, :].rearrange("p (j t) -> p j t", t=128),
                        func=AF.Copy), reads=[("ps", pb)], writes=[("qTp", b, g4)])
                else:
                    S.op("dve", lambda e, b=b, g4=g4, pb=pb: e.tensor_copy(
                        out=qTp[b][:, g4 * 4:(g4 + 1) * 4, :], in_=ps[pb][:, :].rearrange("p (j t) -> p j t", t=128)),
                        reads=[("ps", pb)], writes=[("qTp", b, g4)])
            for g4 in range(4):
                pb = 4 + (g4 % 2)
                for j in range(4):
                    pc = g4 * 4 + j
                    S.op("pe", lambda e, b=b, pc=pc, j=j, pb=pb: e.matmul(
                        ps[pb][:, j * 128:(j + 1) * 128], qTp[b][:, pc, :], kTb[:, pc, :], start=True, stop=True),
                        reads=[("qTp", b, g4), "kTb"], writes=[("ps", pb)])
                S.op("act", lambda e, g4=g4, pb=pb: e.activation(
                    out=ssb[:, g4 * 4:(g4 + 1) * 4, :], in_=ps[pb][:, :].rearrange("p (j t) -> p j t", t=128),
                    func=AF.Copy), reads=[("ps", pb)], writes=[("ssb", g4)])
            for pc in range(16):
                S.op("dve", lambda e, pc=pc: e.max(out=top[:, pc, 0:8], in_=ssb[:, pc, :]),
                     reads=[("ssb", pc // 4)], writes=[("top", pc, 0)])
            for pc in range(16):
                S.op("dve", lambda e, pc=pc: e.match_replace(out=s2[:, pc, :], in_to_replace=top[:, pc, 0:8],
                                                             in_values=ssb[:, pc, :], imm_value=NEG),
                     reads=[("ssb", pc // 4), ("top", pc, 0)], writes=[("s2", pc)])
            for pc in range(16):
                S.op("dve", lambda e, pc=pc: e.max(out=top[:, pc, 8:16], in_=s2[:, pc, :]),
                     reads=[("s2", pc)], writes=[("top", pc, 1)])
            for pc in range(16):
                S.op("dve", lambda e, pc=pc: e.max_index(out=itop[:, pc, 0:8], in_max=top[:, pc, 0:8], in_values=ssb[:, pc, :]),
                     reads=[("ssb", pc // 4), ("top", pc, 0)], writes=[("itop", pc, 0)])
            for pc in range(16):
                S.op("dve", lambda e, pc=pc: e.max_index(out=itop[:, pc, 8:16], in_max=top[:, pc, 8:16], in_values=ssb[:, pc, :]),
                     reads=[("ssb", pc // 4), ("top", pc, 1)], writes=[("itop", pc, 1)])
            tops = [("top", pc, j) for pc in range(16) for j in range(2)]
            itops = [("itop", pc, j) for pc in range(16) for j in range(2)]
            S.op("dve", lambda e: e.tensor_copy(out=itf[:], in_=itop[:]), reads=itops, writes=["itf"])
            topv = top[:].rearrange("p (h c) a -> p h c a", c=2)
            S.op("dve", lambda e: e.tensor_tensor(
                out=cand[:].rearrange("p h (a b) -> p h a b", b=16),
                in0=topv[:, :, 0, :].unsqueeze(3).to_broadcast([128, 8, 16, 16]),
                in1=topv[:, :, 1, :].unsqueeze(2).to_broadcast([128, 8, 16, 16]), op=ALU.add),
                reads=tops, writes=["cand"])
            for p in range(8):
                S.op("dve", lambda e, p=p: e.max(out=ctop[:, p, 0:8], in_=cand[:, p, :]), reads=["cand"], writes=[("ctop", p, 0)])
            for p in range(8):
                S.op("dve", lambda e, p=p: e.match_replace(out=cand2[:, p, :], in_to_replace=ctop[:, p, 0:8],
                                                           in_values=cand[:, p, :], imm_value=NEG),
                     reads=["cand", ("ctop", p, 0)], writes=[("cand2", p)])
            for p in range(8):
                S.op("dve", lambda e, p=p: e.max(out=ctop[:, p, 8:16], in_=cand2[:, p, :]), reads=[("cand2", p)], writes=[("ctop", p, 1)])
            for p in range(8):
                S.op("dve", lambda e, p=p: e.max_index(out=cpos[:, p, 0:8], in_max=ctop[:, p, 0:8], in_values=cand[:, p, :]),
                     reads=["cand", ("ctop", p, 0)], writes=[("cpos", p, 0)])
            for p in range(8):
                S.op("dve", lambda e, p=p: e.max_index(out=cpos[:, p, 8:16], in_max=ctop[:, p, 8:16], in_values=cand[:, p, :]),
                     reads=["cand", ("ctop", p, 1)], writes=[("cpos", p, 1)])
            ctops = [("ctop", p, j) for p in range(8) for j in range(2)]
            cposs = [("cpos", p, j) for p in range(8) for j in range(2)]
            cposf = cpos[:].rearrange("p h j -> p (h j)")
            S.op("dve", lambda e: e.tensor_copy(out=selA[:], in_=cposf), reads=cposs + [("sel", 0)], writes=["posf"])
            S.op("dve", lambda e: e.tensor_scalar(out=pbf[:], in0=selA[:], scalar1=0.0625, scalar2=None, op0=ALU.mult),
                 reads=["posf"], writes=["pbf"])
            S.op("dve", lambda e: e.tensor_copy(out=pai[:], in_=pbf[:]), reads=["pbf"], writes=["pai"])
            S.op("dve", lambda e: e.tensor_copy(out=paf[:], in_=pai[:]), reads=["pai"], writes=["paf"])
            S.op("dve", lambda e: e.scalar_tensor_tensor(out=pbf[:], in0=paf[:], scalar=16.0, in1=selA[:],
                                                         op0=ALU.mult, op1=ALU.is_gt), reads=["paf", "posf", "pai"], writes=["pbf"])
            S.op("dve", lambda e: e.tensor_tensor(out=paf[:], in0=paf[:], in1=pbf[:], op=ALU.subtract),
                 reads=["paf", "pbf"], writes=["paf"])
            S.op("dve", lambda e: e.scalar_tensor_tensor(out=pbf[:], in0=paf[:], scalar=-16.0, in1=selA[:],
                                                         op0=ALU.mult, op1=ALU.add), reads=["paf", "posf"], writes=["pbf"])
            itv = itf[:].rearrange("p (h c) a -> p h c a", c=2)
            for which, pf, sel in ((0, paf, selA), (1, pbf, selB)):
                S.op("dve", lambda e, pf=pf: e.tensor_tensor(
                    out=oh[:], in0=pf[:].unsqueeze(2).to_broadcast([128, 128, 16]),
                    in1=IOTA16.unsqueeze(1).to_broadcast([128, 128, 16]), op=ALU.is_equal),
                    reads=["paf", "pbf", "cA", "oh"], writes=["oh"])
                S.op("dve", lambda e, which=which: e.tensor_tensor(
                    out=oh[:].rearrange("p (h j) a -> p h j a", j=16),
                    in0=oh[:].rearrange("p (h j) a -> p h j a", j=16),
                    in1=itv[:, :, which, :].unsqueeze(2).to_broadcast([128, 8, 16, 16]), op=ALU.mult),
                    reads=["oh", "itf"], writes=["oh"])
                S.op("dve", lambda e, sel=sel: e.tensor_reduce(out=sel[:], in_=oh[:], axis=AX.X, op=ALU.add),
                     reads=["oh", "posf"], writes=[("sel", which)])
            S.op("dve", lambda e: e.scalar_tensor_tensor(out=ef[:], in0=selA[:], scalar=128.0, in1=selB[:],
                                                         op0=ALU.mult, op1=ALU.add),
                 reads=[("sel", 0), ("sel", 1)], writes=["ef"])
            S.op("dve", lambda e, T=T: e.tensor_copy(out=eidx[:, T, :], in_=ef[:]), reads=["ef"], writes=[("eidx", T)])
            S.op("dve", lambda e: e.tensor_tensor(out=ee[:], in0=ctop[:], in1=ctop[:, :, 0:1].to_broadcast([128, 8, 16]),
                                                  op=ALU.subtract), reads=ctops, writes=["ee"])
            S.op("act", lambda e: e.activation(out=ee[:], in_=ee[:], func=AF.Exp), reads=["ee"], writes=["ee"])
            S.op("dve", lambda e: e.tensor_reduce(out=zz[:, 0:8], in_=ee[:], axis=AX.X, op=ALU.add),
                 reads=["ee"], writes=["zz"])
            S.op("dve", lambda e: e.reciprocal(out=zz[:, 8:16], in_=zz[:, 0:8]), reads=["zz"], writes=["zz2"])
            S.op("dve", lambda e, T=T: e.tensor_tensor(
                out=gw[:, T, :].rearrange("p (h j) -> p h j", j=16), in0=ee[:],
                in1=zz[:, 8:16].unsqueeze(2).to_broadcast([128, 8, 16]), op=ALU.mult),
                reads=["ee", "zz2"], writes=[("gw", T)])
        if debug:
            S.dma("sp", lambda e: e.dma_start(out=dbg["eidx"], in_=eidx[:].rearrange("p t s -> p (t s)")),
                  reads=[("eidx", T) for T in range(NT)])
            S.dma("sp", lambda e: e.dma_start(out=dbg["gw"], in_=gw[:].rearrange("p t s -> p (t s)")),
                  reads=[("gw", T) for T in range(NT)])
        phase_end("p5a")

    with contextlib.ExitStack() as p6:
        NB = 12
        ring = [sb(f"ring{j}", [128, 2 * D], BF16, stack=p6) for j in range(NB)]
        NDG = 6
        dg = [sb(f"dg{j}", [128, 128], BF16, stack=p6) for j in range(NDG)]
        bc = sb("bc5", [128, 4, D], stack=p6)
        h2 = [sb(f"h2_{j}", [128, D], stack=p6) for j in range(2)]
        junk = sb("junk6", [128, D], BF16, stack=p6)
        accs = sb("accs", [128, D], stack=p6)
        aa = [sb(f"aa{j}", [128, 128], stack=p6) for j in range(2)]
        gl = [sb(f"gl{j}", [128, 128], stack=p6) for j in range(2)]
        ww = [sb(f"ww{j}", [128, 128], stack=p6) for j in range(2)]
        st6 = sb("st6", [128, 2 * NT], stack=p6)
        S.dma("sp", lambda e: e.dma_start(out=bc[:, 0, :], in_=scr_bc[1]), writes=[("bc", 0)])
        S.dma("sp", lambda e: e.dma_start(out=bc[:, 1, :], in_=scr_bc[2]), writes=[("bc", 1)])
        S.dma("sp", lambda e: e.dma_start(out=bc[:, 2, :], in_=scr_bc[3]), writes=[("bc", 2)])
        S.dma("sp", lambda e: e.dma_start(out=bc[:, 3, :], in_=nfb), writes=[("bc", 3)])
        gi = gd = 0
        for T in range(NT):
            pu = T % 2
            S.op("dve", lambda e, T=T, pu=pu: e.scalar_tensor_tensor(out=h2[pu][:], in0=x1[:, T, :], scalar=rs2[:, T:T + 1],
                                                                     in1=bc[:, 1, :], op0=ALU.mult, op1=ALU.mult),
                 reads=[("bc", 1)], writes=[("h2", pu)])
            S.op("dve", lambda e, pu=pu: e.tensor_tensor(out=h2[pu][:], in0=h2[pu][:], in1=bc[:, 2, :], op=ALU.add),
                 reads=[("h2", pu), ("bc", 2)], writes=[("h2", pu)])
            S.op("dve", lambda e, pu=pu: e.memset(aa[pu][:], 0.0), writes=[("aa", pu)])
            for s_ in range(128):
                r = gi % NB
                gi += 1
                S.dma("pool", lambda e, T=T, s_=s_, r=r: e.indirect_dma_start(
                    out=ring[r][:], out_offset=None, in_=uv_bf,
                    in_offset=bass.IndirectOffsetOnAxis(ap=eidx[:, T, s_:s_ + 1], axis=0)),
                    reads=[], writes=[("ring", r)])
                S.op("dve", lambda e, s_=s_, r=r, pu=pu: e.scalar_tensor_tensor(
                    out=junk[:], in0=ring[r][:, 0:D], scalar=1.0, in1=h2[pu][:], op0=ALU.mult, op1=ALU.mult,
                    accum_out=aa[pu][:, s_:s_ + 1]), reads=[("ring", r), ("h2", pu), ("aa", pu)],
                    writes=["junk6", ("aas", pu, s_)])
                S.op("act", lambda e, s_=s_, pu=pu: e.activation(out=gl[pu][:, s_:s_ + 1], in_=aa[pu][:, s_:s_ + 1], func=AF.Gelu),
                     reads=[("aas", pu, s_)], writes=[("gl", pu, s_)])
                S.op("act", lambda e, T=T, s_=s_, pu=pu: e.activation(out=ww[pu][:, s_:s_ + 1], in_=gl[pu][:, s_:s_ + 1],
                                                                     func=AF.Copy, scale=gw[:, T, s_:s_ + 1]),
                     reads=[("gl", pu, s_)], writes=[("ww", pu, s_)])
                dj = gd % NDG
                gd += 1
                S.op("act", lambda e, s_=s_, dj=dj, pu=pu: e.activation(
                    out=dg[dj][:], in_=identb[:], func=AF.Copy, scale=ww[pu][:, s_:s_ + 1]),
                    reads=[("ww", pu, s_)], writes=[("dg", dj)])
                for hf in range(2):
                    S.op("pe", lambda e, s_=s_, dj=dj, r=r, pu=pu, hf=hf: e.matmul(
                        ps[2 * pu + hf][:, :], dg[dj][:], ring[r][:, D + hf * 512: D + (hf + 1) * 512],
                        start=(s_ == 0), stop=(s_ == 127)),
                        reads=[("dg", dj), ("ring", r)], writes=[("accP", pu, hf)])
            for hf in range(2):
                S.op("dve", lambda e, pu=pu, hf=hf: e.tensor_tensor(
                    out=accs[:, hf * 512:(hf + 1) * 512], in0=ps[2 * pu + hf][:, :], in1=bc[:, 0, hf * 512:(hf + 1) * 512],
                    op=ALU.mult), reads=[("accP", pu, hf), ("bc", 0)], writes=[("accs", hf)])
            S.op("dve", lambda e, T=T: e.tensor_tensor(out=accs[:], in0=accs[:], in1=x1[:, T, :], op=ALU.add),
                 reads=[("accs", 0), ("accs", 1)], writes=["accsum"])
            S.op("act", lambda e, T=T: e.activation(out=junk[:], in_=accs[:], func=AF.Square, accum_out=st6[:, T:T + 1]),
                 reads=["accsum"], writes=["junk6", ("ssq6", T)])
            S.op("dve", lambda e, T=T: e.tensor_scalar(out=st6[:, NT + T:NT + T + 1], in0=st6[:, T:T + 1],
                                                       scalar1=1.0 / D, scalar2=EPS, op0=ALU.mult, op1=ALU.add),
                 reads=[("ssq6", T)], writes=[("ms6", T)])
            S.op("act", lambda e, T=T: e.activation(out=st6[:, NT + T:NT + T + 1], in_=st6[:, NT + T:NT + T + 1],
                                                    func=AF.Sqrt), reads=[("ms6", T)], writes=[("ms6", T)])
            S.op("dve", lambda e, T=T: e.reciprocal(out=st6[:, T:T + 1], in_=st6[:, NT + T:NT + T + 1]),
                 reads=[("ms6", T)], writes=[("rs6", T)])
            S.op("dve", lambda e, T=T: e.scalar_tensor_tensor(out=x1[:, T, :], in0=accs[:], scalar=st6[:, T:T + 1],
                                                              in1=bc[:, 3, :], op0=ALU.mult, op1=ALU.mult),
                 reads=["accsum", ("rs6", T), ("bc", 3)], writes=[("xo", T), ("accs", 0), ("accs", 1)])
            S.dma("sp", lambda e, T=T: e.dma_start(out=out[T * 128:(T + 1) * 128, :], in_=x1[:, T, :]),
                  reads=[("xo", T)], writes=[("out", T)])
        phase_end("p5b")
    S.finish()
    es.close()
    return nc


def _col(v):
    return np.ascontiguousarray(np.asarray(v, np.float32).reshape(8, 128).T)


def make_inputs(inp):
    global _BIAS_IDX
    f = lambda a: np.ascontiguousarray(np.asarray(a, dtype=np.float32))
    if _BIAS_IDX is None:
        _BIAS_IDX = build_bias_index()
    rpb = f(inp["na_rpb"])[0]
    ext = np.concatenate([rpb.reshape(8, -1), np.full((8, 1), MASKV, np.float32)], axis=1)
    biasT = np.stack([ext[h][_BIAS_IDX] for h in range(8)], axis=1)
    cstm = np.zeros((128, 528), np.float32)
    cstm[:, 0:128] = np.eye(128, dtype=np.float32)
    sidx = np.arange(128)
    blk = (sidx[:, None] // 64) == (sidx[None, :] // 64)
    cstm[:, 128:256] = (blk & (sidx[:, None] <= sidx[None, :])).astype(np.float32)
    cstm[:, 256:384] = (blk & (sidx[:, None] >= sidx[None, :])).astype(np.float32)
    cstm[:, 384:512] = 1.0
    cstm[:, 512:528] = np.arange(16, dtype=np.float32)[None, :]
    rmask = np.ones((128, TOK), np.float32)
    rmask[:, ::64] = 0.0
    c_ctx = f(inp["c_ctx"])
    hg_lb = f(inp["hg_lb"])
    lbraw = np.ascontiguousarray(hg_lb.reshape(2, 2, 4, 128).transpose(3, 0, 1, 2).reshape(128, 16))
    keys = f(inp["peer_keys"])[0]
    keysT = np.ascontiguousarray(keys.transpose(3, 0, 1, 2).reshape(128, 16 * 128))
    shared = dict(
        w_mod=f(inp["w_mod"])[0], b_mod=f(inp["b_mod"])[0].reshape(1, -1),
        n1c=_col(f(inp["norm1"])[0]), n2c=_col(f(inp["norm2"])[0]),
        n2b=np.ascontiguousarray(np.broadcast_to(f(inp["norm2"])[0][None, :], (128, D))),
        nfb=np.ascontiguousarray(np.broadcast_to(f(inp["norm_f"])[None, :], (128, D))),
        w_in=f(inp["w_in"])[0], w_out=f(inp["w_out"])[0], wq=f(inp["peer_wq"])[0],
        keysT=keysT, u=f(inp["peer_u"])[0], v=f(inp["peer_v"])[0], lbraw=lbraw,
        hgn=np.ascontiguousarray(np.broadcast_to(f(inp["hg_norm"])[0][None, :], (128, 512))),
        biasT=np.ascontiguousarray(biasT.reshape(128, -1)), cst=cstm, rmask=rmask,
    )
    xs = f(inp["x"]); cs = f(inp["c"]); ctxs = f(inp["ctx"])
    maps = []
    for b in range(xs.shape[0]):
        cc = np.stack([_col(cs[b]), _col(c_ctx)], axis=2).reshape(128, 16)
        m = dict(shared)
        m.update(x=xs[b], ctx=ctxs[b], ccol=np.ascontiguousarray(cc))
        maps.append(m)
    return maps


def kernel(**inputs):
    maps = make_inputs(inputs)
    nc = build()
    res = run_bass_kernel_spmd(nc, maps, core_ids=list(range(len(maps))))
    return np.stack([np.asarray(r["out"], dtype=np.float32) for r in res.results], axis=0)
```

```python
import contextlib
import numpy as np
import concourse.bass as bass
import concourse.mybir as mybir
from concourse.bass_utils import run_bass_kernel_spmd

F32 = mybir.dt.float32
BF16 = mybir.dt.bfloat16
I32 = mybir.dt.int32
U32 = mybir.dt.uint32
ALU = mybir.AluOpType
AF = mybir.ActivationFunctionType
AX = mybir.AxisListType

D = 1024
SEQ = 2048
CTX = 256
NT = 16
NTT = 18
TOK = SEQ + CTX
NCH = TOK // 64
EPS = 1e-6
MASKV = -30000.0
NEG = -1.0e30


class Sched:
    COMPUTE = ("pe", "dve", "act", "pool")

    def __init__(self, nc, n_dsem=None):
        self.nc = nc
        self.engs = {"pe": nc.tensor, "dve": nc.vector, "act": nc.scalar,
                     "pool": nc.gpsimd, "sp": nc.sync}
        self.n_dsem = n_dsem or {"sp": 8, "act": 4, "pool": 16}
        self.es = contextlib.ExitStack()
        self.csem = {e: self.es.enter_context(nc.semaphore("cs_" + e)) for e in self.COMPUTE}
        self.dsem = {q: [self.es.enter_context(nc.semaphore(f"ds_{q}{j}")) for j in range(n)]
                     for q, n in self.n_dsem.items()}
        self.ccount = {e: 0 for e in self.COMPUTE}
        self.dcount = {q: 0 for q in self.n_dsem}
        self.clock = {e: {} for e in self.engs}
        self.bar_sig = 0
        self.bar_clock = {}
        self.bar_tile = None
        self.ops = []
        self.last_writer = {}
        self.readers = {}
        self.total_ops = 0

    def op(self, eng, fn, reads=(), writes=(), dma=False):
        deps = set()
        for r in reads:
            w = self.last_writer.get(r)
            if w is not None:
                deps.add(w)
        for w_ in writes:
            w = self.last_writer.get(w_)
            if w is not None:
                deps.add(w)
            for rd in self.readers.get(w_, ()):
                deps.add(rd)
        i = len(self.ops)
        deps.discard(i)
        self.ops.append(dict(eng=eng, fn=fn, deps=deps, dma=dma))
        for r in reads:
            self.readers.setdefault(r, []).append(i)
        for w_ in writes:
            self.last_writer[w_] = i
            self.readers[w_] = []
        return i

    def dma(self, q, fn, reads=(), writes=()):
        return self.op(q, fn, reads, writes, dma=True)

    def _wait(self, E, key, sem, val):
        ck = self.clock[E]
        if ck.get(key, 0) < val:
            self.engs[E].wait_ge(sem, val)
            ck[key] = val

    def _merge(self, E, clk):
        ck = self.clock[E]
        for k, v in clk.items():
            if ck.get(k, 0) < v:
                ck[k] = v

    def flush(self, barrier=True):
        ops = self.ops
        need_sig = [False] * len(ops)
        for i, o in enumerate(ops):
            for d in o["deps"]:
                od = ops[d]
                if od["dma"]:
                    continue
                if od["eng"] == "pe" and o["eng"] == "pe" and not o["dma"]:
                    continue
                need_sig[d] = True
        if barrier:
            last = {}
            for i, o in enumerate(ops):
                if not o["dma"]:
                    last[o["eng"]] = i
            for e, i in last.items():
                need_sig[i] = True
        for i, o in enumerate(ops):
            E = o["eng"]
            eng = self.engs[E]
            if self.bar_sig:
                self._wait(E, ("c", "dve"), self.csem["dve"], self.bar_sig)
                self._merge(E, self.bar_clock)
            for d in sorted(o["deps"]):
                od = ops[d]
                if od["dma"]:
                    q = od["eng"]
                    self._wait(E, ("d", q, od["dsem_idx"]), self.dsem[q][od["dsem_idx"]], od["dval"])
                else:
                    F = od["eng"]
                    if F == "pe" and E == "pe" and not o["dma"]:
                        continue
                    self._wait(E, ("c", F), self.csem[F], od["sig"])
                self._merge(E, od["clk"])
            if o["dma"]:
                n = self.n_dsem[E]
                j = self.dcount[E] % n
                prev = self.dcount[E] // n
                if prev > 0:
                    self._wait(E, ("d", E, j), self.dsem[E][j], 16 * prev)
                ins = o["fn"](eng)
                ins.then_inc(self.dsem[E][j], 16)
                o["dsem_idx"] = j
                o["dval"] = 16 * (prev + 1)
                self.dcount[E] += 1
                o["clk"] = dict(self.clock[E])
            else:
                ins = o["fn"](eng)
                if need_sig[i]:
                    self.ccount[E] += 1
                    ins.then_inc(self.csem[E], 1)
                    o["sig"] = self.ccount[E]
                else:
                    o["sig"] = None
                o["clk"] = dict(self.clock[E])
            o["fn"] = None
        self.total_ops += len(ops)
        if barrier:
            self._barrier()
        self.ops = []
        self.last_writer = {}
        self.readers = {}

    def _wait_all(self, E):
        for q, n in self.n_dsem.items():
            for j in range(n):
                uses = (self.dcount[q] - j + n - 1) // n if self.dcount[q] > j else 0
                if uses > 0:
                    self._wait(E, ("d", q, j), self.dsem[q][j], 16 * uses)
        for e in self.COMPUTE:
            if self.ccount[e] > 0:
                self._wait(E, ("c", e), self.csem[e], self.ccount[e])

    def _barrier(self):
        self._wait_all("dve")
        ins = self.engs["dve"].memset(self.bar_tile, 0.0)
        self.ccount["dve"] += 1
        ins.then_inc(self.csem["dve"], 1)
        self.bar_sig = self.ccount["dve"]
        self.bar_clock = dict(self.clock["dve"])

    def finish(self, eng="sp"):
        self.flush(barrier=True)
        self._wait_all(eng)
        self.es.close()


NA_VARIANTS = [(-2, True), (-1, False), (0, False), (1, False), (2, True),
               (-3, False), (-2, False), (2, False), (3, False)]


def na_chunks(i):
    if i in (0, 1, 14, 15):
        cs = range(0, 4) if i < 2 else range(12, 16)
        out = []
        for c in cs:
            d = c - i
            t = {(-3): 5, (-2): 6, (-1): 1, 0: 2, 1: 3, 2: 7, 3: 8}[d]
            out.append((c, t))
        return out
    return [(i + d, d + 2) for d in range(-2, 3)]


def build_bias_index():
    idx = np.full((128, 9, 128), 15 * 31, dtype=np.int64)
    for t, (d, partial) in enumerate(NA_VARIANTS):
        for j in range(2):
            for jq in range(2):
                dr = 2 * d + j - jq
                if abs(dr) > 7:
                    continue
                if partial:
                    if d == -2 and not (j >= jq):
                        continue
                    if d == 2 and not (j == 0 and jq == 1):
                        continue
                for cq in range(64):
                    cstart = min(max(cq - 8, 0), 48)
                    for ck in range(cstart, cstart + 16):
                        idx[j * 64 + ck, t, jq * 64 + cq] = (dr + 7) * 31 + (ck - cq + 15)
    return idx


_BIAS_IDX = None


import os as _os
_NOCONV = bool(_os.environ.get('NOCONV'))


class _Stop(Exception):
    pass


def build(debug=False, stop=None):
    nc = bass.Bass("TRN2", target_bir_lowering=False)
    try:
        return _build(nc, debug, stop)
    except _Stop:
        return nc


def _build(nc, debug, stop):

    def din(name, shape, dt=F32):
        return nc.dram_tensor(name, shape, dt, kind="ExternalInput").ap()

    x = din("x", [SEQ, D])
    ctx = din("ctx", [CTX, D])
    ccol = din("ccol", [128, 16])
    w_mod = din("w_mod", [D, 6 * D])
    b_mod = din("b_mod", [1, 6 * D])
    n1c = din("n1c", [128, 8])
    n2c = din("n2c", [128, 8])
    n2b = din("n2b", [128, D])
    nfb = din("nfb", [128, D])
    w_in = din("w_in", [D, 4096])
    w_out = din("w_out", [D, D])
    wq = din("wq", [D, 2048])
    keysT = din("keysT", [128, 2048])
    u_t = din("u", [16384, D])
    v_t = din("v", [16384, D])
    lbraw = din("lbraw", [128, 16])
    hgn = din("hgn", [128, 512])
    biasT = din("biasT", [128, 8 * 9 * 128])
    cst = din("cst", [128, 528])
    rmask_d = din("rmask", [128, TOK])
    out = nc.dram_tensor("out", [SEQ, D], F32, kind="ExternalOutput").ap()
    scr_bc = nc.dram_tensor("scr_bc", [4, 128, D], F32, kind="Internal").ap()
    uv_bf = nc.dram_tensor("uv_bf", [16384, 2 * D], BF16, kind="Internal").ap()
    dbg = {}
    if debug:
        dbg["hT"] = nc.dram_tensor("d_hT", [128, 8 * TOK], BF16, kind="ExternalOutput").ap()
        dbg["mixT"] = nc.dram_tensor("d_mixT", [128, 8 * SEQ], BF16, kind="ExternalOutput").ap()
        dbg["x1"] = nc.dram_tensor("d_x1", [128, NT * D], F32, kind="ExternalOutput").ap()
        dbg["mc"] = nc.dram_tensor("d_mc", [128, 48], F32, kind="ExternalOutput").ap()
        dbg["eidx"] = nc.dram_tensor("d_eidx", [128, NT * 128], I32, kind="ExternalOutput").ap()
        dbg["gw"] = nc.dram_tensor("d_gw", [128, NT * 128], F32, kind="ExternalOutput").ap()

    es = contextlib.ExitStack()

    def sb(name, shape, dt=F32, stack=None):
        return (stack or es).enter_context(nc.sbuf_tensor(name, shape, dt))

    bar = sb("bar", [128, 1])
    cA = sb("cA", [128, 528])
    identb = sb("identb", [128, 128], BF16)
    mc = sb("mc", [128, 48])
    lb = sb("lb", [128, 8])
    oml = sb("oml", [128, 8])
    ps = [es.enter_context(nc.psum_tensor(f"ps{j}", [128, 512], F32)) for j in range(6)]
    pT = [es.enter_context(nc.psum_tensor(f"pT{j}", [128, 1024], BF16)) for j in range(2)]

    S = Sched(nc)
    S.bar_tile = bar[:]

    def phase_end(name):
        S.flush()
        if stop == name:
            S.finish()
            raise _Stop()

    IDENT = cA[:, 0:128]
    TRIF = cA[:, 128:256]
    TRIB = cA[:, 256:384]
    ONES = cA[:, 384:512]
    IOTA16 = cA[:, 512:528]

    with contextlib.ExitStack() as p0:
        cc = sb("cc", [128, 16], stack=p0)
        scl = sb("scl", [128, 8, 33], BF16, stack=p0)
        wm = [sb(f"wm{j}", [128, 8, 512], stack=p0) for j in range(3)]
        wmb = [sb(f"wmb{j}", [128, 8, 512], BF16, stack=p0) for j in range(2)]
        bm = sb("bm", [33, 6 * D], stack=p0)
        modrow = sb("modrow", [33, 6 * D], stack=p0)
        mcol = sb("mcol", [128, 48], stack=p0)
        n1 = sb("n1", [128, 8], stack=p0)
        n2 = sb("n2", [128, 8], stack=p0)
        n2bt = sb("n2bt", [128, D], stack=p0)
        bct = [sb(f"bct{j}", [128, D], stack=p0) for j in range(2)]
        lbr = sb("lbr", [128, 16], stack=p0)

        S.dma("sp", lambda e: e.dma_start(out=cA[:], in_=cst), writes=["cA"])
        S.dma("sp", lambda e: e.dma_start(out=cc[:], in_=ccol), writes=["cc"])
        S.dma("sp", lambda e: e.dma_start(out=n1[:], in_=n1c), writes=["n1"])
        S.dma("sp", lambda e: e.dma_start(out=n2[:], in_=n2c), writes=["n2"])
        S.dma("sp", lambda e: e.dma_start(out=lbr[:], in_=lbraw), writes=["lbr"])
        S.dma("sp", lambda e: e.dma_start(out=n2bt[:], in_=n2b), writes=["n2bt"])
        S.op("dve", lambda e: e.memset(bm[:], 0.0), writes=["bm"])
        S.dma("sp", lambda e: e.dma_start(out=bm[0:1, :], in_=b_mod), reads=["bm"], writes=["bm0"])
        S.dma("sp", lambda e: e.dma_start(out=bm[32:33, :], in_=b_mod), reads=["bm"], writes=["bm32"])
        S.op("dve", lambda e: e.tensor_copy(out=identb[:], in_=IDENT), reads=["cA"], writes=["identb"])
        S.op("dve", lambda e: e.memset(scl[:], 0.0), writes=["scl"])
        ccv = cc[:].rearrange("p (k t) -> p k t", t=2)
        S.op("act", lambda e: e.activation(out=scl[:, :, 0:1], in_=ccv[:, :, 0:1], func=AF.Silu),
             reads=["cc", "scl"], writes=["scl"])
        S.op("act", lambda e: e.activation(out=scl[:, :, 32:33], in_=ccv[:, :, 1:2], func=AF.Silu),
             reads=["cc", "scl"], writes=["scl"])
        S.op("dve", lambda e: e.tensor_tensor(out=lb[:], in0=lbr[:, 0:8], in1=lbr[:, 8:16], op=ALU.subtract),
             reads=["lbr"], writes=["lb"])
        S.op("act", lambda e: e.activation(out=lb[:], in_=lb[:], func=AF.Sigmoid), reads=["lb"], writes=["lb"])
        S.op("dve", lambda e: e.tensor_scalar(out=oml[:], in0=lb[:], scalar1=-1.0, scalar2=1.0,
                                              op0=ALU.mult, op1=ALU.add), reads=["lb"], writes=["oml"])
        for n in range(12):
            wb = wm[n % 3]
            S.dma("sp" if n % 2 == 0 else "act",
                  lambda e, n=n, wb=wb: e.dma_start(
                      out=wb[:], in_=w_mod[:, n * 512:(n + 1) * 512].rearrange("(k p) n -> p k n", p=128)),
                  writes=[("wm", n % 3)])
            wbb = wmb[n % 2]
            if n % 2 == 0:
                S.op("dve", lambda e, wb=wb, wbb=wbb: e.tensor_copy(out=wbb[:], in_=wb[:]),
                     reads=[("wm", n % 3)], writes=[("wmb", n % 2)])
            else:
                S.op("act", lambda e, wb=wb, wbb=wbb: e.activation(out=wbb[:], in_=wb[:], func=AF.Copy),
                     reads=[("wm", n % 3)], writes=[("wmb", n % 2)])
            pb = ps[n % 2]
            for k in range(8):
                S.op("pe", lambda e, k=k, wbb=wbb, pb=pb: e.matmul(pb[0:33, :], scl[:, k, :], wbb[:, k, :],
                                                                  start=(k == 0), stop=(k == 7)),
                     reads=["scl", ("wmb", n % 2)], writes=[("ps", n % 2)])
            S.op("dve", lambda e, n=n, pb=pb: e.tensor_tensor(out=modrow[:, n * 512:(n + 1) * 512], in0=pb[0:33, :],
                                                             in1=bm[:, n * 512:(n + 1) * 512], op=ALU.add),
                 reads=[("ps", n % 2), "bm", "bm0", "bm32"], writes=[("modrow", n)])
        mr_all = [("modrow", n) for n in range(12)]
        col_specs = [(0, 0), (0, 1), (0, 3), (0, 4), (32, 0), (32, 1)]
        for si, (r, vi) in enumerate(col_specs):
            for k in range(8):
                c0 = 2 * (si * 8 + k)
                S.op("pe", lambda e, r=r, vi=vi, k=k, c0=c0: e.matmul(
                    ps[2][:, c0:c0 + 2], modrow[r:r + 1, vi * D + k * 128: vi * D + (k + 1) * 128],
                    cA[r:r + 1, 384:386], start=True, stop=True),
                    reads=mr_all + ["cA"], writes=[("ps", 2)])
        S.op("dve", lambda e: e.tensor_copy(out=mcol[:].unsqueeze(2), in_=ps[2][:, 0:96].rearrange("p (c two) -> p c two", two=2)[:, :, 0:1]), reads=[("ps", 2)], writes=["mcol"])
        S.op("dve", lambda e: e.scalar_tensor_tensor(out=mc[:, 0:8], in0=mcol[:, 8:16], scalar=1.0, in1=n1[:],
                                                     op0=ALU.add, op1=ALU.mult), reads=["mcol", "n1"], writes=["mc0"])
        S.op("dve", lambda e: e.tensor_copy(out=mc[:, 8:16], in_=mcol[:, 0:8]), reads=["mcol"], writes=["mc1"])
        S.op("dve", lambda e: e.scalar_tensor_tensor(out=mc[:, 16:24], in0=mcol[:, 40:48], scalar=1.0, in1=n1[:],
                                                     op0=ALU.add, op1=ALU.mult), reads=["mcol", "n1"], writes=["mc2"])
        S.op("dve", lambda e: e.tensor_copy(out=mc[:, 24:32], in_=mcol[:, 32:40]), reads=["mcol"], writes=["mc3"])
        S.op("dve", lambda e: e.scalar_tensor_tensor(out=mc[:, 32:40], in0=mcol[:, 24:32], scalar=1.0, in1=n2[:],
                                                     op0=ALU.add, op1=ALU.mult), reads=["mcol", "n2"], writes=["mc4"])
        S.op("dve", lambda e: e.tensor_copy(out=mc[:, 40:48], in_=mcol[:, 16:24]), reads=["mcol"], writes=["mc5"])
        for j, (vi, kind) in enumerate([(2, "copy"), (5, "copy"), (4, "g2"), (3, "copy")]):
            bt_ = bct[j % 2]
            for hf in range(2):
                pb = ps[3 + hf]
                S.op("pe", lambda e, vi=vi, hf=hf, pb=pb: e.matmul(
                    pb[:, :], cA[0:1, 384:512], modrow[0:1, vi * D + hf * 512: vi * D + (hf + 1) * 512],
                    start=True, stop=True), reads=mr_all + ["cA"], writes=[("ps", 3 + hf)])
                if kind == "copy":
                    S.op("dve", lambda e, bt_=bt_, hf=hf, pb=pb: e.tensor_copy(out=bt_[:, hf * 512:(hf + 1) * 512], in_=pb[:, :]),
                         reads=[("ps", 3 + hf)], writes=[("bct", j % 2, hf)])
                else:
                    S.op("dve", lambda e, bt_=bt_, hf=hf, pb=pb: e.scalar_tensor_tensor(
                        out=bt_[:, hf * 512:(hf + 1) * 512], in0=pb[:, :], scalar=1.0,
                        in1=n2bt[:, hf * 512:(hf + 1) * 512], op0=ALU.add, op1=ALU.mult),
                        reads=[("ps", 3 + hf), "n2bt"], writes=[("bct", j % 2, hf)])
            S.dma("sp", lambda e, j=j, bt_=bt_: e.dma_start(out=scr_bc[j], in_=bt_[:]),
                  reads=[("bct", j % 2, 0), ("bct", j % 2, 1)], writes=[("scr", j)])
        if debug:
            S.dma("sp", lambda e: e.dma_start(out=dbg["mc"], in_=mc[:]), reads=[f"mc{j}" for j in range(6)])
        phase_end("p0")

    R = sb("R", [128, NT * D])
    x1 = R[:].rearrange("p (t d) -> p t d", d=D)
    hT = R[:, 0:9216].bitcast(BF16).rearrange("p (k t) -> p k t", k=8)
    vtok = R[:, 9216:13824].bitcast(BF16).rearrange("p (t d) -> p t d", d=512)
    with contextlib.ExitStack() as pm:
        mixT = sb("mixT", [128, 8, SEQ], BF16, stack=pm)

        with contextlib.ExitStack() as p1:
            xt = [sb(f"xt{j}", [128, D], stack=p1) for j in range(2)]
            xs = [sb(f"xs{j}", [128, D], BF16, stack=p1) for j in range(2)]
            junk = sb("junk1", [128, D], BF16, stack=p1)
            st = sb("st1", [128, 3 * NTT], stack=p1)
            for T in range(NTT):
                b = T % 2
                src = x[T * 128:(T + 1) * 128, :] if T < NT else ctx[(T - NT) * 128:(T - NT + 1) * 128, :]
                S.dma("sp" if b == 0 else "act", lambda e, b=b, src=src: e.dma_start(out=xt[b][:], in_=src),
                      writes=[("xt", b)])
                S.op("act", lambda e, b=b, T=T: e.activation(out=junk[:], in_=xt[b][:], func=AF.Square,
                                                             accum_out=st[:, T:T + 1]),
                     reads=[("xt", b)], writes=["junk", ("ssq", T)])
                S.op("dve", lambda e, T=T: e.tensor_scalar(out=st[:, NTT + T:NTT + T + 1], in0=st[:, T:T + 1],
                                                           scalar1=1.0 / D, scalar2=EPS, op0=ALU.mult, op1=ALU.add),
                     reads=[("ssq", T)], writes=[("ms", T)])
                S.op("act", lambda e, T=T: e.activation(out=st[:, NTT + T:NTT + T + 1], in_=st[:, NTT + T:NTT + T + 1],
                                                        func=AF.Sqrt), reads=[("ms", T)], writes=[("ms", T)])
                S.op("dve", lambda e, T=T: e.reciprocal(out=st[:, 2 * NTT + T:2 * NTT + T + 1],
                                                        in_=st[:, NTT + T:NTT + T + 1]),
                     reads=[("ms", T)], writes=[("rstd", T)])
                S.op("act", lambda e, b=b, T=T: e.activation(out=xs[b][:], in_=xt[b][:], func=AF.Copy,
                                                             scale=st[:, 2 * NTT + T:2 * NTT + T + 1]),
                     reads=[("xt", b), ("rstd", T)], writes=[("xs", b)])
                for k in range(8):
                    S.op("pe", lambda e, b=b, k=k: e.transpose(pT[b][:, k * 128:(k + 1) * 128],
                                                               xs[b][:, k * 128:(k + 1) * 128], identb[:]),
                         reads=[("xs", b), "identb"], writes=[("pT", b)])
                go, so = (0, 8) if T < NT else (16, 24)
                for k in range(8):
                    S.op("dve", lambda e, b=b, k=k, T=T, go=go, so=so: e.tensor_scalar(
                        out=hT[:, k, T * 128:(T + 1) * 128], in0=pT[b][:, k * 128:(k + 1) * 128],
                        scalar1=mc[:, go + k:go + k + 1], scalar2=mc[:, so + k:so + k + 1],
                        op0=ALU.mult, op1=ALU.add),
                        reads=[("pT", b)], writes=[("hT", T)])
            if debug:
                S.dma("sp", lambda e: e.dma_start(out=dbg["hT"], in_=R[:, 0:9216].bitcast(BF16)),
                      reads=[("hT", T) for T in range(NTT)])
            phase_end("p1")
        hT_all = [("hT", T) for T in range(NTT)]

        def load_w(tile_ap, dram_w, col0, ncols, key):
            for k0 in range(0, 8, 4):
                S.dma("pool", lambda e, k0=k0: e.dma_start(
                    out=tile_ap[:, k0:k0 + 4, :],
                    in_=dram_w[k0 * 128:(k0 + 4) * 128, col0:col0 + ncols].rearrange("(k p) n -> p k n", p=128)),
                    writes=[(key, k0)])
            return [(key, 0), (key, 4)]

        with contextlib.ExitStack() as p2:
            qT = sb("qT", [128, 4, SEQ], BF16, stack=p2)
            kT = sb("kT", [128, 4, TOK], BF16, stack=p2)
            vaug = sb("vaug", [128, NTT, 8, 65], BF16, stack=p2)
            p2a = contextlib.ExitStack()
            wna = sb("wna", [128, 8, 1536], BF16, stack=p2a)
            wk = load_w(wna, w_in, 0, 1536, "wna")
            S.op("pool", lambda e: e.memset(vaug[:, :, :, 64:65], 1.0), writes=["vones"])
            cnt = 0
            for which, dst, ntok, cbase in (("q", qT, SEQ, 0), ("k", kT, TOK, 512)):
                for hp in range(4):
                    for t0 in range(0, ntok, 512):
                        tw = min(512, ntok - t0)
                        pb = cnt % 4
                        for k in range(8):
                            S.op("pe", lambda e, k=k, hp=hp, t0=t0, tw=tw, pb=pb, cbase=cbase: e.matmul(
                                ps[pb][:, 0:tw], wna[:, k, cbase + hp * 128: cbase + (hp + 1) * 128],
                                hT[:, k, t0:t0 + tw], start=(k == 0), stop=(k == 7)),
                                reads=wk + hT_all, writes=[("ps", pb)])
                        eng = "act" if cnt % 2 == 0 else "dve"
                        if eng == "act":
                            S.op("act", lambda e, dst=dst, hp=hp, t0=t0, tw=tw, pb=pb: e.activation(
                                out=dst[:, hp, t0:t0 + tw], in_=ps[pb][:, 0:tw], func=AF.Copy),
                                reads=[("ps", pb)], writes=[(which, hp, t0)])
                        else:
                            S.op("dve", lambda e, dst=dst, hp=hp, t0=t0, tw=tw, pb=pb: e.tensor_copy(
                                out=dst[:, hp, t0:t0 + tw], in_=ps[pb][:, 0:tw]),
                                reads=[("ps", pb)], writes=[(which, hp, t0)])
                        cnt += 1
            for T in range(NTT):
                pb = cnt % 4
                for k in range(8):
                    S.op("pe", lambda e, k=k, T=T, pb=pb: e.matmul(
                        ps[pb][:, :], hT[:, k, T * 128:(T + 1) * 128], wna[:, k, 1024:1536],
                        start=(k == 0), stop=(k == 7)), reads=wk + hT_all, writes=[("ps", pb)])
                if cnt % 2 == 0:
                    S.op("act", lambda e, T=T, pb=pb: e.activation(
                        out=vaug[:, T, :, 0:64], in_=ps[pb][:, :].rearrange("p (h d) -> p h d", d=64), func=AF.Copy),
                        reads=[("ps", pb)], writes=[("v", T)])
                else:
                    S.op("dve", lambda e, T=T, pb=pb: e.tensor_copy(
                        out=vaug[:, T, :, 0:64], in_=ps[pb][:, :].rearrange("p (h d) -> p h d", d=64)),
                        reads=[("ps", pb)], writes=[("v", T)])
                cnt += 1
            phase_end("p2a")
            p2a.close()
            bt = sb("bt", [128, 8, 9, 128], stack=p2)
            Ssb = [sb(f"Ssb{j}", [128, 640], stack=p2) for j in range(2)]
            Pb = [sb(f"Pb{j}", [128, 896], BF16, stack=p2) for j in range(2)]
            rden = sb("rden", [128, 16], stack=p2)
            natok = [sb(f"natok{j}", [128, 512], BF16, stack=p2) for j in range(2)]
            S.dma("sp", lambda e: e.dma_start(out=bt[:].rearrange("p h t q -> p (h t q)"), in_=biasT), writes=["bt"])

            qk_all_r = []
            it = 0
            for i in range(NT):
                chunks = na_chunks(i)
                nw = len(chunks)
                nb = i % 2
                for h in range(8):
                    hp, po = h // 2, (h % 2) * 64
                    sbuf_i = it % 2
                    b0, b1 = ps[2 * sbuf_i], ps[2 * sbuf_i + 1]

                    def sloc(j):
                        return (b0, j * 128) if j < 4 else (b1, (j - 4) * 128)
                    for j, (c, t) in enumerate(chunks):
                        bk, co = sloc(j)
                        S.op("pe", lambda e, bk=bk, co=co, c=c, hp=hp, po=po, i=i: e.matmul(
                            bk[:, co:co + 128], kT[po:po + 64, hp, c * 128:(c + 1) * 128],
                            qT[po:po + 64, hp, i * 128:(i + 1) * 128], start=True, stop=True),
                            reads=[], writes=[("psS", sbuf_i, j // 4)])
                    for cc_ in range(2):
                        S.op("pe", lambda e, cc_=cc_, hp=hp, po=po, i=i, b1=b1: e.matmul(
                            b1[:, 128 + cc_ * 128: 256 + cc_ * 128],
                            kT[po:po + 64, hp, SEQ + cc_ * 128: SEQ + (cc_ + 1) * 128],
                            qT[po:po + 64, hp, i * 128:(i + 1) * 128], start=True, stop=True),
                            reads=[], writes=[("psS", sbuf_i, 1)])
                    for j, (c, t) in enumerate(chunks):
                        bk, co = sloc(j)
                        S.op("dve", lambda e, bk=bk, co=co, j=j, t=t, h=h, sbuf_i=sbuf_i: e.scalar_tensor_tensor(
                            out=Ssb[sbuf_i][:, j * 128:(j + 1) * 128], in0=bk[:, co:co + 128], scalar=0.125,
                            in1=bt[:, h, t, :], op0=ALU.mult, op1=ALU.add),
                            reads=[("psS", sbuf_i, j // 4), "bt"], writes=[("Ssb", sbuf_i)])
                    S.op("act", lambda e, nw=nw, sbuf_i=sbuf_i: e.activation(
                        out=Pb[sbuf_i][:, 0:nw * 128], in_=Ssb[sbuf_i][:, 0:nw * 128], func=AF.Exp),
                        reads=[("Ssb", sbuf_i)], writes=[("Pw", sbuf_i)])
                    S.op("act", lambda e, sbuf_i=sbuf_i, b1=b1: e.activation(
                        out=Pb[sbuf_i][:, 640:896], in_=b1[:, 128:384], func=AF.Exp, scale=0.125),
                        reads=[("psS", sbuf_i, 1)], writes=[("Pc", sbuf_i)])
                    ob = ps[4 + h // 4]
                    oc = (h % 4) * 128
                    nmm = nw + 2
                    for j, (c, t) in enumerate(chunks):
                        S.op("pe", lambda e, j=j, c=c, h=h, ob=ob, oc=oc, sbuf_i=sbuf_i, nmm=nmm: e.matmul(
                            ob[:, oc:oc + 65], Pb[sbuf_i][:, j * 128:(j + 1) * 128], vaug[:, c, h, :],
                            start=(j == 0), stop=False),
                            reads=[("Pw", sbuf_i)], writes=[("psO", h)])
                    for cc_ in range(2):
                        S.op("pe", lambda e, cc_=cc_, h=h, ob=ob, oc=oc, sbuf_i=sbuf_i: e.matmul(
                            ob[:, oc:oc + 65], Pb[sbuf_i][:, 640 + cc_ * 128: 768 + cc_ * 128], vaug[:, NT + cc_, h, :],
                            start=False, stop=(cc_ == 1)),
                            reads=[("Pc", sbuf_i)], writes=[("psO", h)])
                    S.op("dve", lambda e, h=h, ob=ob, oc=oc: e.reciprocal(out=rden[:, h:h + 1], in_=ob[:, oc + 64:oc + 65]),
                         reads=[("psO", h)], writes=[("rden", h)])
                    S.op("dve", lambda e, h=h, ob=ob, oc=oc, nb=nb: e.tensor_scalar(
                        out=natok[nb][:, h * 64:(h + 1) * 64], in0=ob[:, oc:oc + 64], scalar1=rden[:, h:h + 1],
                        scalar2=None, op0=ALU.mult),
                        reads=[("psO", h), ("rden", h)], writes=[("natok", nb)])
                    it += 1
                for j in range(4):
                    S.op("pe", lambda e, j=j, nb=nb: e.transpose(pT[nb][:, j * 128:(j + 1) * 128],
                                                                natok[nb][:, j * 128:(j + 1) * 128], identb[:]),
                         reads=[("natok", nb)], writes=[("pT", nb)])
                S.op("act", lambda e, i=i, nb=nb: e.activation(
                    out=mixT[:, 0:4, i * 128:(i + 1) * 128],
                    in_=pT[nb][:, 0:512].rearrange("p (j t) -> p j t", t=128), func=AF.Copy),
                    reads=[("pT", nb)], writes=[("mixna", i)])
            phase_end("p2")

        with contextlib.ExitStack() as p3:
            oacc = sb("oacc", [128, NT, 512], stack=p3)
            with contextlib.ExitStack() as p3a:
                wv = sb("wv", [128, 8, 512], BF16, stack=p3a)
                wk = load_w(wv, w_in, 3 * 512 + 3 * 512, 512, "wv")
                for T in range(NTT):
                    pb = T % 4
                    for k in range(8):
                        S.op("pe", lambda e, k=k, T=T, pb=pb: e.matmul(
                            ps[pb][:, :], hT[:, k, T * 128:(T + 1) * 128], wv[:, k, :],
                            start=(k == 0), stop=(k == 7)), reads=wk, writes=[("ps", pb)])
                    if T % 2 == 0:
                        S.op("act", lambda e, T=T, pb=pb: e.activation(out=vtok[:, T, :], in_=ps[pb][:, :], func=AF.Copy),
                             reads=[("ps", pb)], writes=[("vtok", T)])
                    else:
                        S.op("dve", lambda e, T=T, pb=pb: e.tensor_copy(out=vtok[:, T, :], in_=ps[pb][:, :]),
                             reads=[("ps", pb)], writes=[("vtok", T)])
                phase_end("p3a")
            with contextlib.ExitStack() as p3b:
                rmask = sb("rmask_sb", [128, TOK], stack=p3b)
                A_ = sb("hgA", [128, TOK], stack=p3b)
                B_ = sb("hgB", [128, TOK], stack=p3b)
                C_ = sb("hgC", [128, TOK], stack=p3b)
                qsb = sb("hgqs", [128, 512], stack=p3b)
                Qt = sb("hgQt", [128, SEQ], BF16, stack=p3b)
                Qs = sb("hgQs", [128, SEQ], BF16, stack=p3b)
                Ks = sb("hgKs", [128, TOK], BF16, stack=p3b)
                Kf = sb("hgKf", [128, TOK], BF16, stack=p3b)
                Ktok = sb("hgKtok", [128, NTT, 128], BF16, stack=p3b)
                wqf = sb("hgwqf", [128, 8, 256], BF16, stack=p3b)
                sc_ = sb("hgsc", [128, 5, NCH], stack=p3b)
                Sst = [sb(f"hgS{j}", [128, 128], stack=p3b) for j in range(2)]
                Usc = [sb(f"hgUsc{j}", [128, 128], stack=p3b) for j in range(4)]
                Smb = [sb(f"hgSmb{j}", [128, 128], BF16, stack=p3b) for j in range(2)]
                Qe = sb("hgQe", [128, SEQ], BF16, stack=p3b)
                Qo = sb("hgQo", [128, SEQ], BF16, stack=p3b)
                ATs = [sb(f"hgATs{j}", [128, 128], BF16, stack=p3b) for j in range(2)]

                S.dma("sp", lambda e: e.dma_start(out=rmask[:], in_=rmask_d), writes=["rmask"])

                def v3(t, n=TOK):
                    return t[:, 0:n].rearrange("p (t s) -> p t s", s=64)

                for dr in range(2):
                    for hh in range(4):
                        dh = dr * 4 + hh
                        S.dma("pool", lambda e, hh=hh: e.dma_start(
                            out=wqf[:, :, 0:128],
                            in_=w_in[:, 1536 + hh * 128:1536 + (hh + 1) * 128].rearrange("(k p) n -> p k n", p=128)),
                            writes=["wq_h"])
                        S.dma("pool", lambda e, hh=hh, dr=dr: e.dma_start(
                            out=wqf[:, :, 128:256],
                            in_=w_in[:, 2048 + dr * 512 + hh * 128:2048 + dr * 512 + (hh + 1) * 128].rearrange(
                                "(k p) n -> p k n", p=128)), writes=["wf_h"])
                        for ci, t0 in enumerate(range(0, TOK, 512)):
                            tw = min(512, TOK - t0)
                            pb = ci % 4
                            for k in range(8):
                                S.op("pe", lambda e, k=k, t0=t0, tw=tw, pb=pb: e.matmul(
                                    ps[pb][:, 0:tw], wqf[:, k, 128:256], hT[:, k, t0:t0 + tw],
                                    start=(k == 0), stop=(k == 7)), reads=["wf_h"], writes=[("ps", pb)])
                            S.op("act", lambda e, t0=t0, tw=tw, pb=pb: e.activation(
                                out=A_[:, t0:t0 + tw], in_=ps[pb][:, 0:tw], func=AF.Sigmoid),
                                reads=[("ps", pb)], writes=["A"])
                        S.op("dve", lambda e, dh=dh: e.tensor_scalar(out=A_[:], in0=A_[:], scalar1=oml[:, dh:dh + 1],
                                                                     scalar2=lb[:, dh:dh + 1], op0=ALU.mult, op1=ALU.add),
                             reads=["A"], writes=["A"])
                        S.op("act", lambda e: e.activation(out=B_[:], in_=A_[:], func=AF.Ln), reads=["A"], writes=["B"])
                        S.op("dve", lambda e: e.tensor_scalar(out=A_[:], in0=A_[:], scalar1=-1.0, scalar2=1.0,
                                                              op0=ALU.mult, op1=ALU.add), reads=["A", "B"], writes=["A"])
                        S.op("dve", lambda e: e.tensor_tensor_scan(out=C_[:], data0=rmask[:], data1=B_[:], initial=0.0,
                                                                   op0=ALU.mult, op1=ALU.add),
                             reads=["B", "rmask"], writes=["C"])
                        if dr == 0:
                            gbuf, gkey = C_, "C"
                            refpos = 31
                        else:
                            S.op("dve", lambda e: e.tensor_tensor(out=B_[:], in0=B_[:], in1=C_[:], op=ALU.subtract),
                                 reads=["B", "C"], writes=["B"])
                            S.op("dve", lambda e: e.tensor_tensor(
                                out=v3(B_), in0=v3(B_), in1=v3(C_)[:, :, 63:64].to_broadcast([128, NCH, 64]), op=ALU.add),
                                reads=["B", "C"], writes=["B"])
                            gbuf, gkey = B_, "B"
                            refpos = 32
                        endpos = 63 if dr == 0 else 0
                        S.op("dve", lambda e, gbuf=gbuf, refpos=refpos: e.tensor_copy(
                            out=sc_[:, 0, :].unsqueeze(2), in_=v3(gbuf)[:, :, refpos:refpos + 1]), reads=[gkey], writes=["sc0"])
                        S.op("dve", lambda e, gbuf=gbuf, endpos=endpos: e.tensor_copy(
                            out=sc_[:, 1, :].unsqueeze(2), in_=v3(gbuf)[:, :, endpos:endpos + 1]), reads=[gkey], writes=["sc1"])
                        S.op("act", lambda e: e.activation(out=sc_[:, 2:4, :], in_=sc_[:, 0:2, :], func=AF.Exp),
                             reads=["sc0", "sc1"], writes=["sc23"])
                        S.op("dve", lambda e: e.tensor_tensor(out=sc_[:, 4, :], in0=sc_[:, 1, :], in1=sc_[:, 0, :],
                                                              op=ALU.subtract), reads=["sc0", "sc1"], writes=["sc4"])
                        S.op("act", lambda e: e.activation(out=sc_[:, 4, :], in_=sc_[:, 4, :], func=AF.Exp),
                             reads=["sc4"], writes=["sc4"])
                        obuf, okey = (B_, "B") if dr == 0 else (C_, "C")
                        S.op("dve", lambda e, gbuf=gbuf: e.tensor_tensor(
                            out=v3(gbuf), in0=v3(gbuf), in1=sc_[:, 0, :].unsqueeze(2).to_broadcast([128, NCH, 64]),
                            op=ALU.subtract), reads=[gkey, "sc0", "sc1"], writes=[gkey])
                        S.op("act", lambda e, gbuf=gbuf, obuf=obuf: e.activation(out=obuf[:, 0:SEQ], in_=gbuf[:, 0:SEQ], func=AF.Exp),
                             reads=[gkey, okey], writes=[okey])
                        S.op("act", lambda e, gbuf=gbuf: e.activation(out=gbuf[:], in_=gbuf[:], func=AF.Exp, scale=-1.0),
                             reads=[gkey, okey], writes=[gkey])
                        S.op("dve", lambda e, gbuf=gbuf: e.tensor_tensor(out=Kf[:], in0=A_[:], in1=gbuf[:], op=ALU.mult),
                             reads=["A", gkey], writes=["Kf"])
                        hk = 1 if dr == 0 else 0
                        hq = 1 - hk
                        def h32(t):
                            return t[:].rearrange("p (t two s) -> p t two s", two=2, s=32)

                        def h64(t):
                            return t[:].rearrange("p (t two s) -> p t two s", two=2, s=64)
                        if hh == 0:
                            S.op("pool", lambda e: e.memset(Ks[:], 0.0), reads=["Ks"], writes=["Ks"])
                            S.op("pool", lambda e: e.memset(Qs[:], 0.0), reads=["Qs"], writes=["Qs"])
                            if dr == 0:
                                S.op("pool", lambda e: e.memset(Qe[:], 0.0), reads=["Qe"], writes=["Qe"])
                                S.op("pool", lambda e: e.memset(Qo[:], 0.0), reads=["Qo"], writes=["Qo"])
                        S.op("dve", lambda e, hk=hk: e.tensor_copy(out=h32(Ks)[:, :, 1 - hk, :], in_=h32(Kf)[:, :, 1 - hk, :]),
                             reads=["Kf", "Ks"], writes=["Ks"])
                        for ci, t0 in enumerate(range(0, SEQ, 512)):
                            pb = ci % 4
                            for k in range(8):
                                S.op("pe", lambda e, k=k, t0=t0, pb=pb: e.matmul(
                                    ps[pb][:, :], wqf[:, k, 0:128], hT[:, k, t0:t0 + 512],
                                    start=(k == 0), stop=(k == 7)), reads=["wq_h"], writes=[("ps", pb)])
                            S.op("act", lambda e, pb=pb: e.activation(out=qsb[:], in_=ps[pb][:, :], func=AF.Silu),
                                 reads=[("ps", pb)], writes=["qsb"])
                            S.op("dve", lambda e, t0=t0, obuf=obuf: e.tensor_tensor(out=Qt[:, t0:t0 + 512], in0=qsb[:],
                                                                                    in1=obuf[:, t0:t0 + 512], op=ALU.mult),
                                 reads=["qsb", okey], writes=["Qt"])
                        S.op("dve", lambda e, hq=hq: e.tensor_copy(out=h32(Qs)[:, :, 1 - hq, :], in_=h32(Qt)[:, :, 1 - hq, :]),
                             reads=["Qt", "Qs"], writes=["Qs"])
                        S.op("act", lambda e: e.activation(out=h64(Qe)[:, :, 0, :], in_=h64(Qt)[:, :, 0, :], func=AF.Copy),
                             reads=["Qt", "Qe"], writes=["Qe"])
                        S.op("act", lambda e: e.activation(out=h64(Qo)[:, :, 1, :], in_=h64(Qt)[:, :, 1, :], func=AF.Copy),
                             reads=["Qt", "Qo"], writes=["Qo"])
                        for T in range(NTT):
                            nb = T % 2
                            S.op("pe", lambda e, T=T, nb=nb: e.transpose(pT[nb][:, 0:128], Kf[:, T * 128:(T + 1) * 128], identb[:]),
                                 reads=["Kf"], writes=[("pT", nb)])
                            S.op("act", lambda e, T=T, nb=nb: e.activation(out=Ktok[:, T, :], in_=pT[nb][:, 0:128], func=AF.Copy),
                                 reads=[("pT", nb)], writes=[("Ktok", T)])
                        S.op("pool", lambda e, hk=hk: e.memset(
                            Kf[:].rearrange("p (t two s) -> p t two s", two=2, s=32)[:, :, 1 - hk, :], 0.0),
                            reads=["Kf"], writes=["Kf"])
                        order = [16, 17] + list(range(NT)) if dr == 0 else [17, 16] + list(range(NT - 1, -1, -1))
                        tri = TRIF if dr == 0 else TRIB
                        kcnt = [0]
                        S.op("dve", lambda e: e.memset(Sst[0][:], 0.0), reads=[("S", 0)], writes=[("S", 0)])

                        def chunks_of(T):
                            return [2 * T, 2 * T + 1] if dr == 0 else [2 * T + 1, 2 * T]

                        def stage_a(n):
                            T = order[n]
                            q = n % 2
                            for ci, c in enumerate(chunks_of(T)):
                                par = c % 2
                                S.op("pe", lambda e, T=T, par=par, ci=ci, hh=hh: e.matmul(
                                    ps[2 + ci][:, 0:128], Ktok[par * 64:(par + 1) * 64, T, :],
                                    vtok[par * 64:(par + 1) * 64, T, hh * 128:(hh + 1) * 128], start=True, stop=True),
                                    reads=[("Ktok", T)], writes=[("ps", 2 + ci)])
                                S.op("dve", lambda e, c=c, ci=ci, q=q: e.tensor_scalar(
                                    out=Usc[2 * q + ci][:], in0=ps[2 + ci][:, 0:128], scalar1=sc_[:, 4, c:c + 1], scalar2=None,
                                    op0=ALU.mult), reads=[("ps", 2 + ci), "sc4"], writes=[("Usc", 2 * q + ci)])
                            if T < NT:
                                S.op("pe", lambda e, T=T: e.matmul(ps[4][:, 0:128], Ks[:, T * 128:(T + 1) * 128],
                                                                   Qt[:, T * 128:(T + 1) * 128], start=True, stop=False),
                                     reads=["Ks", "Qt"], writes=[("ps", 4)])
                                S.op("pe", lambda e, T=T: e.matmul(ps[4][:, 0:128], Kf[:, T * 128:(T + 1) * 128],
                                                                   Qs[:, T * 128:(T + 1) * 128], start=False, stop=True),
                                     reads=["Kf", "Qs"], writes=[("ps", 4)])

                        def stage_a2(n):
                            T = order[n]
                            q = n % 2
                            if T < NT:
                                S.op("dve", lambda e, q=q, tri=tri: e.tensor_tensor(out=ATs[q][:], in0=ps[4][:, 0:128], in1=tri, op=ALU.mult),
                                     reads=[("ps", 4), "cA"], writes=[("ATs", q)])
                                ob = ps[5] if q == 0 else ps[1]
                                S.op("pe", lambda e, T=T, q=q, ob=ob, hh=hh: e.matmul(ob[:, 0:128], ATs[q][:], vtok[:, T, hh * 128:(hh + 1) * 128],
                                                                               start=True, stop=False),
                                     reads=[("ATs", q)], writes=[("ps", 5 if q == 0 else 1)])

                        def stage_b(n):
                            T = order[n]
                            q = n % 2
                            lat = T < NT
                            ob = ps[5] if q == 0 else ps[1]
                            for ci, c in enumerate(chunks_of(T)):
                                par = c % 2
                                k = kcnt[0]
                                kcnt[0] += 1
                                si, so = k % 2, (k + 1) % 2
                                if lat:
                                    S.op("act", lambda e, c=c, ci=ci, si=si: e.activation(
                                        out=Smb[ci][:], in_=Sst[si][:], func=AF.Copy, scale=sc_[:, 2, c:c + 1]),
                                        reads=[("S", si), "sc23"], writes=[("Smb", ci)])
                                    Qz, qzk = (Qe, "Qe") if par == 0 else (Qo, "Qo")
                                    S.op("pe", lambda e, T=T, ci=ci, Qz=Qz, ob=ob: e.matmul(
                                        ob[:, 0:128], Qz[:, T * 128:(T + 1) * 128], Smb[ci][:], start=False, stop=(ci == 1)),
                                        reads=[("Smb", ci), qzk], writes=[("ps", 5 if q == 0 else 1)])
                                S.op("dve", lambda e, c=c, ci=ci, q=q, si=si, so=so: e.scalar_tensor_tensor(
                                    out=Sst[so][:], in0=Sst[si][:], scalar=sc_[:, 3, c:c + 1], in1=Usc[2 * q + ci][:],
                                    op0=ALU.mult, op1=ALU.add), reads=[("Usc", 2 * q + ci), ("S", si), "sc23"], writes=[("S", so)])
                            if lat:
                                if dr == 0:
                                    S.op("act", lambda e, T=T, ob=ob, hh=hh: e.activation(
                                        out=oacc[:, T, hh * 128:(hh + 1) * 128], in_=ob[:, 0:128], func=AF.Copy),
                                        reads=[("ps", 5 if q == 0 else 1)], writes=[("oacc", T, hh)])
                                else:
                                    S.op("dve", lambda e, T=T, ob=ob, hh=hh: e.tensor_tensor(
                                        out=oacc[:, T, hh * 128:(hh + 1) * 128], in0=ob[:, 0:128],
                                        in1=oacc[:, T, hh * 128:(hh + 1) * 128], op=ALU.add),
                                        reads=[("ps", 5 if q == 0 else 1), ("oacc", T, hh)], writes=[("oacc", T, hh)])

                        stage_a(0)
                        stage_a2(0)
                        for n in range(len(order)):
                            if n + 1 < len(order):
                                stage_a(n + 1)
                            stage_b(n)
                            if n + 1 < len(order):
                                stage_a2(n + 1)
                        if kcnt[0] % 2 == 1:
                            pass
                phase_end("p3b")
            with contextlib.ExitStack() as p3c:
                wg = sb("wg", [128, 8, 512], BF16, stack=p3c)
                hgnb = sb("hgnb", [128, 512], stack=p3c)
                sg = [sb(f"sg{j}", [128, 512], stack=p3c) for j in range(2)]
                yb = [sb(f"yb{j}", [128, 512], stack=p3c) for j in range(2)]
                yt = [sb(f"yt{j}", [128, 512], BF16, stack=p3c) for j in range(2)]
                jk = sb("jk3", [128, 128], BF16, stack=p3c)
                st3 = sb("st3", [128, NT, 8], stack=p3c)
                wk = load_w(wg, w_in, 1536 + 4 * 512, 512, "wg")
                S.dma("sp", lambda e: e.dma_start(out=hgnb[:], in_=hgn), writes=["hgnb"])
                for T in range(NT):
                    b = T % 2
                    for k in range(8):
                        S.op("pe", lambda e, k=k, T=T, b=b: e.matmul(
                            ps[b][:, :], hT[:, k, T * 128:(T + 1) * 128], wg[:, k, :],
                            start=(k == 0), stop=(k == 7)), reads=wk, writes=[("ps", b)])
                    S.op("act", lambda e, b=b: e.activation(out=sg[b][:], in_=ps[b][:, :], func=AF.Silu),
                         reads=[("ps", b)], writes=[("sg", b)])
                    for hh in range(4):
                        S.op("act", lambda e, T=T, hh=hh: e.activation(
                            out=jk[:], in_=oacc[:, T, hh * 128:(hh + 1) * 128], func=AF.Square,
                            accum_out=st3[:, T, hh:hh + 1]), reads=[], writes=["jk3", ("ss3", T)])
                    S.op("dve", lambda e, T=T: e.tensor_scalar(out=st3[:, T, 4:8], in0=st3[:, T, 0:4], scalar1=1.0 / 128,
                                                               scalar2=EPS, op0=ALU.mult, op1=ALU.add),
                         reads=[("ss3", T)], writes=[("ms3", T)])
                    S.op("act", lambda e, T=T: e.activation(out=st3[:, T, 4:8], in_=st3[:, T, 4:8], func=AF.Sqrt),
                         reads=[("ms3", T)], writes=[("ms3", T)])
                    S.op("dve", lambda e, T=T: e.reciprocal(out=st3[:, T, 0:4], in_=st3[:, T, 4:8]),
                         reads=[("ms3", T)], writes=[("rs3", T)])
                    S.op("dve", lambda e, T=T, b=b: e.tensor_tensor(
                        out=yb[b][:].rearrange("p (h d) -> p h d", d=128),
                        in0=oacc[:, T, :].rearrange("p (h d) -> p h d", d=128),
                        in1=st3[:, T, 0:4].unsqueeze(2).to_broadcast([128, 4, 128]), op=ALU.mult),
                        reads=[("rs3", T)], writes=[("yb", b)])
                    S.op("dve", lambda e, b=b: e.tensor_tensor(out=yb[b][:], in0=yb[b][:], in1=hgnb[:], op=ALU.mult),
                         reads=[("yb", b), "hgnb"], writes=[("yb", b)])
                    S.op("dve", lambda e, b=b: e.tensor_tensor(out=yt[b][:], in0=yb[b][:], in1=sg[b][:], op=ALU.mult),
                         reads=[("yb", b), ("sg", b)], writes=[("yt", b)])
                    for j in range(4):
                        S.op("pe", lambda e, j=j, b=b: e.transpose(pT[b][:, j * 128:(j + 1) * 128],
                                                                   yt[b][:, j * 128:(j + 1) * 128], identb[:]),
                             reads=[("yt", b)], writes=[("pT", b)])
                    S.op("act", lambda e, T=T, b=b: e.activation(
                        out=mixT[:, 4:8, T * 128:(T + 1) * 128],
                        in_=pT[b][:, 0:512].rearrange("p (j t) -> p j t", t=128), func=AF.Copy),
                        reads=[("pT", b)], writes=[("mixhg", T)])
                if debug:
                    S.dma("sp", lambda e: e.dma_start(out=dbg["mixT"], in_=mixT[:].rearrange("p k t -> p (k t)")),
                          reads=[("mixhg", T) for T in range(NT)])
                phase_end("p3")

        with contextlib.ExitStack() as p4:
            wo32 = sb("wo32", [128, 8, D], stack=p4)
            wob = sb("wob", [128, 8, D], BF16, stack=p4)
            g1b = sb("g1b", [128, D], stack=p4)
            S.dma("sp", lambda e: e.dma_start(out=g1b[:], in_=scr_bc[0]), writes=["g1b"])
            for k in range(8):
                S.dma("sp" if k % 2 == 0 else "act", lambda e, k=k: e.dma_start(
                    out=wo32[:, k, :], in_=w_out[k * 128:(k + 1) * 128, :]), writes=[("wo32", k)])
                S.op("dve" if k % 2 == 0 else "pool", lambda e, k=k: e.tensor_tensor(
                    out=wob[:, k, :], in0=wo32[:, k, :], in1=g1b[:], op=ALU.mult),
                    reads=[("wo32", k), "g1b"], writes=[("wob", k)])
            wob_all = [("wob", k) for k in range(8)]
            for T in range(NT):
                S.dma("sp" if T % 2 == 0 else "act", lambda e, T=T: e.dma_start(
                    out=x1[:, T, :], in_=x[T * 128:(T + 1) * 128, :]), writes=[("x1", T)])
                for hf in range(2):
                    pb = (2 * T + hf) % 4
                    for k in range(8):
                        S.op("pe", lambda e, k=k, T=T, hf=hf, pb=pb: e.matmul(
                            ps[pb][:, :], mixT[:, k, T * 128:(T + 1) * 128], wob[:, k, hf * 512:(hf + 1) * 512],
                            start=(k == 0), stop=(k == 7)), reads=wob_all, writes=[("ps", pb)])
                    S.op("dve", lambda e, T=T, hf=hf, pb=pb: e.tensor_tensor(
                        out=x1[:, T, hf * 512:(hf + 1) * 512], in0=ps[pb][:, :], in1=x1[:, T, hf * 512:(hf + 1) * 512],
                        op=ALU.add), reads=[("ps", pb), ("x1", T)], writes=[("x1", T)])
            if debug:
                S.dma("sp", lambda e: e.dma_start(out=dbg["x1"], in_=R[:]),
                      reads=[("x1", T) for T in range(NT)])
            phase_end("p4")

    eidx = sb("eidx", [128, NT, 128], I32)
    gw = sb("gw", [128, NT, 128])
    rs2 = sb("rs2", [128, NT])
    with contextlib.ExitStack() as p5:
        wqb = sb("wqb", [128, 8, 2048], BF16, stack=p5)
        kTb = sb("kTb", [128, 16, 128], BF16, stack=p5)
        junk = sb("junk5", [128, D], BF16, stack=p5)
        xs = [sb(f"xs5{j}", [128, D], BF16, stack=p5) for j in range(2)]
        h2T = [sb(f"h2T{j}", [128, 8, 128], BF16, stack=p5) for j in range(2)]
        qTp = [sb(f"qTp{j}", [128, 16, 128], BF16, stack=p5) for j in range(2)]
        ssb = sb("ssb", [128, 16, 128], stack=p5)
        s2 = sb("s2", [128, 16, 128], stack=p5)
        top = sb("top", [128, 16, 16], stack=p5)
        itop = sb("itop", [128, 16, 16], U32, stack=p5)
        itf = sb("itf", [128, 16, 16], stack=p5)
        cand = sb("cand", [128, 8, 256], stack=p5)
        cand2 = sb("cand2", [128, 8, 256], stack=p5)
        ctop = sb("ctop", [128, 8, 16], stack=p5)
        cpos = sb("cpos", [128, 8, 16], U32, stack=p5)
        paf = sb("paf", [128, 128], stack=p5)
        pai = sb("pai", [128, 128], I32, stack=p5)
        pbf = sb("pbf", [128, 128], stack=p5)
        oh = sb("oh", [128, 128, 16], stack=p5)
        selA = sb("selA", [128, 128], stack=p5)
        selB = sb("selB", [128, 128], stack=p5)
        ef = sb("ef", [128, 128], stack=p5)
        ee = sb("ee", [128, 8, 16], stack=p5)
        zz = sb("zz", [128, 16], stack=p5)
        st5 = sb("st5", [128, 2 * NT], stack=p5)

        stg = [sb(f"stg{j}", [128, 4, D], BF16, stack=p5) for j in range(2)]
        conv_steps = [(tab, c) for tab in range(2) for c in range(32)]

        def emit_conv(si):
            tab, c = conv_steps[si]
            src = (u_t, v_t)[tab].rearrange("(p j c) d -> c p j d", p=128, j=4, c=32)[c]
            dst = uv_bf.rearrange("(p j c) d -> c p j d", p=128, j=4, c=32)[c][:, :, tab * D:(tab + 1) * D]
            b = si % 2
            S.dma("pool", lambda e: e.dma_start(out=stg[b][:], in_=src), writes=[("stg", b)])
            S.dma("sp", lambda e: e.dma_start(out=dst, in_=stg[b][:]), reads=[("stg", b)], writes=[("tab", tab, c)])

        wkq = load_w(wqb[:, :, 0:1024], wq, 0, 1024, "wqa") + load_w(wqb[:, :, 1024:2048], wq, 1024, 1024, "wqb")
        S.dma("pool", lambda e: e.dma_start(out=kTb[:].rearrange("p c k -> p (c k)"), in_=keysT), writes=["kTb"])
        for T in range(NT):
            b = T % 2
            if not _NOCONV:
                for q_ in range(4):
                    emit_conv(4 * T + q_)
            S.op("act", lambda e, T=T: e.activation(out=junk[:], in_=x1[:, T, :], func=AF.Square,
                                                    accum_out=st5[:, T:T + 1]), reads=[], writes=["junk5", ("ssq5", T)])
            S.op("dve", lambda e, T=T: e.tensor_scalar(out=st5[:, NT + T:NT + T + 1], in0=st5[:, T:T + 1],
                                                       scalar1=1.0 / D, scalar2=EPS, op0=ALU.mult, op1=ALU.add),
                 reads=[("ssq5", T)], writes=[("ms5", T)])
            S.op("act", lambda e, T=T: e.activation(out=st5[:, NT + T:NT + T + 1], in_=st5[:, NT + T:NT + T + 1],
                                                    func=AF.Sqrt), reads=[("ms5", T)], writes=[("ms5", T)])
            S.op("dve", lambda e, T=T: e.reciprocal(out=rs2[:, T:T + 1], in_=st5[:, NT + T:NT + T + 1]),
                 reads=[("ms5", T)], writes=[("rs2", T)])
            S.op("act", lambda e, b=b, T=T: e.activation(out=xs[b][:], in_=x1[:, T, :], func=AF.Copy,
                                                         scale=rs2[:, T:T + 1]), reads=[("rs2", T)], writes=[("xs5", b)])
            for k in range(8):
                S.op("pe", lambda e, b=b, k=k: e.transpose(pT[b][:, k * 128:(k + 1) * 128],
                                                           xs[b][:, k * 128:(k + 1) * 128], identb[:]),
                     reads=[("xs5", b)], writes=[("pT", b)])
            for k in range(8):
                S.op("dve" if k % 2 == 0 else "act", (lambda e, b=b, k=k: e.tensor_scalar(
                    out=h2T[b][:, k, :], in0=pT[b][:, k * 128:(k + 1) * 128],
                    scalar1=mc[:, 32 + k:33 + k], scalar2=mc[:, 40 + k:41 + k], op0=ALU.mult, op1=ALU.add))
                    if k % 2 == 0 else (lambda e, b=b, k=k: e.activation(
                        out=h2T[b][:, k, :], in_=pT[b][:, k * 128:(k + 1) * 128], func=AF.Identity,
                        scale=mc[:, 32 + k:33 + k], bias=mc[:, 40 + k:41 + k])),
                    reads=[("pT", b)], writes=[("h2T", b)])
            for g4 in range(4):
                pb = g4
                for j in range(4):
                    pc = g4 * 4 + j
                    for k in range(8):
                        S.op("pe", lambda e, b=b, k=k, pc=pc, j=j, pb=pb: e.matmul(
                            ps[pb][:, j * 128:(j + 1) * 128], wqb[:, k, pc * 128:(pc + 1) * 128], h2T[b][:, k, :],
                            start=(k == 0), stop=(k == 7)), reads=wkq + [("h2T", b)], writes=[("ps", pb)])
                if g4 % 2 == 0:
                    S.op("act", lambda e, b=b, g4=g4, pb=pb: e.activation(
                        out=qTp[b][:, g4 * 4:(g4 + 1) * 4, :], in_=ps[pb][:, :].rearrange("p (j t) -> p j t", t=128),
                        func=AF.Copy), reads=[("ps", pb)], writes=[("qTp", b, g4)])
                else:
                    S.op("dve", lambda e, b=b, g4=g4, pb=pb: e.tensor_copy(
                        out=qTp[b][:, g4 * 4:(g4 + 1) * 4, :], in_=ps[pb][:, :].rearrange("p (j t) -> p j t", t=128)),
                        reads=[("ps", pb)], writes=[("qTp", b, g4)])
            for g4 in range(4):
                pb = 4 + (g4 % 2)
                for j in range(4):
                    pc = g4 * 4 + j
                    S.op("pe", lambda e, b=b, pc=pc, j=j, pb=pb: e.matmul(
                        ps[pb][:, j * 128:(j + 1) * 128], qTp[b][:, pc, :], kTb[:, pc, :], start=True, stop=True),
                        reads=[("qTp", b, g4), "kTb"], writes=[("ps", pb)])
                S.op("act", lambda e, g4=g4, pb=pb: e.activation(
                    out=ssb[:, g4 * 4:(g4 + 1) * 4, :], in_=ps[pb][:, :].rearrange("p (j t) -> p j t", t=128),
                    func=AF.Copy), reads=[("ps", pb)], writes=[("ssb", g4)])
            for pc in range(16):
                S.op("dve", lambda e, pc=pc: e.max(out=top[:, pc, 0:8], in_=ssb[:, pc, :]),
                     reads=[("ssb", pc // 4)], writes=[("top", pc, 0)])
            for pc in range(16):
                S.op("dve", lambda e, pc=pc: e.match_replace(out=s2[:, pc, :], in_to_replace=top[:, pc, 0:8],
                                                             in_values=ssb[:, pc, :], imm_value=NEG),
                     reads=[("ssb", pc // 4), ("top", pc, 0)], writes=[("s2", pc)])
            for pc in range(16):
                S.op("dve", lambda e, pc=pc: e.max(out=top[:, pc, 8:16], in_=s2[:, pc, :]),
                     reads=[("s2", pc)], writes=[("top", pc, 1)])
            for pc in range(16):
                S.op("dve", lambda e, pc=pc: e.max_index(out=itop[:, pc, 0:8], in_max=top[:, pc, 0:8], in_values=ssb[:, pc, :]),
                     reads=[("ssb", pc // 4), ("top", pc, 0)], writes=[("itop", pc, 0)])
            for pc in range(16):
                S.op("dve", lambda e, pc=pc: e.max_index(out=itop[:, pc, 8:16], in_max=top[:, pc, 8:16], in_values=ssb[:, pc, :]),
                     reads=[("ssb", pc // 4), ("top", pc, 1)], writes=[("itop", pc, 1)])
            tops = [("top", pc, j) for pc in range(16) for j in range(2)]
            itops = [("itop", pc, j) for pc in range(16) for j in range(2)]
            S.op("dve", lambda e: e.tensor_copy(out=itf[:], in_=itop[:]), reads=itops, writes=["itf"])
            topv = top[:].rearrange("p (h c) a -> p h c a", c=2)
            S.op("dve", lambda e: e.tensor_tensor(
                out=cand[:].rearrange("p h (a b) -> p h a b", b=16),
                in0=topv[:, :, 0, :].unsqueeze(3).to_broadcast([128, 8, 16, 16]),
                in1=topv[:, :, 1, :].unsqueeze(2).to_broadcast([128, 8, 16, 16]), op=ALU.add),
                reads=tops, writes=["cand"])
            for p in range(8):
                S.op("dve", lambda e, p=p: e.max(out=ctop[:, p, 0:8], in_=cand[:, p, :]), reads=["cand"], writes=[("ctop", p, 0)])
            for p in range(8):
                S.op("dve", lambda e, p=p: e.match_replace(out=cand2[:, p, :], in_to_replace=ctop[:, p, 0:8],
                                                           in_values=cand[:, p, :], imm_value=NEG),
                     reads=["cand", ("ctop", p, 0)], writes=[("cand2", p)])
            for p in range(8):
                S.op("dve", lambda e, p=p: e.max(out=ctop[:, p, 8:16], in_=cand2[:, p, :]), reads=[("cand2", p)], writes=[("ctop", p, 1)])
            for p in range(8):
                S.op("dve", lambda e, p=p: e.max_index(out=cpos[:, p, 0:8], in_max=ctop[:, p, 0:8], in_values=cand[:, p, :]),
                     reads=["cand", ("ctop", p, 0)], writes=[("cpos", p, 0)])
            for p in range(8):
                S.op("dve", lambda e, p=p: e.max_index(out=cpos[:, p, 8:16], in_max=ctop[:, p, 8:16], in_values=cand[:, p, :]),
                     reads=["cand", ("ctop", p, 1)], writes=[("cpos", p, 1)])
            ctops = [("ctop", p, j) for p in range(8) for j in range(2)]
            cposs = [("cpos", p, j) for p in range(8) for j in range(2)]
            cposf = cpos[:].rearrange("p h j -> p (h j)")
            S.op("dve", lambda e: e.tensor_copy(out=selA[:], in_=cposf), reads=cposs + [("sel", 0)], writes=["posf"])
            S.op("dve", lambda e: e.tensor_scalar(out=pbf[:], in0=selA[:], scalar1=0.0625, scalar2=None, op0=ALU.mult),
                 reads=["posf"], writes=["pbf"])
            S.op("dve", lambda e: e.tensor_copy(out=pai[:], in_=pbf[:]), reads=["pbf"], writes=["pai"])
            S.op("dve", lambda e: e.tensor_copy(out=paf[:], in_=pai[:]), reads=["pai"], writes=["paf"])
            S.op("dve", lambda e: e.scalar_tensor_tensor(out=pbf[:], in0=paf[:], scalar=16.0, in1=selA[:],
                                                         op0=ALU.mult, op1=ALU.is_gt), reads=["paf", "posf", "pai"], writes=["pbf"])
            S.op("dve", lambda e: e.tensor_tensor(out=paf[:], in0=paf[:], in1=pbf[:], op=ALU.subtract),
                 reads=["paf", "pbf"], writes=["paf"])
            S.op("dve", lambda e: e.scalar_tensor_tensor(out=pbf[:], in0=paf[:], scalar=-16.0, in1=selA[:],
                                                         op0=ALU.mult, op1=ALU.add), reads=["paf", "posf"], writes=["pbf"])
            itv = itf[:].rearrange("p (h c) a -> p h c a", c=2)
            for which, pf, sel in ((0, paf, selA), (1, pbf, selB)):
                S.op("dve", lambda e, pf=pf: e.tensor_tensor(
                    out=oh[:], in0=pf[:].unsqueeze(2).to_broadcast([128, 128, 16]),
                    in1=IOTA16.unsqueeze(1).to_broadcast([128, 128, 16]), op=ALU.is_equal),
                    reads=["paf", "pbf", "cA", "oh"], writes=["oh"])
                S.op("dve", lambda e, which=which: e.tensor_tensor(
                    out=oh[:].rearrange("p (h j) a -> p h j a", j=16),
                    in0=oh[:].rearrange("p (h j) a -> p h j a", j=16),
                    in1=itv[:, :, which, :].unsqueeze(2).to_broadcast([128, 8, 16, 16]), op=ALU.mult),
                    reads=["oh", "itf"], writes=["oh"])
                S.op("dve", lambda e, sel=sel: e.tensor_reduce(out=sel[:], in_=oh[:], axis=AX.X, op=ALU.add),
                     reads=["oh", "posf"], writes=[("sel", which)])
            S.op("dve", lambda e: e.scalar_tensor_tensor(out=ef[:], in0=selA[:], scalar=128.0, in1=selB[:],
                                                         op0=ALU.mult, op1=ALU.add),
                 reads=[("sel", 0), ("sel", 1)], writes=["ef"])
            S.op("dve", lambda e, T=T: e.tensor_copy(out=eidx[:, T, :], in_=ef[:]), reads=["ef"], writes=[("eidx", T)])
            S.op("dve", lambda e: e.tensor_tensor(out=ee[:], in0=ctop[:], in1=ctop[:, :, 0:1].to_broadcast([128, 8, 16]),
                                                  op=ALU.subtract), reads=ctops, writes=["ee"])
            S.op("act", lambda e: e.activation(out=ee[:], in_=ee[:], func=AF.Exp), reads=["ee"], writes=["ee"])
            S.op("dve", lambda e: e.tensor_reduce(out=zz[:, 0:8], in_=ee[:], axis=AX.X, op=ALU.add),
                 reads=["ee"], writes=["zz"])
            S.op("dve", lambda e: e.reciprocal(out=zz[:, 8:16], in_=zz[:, 0:8]), reads=["zz"], writes=["zz2"])
            S.op("dve", lambda e, T=T: e.tensor_tensor(
                out=gw[:, T, :].rearrange("p (h j) -> p h j", j=16), in0=ee[:],
                in1=zz[:, 8:16].unsqueeze(2).to_broadcast([128, 8, 16]), op=ALU.mult),
                reads=["ee", "zz2"], writes=[("gw", T)])
        if debug:
            S.dma("sp", lambda e: e.dma_start(out=dbg["eidx"], in_=eidx[:].rearrange("p t s -> p (t s)")),
                  reads=[("eidx", T) for T in range(NT)])
            S.dma("sp", lambda e: e.dma_start(out=dbg["gw"], in_=gw[:].rearrange("p t s -> p (t s)")),
                  reads=[("gw", T) for T in range(NT)])
        phase_end("p5a")

    with contextlib.ExitStack() as p6:
        NB = 12
        ring = [sb(f"ring{j}", [128, 2 * D], BF16, stack=p6) for j in range(NB)]
        NDG = 6
        dg = [sb(f"dg{j}", [128, 128], BF16, stack=p6) for j in range(NDG)]
        bc = sb("bc5", [128, 4, D], stack=p6)
        h2 = [sb(f"h2_{j}", [128, D], stack=p6) for j in range(2)]
        junk = sb("junk6", [128, D], BF16, stack=p6)
        accs = sb("accs", [128, D], stack=p6)
        aa = [sb(f"aa{j}", [128, 128], stack=p6) for j in range(2)]
        gl = [sb(f"gl{j}", [128, 128], stack=p6) for j in range(2)]
        ww = [sb(f"ww{j}", [128, 128], stack=p6) for j in range(2)]
        st6 = sb("st6", [128, 2 * NT], stack=p6)
        S.dma("sp", lambda e: e.dma_start(out=bc[:, 0, :], in_=scr_bc[1]), writes=[("bc", 0)])
        S.dma("sp", lambda e: e.dma_start(out=bc[:, 1, :], in_=scr_bc[2]), writes=[("bc", 1)])
        S.dma("sp", lambda e: e.dma_start(out=bc[:, 2, :], in_=scr_bc[3]), writes=[("bc", 2)])
        S.dma("sp", lambda e: e.dma_start(out=bc[:, 3, :], in_=nfb), writes=[("bc", 3)])
        gi = gd = 0
        for T in range(NT):
            pu = T % 2
            S.op("dve", lambda e, T=T, pu=pu: e.scalar_tensor_tensor(out=h2[pu][:], in0=x1[:, T, :], scalar=rs2[:, T:T + 1],
                                                                     in1=bc[:, 1, :], op0=ALU.mult, op1=ALU.mult),
                 reads=[("bc", 1)], writes=[("h2", pu)])
            S.op("dve", lambda e, pu=pu: e.tensor_tensor(out=h2[pu][:], in0=h2[pu][:], in1=bc[:, 2, :], op=ALU.add),
                 reads=[("h2", pu), ("bc", 2)], writes=[("h2", pu)])
            S.op("dve", lambda e, pu=pu: e.memset(aa[pu][:], 0.0), writes=[("aa", pu)])
            for s_ in range(128):
                r = gi % NB
                gi += 1
                S.dma("pool", lambda e, T=T, s_=s_, r=r: e.indirect_dma_start(
                    out=ring[r][:], out_offset=None, in_=uv_bf,
                    in_offset=bass.IndirectOffsetOnAxis(ap=eidx[:, T, s_:s_ + 1], axis=0)),
                    reads=[], writes=[("ring", r)])
                S.op("dve", lambda e, s_=s_, r=r, pu=pu: e.scalar_tensor_tensor(
                    out=junk[:], in0=ring[r][:, 0:D], scalar=1.0, in1=h2[pu][:], op0=ALU.mult, op1=ALU.mult,
                    accum_out=aa[pu][:, s_:s_ + 1]), reads=[("ring", r), ("h2", pu), ("aa", pu)],
                    writes=["junk6", ("aas", pu, s_)])
                S.op("act", lambda e, s_=s_, pu=pu: e.activation(out=gl[pu][:, s_:s_ + 1], in_=aa[pu][:, s_:s_ + 1], func=AF.Gelu),
                     reads=[("aas", pu, s_)], writes=[("gl", pu, s_)])
                S.op("act", lambda e, T=T, s_=s_, pu=pu: e.activation(out=ww[pu][:, s_:s_ + 1], in_=gl[pu][:, s_:s_ + 1],
                                                                     func=AF.Copy, scale=gw[:, T, s_:s_ + 1]),
                     reads=[("gl", pu, s_)], writes=[("ww", pu, s_)])
                dj = gd % NDG
                gd += 1
                S.op("act", lambda e, s_=s_, dj=dj, pu=pu: e.activation(
                    out=dg[dj][:], in_=identb[:], func=AF.Copy, scale=ww[pu][:, s_:s_ + 1]),
                    reads=[("ww", pu, s_)], writes=[("dg", dj)])
                for hf in range(2):
                    S.op("pe", lambda e, s_=s_, dj=dj, r=r, pu=pu, hf=hf: e.matmul(
                        ps[2 * pu + hf][:, :], dg[dj][:], ring[r][:, D + hf * 512: D + (hf + 1) * 512],
                        start=(s_ == 0), stop=(s_ == 127)),
                        reads=[("dg", dj), ("ring", r)], writes=[("accP", pu, hf)])
            for hf in range(2):
                S.op("dve", lambda e, pu=pu, hf=hf: e.tensor_tensor(
                    out=accs[:, hf * 512:(hf + 1) * 512], in0=ps[2 * pu + hf][:, :], in1=bc[:, 0, hf * 512:(hf + 1) * 512],
                    op=ALU.mult), reads=[("accP", pu, hf), ("bc", 0)], writes=[("accs", hf)])
            S.op("dve", lambda e, T=T: e.tensor_tensor(out=accs[:], in0=accs[:], in1=x1[:, T, :], op=ALU.add),
                 reads=[("accs", 0), ("accs", 1)], writes=["accsum"])
            S.op("act", lambda e, T=T: e.activation(out=junk[:], in_=accs[:], func=AF.Square, accum_out=st6[:, T:T + 1]),
                 reads=["accsum"], writes=["junk6", ("ssq6", T)])
            S.op("dve", lambda e, T=T: e.tensor_scalar(out=st6[:, NT + T:NT + T + 1], in0=st6[:, T:T + 1],
                                                       scalar1=1.0 / D, scalar2=EPS, op0=ALU.mult, op1=ALU.add),
                 reads=[("ssq6", T)], writes=[("ms6", T)])
            S.op("act", lambda e, T=T: e.activation(out=st6[:, NT + T:NT + T + 1], in_=st6[:, NT + T:NT + T + 1],
                                                    func=AF.Sqrt), reads=[("ms6", T)], writes=[("ms6", T)])
            S.op("dve", lambda e, T=T: e.reciprocal(out=st6[:, T:T + 1], in_=st6[:, NT + T:NT + T + 1]),
                 reads=[("ms6", T)], writes=[("rs6", T)])
            S.op("dve", lambda e, T=T: e.scalar_tensor_tensor(out=x1[:, T, :], in0=accs[:], scalar=st6[:, T:T + 1],
                                                              in1=bc[:, 3, :], op0=ALU.mult, op1=ALU.mult),
                 reads=["accsum", ("rs6", T), ("bc", 3)], writes=[("xo", T), ("accs", 0), ("accs", 1)])
            S.dma("sp", lambda e, T=T: e.dma_start(out=out[T * 128:(T + 1) * 128, :], in_=x1[:, T, :]),
                  reads=[("xo", T)], writes=[("out", T)])
        phase_end("p5b")
    S.finish()
    es.close()
    return nc


def _col(v):
    return np.ascontiguousarray(np.asarray(v, np.float32).reshape(8, 128).T)


def make_inputs(inp):
    global _BIAS_IDX
    f = lambda a: np.ascontiguousarray(np.asarray(a, dtype=np.float32))
    if _BIAS_IDX is None:
        _BIAS_IDX = build_bias_index()
    rpb = f(inp["na_rpb"])[0]
    ext = np.concatenate([rpb.reshape(8, -1), np.full((8, 1), MASKV, np.float32)], axis=1)
    biasT = np.stack([ext[h][_BIAS_IDX] for h in range(8)], axis=1)
    cstm = np.zeros((128, 528), np.float32)
    cstm[:, 0:128] = np.eye(128, dtype=np.float32)
    sidx = np.arange(128)
    blk = (sidx[:, None] // 64) == (sidx[None, :] // 64)
    cstm[:, 128:256] = (blk & (sidx[:, None] <= sidx[None, :])).astype(np.float32)
    cstm[:, 256:384] = (blk & (sidx[:, None] >= sidx[None, :])).astype(np.float32)
    cstm[:, 384:512] = 1.0
    cstm[:, 512:528] = np.arange(16, dtype=np.float32)[None, :]
    rmask = np.ones((128, TOK), np.float32)
    rmask[:, ::64] = 0.0
    c_ctx = f(inp["c_ctx"])
    hg_lb = f(inp["hg_lb"])
    lbraw = np.ascontiguousarray(hg_lb.reshape(2, 2, 4, 128).transpose(3, 0, 1, 2).reshape(128, 16))
    keys = f(inp["peer_keys"])[0]
    keysT = np.ascontiguousarray(keys.transpose(3, 0, 1, 2).reshape(128, 16 * 128))
    shared = dict(
        w_mod=f(inp["w_mod"])[0], b_mod=f(inp["b_mod"])[0].reshape(1, -1),
        n1c=_col(f(inp["norm1"])[0]), n2c=_col(f(inp["norm2"])[0]),
        n2b=np.ascontiguousarray(np.broadcast_to(f(inp["norm2"])[0][None, :], (128, D))),
        nfb=np.ascontiguousarray(np.broadcast_to(f(inp["norm_f"])[None, :], (128, D))),
        w_in=f(inp["w_in"])[0], w_out=f(inp["w_out"])[0], wq=f(inp["peer_wq"])[0],
        keysT=keysT, u=f(inp["peer_u"])[0], v=f(inp["peer_v"])[0], lbraw=lbraw,
        hgn=np.ascontiguousarray(np.broadcast_to(f(inp["hg_norm"])[0][None, :], (128, 512))),
        biasT=np.ascontiguousarray(biasT.reshape(128, -1)), cst=cstm, rmask=rmask,
    )
    xs = f(inp["x"]); cs = f(inp["c"]); ctxs = f(inp["ctx"])
    maps = []
    for b in range(xs.shape[0]):
        cc = np.stack([_col(cs[b]), _col(c_ctx)], axis=2).reshape(128, 16)
        m = dict(shared)
        m.update(x=xs[b], ctx=ctxs[b], ccol=np.ascontiguousarray(cc))
        maps.append(m)
    return maps


def kernel(**inputs):
    maps = make_inputs(inputs)
    nc = build()
    res = run_bass_kernel_spmd(nc, maps, core_ids=list(range(len(maps))))
    return np.stack([np.asarray(r["out"], dtype=np.float32) for r in res.results], axis=0)
```

```python
import contextlib
import numpy as np
import concourse.bass as bass
import concourse.mybir as mybir
from concourse.bass_utils import run_bass_kernel_spmd

F32 = mybir.dt.float32
BF16 = mybir.dt.bfloat16
I32 = mybir.dt.int32
U32 = mybir.dt.uint32
ALU = mybir.AluOpType
AF = mybir.ActivationFunctionType
AX = mybir.AxisListType

D = 1024
SEQ = 2048
CTX = 256
NT = 16
NTT = 18
TOK = SEQ + CTX
NCH = TOK // 64
EPS = 1e-6
MASKV = -30000.0
NEG = -1.0e30


class Sched:
    COMPUTE = ("pe", "dve", "act", "pool")

    def __init__(self, nc, n_dsem=None):
        self.nc = nc
        self.engs = {"pe": nc.tensor, "dve": nc.vector, "act": nc.scalar,
                     "pool": nc.gpsimd, "sp": nc.sync}
        self.n_dsem = n_dsem or {"sp": 8, "act": 4, "pool": 16}
        self.es = contextlib.ExitStack()
        self.csem = {e: self.es.enter_context(nc.semaphore("cs_" + e)) for e in self.COMPUTE}
        self.dsem = {q: [self.es.enter_context(nc.semaphore(f"ds_{q}{j}")) for j in range(n)]
                     for q, n in self.n_dsem.items()}
        self.ccount = {e: 0 for e in self.COMPUTE}
        self.dcount = {q: 0 for q in self.n_dsem}
        self.clock = {e: {} for e in self.engs}
        self.bar_sig = 0
        self.bar_clock = {}
        self.bar_tile = None
        self.ops = []
        self.last_writer = {}
        self.readers = {}
        self.total_ops = 0

    def op(self, eng, fn, reads=(), writes=(), dma=False):
        deps = set()
        for r in reads:
            w = self.last_writer.get(r)
            if w is not None:
                deps.add(w)
        for w_ in writes:
            w = self.last_writer.get(w_)
            if w is not None:
                deps.add(w)
            for rd in self.readers.get(w_, ()):
                deps.add(rd)
        i = len(self.ops)
        deps.discard(i)
        self.ops.append(dict(eng=eng, fn=fn, deps=deps, dma=dma))
        for r in reads:
            self.readers.setdefault(r, []).append(i)
        for w_ in writes:
            self.last_writer[w_] = i
            self.readers[w_] = []
        return i

    def dma(self, q, fn, reads=(), writes=()):
        return self.op(q, fn, reads, writes, dma=True)

    def _wait(self, E, key, sem, val):
        ck = self.clock[E]
        if ck.get(key, 0) < val:
            self.engs[E].wait_ge(sem, val)
            ck[key] = val

    def _merge(self, E, clk):
        ck = self.clock[E]
        for k, v in clk.items():
            if ck.get(k, 0) < v:
                ck[k] = v

    def flush(self, barrier=True):
        ops = self.ops
        need_sig = [False] * len(ops)
        for i, o in enumerate(ops):
            for d in o["deps"]:
                od = ops[d]
                if od["dma"]:
                    continue
                if od["eng"] == "pe" and o["eng"] == "pe" and not o["dma"]:
                    continue
                need_sig[d] = True
        if barrier:
            last = {}
            for i, o in enumerate(ops):
                if not o["dma"]:
                    last[o["eng"]] = i
            for e, i in last.items():
                need_sig[i] = True
        for i, o in enumerate(ops):
            E = o["eng"]
            eng = self.engs[E]
            if self.bar_sig:
                self._wait(E, ("c", "dve"), self.csem["dve"], self.bar_sig)
                self._merge(E, self.bar_clock)
            for d in sorted(o["deps"]):
                od = ops[d]
                if od["dma"]:
                    q = od["eng"]
                    self._wait(E, ("d", q, od["dsem_idx"]), self.dsem[q][od["dsem_idx"]], od["dval"])
                else:
                    F = od["eng"]
                    if F == "pe" and E == "pe" and not o["dma"]:
                        continue
                    self._wait(E, ("c", F), self.csem[F], od["sig"])
                self._merge(E, od["clk"])
            if o["dma"]:
                n = self.n_dsem[E]
                j = self.dcount[E] % n
                prev = self.dcount[E] // n
                if prev > 0:
                    self._wait(E, ("d", E, j), self.dsem[E][j], 16 * prev)
                ins = o["fn"](eng)
                ins.then_inc(self.dsem[E][j], 16)
                o["dsem_idx"] = j
                o["dval"] = 16 * (prev + 1)
                self.dcount[E] += 1
                o["clk"] = dict(self.clock[E])
            else:
                ins = o["fn"](eng)
                if need_sig[i]:
                    self.ccount[E] += 1
                    ins.then_inc(self.csem[E], 1)
                    o["sig"] = self.ccount[E]
                else:
                    o["sig"] = None
                o["clk"] = dict(self.clock[E])
            o["fn"] = None
        self.total_ops += len(ops)
        if barrier:
            self._barrier()
        self.ops = []
        self.last_writer = {}
        self.readers = {}

    def _wait_all(self, E):
        for q, n in self.n_dsem.items():
            for j in range(n):
                uses = (self.dcount[q] - j + n - 1) // n if self.dcount[q] > j else 0
                if uses > 0:
                    self._wait(E, ("d", q, j), self.dsem[q][j], 16 * uses)
        for e in self.COMPUTE:
            if self.ccount[e] > 0:
                self._wait(E, ("c", e), self.csem[e], self.ccount[e])

    def _barrier(self):
        self._wait_all("dve")
        ins = self.engs["dve"].memset(self.bar_tile, 0.0)
        self.ccount["dve"] += 1
        ins.then_inc(self.csem["dve"], 1)
        self.bar_sig = self.ccount["dve"]
        self.bar_clock = dict(self.clock["dve"])

    def finish(self, eng="sp"):
        self.flush(barrier=True)
        self._wait_all(eng)
        self.es.close()


NA_VARIANTS = [(-2, True), (-1, False), (0, False), (1, False), (2, True),
               (-3, False), (-2, False), (2, False), (3, False)]


def na_chunks(i):
    if i in (0, 1, 14, 15):
        cs = range(0, 4) if i < 2 else range(12, 16)
        out = []
        for c in cs:
            d = c - i
            t = {(-3): 5, (-2): 6, (-1): 1, 0: 2, 1: 3, 2: 7, 3: 8}[d]
            out.append((c, t))
        return out
    return [(i + d, d + 2) for d in range(-2, 3)]


def build_bias_index():
    idx = np.full((128, 9, 128), 15 * 31, dtype=np.int64)
    for t, (d, partial) in enumerate(NA_VARIANTS):
        for j in range(2):
            for jq in range(2):
                dr = 2 * d + j - jq
                if abs(dr) > 7:
                    continue
                if partial:
                    if d == -2 and not (j >= jq):
                        continue
                    if d == 2 and not (j == 0 and jq == 1):
                        continue
                for cq in range(64):
                    cstart = min(max(cq - 8, 0), 48)
                    for ck in range(cstart, cstart + 16):
                        idx[j * 64 + ck, t, jq * 64 + cq] = (dr + 7) * 31 + (ck - cq + 15)
    return idx


_BIAS_IDX = None


import os as _os
_NOCONV = bool(_os.environ.get('NOCONV'))


class _Stop(Exception):
    pass


def build(debug=False, stop=None):
    nc = bass.Bass("TRN2", target_bir_lowering=False)
    try:
        return _build(nc, debug, stop)
    except _Stop:
        return nc


def _build(nc, debug, stop):

    def din(name, shape, dt=F32):
        return nc.dram_tensor(name, shape, dt, kind="ExternalInput").ap()

    x = din("x", [SEQ, D])
    ctx = din("ctx", [CTX, D])
    ccol = din("ccol", [128, 16])
    w_mod = din("w_mod", [D, 6 * D])
    b_mod = din("b_mod", [1, 6 * D])
    n1c = din("n1c", [128, 8])
    n2c = din("n2c", [128, 8])
    n2b = din("n2b", [128, D])
    nfb = din("nfb", [128, D])
    w_in = din("w_in", [D, 4096])
    w_out = din("w_out", [D, D])
    wq = din("wq", [D, 2048])
    keysT = din("keysT", [128, 2048])
    u_t = din("u", [16384, D])
    v_t = din("v", [16384, D])
    lbraw = din("lbraw", [128, 16])
    hgn = din("hgn", [128, 512])
    biasT = din("biasT", [128, 8 * 9 * 128])
    cst = din("cst", [128, 528])
    rmask_d = din("rmask", [128, TOK])
    out = nc.dram_tensor("out", [SEQ, D], F32, kind="ExternalOutput").ap()
    scr_bc = nc.dram_tensor("scr_bc", [4, 128, D], F32, kind="Internal").ap()
    uv_bf = nc.dram_tensor("uv_bf", [16384, 2 * D], BF16, kind="Internal").ap()
    dbg = {}
    if debug:
        dbg["hT"] = nc.dram_tensor("d_hT", [128, 8 * TOK], BF16, kind="ExternalOutput").ap()
        dbg["mixT"] = nc.dram_tensor("d_mixT", [128, 8 * SEQ], BF16, kind="ExternalOutput").ap()
        dbg["x1"] = nc.dram_tensor("d_x1", [128, NT * D], F32, kind="ExternalOutput").ap()
        dbg["mc"] = nc.dram_tensor("d_mc", [128, 48], F32, kind="ExternalOutput").ap()
        dbg["eidx"] = nc.dram_tensor("d_eidx", [128, NT * 128], I32, kind="ExternalOutput").ap()
        dbg["gw"] = nc.dram_tensor("d_gw", [128, NT * 128], F32, kind="ExternalOutput").ap()

    es = contextlib.ExitStack()

    def sb(name, shape, dt=F32, stack=None):
        return (stack or es).enter_context(nc.sbuf_tensor(name, shape, dt))

    bar = sb("bar", [128, 1])
    cA = sb("cA", [128, 528])
    identb = sb("identb", [128, 128], BF16)
    mc = sb("mc", [128, 48])
    lb = sb("lb", [128, 8])
    oml = sb("oml", [128, 8])
    ps = [es.enter_context(nc.psum_tensor(f"ps{j}", [128, 512], F32)) for j in range(6)]
    pT = [es.enter_context(nc.psum_tensor(f"pT{j}", [128, 1024], BF16)) for j in range(2)]

    S = Sched(nc)
    S.bar_tile = bar[:]

    def phase_end(name):
        S.flush()
        if stop == name:
            S.finish()
            raise _Stop()

    IDENT = cA[:, 0:128]
    TRIF = cA[:, 128:256]
    TRIB = cA[:, 256:384]
    ONES = cA[:, 384:512]
    IOTA16 = cA[:, 512:528]

    with contextlib.ExitStack() as p0:
        cc = sb("cc", [128, 16], stack=p0)
        scl = sb("scl", [128, 8, 33], BF16, stack=p0)
        wm = [sb(f"wm{j}", [128, 8, 512], stack=p0) for j in range(3)]
        wmb = [sb(f"wmb{j}", [128, 8, 512], BF16, stack=p0) for j in range(2)]
        bm = sb("bm", [33, 6 * D], stack=p0)
        modrow = sb("modrow", [33, 6 * D], stack=p0)
        mcol = sb("mcol", [128, 48], stack=p0)
        n1 = sb("n1", [128, 8], stack=p0)
        n2 = sb("n2", [128, 8], stack=p0)
        n2bt = sb("n2bt", [128, D], stack=p0)
        bct = [sb(f"bct{j}", [128, D], stack=p0) for j in range(2)]
        lbr = sb("lbr", [128, 16], stack=p0)

        S.dma("sp", lambda e: e.dma_start(out=cA[:], in_=cst), writes=["cA"])
        S.dma("sp", lambda e: e.dma_start(out=cc[:], in_=ccol), writes=["cc"])
        S.dma("sp", lambda e: e.dma_start(out=n1[:], in_=n1c), writes=["n1"])
        S.dma("sp", lambda e: e.dma_start(out=n2[:], in_=n2c), writes=["n2"])
        S.dma("sp", lambda e: e.dma_start(out=lbr[:], in_=lbraw), writes=["lbr"])
        S.dma("sp", lambda e: e.dma_start(out=n2bt[:], in_=n2b), writes=["n2bt"])
        S.op("dve", lambda e: e.memset(bm[:], 0.0), writes=["bm"])
        S.dma("sp", lambda e: e.dma_start(out=bm[0:1, :], in_=b_mod), reads=["bm"], writes=["bm0"])
        S.dma("sp", lambda e: e.dma_start(out=bm[32:33, :], in_=b_mod), reads=["bm"], writes=["bm32"])
        S.op("dve", lambda e: e.tensor_copy(out=identb[:], in_=IDENT), reads=["cA"], writes=["identb"])
        S.op("dve", lambda e: e.memset(scl[:], 0.0), writes=["scl"])
        ccv = cc[:].rearrange("p (k t) -> p k t", t=2)
        S.op("act", lambda e: e.activation(out=scl[:, :, 0:1], in_=ccv[:, :, 0:1], func=AF.Silu),
             reads=["cc", "scl"], writes=["scl"])
        S.op("act", lambda e: e.activation(out=scl[:, :, 32:33], in_=ccv[:, :, 1:2], func=AF.Silu),
             reads=["cc", "scl"], writes=["scl"])
        S.op("dve", lambda e: e.tensor_tensor(out=lb[:], in0=lbr[:, 0:8], in1=lbr[:, 8:16], op=ALU.subtract),
             reads=["lbr"], writes=["lb"])
        S.op("act", lambda e: e.activation(out=lb[:], in_=lb[:], func=AF.Sigmoid), reads=["lb"], writes=["lb"])
        S.op("dve", lambda e: e.tensor_scalar(out=oml[:], in0=lb[:], scalar1=-1.0, scalar2=1.0,
                                              op0=ALU.mult, op1=ALU.add), reads=["lb"], writes=["oml"])
        for n in range(12):
            wb = wm[n % 3]
            S.dma("sp" if n % 2 == 0 else "act",
                  lambda e, n=n, wb=wb: e.dma_start(
                      out=wb[:], in_=w_mod[:, n * 512:(n + 1) * 512].rearrange("(k p) n -> p k n", p=128)),
                  writes=[("wm", n % 3)])
            wbb = wmb[n % 2]
            if n % 2 == 0:
                S.op("dve", lambda e, wb=wb, wbb=wbb: e.tensor_copy(out=wbb[:], in_=wb[:]),
                     reads=[("wm", n % 3)], writes=[("wmb", n % 2)])
            else:
                S.op("act", lambda e, wb=wb, wbb=wbb: e.activation(out=wbb[:], in_=wb[:], func=AF.Copy),
                     reads=[("wm", n % 3)], writes=[("wmb", n % 2)])
            pb = ps[n % 2]
            for k in range(8):
                S.op("pe", lambda e, k=k, wbb=wbb, pb=pb: e.matmul(pb[0:33, :], scl[:, k, :], wbb[:, k, :],
                                                                  start=(k == 0), stop=(k == 7)),
                     reads=["scl", ("wmb", n % 2)], writes=[("ps", n % 2)])
            S.op("dve", lambda e, n=n, pb=pb: e.tensor_tensor(out=modrow[:, n * 512:(n + 1) * 512], in0=pb[0:33, :],
                                                             in1=bm[:, n * 512:(n + 1) * 512], op=ALU.add),
                 reads=[("ps", n % 2), "bm", "bm0", "bm32"], writes=[("modrow", n)])
        mr_all = [("modrow", n) for n in range(12)]
        col_specs = [(0, 0), (0, 1), (0, 3), (0, 4), (32, 0), (32, 1)]
        for si, (r, vi) in enumerate(col_specs):
            for k in range(8):
                c0 = 2 * (si * 8 + k)
                S.op("pe", lambda e, r=r, vi=vi, k=k, c0=c0: e.matmul(
                    ps[2][:, c0:c0 + 2], modrow[r:r + 1, vi * D + k * 128: vi * D + (k + 1) * 128],
                    cA[r:r + 1, 384:386], start=True, stop=True),
                    reads=mr_all + ["cA"], writes=[("ps", 2)])
        S.op("dve", lambda e: e.tensor_copy(out=mcol[:].unsqueeze(2), in_=ps[2][:, 0:96].rearrange("p (c two) -> p c two", two=2)[:, :, 0:1]), reads=[("ps", 2)], writes=["mcol"])
        S.op("dve", lambda e: e.scalar_tensor_tensor(out=mc[:, 0:8], in0=mcol[:, 8:16], scalar=1.0, in1=n1[:],
                                                     op0=ALU.add, op1=ALU.mult), reads=["mcol", "n1"], writes=["mc0"])
        S.op("dve", lambda e: e.tensor_copy(out=mc[:, 8:16], in_=mcol[:, 0:8]), reads=["mcol"], writes=["mc1"])
        S.op("dve", lambda e: e.scalar_tensor_tensor(out=mc[:, 16:24], in0=mcol[:, 40:48], scalar=1.0, in1=n1[:],
                                                     op0=ALU.add, op1=ALU.mult), reads=["mcol", "n1"], writes=["mc2"])
        S.op("dve", lambda e: e.tensor_copy(out=mc[:, 24:32], in_=mcol[:, 32:40]), reads=["mcol"], writes=["mc3"])
        S.op("dve", lambda e: e.scalar_tensor_tensor(out=mc[:, 32:40], in0=mcol[:, 24:32], scalar=1.0, in1=n2[:],
                                                     op0=ALU.add, op1=ALU.mult), reads=["mcol", "n2"], writes=["mc4"])
        S.op("dve", lambda e: e.tensor_copy(out=mc[:, 40:48], in_=mcol[:, 16:24]), reads=["mcol"], writes=["mc5"])
        for j, (vi, kind) in enumerate([(2, "copy"), (5, "copy"), (4, "g2"), (3, "copy")]):
            bt_ = bct[j % 2]
            for hf in range(2):
                pb = ps[3 + hf]
                S.op("pe", lambda e, vi=vi, hf=hf, pb=pb: e.matmul(
                    pb[:, :], cA[0:1, 384:512], modrow[0:1, vi * D + hf * 512: vi * D + (hf + 1) * 512],
                    start=True, stop=True), reads=mr_all + ["cA"], writes=[("ps", 3 + hf)])
                if kind == "copy":
                    S.op("dve", lambda e, bt_=bt_, hf=hf, pb=pb: e.tensor_copy(out=bt_[:, hf * 512:(hf + 1) * 512], in_=pb[:, :]),
                         reads=[("ps", 3 + hf)], writes=[("bct", j % 2, hf)])
                else:
                    S.op("dve", lambda e, bt_=bt_, hf=hf, pb=pb: e.scalar_tensor_tensor(
                        out=bt_[:, hf * 512:(hf + 1) * 512], in0=pb[:, :], scalar=1.0,
                        in1=n2bt[:, hf * 512:(hf + 1) * 512], op0=ALU.add, op1=ALU.mult),
                        reads=[("ps", 3 + hf), "n2bt"], writes=[("bct", j % 2, hf)])
            S.dma("sp", lambda e, j=j, bt_=bt_: e.dma_start(out=scr_bc[j], in_=bt_[:]),
                  reads=[("bct", j % 2, 0), ("bct", j % 2, 1)], writes=[("scr", j)])
        if debug:
            S.dma("sp", lambda e: e.dma_start(out=dbg["mc"], in_=mc[:]), reads=[f"mc{j}" for j in range(6)])
        phase_end("p0")

    R = sb("R", [128, NT * D])
    x1 = R[:].rearrange("p (t d) -> p t d", d=D)
    hT = R[:, 0:9216].bitcast(BF16).rearrange("p (k t) -> p k t", k=8)
    vtok = R[:, 9216:13824].bitcast(BF16).rearrange("p (t d) -> p t d", d=512)
    with contextlib.ExitStack() as pm:
        mixT = sb("mixT", [128, 8, SEQ], BF16, stack=pm)

        with contextlib.ExitStack() as p1:
            xt = [sb(f"xt{j}", [128, D], stack=p1) for j in range(2)]
            xs = [sb(f"xs{j}", [128, D], BF16, stack=p1) for j in range(2)]
            junk = sb("junk1", [128, D], BF16, stack=p1)
            st = sb("st1", [128, 3 * NTT], stack=p1)
            for T in range(NTT):
                b = T % 2
                src = x[T * 128:(T + 1) * 128, :] if T < NT else ctx[(T - NT) * 128:(T - NT + 1) * 128, :]
                S.dma("sp" if b == 0 else "act", lambda e, b=b, src=src: e.dma_start(out=xt[b][:], in_=src),
                      writes=[("xt", b)])
                S.op("act", lambda e, b=b, T=T: e.activation(out=junk[:], in_=xt[b][:], func=AF.Square,
                                                             accum_out=st[:, T:T + 1]),
                     reads=[("xt", b)], writes=["junk", ("ssq", T)])
                S.op("dve", lambda e, T=T: e.tensor_scalar(out=st[:, NTT + T:NTT + T + 1], in0=st[:, T:T + 1],
                                                           scalar1=1.0 / D, scalar2=EPS, op0=ALU.mult, op1=ALU.add),
                     reads=[("ssq", T)], writes=[("ms", T)])
                S.op("act", lambda e, T=T: e.activation(out=st[:, NTT + T:NTT + T + 1], in_=st[:, NTT + T:NTT + T + 1],
                                                        func=AF.Sqrt), reads=[("ms", T)], writes=[("ms", T)])
                S.op("dve", lambda e, T=T: e.reciprocal(out=st[:, 2 * NTT + T:2 * NTT + T + 1],
                                                        in_=st[:, NTT + T:NTT + T + 1]),
                     reads=[("ms", T)], writes=[("rstd", T)])
                S.op("act", lambda e, b=b, T=T: e.activation(out=xs[b][:], in_=xt[b][:], func=AF.Copy,
                                                             scale=st[:, 2 * NTT + T:2 * NTT + T + 1]),
                     reads=[("xt", b), ("rstd", T)], writes=[("xs", b)])
                for k in range(8):
                    S.op("pe", lambda e, b=b, k=k: e.transpose(pT[b][:, k * 128:(k + 1) * 128],
                                                               xs[b][:, k * 128:(k + 1) * 128], identb[:]),
                         reads=[("xs", b), "identb"], writes=[("pT", b)])
                go, so = (0, 8) if T < NT else (16, 24)
                for k in range(8):
                    S.op("dve", lambda e, b=b, k=k, T=T, go=go, so=so: e.tensor_scalar(
                        out=hT[:, k, T * 128:(T + 1) * 128], in0=pT[b][:, k * 128:(k + 1) * 128],
                        scalar1=mc[:, go + k:go + k + 1], scalar2=mc[:, so + k:so + k + 1],
                        op0=ALU.mult, op1=ALU.add),
                        reads=[("pT", b)], writes=[("hT", T)])
            if debug:
                S.dma("sp", lambda e: e.dma_start(out=dbg["hT"], in_=R[:, 0:9216].bitcast(BF16)),
                      reads=[("hT", T) for T in range(NTT)])
            phase_end("p1")
        hT_all = [("hT", T) for T in range(NTT)]

        def load_w(tile_ap, dram_w, col0, ncols, key):
            for k0 in range(0, 8, 4):
                S.dma("pool", lambda e, k0=k0: e.dma_start(
                    out=tile_ap[:, k0:k0 + 4, :],
                    in_=dram_w[k0 * 128:(k0 + 4) * 128, col0:col0 + ncols].rearrange("(k p) n -> p k n", p=128)),
                    writes=[(key, k0)])
            return [(key, 0), (key, 4)]

        with contextlib.ExitStack() as p2:
            qT = sb("qT", [128, 4, SEQ], BF16, stack=p2)
            kT = sb("kT", [128, 4, TOK], BF16, stack=p2)
            vaug = sb("vaug", [128, NTT, 8, 65], BF16, stack=p2)
            p2a = contextlib.ExitStack()
            wna = sb("wna", [128, 8, 1536], BF16, stack=p2a)
            wk = load_w(wna, w_in, 0, 1536, "wna")
            S.op("pool", lambda e: e.memset(vaug[:, :, :, 64:65], 1.0), writes=["vones"])
            cnt = 0
            for which, dst, ntok, cbase in (("q", qT, SEQ, 0), ("k", kT, TOK, 512)):
                for hp in range(4):
                    for t0 in range(0, ntok, 512):
                        tw = min(512, ntok - t0)
                        pb = cnt % 4
                        for k in range(8):
                            S.op("pe", lambda e, k=k, hp=hp, t0=t0, tw=tw, pb=pb, cbase=cbase: e.matmul(
                                ps[pb][:, 0:tw], wna[:, k, cbase + hp * 128: cbase + (hp + 1) * 128],
                                hT[:, k, t0:t0 + tw], start=(k == 0), stop=(k == 7)),
                                reads=wk + hT_all, writes=[("ps", pb)])
                        eng = "act" if cnt % 2 == 0 else "dve"
                        if eng == "act":
                            S.op("act", lambda e, dst=dst, hp=hp, t0=t0, tw=tw, pb=pb: e.activation(
                                out=dst[:, hp, t0:t0 + tw], in_=ps[pb][:, 0:tw], func=AF.Copy),
                                reads=[("ps", pb)], writes=[(which, hp, t0)])
                        else:
                            S.op("dve", lambda e, dst=dst, hp=hp, t0=t0, tw=tw, pb=pb: e.tensor_copy(
                                out=dst[:, hp, t0:t0 + tw], in_=ps[pb][:, 0:tw]),
                                reads=[("ps", pb)], writes=[(which, hp, t0)])
                        cnt += 1
            for T in range(NTT):
                pb = cnt % 4
                for k in range(8):
                    S.op("pe", lambda e, k=k, T=T, pb=pb: e.matmul(
                        ps[pb][:, :], hT[:, k, T * 128:(T + 1) * 128], wna[:, k, 1024:1536],
                        start=(k == 0), stop=(k == 7)), reads=wk + hT_all, writes=[("ps", pb)])
                if cnt % 2 == 0:
                    S.op("act", lambda e, T=T, pb=pb: e.activation(
                        out=vaug[:, T, :, 0:64], in_=ps[pb][:, :].rearrange("p (h d) -> p h d", d=64), func=AF.Copy),
                        reads=[("ps", pb)], writes=[("v", T)])
                else:
                    S.op("dve", lambda e, T=T, pb=pb: e.tensor_copy(
                        out=vaug[:, T, :, 0:64], in_=ps[pb][:, :].rearrange("p (h d) -> p h d", d=64)),
                        reads=[("ps", pb)], writes=[("v", T)])
                cnt += 1
            phase_end("p2a")
            p2a.close()
            bt = sb("bt", [128, 8, 9, 128], stack=p2)
            Ssb = [sb(f"Ssb{j}", [128, 640], stack=p2) for j in range(2)]
            Pb = [sb(f"Pb{j}", [128, 896], BF16, stack=p2) for j in range(2)]
            rden = sb("rden", [128, 16], stack=p2)
            natok = [sb(f"natok{j}", [128, 512], BF16, stack=p2) for j in range(2)]
            S.dma("sp", lambda e: e.dma_start(out=bt[:].rearrange("p h t q -> p (h t q)"), in_=biasT), writes=["bt"])

            qk_all_r = []
            it = 0
            for i in range(NT):
                chunks = na_chunks(i)
                nw = len(chunks)
                nb = i % 2
                for h in range(8):
                    hp, po = h // 2, (h % 2) * 64
                    sbuf_i = it % 2
                    b0, b1 = ps[2 * sbuf_i], ps[2 * sbuf_i + 1]

                    def sloc(j):
                        return (b0, j * 128) if j < 4 else (b1, (j - 4) * 128)
                    for j, (c, t) in enumerate(chunks):
                        bk, co = sloc(j)
                        S.op("pe", lambda e, bk=bk, co=co, c=c, hp=hp, po=po, i=i: e.matmul(
                            bk[:, co:co + 128], kT[po:po + 64, hp, c * 128:(c + 1) * 128],
                            qT[po:po + 64, hp, i * 128:(i + 1) * 128], start=True, stop=True),
                            reads=[], writes=[("psS", sbuf_i, j // 4)])
                    for cc_ in range(2):
                        S.op("pe", lambda e, cc_=cc_, hp=hp, po=po, i=i, b1=b1: e.matmul(
                            b1[:, 128 + cc_ * 128: 256 + cc_ * 128],
                            kT[po:po + 64, hp, SEQ + cc_ * 128: SEQ + (cc_ + 1) * 128],
                            qT[po:po + 64, hp, i * 128:(i + 1) * 128], start=True, stop=True),
                            reads=[], writes=[("psS", sbuf_i, 1)])
                    for j, (c, t) in enumerate(chunks):
                        bk, co = sloc(j)
                        S.op("dve", lambda e, bk=bk, co=co, j=j, t=t, h=h, sbuf_i=sbuf_i: e.scalar_tensor_tensor(
                            out=Ssb[sbuf_i][:, j * 128:(j + 1) * 128], in0=bk[:, co:co + 128], scalar=0.125,
                            in1=bt[:, h, t, :], op0=ALU.mult, op1=ALU.add),
                            reads=[("psS", sbuf_i, j // 4), "bt"], writes=[("Ssb", sbuf_i)])
                    S.op("act", lambda e, nw=nw, sbuf_i=sbuf_i: e.activation(
                        out=Pb[sbuf_i][:, 0:nw * 128], in_=Ssb[sbuf_i][:, 0:nw * 128], func=AF.Exp),
                        reads=[("Ssb", sbuf_i)], writes=[("Pw", sbuf_i)])
                    S.op("act", lambda e, sbuf_i=sbuf_i, b1=b1: e.activation(
                        out=Pb[sbuf_i][:, 640:896], in_=b1[:, 128:384], func=AF.Exp, scale=0.125),
                        reads=[("psS", sbuf_i, 1)], writes=[("Pc", sbuf_i)])
                    ob = ps[4 + h // 4]
                    oc = (h % 4) * 128
                    nmm = nw + 2
                    for j, (c, t) in enumerate(chunks):
                        S.op("pe", lambda e, j=j, c=c, h=h, ob=ob, oc=oc, sbuf_i=sbuf_i, nmm=nmm: e.matmul(
                            ob[:, oc:oc + 65], Pb[sbuf_i][:, j * 128:(j + 1) * 128], vaug[:, c, h, :],
                            start=(j == 0), stop=False),
                            reads=[("Pw", sbuf_i)], writes=[("psO", h)])
                    for cc_ in range(2):
                        S.op("pe", lambda e, cc_=cc_, h=h, ob=ob, oc=oc, sbuf_i=sbuf_i: e.matmul(
                            ob[:, oc:oc + 65], Pb[sbuf_i][:, 640 + cc_ * 128: 768 + cc_ * 128], vaug[:, NT + cc_, h, :],
                            start=False, stop=(cc_ == 1)),
                            reads=[("Pc", sbuf_i)], writes=[("psO", h)])
                    S.op("dve", lambda e, h=h, ob=ob, oc=oc: e.reciprocal(out=rden[:, h:h + 1], in_=ob[:, oc + 64:oc + 65]),
                         reads=[("psO", h)], writes=[("rden", h)])
                    S.op("dve", lambda e, h=h, ob=ob, oc=oc, nb=nb: e.tensor_scalar(
                        out=natok[nb][:, h * 64:(h + 1) * 64], in0=ob[:, oc:oc + 64], scalar1=rden[:, h:h + 1],
                        scalar2=None, op0=ALU.mult),
                        reads=[("psO", h), ("rden", h)], writes=[("natok", nb)])
                    it += 1
                for j in range(4):
                    S.op("pe", lambda e, j=j, nb=nb: e.transpose(pT[nb][:, j * 128:(j + 1) * 128],
                                                                natok[nb][:, j * 128:(j + 1) * 128], identb[:]),
                         reads=[("natok", nb)], writes=[("pT", nb)])
                S.op("act", lambda e, i=i, nb=nb: e.activation(
                    out=mixT[:, 0:4, i * 128:(i + 1) * 128],
                    in_=pT[nb][:, 0:512].rearrange("p (j t) -> p j t", t=128), func=AF.Copy),
                    reads=[("pT", nb)], writes=[("mixna", i)])
            phase_end("p2")

        with contextlib.ExitStack() as p3:
            oacc = sb("oacc", [128, NT, 512], stack=p3)
            with contextlib.ExitStack() as p3a:
                wv = sb("wv", [128, 8, 512], BF16, stack=p3a)
                wk = load_w(wv, w_in, 3 * 512 + 3 * 512, 512, "wv")
                for T in range(NTT):
                    pb = T % 4
                    for k in range(8):
                        S.op("pe", lambda e, k=k, T=T, pb=pb: e.matmul(
                            ps[pb][:, :], hT[:, k, T * 128:(T + 1) * 128], wv[:, k, :],
                            start=(k == 0), stop=(k == 7)), reads=wk, writes=[("ps", pb)])
                    if T % 2 == 0:
                        S.op("act", lambda e, T=T, pb=pb: e.activation(out=vtok[:, T, :], in_=ps[pb][:, :], func=AF.Copy),
                             reads=[("ps", pb)], writes=[("vtok", T)])
                    else:
                        S.op("dve", lambda e, T=T, pb=pb: e.tensor_copy(out=vtok[:, T, :], in_=ps[pb][:, :]),
                             reads=[("ps", pb)], writes=[("vtok", T)])
                phase_end("p3a")
            with contextlib.ExitStack() as p3b:
                rmask = sb("rmask_sb", [128, TOK], stack=p3b)
                A_ = sb("hgA", [128, TOK], stack=p3b)
                B_ = sb("hgB", [128, TOK], stack=p3b)
                C_ = sb("hgC", [128, TOK], stack=p3b)
                qsb = sb("hgqs", [128, 512], stack=p3b)
                Qt = sb("hgQt", [128, SEQ], BF16, stack=p3b)
                Qs = sb("hgQs", [128, SEQ], BF16, stack=p3b)
                Ks = sb("hgKs", [128, TOK], BF16, stack=p3b)
                Kf = sb("hgKf", [128, TOK], BF16, stack=p3b)
                Ktok = sb("hgKtok", [128, NTT, 128], BF16, stack=p3b)
                wqf = sb("hgwqf", [128, 8, 256], BF16, stack=p3b)
                sc_ = sb("hgsc", [128, 5, NCH], stack=p3b)
                Sst = [sb(f"hgS{j}", [128, 128], stack=p3b) for j in range(2)]
                Usc = [sb(f"hgUsc{j}", [128, 128], stack=p3b) for j in range(4)]
                Smb = [sb(f"hgSmb{j}", [128, 128], BF16, stack=p3b) for j in range(2)]
                Qe = sb("hgQe", [128, SEQ], BF16, stack=p3b)
                Qo = sb("hgQo", [128, SEQ], BF16, stack=p3b)
                ATs = [sb(f"hgATs{j}", [128, 128], BF16, stack=p3b) for j in range(2)]

                S.dma("sp", lambda e: e.dma_start(out=rmask[:], in_=rmask_d), writes=["rmask"])

                def v3(t, n=TOK):
                    return t[:, 0:n].rearrange("p (t s) -> p t s", s=64)

                for dr in range(2):
                    for hh in range(4):
                        dh = dr * 4 + hh
                        S.dma("pool", lambda e, hh=hh: e.dma_start(
                            out=wqf[:, :, 0:128],
                            in_=w_in[:, 1536 + hh * 128:1536 + (hh + 1) * 128].rearrange("(k p) n -> p k n", p=128)),
                            writes=["wq_h"])
                        S.dma("pool", lambda e, hh=hh, dr=dr: e.dma_start(
                            out=wqf[:, :, 128:256],
                            in_=w_in[:, 2048 + dr * 512 + hh * 128:2048 + dr * 512 + (hh + 1) * 128].rearrange(
                                "(k p) n -> p k n", p=128)), writes=["wf_h"])
                        for ci, t0 in enumerate(range(0, TOK, 512)):
                            tw = min(512, TOK - t0)
                            pb = ci % 4
                            for k in range(8):
                                S.op("pe", lambda e, k=k, t0=t0, tw=tw, pb=pb: e.matmul(
                                    ps[pb][:, 0:tw], wqf[:, k, 128:256], hT[:, k, t0:t0 + tw],
                                    start=(k == 0), stop=(k == 7)), reads=["wf_h"], writes=[("ps", pb)])
                            S.op("act", lambda e, t0=t0, tw=tw, pb=pb: e.activation(
                                out=A_[:, t0:t0 + tw], in_=ps[pb][:, 0:tw], func=AF.Sigmoid),
                                reads=[("ps", pb)], writes=["A"])
                        S.op("dve", lambda e, dh=dh: e.tensor_scalar(out=A_[:], in0=A_[:], scalar1=oml[:, dh:dh + 1],
                                                                     scalar2=lb[:, dh:dh + 1], op0=ALU.mult, op1=ALU.add),
                             reads=["A"], writes=["A"])
                        S.op("act", lambda e: e.activation(out=B_[:], in_=A_[:], func=AF.Ln), reads=["A"], writes=["B"])
                        S.op("dve", lambda e: e.tensor_scalar(out=A_[:], in0=A_[:], scalar1=-1.0, scalar2=1.0,
                                                              op0=ALU.mult, op1=ALU.add), reads=["A", "B"], writes=["A"])
                        S.op("dve", lambda e: e.tensor_tensor_scan(out=C_[:], data0=rmask[:], data1=B_[:], initial=0.0,
                                                                   op0=ALU.mult, op1=ALU.add),
                             reads=["B", "rmask"], writes=["C"])
                        if dr == 0:
                            gbuf, gkey = C_, "C"
                            refpos = 31
                        else:
                            S.op("dve", lambda e: e.tensor_tensor(out=B_[:], in0=B_[:], in1=C_[:], op=ALU.subtract),
                                 reads=["B", "C"], writes=["B"])
                            S.op("dve", lambda e: e.tensor_tensor(
                                out=v3(B_), in0=v3(B_), in1=v3(C_)[:, :, 63:64].to_broadcast([128, NCH, 64]), op=ALU.add),
                                reads=["B", "C"], writes=["B"])
                            gbuf, gkey = B_, "B"
                            refpos = 32
                        endpos = 63 if dr == 0 else 0
                        S.op("dve", lambda e, gbuf=gbuf, refpos=refpos: e.tensor_copy(
                            out=sc_[:, 0, :].unsqueeze(2), in_=v3(gbuf)[:, :, refpos:refpos + 1]), reads=[gkey], writes=["sc0"])
                        S.op("dve", lambda e, gbuf=gbuf, endpos=endpos: e.tensor_copy(
                            out=sc_[:, 1, :].unsqueeze(2), in_=v3(gbuf)[:, :, endpos:endpos + 1]), reads=[gkey], writes=["sc1"])
                        S.op("act", lambda e: e.activation(out=sc_[:, 2:4, :], in_=sc_[:, 0:2, :], func=AF.Exp),
                             reads=["sc0", "sc1"], writes=["sc23"])
                        S.op("dve", lambda e: e.tensor_tensor(out=sc_[:, 4, :], in0=sc_[:, 1, :], in1=sc_[:, 0, :],
                                                              op=ALU.subtract), reads=["sc0", "sc1"], writes=["sc4"])
                        S.op("act", lambda e: e.activation(out=sc_[:, 4, :], in_=sc_[:, 4, :], func=AF.Exp),
                             reads=["sc4"], writes=["sc4"])
                        obuf, okey = (B_, "B") if dr == 0 else (C_, "C")
                        S.op("dve", lambda e, gbuf=gbuf: e.tensor_tensor(
                            out=v3(gbuf), in0=v3(gbuf), in1=sc_[:, 0, :].unsqueeze(2).to_broadcast([128, NCH, 64]),
                            op=ALU.subtract), reads=[gkey, "sc0", "sc1"], writes=[gkey])
                        S.op("act", lambda e, gbuf=gbuf, obuf=obuf: e.activation(out=obuf[:, 0:SEQ], in_=gbuf[:, 0:SEQ], func=AF.Exp),
                             reads=[gkey, okey], writes=[okey])
                        S.op("act", lambda e, gbuf=gbuf: e.activation(out=gbuf[:], in_=gbuf[:], func=AF.Exp, scale=-1.0),
                             reads=[gkey, okey], writes=[gkey])
                        S.op("dve", lambda e, gbuf=gbuf: e.tensor_tensor(out=Kf[:], in0=A_[:], in1=gbuf[:], op=ALU.mult),
                             reads=["A", gkey], writes=["Kf"])
                        hk = 1 if dr == 0 else 0
                        hq = 1 - hk
                        def h32(t):
                            return t[:].rearrange("p (t two s) -> p t two s", two=2, s=32)

                        def h64(t):
                            return t[:].rearrange("p (t two s) -> p t two s", two=2, s=64)
                        if hh == 0:
                            S.op("pool", lambda e: e.memset(Ks[:], 0.0), reads=["Ks"], writes=["Ks"])
                            S.op("pool", lambda e: e.memset(Qs[:], 0.0), reads=["Qs"], writes=["Qs"])
                            if dr == 0:
                                S.op("pool", lambda e: e.memset(Qe[:], 0.0), reads=["Qe"], writes=["Qe"])
                                S.op("pool", lambda e: e.memset(Qo[:], 0.0), reads=["Qo"], writes=["Qo"])
                        S.op("dve", lambda e, hk=hk: e.tensor_copy(out=h32(Ks)[:, :, 1 - hk, :], in_=h32(Kf)[:, :, 1 - hk, :]),
                             reads=["Kf", "Ks"], writes=["Ks"])
                        for ci, t0 in enumerate(range(0, SEQ, 512)):
                            pb = ci % 4
                            for k in range(8):
                                S.op("pe", lambda e, k=k, t0=t0, pb=pb: e.matmul(
                                    ps[pb][:, :], wqf[:, k, 0:128], hT[:, k, t0:t0 + 512],
                                    start=(k == 0), stop=(k == 7)), reads=["wq_h"], writes=[("ps", pb)])
                            S.op("act", lambda e, pb=pb: e.activation(out=qsb[:], in_=ps[pb][:, :], func=AF.Silu),
                                 reads=[("ps", pb)], writes=["qsb"])
                            S.op("dve", lambda e, t0=t0, obuf=obuf: e.tensor_tensor(out=Qt[:, t0:t0 + 512], in0=qsb[:],
                                                                                    in1=obuf[:, t0:t0 + 512], op=ALU.mult),
                                 reads=["qsb", okey], writes=["Qt"])
                        S.op("dve", lambda e, hq=hq: e.tensor_copy(out=h32(Qs)[:, :, 1 - hq, :], in_=h32(Qt)[:, :, 1 - hq, :]),
                             reads=["Qt", "Qs"], writes=["Qs"])
                        S.op("act", lambda e: e.activation(out=h64(Qe)[:, :, 0, :], in_=h64(Qt)[:, :, 0, :], func=AF.Copy),
                             reads=["Qt", "Qe"], writes=["Qe"])
                        S.op("act", lambda e: e.activation(out=h64(Qo)[:, :, 1, :], in_=h64(Qt)[:, :, 1, :], func=AF.Copy),
                             reads=["Qt", "Qo"], writes=["Qo"])
                        for T in range(NTT):
                            nb = T % 2
                            S.op("pe", lambda e, T=T, nb=nb: e.transpose(pT[nb][:, 0:128], Kf[:, T * 128:(T + 1) * 128], identb[:]),
                                 reads=["Kf"], writes=[("pT", nb)])
                            S.op("act", lambda e, T=T, nb=nb: e.activation(out=Ktok[:, T, :], in_=pT[nb][:, 0:128], func=AF.Copy),
                                 reads=[("pT", nb)], writes=[("Ktok", T)])
                        S.op("pool", lambda e, hk=hk: e.memset(
                            Kf[:].rearrange("p (t two s) -> p t two s", two=2, s=32)[:, :, 1 - hk, :], 0.0),
                            reads=["Kf"], writes=["Kf"])
                        order = [16, 17] + list(range(NT)) if dr == 0 else [17, 16] + list(range(NT - 1, -1, -1))
                        tri = TRIF if dr == 0 else TRIB
                        kcnt = [0]
                        S.op("dve", lambda e: e.memset(Sst[0][:], 0.0), reads=[("S", 0)], writes=[("S", 0)])

                        def chunks_of(T):
                            return [2 * T, 2 * T + 1] if dr == 0 else [2 * T + 1, 2 * T]

                        def stage_a(n):
                            T = order[n]
                            q = n % 2
                            for ci, c in enumerate(chunks_of(T)):
                                par = c % 2
                                S.op("pe", lambda e, T=T, par=par, ci=ci, hh=hh: e.matmul(
                                    ps[2 + ci][:, 0:128], Ktok[par * 64:(par + 1) * 64, T, :],
                                    vtok[par * 64:(par + 1) * 64, T, hh * 128:(hh + 1) * 128], start=True, stop=True),
                                    reads=[("Ktok", T)], writes=[("ps", 2 + ci)])
                                S.op("dve", lambda e, c=c, ci=ci, q=q: e.tensor_scalar(
                                    out=Usc[2 * q + ci][:], in0=ps[2 + ci][:, 0:128], scalar1=sc_[:, 4, c:c + 1], scalar2=None,
                                    op0=ALU.mult), reads=[("ps", 2 + ci), "sc4"], writes=[("Usc", 2 * q + ci)])
                            if T < NT:
                                S.op("pe", lambda e, T=T: e.matmul(ps[4][:, 0:128], Ks[:, T * 128:(T + 1) * 128],
                                                                   Qt[:, T * 128:(T + 1) * 128], start=True, stop=False),
                                     reads=["Ks", "Qt"], writes=[("ps", 4)])
                                S.op("pe", lambda e, T=T: e.matmul(ps[4][:, 0:128], Kf[:, T * 128:(T + 1) * 128],
                                                                   Qs[:, T * 128:(T + 1) * 128], start=False, stop=True),
                                     reads=["Kf", "Qs"], writes=[("ps", 4)])

                        def stage_a2(n):
                            T = order[n]
                            q = n % 2
                            if T < NT:
                                S.op("dve", lambda e, q=q, tri=tri: e.tensor_tensor(out=ATs[q][:], in0=ps[4][:, 0:128], in1=tri, op=ALU.mult),
                                     reads=[("ps", 4), "cA"], writes=[("ATs", q)])
                                ob = ps[5] if q == 0 else ps[1]
                                S.op("pe", lambda e, T=T, q=q, ob=ob, hh=hh: e.matmul(ob[:, 0:128], ATs[q][:], vtok[:, T, hh * 128:(hh + 1) * 128],
                                                                               start=True, stop=False),
                                     reads=[("ATs", q)], writes=[("ps", 5 if q == 0 else 1)])

                        def stage_b(n):
                            T = order[n]
                            q = n % 2
                            lat = T < NT
                            ob = ps[5] if q == 0 else ps[1]
                            for ci, c in enumerate(chunks_of(T)):
                                par = c % 2
                                k = kcnt[0]
                                kcnt[0] += 1
                                si, so = k % 2, (k + 1) % 2
                                if lat:
                                    S.op("act", lambda e, c=c, ci=ci, si=si: e.activation(
                                        out=Smb[ci][:], in_=Sst[si][:], func=AF.Copy, scale=sc_[:, 2, c:c + 1]),
                                        reads=[("S", si), "sc23"], writes=[("Smb", ci)])
                                    Qz, qzk = (Qe, "Qe") if par == 0 else (Qo, "Qo")
                                    S.op("pe", lambda e, T=T, ci=ci, Qz=Qz, ob=ob: e.matmul(
                                        ob[:, 0:128], Qz[:, T * 128:(T + 1) * 128], Smb[ci][:], start=False, stop=(ci == 1)),
                                        reads=[("Smb", ci), qzk], writes=[("ps", 5 if q == 0 else 1)])
                                S.op("dve", lambda e, c=c, ci=ci, q=q, si=si, so=so: e.scalar_tensor_tensor(
                                    out=Sst[so][:], in0=Sst[si][:], scalar=sc_[:, 3, c:c + 1], in1=Usc[2 * q + ci][:],
                                    op0=ALU.mult, op1=ALU.add), reads=[("Usc", 2 * q + ci), ("S", si), "sc23"], writes=[("S", so)])
                            if lat:
                                if dr == 0:
                                    S.op("act", lambda e, T=T, ob=ob, hh=hh: e.activation(
                                        out=oacc[:, T, hh * 128:(hh + 1) * 128], in_=ob[:, 0:128], func=AF.Copy),
                                        reads=[("ps", 5 if q == 0 else 1)], writes=[("oacc", T, hh)])
                                else:
                                    S.op("dve", lambda e, T=T, ob=ob, hh=hh: e.tensor_tensor(
                                        out=oacc[:, T, hh * 128:(hh + 1) * 128], in0=ob[:, 0:128],
                                        in1=oacc[:, T, hh * 128:(hh + 1) * 128], op=ALU.add),
                                        reads=[("ps", 5 if q == 0 else 1), ("oacc", T, hh)], writes=[("oacc", T, hh)])

                        stage_a(0)
                        stage_a2(0)
                        for n in range(len(order)):
                            if n + 1 < len(order):
                                stage_a(n + 1)
                            stage_b(n)
                            if n + 1 < len(order):
                                stage_a2(n + 1)
                        if kcnt[0] % 2 == 1:
                            pass
                phase_end("p3b")
            with contextlib.ExitStack() as p3c:
                wg = sb("wg", [128, 8, 512], BF16, stack=p3c)
                hgnb = sb("hgnb", [128, 512], stack=p3c)
                sg = [sb(f"sg{j}", [128, 512], stack=p3c) for j in range(2)]
                yb = [sb(f"yb{j}", [128, 512], stack=p3c) for j in range(2)]
                yt = [sb(f"yt{j}", [128, 512], BF16, stack=p3c) for j in range(2)]
                jk = sb("jk3", [128, 128], BF16, stack=p3c)
                st3 = sb("st3", [128, NT, 8], stack=p3c)
                wk = load_w(wg, w_in, 1536 + 4 * 512, 512, "wg")
                S.dma("sp", lambda e: e.dma_start(out=hgnb[:], in_=hgn), writes=["hgnb"])
                for T in range(NT):
                    for hh in range(4):
                        S.op("act", lambda e, T=T, hh=hh: e.activation(
                            out=jk[:], in_=oacc[:, T, hh * 128:(hh + 1) * 128], func=AF.Square,
                            accum_out=st3[:, T, hh:hh + 1]), reads=[], writes=["jk3", ("ss3", T)])
                ss_all = [("ss3", T) for T in range(NT)]
                S.op("dve", lambda e: e.tensor_scalar(out=st3[:, :, 4:8], in0=st3[:, :, 0:4], scalar1=1.0 / 128,
                                                      scalar2=EPS, op0=ALU.mult, op1=ALU.add),
                     reads=ss_all, writes=["ms3"])
                S.op("act", lambda e: e.activation(out=st3[:, :, 4:8], in_=st3[:, :, 4:8], func=AF.Sqrt),
                     reads=["ms3"], writes=["ms3"])
                S.op("dve", lambda e: e.reciprocal(out=st3[:, :, 0:4], in_=st3[:, :, 4:8]),
                     reads=["ms3"] + ss_all, writes=["rs3"])
                for T in range(NT):
                    b = T % 2
                    for k in range(8):
                        S.op("pe", lambda e, k=k, T=T, b=b: e.matmul(
                            ps[b][:, :], hT[:, k, T * 128:(T + 1) * 128], wg[:, k, :],
                            start=(k == 0), stop=(k == 7)), reads=wk, writes=[("ps", b)])
                    S.op("act", lambda e, b=b: e.activation(out=sg[b][:], in_=ps[b][:, :], func=AF.Silu),
                         reads=[("ps", b)], writes=[("sg", b)])
                    S.op("dve", lambda e, T=T, b=b: e.tensor_tensor(
                        out=yb[b][:].rearrange("p (h d) -> p h d", d=128),
                        in0=oacc[:, T, :].rearrange("p (h d) -> p h d", d=128),
                        in1=st3[:, T, 0:4].unsqueeze(2).to_broadcast([128, 4, 128]), op=ALU.mult),
                        reads=["rs3"], writes=[("yb", b)])
                    S.op("dve", lambda e, b=b: e.tensor_tensor(out=yb[b][:], in0=yb[b][:], in1=hgnb[:], op=ALU.mult),
                         reads=[("yb", b), "hgnb"], writes=[("yb", b)])
                    S.op("dve", lambda e, b=b: e.tensor_tensor(out=yt[b][:], in0=yb[b][:], in1=sg[b][:], op=ALU.mult),
                         reads=[("yb", b), ("sg", b)], writes=[("yt", b)])
                    for j in range(4):
                        S.op("pe", lambda e, j=j, b=b: e.transpose(pT[b][:, j * 128:(j + 1) * 128],
                                                                   yt[b][:, j * 128:(j + 1) * 128], identb[:]),
                             reads=[("yt", b)], writes=[("pT", b)])
                    S.op("act", lambda e, T=T, b=b: e.activation(
                        out=mixT[:, 4:8, T * 128:(T + 1) * 128],
                        in_=pT[b][:, 0:512].rearrange("p (j t) -> p j t", t=128), func=AF.Copy),
                        reads=[("pT", b)], writes=[("mixhg", T)])
                if debug:
                    S.dma("sp", lambda e: e.dma_start(out=dbg["mixT"], in_=mixT[:].rearrange("p k t -> p (k t)")),
                          reads=[("mixhg", T) for T in range(NT)])
                phase_end("p3")

        with contextlib.ExitStack() as p4:
            wo32 = sb("wo32", [128, 8, D], stack=p4)
            wob = sb("wob", [128, 8, D], BF16, stack=p4)
            g1b = sb("g1b", [128, D], stack=p4)
            S.dma("sp", lambda e: e.dma_start(out=g1b[:], in_=scr_bc[0]), writes=["g1b"])
            for k in range(8):
                S.dma("sp" if k % 2 == 0 else "act", lambda e, k=k: e.dma_start(
                    out=wo32[:, k, :], in_=w_out[k * 128:(k + 1) * 128, :]), writes=[("wo32", k)])
                S.op("dve" if k % 2 == 0 else "pool", lambda e, k=k: e.tensor_tensor(
                    out=wob[:, k, :], in0=wo32[:, k, :], in1=g1b[:], op=ALU.mult),
                    reads=[("wo32", k), "g1b"], writes=[("wob", k)])
            wob_all = [("wob", k) for k in range(8)]
            for T in range(NT):
                S.dma("sp" if T % 2 == 0 else "act", lambda e, T=T: e.dma_start(
                    out=x1[:, T, :], in_=x[T * 128:(T + 1) * 128, :]), writes=[("x1", T)])
                for hf in range(2):
                    pb = (2 * T + hf) % 4
                    for k in range(8):
                        S.op("pe", lambda e, k=k, T=T, hf=hf, pb=pb: e.matmul(
                            ps[pb][:, :], mixT[:, k, T * 128:(T + 1) * 128], wob[:, k, hf * 512:(hf + 1) * 512],
                            start=(k == 0), stop=(k == 7)), reads=wob_all, writes=[("ps", pb)])
                    S.op("dve", lambda e, T=T, hf=hf, pb=pb: e.tensor_tensor(
                        out=x1[:, T, hf * 512:(hf + 1) * 512], in0=ps[pb][:, :], in1=x1[:, T, hf * 512:(hf + 1) * 512],
                        op=ALU.add), reads=[("ps", pb), ("x1", T)], writes=[("x1", T)])
            if debug:
                S.dma("sp", lambda e: e.dma_start(out=dbg["x1"], in_=R[:]),
                      reads=[("x1", T) for T in range(NT)])
            phase_end("p4")

    eidx = sb("eidx", [128, NT, 128], I32)
    gw = sb("gw", [128, NT, 128])
    rs2 = sb("rs2", [128, NT])
    with contextlib.ExitStack() as p5:
        wqb = sb("wqb", [128, 8, 2048], BF16, stack=p5)
        kTb = sb("kTb", [128, 16, 128], BF16, stack=p5)
        junk = sb("junk5", [128, D], BF16, stack=p5)
        xs = [sb(f"xs5{j}", [128, D], BF16, stack=p5) for j in range(2)]
        h2T = [sb(f"h2T{j}", [128, 8, 128], BF16, stack=p5) for j in range(2)]
        qTp = [sb(f"qTp{j}", [128, 16, 128], BF16, stack=p5) for j in range(2)]
        ssb = sb("ssb", [128, 16, 128], stack=p5)
        s2 = sb("s2", [128, 16, 128], stack=p5)
        top = sb("top", [128, 16, 16], stack=p5)
        itop = sb("itop", [128, 16, 16], U32, stack=p5)
        itf = sb("itf", [128, 16, 16], stack=p5)
        cand = sb("cand", [128, 8, 256], stack=p5)
        cand2 = sb("cand2", [128, 8, 256], stack=p5)
        ctop = sb("ctop", [128, 8, 16], stack=p5)
        cpos = sb("cpos", [128, 8, 16], U32, stack=p5)
        paf = sb("paf", [128, 128], stack=p5)
        pai = sb("pai", [128, 128], I32, stack=p5)
        pbf = sb("pbf", [128, 128], stack=p5)
        oh = sb("oh", [128, 128, 16], stack=p5)
        selA = sb("selA", [128, 128], stack=p5)
        selB = sb("selB", [128, 128], stack=p5)
        ef = sb("ef", [128, 128], stack=p5)
        ee = sb("ee", [128, 8, 16], stack=p5)
        zz = sb("zz", [128, 16], stack=p5)
        st5 = sb("st5", [128, 2 * NT], stack=p5)

        stg = [sb(f"stg{j}", [128, 4, D], BF16, stack=p5) for j in range(2)]
        conv_steps = [(tab, c) for tab in range(2) for c in range(32)]

        def emit_conv(si):
            tab, c = conv_steps[si]
            src = (u_t, v_t)[tab].rearrange("(p j c) d -> c p j d", p=128, j=4, c=32)[c]
            dst = uv_bf.rearrange("(p j c) d -> c p j d", p=128, j=4, c=32)[c][:, :, tab * D:(tab + 1) * D]
            b = si % 2
            S.dma("pool", lambda e: e.dma_start(out=stg[b][:], in_=src), writes=[("stg", b)])
            S.dma("sp", lambda e: e.dma_start(out=dst, in_=stg[b][:]), reads=[("stg", b)], writes=[("tab", tab, c)])

        wkq = load_w(wqb[:, :, 0:1024], wq, 0, 1024, "wqa") + load_w(wqb[:, :, 1024:2048], wq, 1024, 1024, "wqb")
        S.dma("pool", lambda e: e.dma_start(out=kTb[:].rearrange("p c k -> p (c k)"), in_=keysT), writes=["kTb"])
        for T in range(NT):
            S.op("act", lambda e, T=T: e.activation(out=junk[:], in_=x1[:, T, :], func=AF.Square,
                                                    accum_out=st5[:, T:T + 1]), reads=[], writes=["junk5", ("ssq5", T)])
        S.op("dve", lambda e: e.tensor_scalar(out=st5[:, NT:2 * NT], in0=st5[:, 0:NT],
                                              scalar1=1.0 / D, scalar2=EPS, op0=ALU.mult, op1=ALU.add),
             reads=[("ssq5", T) for T in range(NT)], writes=["ms5"])
        S.op("act", lambda e: e.activation(out=st5[:, NT:2 * NT], in_=st5[:, NT:2 * NT], func=AF.Sqrt),
             reads=["ms5"], writes=["ms5"])
        S.op("dve", lambda e: e.reciprocal(out=rs2[:, :], in_=st5[:, NT:2 * NT]), reads=["ms5"], writes=["rs2"])
        for T in range(NT):
            b = T % 2
            if not _NOCONV:
                for q_ in range(4):
                    emit_conv(4 * T + q_)
            S.op("act", lambda e, b=b, T=T: e.activation(out=xs[b][:], in_=x1[:, T, :], func=AF.Copy,
                                                         scale=rs2[:, T:T + 1]), reads=["rs2"], writes=[("xs5", b)])
            for k in range(8):
                S.op("pe", lambda e, b=b, k=k: e.transpose(pT[b][:, k * 128:(k + 1) * 128],
                                                           xs[b][:, k * 128:(k + 1) * 128], identb[:]),
                     reads=[("xs5", b)], writes=[("pT", b)])
            for k in range(8):
                S.op("dve" if k % 2 == 0 else "act", (lambda e, b=b, k=k: e.tensor_scalar(
                    out=h2T[b][:, k, :], in0=pT[b][:, k * 128:(k + 1) * 128],
                    scalar1=mc[:, 32 + k:33 + k], scalar2=mc[:, 40 + k:41 + k], op0=ALU.mult, op1=ALU.add))
                    if k % 2 == 0 else (lambda e, b=b, k=k: e.activation(
                        out=h2T[b][:, k, :], in_=pT[b][:, k * 128:(k + 1) * 128], func=AF.Identity,
                        scale=mc[:, 32 + k:33 + k], bias=mc[:, 40 + k:41 + k])),
                    reads=[("pT", b)], writes=[("h2T", b)])
            for g4 in range(4):
                pb = g4
                for j in range(4):
                    pc = g4 * 4 + j
                    for k in range(8):
                        S.op("pe", lambda e, b=b, k=k, pc=pc, j=j, pb=pb: e.matmul(
                            ps[pb][:, j * 128:(j + 1) * 128], wqb[:, k, pc * 128:(pc + 1) * 128], h2T[b][:, k, :],
                            start=(k == 0), stop=(k == 7)), reads=wkq + [("h2T", b)], writes=[("ps", pb)])
                if g4 % 2 == 0:
                    S.op("act", lambda e, b=b, g4=g4, pb=pb: e.activation(
                        out=qTp[b][:, g4 * 4:(g4 + 1) * 4, :], in_=ps[pb][:, :].rearrange("p (j t) -> p j t", t=128),
                        func=AF.Copy), reads=[("ps", pb)], writes=[("qTp", b, g4)])
                else:
                    S.op("dve", lambda e, b=b, g4=g4, pb=pb: e.tensor_copy(
                        out=qTp[b][:, g4 * 4:(g4 + 1) * 4, :], in_=ps[pb][:, :].rearrange("p (j t) -> p j t", t=128)),
                        reads=[("ps", pb)], writes=[("qTp", b, g4)])
            for g4 in range(4):
                pb = 4 + (g4 % 2)
                for j in range(4):
                    pc = g4 * 4 + j
                    S.op("pe", lambda e, b=b, pc=pc, j=j, pb=pb: e.matmul(
                        ps[pb][:, j * 128:(j + 1) * 128], qTp[b][:, pc, :], kTb[:, pc, :], start=True, stop=True),
                        reads=[("qTp", b, g4), "kTb"], writes=[("ps", pb)])
                S.op("act", lambda e, g4=g4, pb=pb: e.activation(
                    out=ssb[:, g4 * 4:(g4 + 1) * 4, :], in_=ps[pb][:, :].rearrange("p (j t) -> p j t", t=128),
                    func=AF.Copy), reads=[("ps", pb)], writes=[("ssb", g4)])
            for pc in range(16):
                S.op("dve", lambda e, pc=pc: e.max(out=top[:, pc, 0:8], in_=ssb[:, pc, :]),
                     reads=[("ssb", pc // 4)], writes=[("top", pc, 0)])
            for pc in range(16):
                S.op("dve", lambda e, pc=pc: e.match_replace(out=s2[:, pc, :], in_to_replace=top[:, pc, 0:8],
                                                             in_values=ssb[:, pc, :], imm_value=NEG),
                     reads=[("ssb", pc // 4), ("top", pc, 0)], writes=[("s2", pc)])
            for pc in range(16):
                S.op("dve", lambda e, pc=pc: e.max(out=top[:, pc, 8:16], in_=s2[:, pc, :]),
                     reads=[("s2", pc)], writes=[("top", pc, 1)])
            for pc in range(16):
                S.op("dve", lambda e, pc=pc: e.max_index(out=itop[:, pc, 0:8], in_max=top[:, pc, 0:8], in_values=ssb[:, pc, :]),
                     reads=[("ssb", pc // 4), ("top", pc, 0)], writes=[("itop", pc, 0)])
            for pc in range(16):
                S.op("dve", lambda e, pc=pc: e.max_index(out=itop[:, pc, 8:16], in_max=top[:, pc, 8:16], in_values=ssb[:, pc, :]),
                     reads=[("ssb", pc // 4), ("top", pc, 1)], writes=[("itop", pc, 1)])
            tops = [("top", pc, j) for pc in range(16) for j in range(2)]
            itops = [("itop", pc, j) for pc in range(16) for j in range(2)]
            S.op("dve", lambda e: e.tensor_copy(out=itf[:], in_=itop[:]), reads=itops, writes=["itf"])
            topv = top[:].rearrange("p (h c) a -> p h c a", c=2)
            S.op("dve", lambda e: e.tensor_tensor(
                out=cand[:].rearrange("p h (a b) -> p h a b", b=16),
                in0=topv[:, :, 0, :].unsqueeze(3).to_broadcast([128, 8, 16, 16]),
                in1=topv[:, :, 1, :].unsqueeze(2).to_broadcast([128, 8, 16, 16]), op=ALU.add),
                reads=tops, writes=["cand"])
            for p in range(8):
                S.op("dve", lambda e, p=p: e.max(out=ctop[:, p, 0:8], in_=cand[:, p, :]), reads=["cand"], writes=[("ctop", p, 0)])
            for p in range(8):
                S.op("dve", lambda e, p=p: e.match_replace(out=cand2[:, p, :], in_to_replace=ctop[:, p, 0:8],
                                                           in_values=cand[:, p, :], imm_value=NEG),
                     reads=["cand", ("ctop", p, 0)], writes=[("cand2", p)])
            for p in range(8):
                S.op("dve", lambda e, p=p: e.max(out=ctop[:, p, 8:16], in_=cand2[:, p, :]), reads=[("cand2", p)], writes=[("ctop", p, 1)])
            for p in range(8):
                S.op("dve", lambda e, p=p: e.max_index(out=cpos[:, p, 0:8], in_max=ctop[:, p, 0:8], in_values=cand[:, p, :]),
                     reads=["cand", ("ctop", p, 0)], writes=[("cpos", p, 0)])
            for p in range(8):
                S.op("dve", lambda e, p=p: e.max_index(out=cpos[:, p, 8:16], in_max=ctop[:, p, 8:16], in_values=cand[:, p, :]),
                     reads=["cand", ("ctop", p, 1)], writes=[("cpos", p, 1)])
            ctops = [("ctop", p, j) for p in range(8) for j in range(2)]
            cposs = [("cpos", p, j) for p in range(8) for j in range(2)]
            cposf = cpos[:].rearrange("p h j -> p (h j)")
            S.op("dve", lambda e: e.tensor_copy(out=selA[:], in_=cposf), reads=cposs + [("sel", 0)], writes=["posf"])
            S.op("dve", lambda e: e.tensor_scalar(out=pbf[:], in0=selA[:], scalar1=0.0625, scalar2=None, op0=ALU.mult),
                 reads=["posf"], writes=["pbf"])
            S.op("dve", lambda e: e.tensor_copy(out=pai[:], in_=pbf[:]), reads=["pbf"], writes=["pai"])
            S.op("dve", lambda e: e.tensor_copy(out=paf[:], in_=pai[:]), reads=["pai"], writes=["paf"])
            S.op("dve", lambda e: e.scalar_tensor_tensor(out=pbf[:], in0=paf[:], scalar=16.0, in1=selA[:],
                                                         op0=ALU.mult, op1=ALU.is_gt), reads=["paf", "posf", "pai"], writes=["pbf"])
            S.op("dve", lambda e: e.tensor_tensor(out=paf[:], in0=paf[:], in1=pbf[:], op=ALU.subtract),
                 reads=["paf", "pbf"], writes=["paf"])
            S.op("dve", lambda e: e.scalar_tensor_tensor(out=pbf[:], in0=paf[:], scalar=-16.0, in1=selA[:],
                                                         op0=ALU.mult, op1=ALU.add), reads=["paf", "posf"], writes=["pbf"])
            itv = itf[:].rearrange("p (h c) a -> p h c a", c=2)
            for which, pf, sel in ((0, paf, selA), (1, pbf, selB)):
                S.op("dve", lambda e, pf=pf: e.tensor_tensor(
                    out=oh[:], in0=pf[:].unsqueeze(2).to_broadcast([128, 128, 16]),
                    in1=IOTA16.unsqueeze(1).to_broadcast([128, 128, 16]), op=ALU.is_equal),
                    reads=["paf", "pbf", "cA", "oh"], writes=["oh"])
                S.op("dve", lambda e, which=which: e.tensor_tensor(
                    out=oh[:].rearrange("p (h j) a -> p h j a", j=16),
                    in0=oh[:].rearrange("p (h j) a -> p h j a", j=16),
                    in1=itv[:, :, which, :].unsqueeze(2).to_broadcast([128, 8, 16, 16]), op=ALU.mult),
                    reads=["oh", "itf"], writes=["oh"])
                S.op("dve", lambda e, sel=sel: e.tensor_reduce(out=sel[:], in_=oh[:], axis=AX.X, op=ALU.add),
                     reads=["oh", "posf"], writes=[("sel", which)])
            S.op("dve", lambda e: e.scalar_tensor_tensor(out=ef[:], in0=selA[:], scalar=128.0, in1=selB[:],
                                                         op0=ALU.mult, op1=ALU.add),
                 reads=[("sel", 0), ("sel", 1)], writes=["ef"])
            S.op("dve", lambda e, T=T: e.tensor_copy(out=eidx[:, T, :], in_=ef[:]), reads=["ef"], writes=[("eidx", T)])
            S.op("dve", lambda e: e.tensor_tensor(out=ee[:], in0=ctop[:], in1=ctop[:, :, 0:1].to_broadcast([128, 8, 16]),
                                                  op=ALU.subtract), reads=ctops, writes=["ee"])
            S.op("act", lambda e: e.activation(out=ee[:], in_=ee[:], func=AF.Exp), reads=["ee"], writes=["ee"])
            S.op("dve", lambda e: e.tensor_reduce(out=zz[:, 0:8], in_=ee[:], axis=AX.X, op=ALU.add),
                 reads=["ee"], writes=["zz"])
            S.op("dve", lambda e: e.reciprocal(out=zz[:, 8:16], in_=zz[:, 0:8]), reads=["zz"], writes=["zz2"])
            S.op("dve", lambda e, T=T: e.tensor_tensor(
                out=gw[:, T, :].rearrange("p (h j) -> p h j", j=16), in0=ee[:],
                in1=zz[:, 8:16].unsqueeze(2).to_broadcast([128, 8, 16]), op=ALU.mult),
                reads=["ee", "zz2"], writes=[("gw", T)])
        if debug:
            S.dma("sp", lambda e: e.dma_start(out=dbg["eidx"], in_=eidx[:].rearrange("p t s -> p (t s)")),
                  reads=[("eidx", T) for T in range(NT)])
            S.dma("sp", lambda e: e.dma_start(out=dbg["gw"], in_=gw[:].rearrange("p t s -> p (t s)")),
                  reads=[("gw", T) for T in range(NT)])
        phase_end("p5a")

    with contextlib.ExitStack() as p6:
        NB = 12
        ring = [sb(f"ring{j}", [128, 2 * D], BF16, stack=p6) for j in range(NB)]
        NDG = 6
        dg = [sb(f"dg{j}", [128, 128], BF16, stack=p6) for j in range(NDG)]
        bc = sb("bc5", [128, 4, D], stack=p6)
        h2 = [sb(f"h2_{j}", [128, D], stack=p6) for j in range(2)]
        junk = sb("junk6", [128, D], BF16, stack=p6)
        accs = sb("accs", [128, D], stack=p6)
        aa = [sb(f"aa{j}", [128, 128], stack=p6) for j in range(2)]
        gl = [sb(f"gl{j}", [128, 128], stack=p6) for j in range(2)]
        ww = [sb(f"ww{j}", [128, 128], stack=p6) for j in range(2)]
        st6 = sb("st6", [128, 2 * NT], stack=p6)
        S.dma("sp", lambda e: e.dma_start(out=bc[:, 0, :], in_=scr_bc[1]), writes=[("bc", 0)])
        S.dma("sp", lambda e: e.dma_start(out=bc[:, 1, :], in_=scr_bc[2]), writes=[("bc", 1)])
        S.dma("sp", lambda e: e.dma_start(out=bc[:, 2, :], in_=scr_bc[3]), writes=[("bc", 2)])
        S.dma("sp", lambda e: e.dma_start(out=bc[:, 3, :], in_=nfb), writes=[("bc", 3)])
        gi = gd = 0
        for T in range(NT):
            pu = T % 2
            S.op("dve", lambda e, T=T, pu=pu: e.scalar_tensor_tensor(out=h2[pu][:], in0=x1[:, T, :], scalar=rs2[:, T:T + 1],
                                                                     in1=bc[:, 1, :], op0=ALU.mult, op1=ALU.mult),
                 reads=[("bc", 1)], writes=[("h2", pu)])
            S.op("dve", lambda e, pu=pu: e.tensor_tensor(out=h2[pu][:], in0=h2[pu][:], in1=bc[:, 2, :], op=ALU.add),
                 reads=[("h2", pu), ("bc", 2)], writes=[("h2", pu)])
            S.op("dve", lambda e, pu=pu: e.memset(aa[pu][:], 0.0), writes=[("aa", pu)])
            for s_ in range(128):
                r = gi % NB
                gi += 1
                S.dma("pool", lambda e, T=T, s_=s_, r=r: e.indirect_dma_start(
                    out=ring[r][:], out_offset=None, in_=uv_bf,
                    in_offset=bass.IndirectOffsetOnAxis(ap=eidx[:, T, s_:s_ + 1], axis=0)),
                    reads=[], writes=[("ring", r)])
                S.op("dve", lambda e, s_=s_, r=r, pu=pu: e.scalar_tensor_tensor(
                    out=junk[:], in0=ring[r][:, 0:D], scalar=1.0, in1=h2[pu][:], op0=ALU.mult, op1=ALU.mult,
                    accum_out=aa[pu][:, s_:s_ + 1]), reads=[("ring", r), ("h2", pu), ("aa", pu)],
                    writes=["junk6", ("aas", pu, s_)])
                S.op("act", lambda e, s_=s_, pu=pu: e.activation(out=gl[pu][:, s_:s_ + 1], in_=aa[pu][:, s_:s_ + 1], func=AF.Gelu),
                     reads=[("aas", pu, s_)], writes=[("gl", pu, s_)])
                S.op("act", lambda e, T=T, s_=s_, pu=pu: e.activation(out=ww[pu][:, s_:s_ + 1], in_=gl[pu][:, s_:s_ + 1],
                                                                     func=AF.Copy, scale=gw[:, T, s_:s_ + 1]),
                     reads=[("gl", pu, s_)], writes=[("ww", pu, s_)])
                dj = gd % NDG
                gd += 1
                S.op("act", lambda e, s_=s_, dj=dj, pu=pu: e.activation(
                    out=dg[dj][:], in_=identb[:], func=AF.Copy, scale=ww[pu][:, s_:s_ + 1]),
                    reads=[("ww", pu, s_)], writes=[("dg", dj)])
                for hf in range(2):
                    S.op("pe", lambda e, s_=s_, dj=dj, r=r, pu=pu, hf=hf: e.matmul(
                        ps[2 * pu + hf][:, :], dg[dj][:], ring[r][:, D + hf * 512: D + (hf + 1) * 512],
                        start=(s_ == 0), stop=(s_ == 127)),
                        reads=[("dg", dj), ("ring", r)], writes=[("accP", pu, hf)])
            for hf in range(2):
                S.op("dve", lambda e, pu=pu, hf=hf: e.tensor_tensor(
                    out=accs[:, hf * 512:(hf + 1) * 512], in0=ps[2 * pu + hf][:, :], in1=bc[:, 0, hf * 512:(hf + 1) * 512],
                    op=ALU.mult), reads=[("accP", pu, hf), ("bc", 0)], writes=[("accs", hf)])
            S.op("dve", lambda e, T=T: e.tensor_tensor(out=accs[:], in0=accs[:], in1=x1[:, T, :], op=ALU.add),
                 reads=[("accs", 0), ("accs", 1)], writes=["accsum"])
            S.op("act", lambda e, T=T: e.activation(out=junk[:], in_=accs[:], func=AF.Square, accum_out=st6[:, T:T + 1]),
                 reads=["accsum"], writes=["junk6", ("ssq6", T)])
            S.op("dve", lambda e, T=T: e.tensor_scalar(out=st6[:, NT + T:NT + T + 1], in0=st6[:, T:T + 1],
                                                       scalar1=1.0 / D, scalar2=EPS, op0=ALU.mult, op1=ALU.add),
                 reads=[("ssq6", T)], writes=[("ms6", T)])
            S.op("act", lambda e, T=T: e.activation(out=st6[:, NT + T:NT + T + 1], in_=st6[:, NT + T:NT + T + 1],
                                                    func=AF.Sqrt), reads=[("ms6", T)], writes=[("ms6", T)])
            S.op("dve", lambda e, T=T: e.reciprocal(out=st6[:, T:T + 1], in_=st6[:, NT + T:NT + T + 1]),
                 reads=[("ms6", T)], writes=[("rs6", T)])
            S.op("dve", lambda e, T=T: e.scalar_tensor_tensor(out=x1[:, T, :], in0=accs[:], scalar=st6[:, T:T + 1],
                                                              in1=bc[:, 3, :], op0=ALU.mult, op1=ALU.mult),
                 reads=["accsum", ("rs6", T), ("bc", 3)], writes=[("xo", T), ("accs", 0), ("accs", 1)])
            S.dma("sp", lambda e, T=T: e.dma_start(out=out[T * 128:(T + 1) * 128, :], in_=x1[:, T, :]),
                  reads=[("xo", T)], writes=[("out", T)])
        phase_end("p5b")
    S.finish()
    es.close()
    return nc


def _col(v):
    return np.ascontiguousarray(np.asarray(v, np.float32).reshape(8, 128).T)


def make_inputs(inp):
    global _BIAS_IDX
    f = lambda a: np.ascontiguousarray(np.asarray(a, dtype=np.float32))
    if _BIAS_IDX is None:
        _BIAS_IDX = build_bias_index()
    rpb = f(inp["na_rpb"])[0]
    ext = np.concatenate([rpb.reshape(8, -1), np.full((8, 1), MASKV, np.float32)], axis=1)
    biasT = np.stack([ext[h][_BIAS_IDX] for h in range(8)], axis=1)
    cstm = np.zeros((128, 528), np.float32)
    cstm[:, 0:128] = np.eye(128, dtype=np.float32)
    sidx = np.arange(128)
    blk = (sidx[:, None] // 64) == (sidx[None, :] // 64)
    cstm[:, 128:256] = (blk & (sidx[:, None] <= sidx[None, :])).astype(np.float32)
    cstm[:, 256:384] = (blk & (sidx[:, None] >= sidx[None, :])).astype(np.float32)
    cstm[:, 384:512] = 1.0
    cstm[:, 512:528] = np.arange(16, dtype=np.float32)[None, :]
    rmask = np.ones((128, TOK), np.float32)
    rmask[:, ::64] = 0.0
    c_ctx = f(inp["c_ctx"])
    hg_lb = f(inp["hg_lb"])
    lbraw = np.ascontiguousarray(hg_lb.reshape(2, 2, 4, 128).transpose(3, 0, 1, 2).reshape(128, 16))
    keys = f(inp["peer_keys"])[0]
    keysT = np.ascontiguousarray(keys.transpose(3, 0, 1, 2).reshape(128, 16 * 128))
    shared = dict(
        w_mod=f(inp["w_mod"])[0], b_mod=f(inp["b_mod"])[0].reshape(1, -1),
        n1c=_col(f(inp["norm1"])[0]), n2c=_col(f(inp["norm2"])[0]),
        n2b=np.ascontiguousarray(np.broadcast_to(f(inp["norm2"])[0][None, :], (128, D))),
        nfb=np.ascontiguousarray(np.broadcast_to(f(inp["norm_f"])[None, :], (128, D))),
        w_in=f(inp["w_in"])[0], w_out=f(inp["w_out"])[0], wq=f(inp["peer_wq"])[0],
        keysT=keysT, u=f(inp["peer_u"])[0], v=f(inp["peer_v"])[0], lbraw=lbraw,
        hgn=np.ascontiguousarray(np.broadcast_to(f(inp["hg_norm"])[0][None, :], (128, 512))),
        biasT=np.ascontiguousarray(biasT.reshape(128, -1)), cst=cstm, rmask=rmask,
    )
    xs = f(inp["x"]); cs = f(inp["c"]); ctxs = f(inp["ctx"])
    maps = []
    for b in range(xs.shape[0]):
        cc = np.stack([_col(cs[b]), _col(c_ctx)], axis=2).reshape(128, 16)
        m = dict(shared)
        m.update(x=xs[b], ctx=ctxs[b], ccol=np.ascontiguousarray(cc))
        maps.append(m)
    return maps


def kernel(**inputs):
    maps = make_inputs(inputs)
    nc = build()
    res = run_bass_kernel_spmd(nc, maps, core_ids=list(range(len(maps))))
    return np.stack([np.asarray(r["out"], dtype=np.float32) for r in res.results], axis=0)
```

```python
import contextlib
import numpy as np
import concourse.bass as bass
import concourse.mybir as mybir
from concourse.bass_utils import run_bass_kernel_spmd

F32 = mybir.dt.float32
BF16 = mybir.dt.bfloat16
I32 = mybir.dt.int32
U32 = mybir.dt.uint32
ALU = mybir.AluOpType
AF = mybir.ActivationFunctionType
AX = mybir.AxisListType

D = 1024
SEQ = 2048
CTX = 256
NT = 16
NTT = 18
TOK = SEQ + CTX
NCH = TOK // 64
EPS = 1e-6
MASKV = -30000.0
NEG = -1.0e30


class Sched:
    COMPUTE = ("pe", "dve", "act", "pool")

    def __init__(self, nc, n_dsem=None):
        self.nc = nc
        self.engs = {"pe": nc.tensor, "dve": nc.vector, "act": nc.scalar,
                     "pool": nc.gpsimd, "sp": nc.sync}
        self.n_dsem = n_dsem or {"sp": 8, "act": 4, "pool": 16}
        self.es = contextlib.ExitStack()
        self.csem = {e: self.es.enter_context(nc.semaphore("cs_" + e)) for e in self.COMPUTE}
        self.dsem = {q: [self.es.enter_context(nc.semaphore(f"ds_{q}{j}")) for j in range(n)]
                     for q, n in self.n_dsem.items()}
        self.ccount = {e: 0 for e in self.COMPUTE}
        self.dcount = {q: 0 for q in self.n_dsem}
        self.clock = {e: {} for e in self.engs}
        self.bar_sig = 0
        self.bar_clock = {}
        self.bar_tile = None
        self.ops = []
        self.last_writer = {}
        self.readers = {}
        self.total_ops = 0

    def op(self, eng, fn, reads=(), writes=(), dma=False):
        deps = set()
        for r in reads:
            w = self.last_writer.get(r)
            if w is not None:
                deps.add(w)
        for w_ in writes:
            w = self.last_writer.get(w_)
            if w is not None:
                deps.add(w)
            for rd in self.readers.get(w_, ()):
                deps.add(rd)
        i = len(self.ops)
        deps.discard(i)
        self.ops.append(dict(eng=eng, fn=fn, deps=deps, dma=dma))
        for r in reads:
            self.readers.setdefault(r, []).append(i)
        for w_ in writes:
            self.last_writer[w_] = i
            self.readers[w_] = []
        return i

    def dma(self, q, fn, reads=(), writes=()):
        return self.op(q, fn, reads, writes, dma=True)

    def _wait(self, E, key, sem, val):
        ck = self.clock[E]
        if ck.get(key, 0) < val:
            self.engs[E].wait_ge(sem, val)
            ck[key] = val

    def _merge(self, E, clk):
        ck = self.clock[E]
        for k, v in clk.items():
            if ck.get(k, 0) < v:
                ck[k] = v

    def flush(self, barrier=True):
        ops = self.ops
        need_sig = [False] * len(ops)
        for i, o in enumerate(ops):
            for d in o["deps"]:
                od = ops[d]
                if od["dma"]:
                    continue
                if od["eng"] == "pe" and o["eng"] == "pe" and not o["dma"]:
                    continue
                need_sig[d] = True
        if barrier:
            last = {}
            for i, o in enumerate(ops):
                if not o["dma"]:
                    last[o["eng"]] = i
            for e, i in last.items():
                need_sig[i] = True
        for i, o in enumerate(ops):
            E = o["eng"]
            eng = self.engs[E]
            if self.bar_sig:
                self._wait(E, ("c", "dve"), self.csem["dve"], self.bar_sig)
                self._merge(E, self.bar_clock)
            for d in sorted(o["deps"]):
                od = ops[d]
                if od["dma"]:
                    q = od["eng"]
                    self._wait(E, ("d", q, od["dsem_idx"]), self.dsem[q][od["dsem_idx"]], od["dval"])
                else:
                    F = od["eng"]
                    if F == "pe" and E == "pe" and not o["dma"]:
                        continue
                    self._wait(E, ("c", F), self.csem[F], od["sig"])
                self._merge(E, od["clk"])
            if o["dma"]:
                n = self.n_dsem[E]
                j = self.dcount[E] % n
                prev = self.dcount[E] // n
                if prev > 0:
                    self._wait(E, ("d", E, j), self.dsem[E][j], 16 * prev)
                ins = o["fn"](eng)
                ins.then_inc(self.dsem[E][j], 16)
                o["dsem_idx"] = j
                o["dval"] = 16 * (prev + 1)
                self.dcount[E] += 1
                o["clk"] = dict(self.clock[E])
            else:
                ins = o["fn"](eng)
                if need_sig[i]:
                    self.ccount[E] += 1
                    ins.then_inc(self.csem[E], 1)
                    o["sig"] = self.ccount[E]
                else:
                    o["sig"] = None
                o["clk"] = dict(self.clock[E])
            o["fn"] = None
        self.total_ops += len(ops)
        if barrier:
            self._barrier()
        self.ops = []
        self.last_writer = {}
        self.readers = {}

    def _wait_all(self, E):
        for q, n in self.n_dsem.items():
            for j in range(n):
                uses = (self.dcount[q] - j + n - 1) // n if self.dcount[q] > j else 0
                if uses > 0:
                    self._wait(E, ("d", q, j), self.dsem[q][j], 16 * uses)
        for e in self.COMPUTE:
            if self.ccount[e] > 0:
                self._wait(E, ("c", e), self.csem[e], self.ccount[e])

    def _barrier(self):
        self._wait_all("dve")
        ins = self.engs["dve"].memset(self.bar_tile, 0.0)
        self.ccount["dve"] += 1
        ins.then_inc(self.csem["dve"], 1)
        self.bar_sig = self.ccount["dve"]
        self.bar_clock = dict(self.clock["dve"])

    def finish(self, eng="sp"):
        self.flush(barrier=True)
        self._wait_all(eng)
        self.es.close()


NA_VARIANTS = [(-2, True), (-1, False), (0, False), (1, False), (2, True),
               (-3, False), (-2, False), (2, False), (3, False)]


def na_chunks(i):
    if i in (0, 1, 14, 15):
        cs = range(0, 4) if i < 2 else range(12, 16)
        out = []
        for c in cs:
            d = c - i
            t = {(-3): 5, (-2): 6, (-1): 1, 0: 2, 1: 3, 2: 7, 3: 8}[d]
            out.append((c, t))
        return out
    return [(i + d, d + 2) for d in range(-2, 3)]


def build_bias_index():
    idx = np.full((128, 9, 128), 15 * 31, dtype=np.int64)
    for t, (d, partial) in enumerate(NA_VARIANTS):
        for j in range(2):
            for jq in range(2):
                dr = 2 * d + j - jq
                if abs(dr) > 7:
                    continue
                if partial:
                    if d == -2 and not (j >= jq):
                        continue
                    if d == 2 and not (j == 0 and jq == 1):
                        continue
                for cq in range(64):
                    cstart = min(max(cq - 8, 0), 48)
                    for ck in range(cstart, cstart + 16):
                        idx[j * 64 + ck, t, jq * 64 + cq] = (dr + 7) * 31 + (ck - cq + 15)
    return idx


_BIAS_IDX = None


import os as _os
_NOCONV = bool(_os.environ.get('NOCONV'))


class _Stop(Exception):
    pass


def build(debug=False, stop=None):
    nc = bass.Bass("TRN2", target_bir_lowering=False)
    try:
        return _build(nc, debug, stop)
    except _Stop:
        return nc


def _build(nc, debug, stop):

    def din(name, shape, dt=F32):
        return nc.dram_tensor(name, shape, dt, kind="ExternalInput").ap()

    x = din("x", [SEQ, D])
    ctx = din("ctx", [CTX, D])
    ccol = din("ccol", [128, 16])
    w_mod = din("w_mod", [D, 6 * D])
    b_mod = din("b_mod", [1, 6 * D])
    n1c = din("n1c", [128, 8])
    n2c = din("n2c", [128, 8])
    n2b = din("n2b", [128, D])
    nfb = din("nfb", [128, D])
    w_in = din("w_in", [D, 4096])
    w_out = din("w_out", [D, D])
    wq = din("wq", [D, 2048])
    keysT = din("keysT", [128, 2048])
    u_t = din("u", [16384, D])
    v_t = din("v", [16384, D])
    lbraw = din("lbraw", [128, 16])
    hgn = din("hgn", [128, 512])
    biasT = din("biasT", [128, 8 * 9 * 128])
    cst = din("cst", [128, 528])
    rmask_d = din("rmask", [128, TOK])
    out = nc.dram_tensor("out", [SEQ, D], F32, kind="ExternalOutput").ap()
    scr_bc = nc.dram_tensor("scr_bc", [4, 128, D], F32, kind="Internal").ap()
    uv_bf = nc.dram_tensor("uv_bf", [16384, 2 * D], BF16, kind="Internal").ap()
    dbg = {}
    if debug:
        dbg["hT"] = nc.dram_tensor("d_hT", [128, 8 * TOK], BF16, kind="ExternalOutput").ap()
        dbg["mixT"] = nc.dram_tensor("d_mixT", [128, 8 * SEQ], BF16, kind="ExternalOutput").ap()
        dbg["x1"] = nc.dram_tensor("d_x1", [128, NT * D], F32, kind="ExternalOutput").ap()
        dbg["mc"] = nc.dram_tensor("d_mc", [128, 48], F32, kind="ExternalOutput").ap()
        dbg["eidx"] = nc.dram_tensor("d_eidx", [128, NT * 128], I32, kind="ExternalOutput").ap()
        dbg["gw"] = nc.dram_tensor("d_gw", [128, NT * 128], F32, kind="ExternalOutput").ap()

    es = contextlib.ExitStack()

    def sb(name, shape, dt=F32, stack=None):
        return (stack or es).enter_context(nc.sbuf_tensor(name, shape, dt))

    bar = sb("bar", [128, 1])
    cA = sb("cA", [128, 528])
    identb = sb("identb", [128, 128], BF16)
    mc = sb("mc", [128, 48])
    lb = sb("lb", [128, 8])
    oml = sb("oml", [128, 8])
    ps = [es.enter_context(nc.psum_tensor(f"ps{j}", [128, 512], F32)) for j in range(6)]
    pT = [es.enter_context(nc.psum_tensor(f"pT{j}", [128, 1024], BF16)) for j in range(2)]

    S = Sched(nc)
    S.bar_tile = bar[:]

    def phase_end(name):
        S.flush()
        if stop == name:
            S.finish()
            raise _Stop()

    IDENT = cA[:, 0:128]
    TRIF = cA[:, 128:256]
    TRIB = cA[:, 256:384]
    ONES = cA[:, 384:512]
    IOTA16 = cA[:, 512:528]

    with contextlib.ExitStack() as p0:
        cc = sb("cc", [128, 16], stack=p0)
        scl = sb("scl", [128, 8, 33], BF16, stack=p0)
        wm = [sb(f"wm{j}", [128, 8, 512], stack=p0) for j in range(3)]
        wmb = [sb(f"wmb{j}", [128, 8, 512], BF16, stack=p0) for j in range(2)]
        bm = sb("bm", [33, 6 * D], stack=p0)
        modrow = sb("modrow", [33, 6 * D], stack=p0)
        mcol = sb("mcol", [128, 48], stack=p0)
        n1 = sb("n1", [128, 8], stack=p0)
        n2 = sb("n2", [128, 8], stack=p0)
        n2bt = sb("n2bt", [128, D], stack=p0)
        bct = [sb(f"bct{j}", [128, D], stack=p0) for j in range(2)]
        lbr = sb("lbr", [128, 16], stack=p0)

        S.dma("sp", lambda e: e.dma_start(out=cA[:], in_=cst), writes=["cA"])
        S.dma("sp", lambda e: e.dma_start(out=cc[:], in_=ccol), writes=["cc"])
        S.dma("sp", lambda e: e.dma_start(out=n1[:], in_=n1c), writes=["n1"])
        S.dma("sp", lambda e: e.dma_start(out=n2[:], in_=n2c), writes=["n2"])
        S.dma("sp", lambda e: e.dma_start(out=lbr[:], in_=lbraw), writes=["lbr"])
        S.dma("sp", lambda e: e.dma_start(out=n2bt[:], in_=n2b), writes=["n2bt"])
        S.op("dve", lambda e: e.memset(bm[:], 0.0), writes=["bm"])
        S.dma("sp", lambda e: e.dma_start(out=bm[0:1, :], in_=b_mod), reads=["bm"], writes=["bm0"])
        S.dma("sp", lambda e: e.dma_start(out=bm[32:33, :], in_=b_mod), reads=["bm"], writes=["bm32"])
        S.op("dve", lambda e: e.tensor_copy(out=identb[:], in_=IDENT), reads=["cA"], writes=["identb"])
        S.op("dve", lambda e: e.memset(scl[:], 0.0), writes=["scl"])
        ccv = cc[:].rearrange("p (k t) -> p k t", t=2)
        S.op("act", lambda e: e.activation(out=scl[:, :, 0:1], in_=ccv[:, :, 0:1], func=AF.Silu),
             reads=["cc", "scl"], writes=["scl"])
        S.op("act", lambda e: e.activation(out=scl[:, :, 32:33], in_=ccv[:, :, 1:2], func=AF.Silu),
             reads=["cc", "scl"], writes=["scl"])
        S.op("dve", lambda e: e.tensor_tensor(out=lb[:], in0=lbr[:, 0:8], in1=lbr[:, 8:16], op=ALU.subtract),
             reads=["lbr"], writes=["lb"])
        S.op("act", lambda e: e.activation(out=lb[:], in_=lb[:], func=AF.Sigmoid), reads=["lb"], writes=["lb"])
        S.op("dve", lambda e: e.tensor_scalar(out=oml[:], in0=lb[:], scalar1=-1.0, scalar2=1.0,
                                              op0=ALU.mult, op1=ALU.add), reads=["lb"], writes=["oml"])
        for n in range(12):
            wb = wm[n % 3]
            S.dma("sp" if n % 2 == 0 else "act",
                  lambda e, n=n, wb=wb: e.dma_start(
                      out=wb[:], in_=w_mod[:, n * 512:(n + 1) * 512].rearrange("(k p) n -> p k n", p=128)),
                  writes=[("wm", n % 3)])
            wbb = wmb[n % 2]
            if n % 2 == 0:
                S.op("dve", lambda e, wb=wb, wbb=wbb: e.tensor_copy(out=wbb[:], in_=wb[:]),
                     reads=[("wm", n % 3)], writes=[("wmb", n % 2)])
            else:
                S.op("act", lambda e, wb=wb, wbb=wbb: e.activation(out=wbb[:], in_=wb[:], func=AF.Copy),
                     reads=[("wm", n % 3)], writes=[("wmb", n % 2)])
            pb = ps[n % 2]
            for k in range(8):
                S.op("pe", lambda e, k=k, wbb=wbb, pb=pb: e.matmul(pb[0:33, :], scl[:, k, :], wbb[:, k, :],
                                                                  start=(k == 0), stop=(k == 7)),
                     reads=["scl", ("wmb", n % 2)], writes=[("ps", n % 2)])
            S.op("dve", lambda e, n=n, pb=pb: e.tensor_tensor(out=modrow[:, n * 512:(n + 1) * 512], in0=pb[0:33, :],
                                                             in1=bm[:, n * 512:(n + 1) * 512], op=ALU.add),
                 reads=[("ps", n % 2), "bm", "bm0", "bm32"], writes=[("modrow", n)])
        mr_all = [("modrow", n) for n in range(12)]
        col_specs = [(0, 0), (0, 1), (0, 3), (0, 4), (32, 0), (32, 1)]
        for si, (r, vi) in enumerate(col_specs):
            for k in range(8):
                c0 = 2 * (si * 8 + k)
                S.op("pe", lambda e, r=r, vi=vi, k=k, c0=c0: e.matmul(
                    ps[2][:, c0:c0 + 2], modrow[r:r + 1, vi * D + k * 128: vi * D + (k + 1) * 128],
                    cA[r:r + 1, 384:386], start=True, stop=True),
                    reads=mr_all + ["cA"], writes=[("ps", 2)])
        S.op("dve", lambda e: e.tensor_copy(out=mcol[:].unsqueeze(2), in_=ps[2][:, 0:96].rearrange("p (c two) -> p c two", two=2)[:, :, 0:1]), reads=[("ps", 2)], writes=["mcol"])
        S.op("dve", lambda e: e.scalar_tensor_tensor(out=mc[:, 0:8], in0=mcol[:, 8:16], scalar=1.0, in1=n1[:],
                                                     op0=ALU.add, op1=ALU.mult), reads=["mcol", "n1"], writes=["mc0"])
        S.op("dve", lambda e: e.tensor_copy(out=mc[:, 8:16], in_=mcol[:, 0:8]), reads=["mcol"], writes=["mc1"])
        S.op("dve", lambda e: e.scalar_tensor_tensor(out=mc[:, 16:24], in0=mcol[:, 40:48], scalar=1.0, in1=n1[:],
                                                     op0=ALU.add, op1=ALU.mult), reads=["mcol", "n1"], writes=["mc2"])
        S.op("dve", lambda e: e.tensor_copy(out=mc[:, 24:32], in_=mcol[:, 32:40]), reads=["mcol"], writes=["mc3"])
        S.op("dve", lambda e: e.scalar_tensor_tensor(out=mc[:, 32:40], in0=mcol[:, 24:32], scalar=1.0, in1=n2[:],
                                                     op0=ALU.add, op1=ALU.mult), reads=["mcol", "n2"], writes=["mc4"])
        S.op("dve", lambda e: e.tensor_copy(out=mc[:, 40:48], in_=mcol[:, 16:24]), reads=["mcol"], writes=["mc5"])
        for j, (vi, kind) in enumerate([(2, "copy"), (5, "copy"), (4, "g2"), (3, "copy")]):
            bt_ = bct[j % 2]
            for hf in range(2):
                pb = ps[3 + hf]
                S.op("pe", lambda e, vi=vi, hf=hf, pb=pb: e.matmul(
                    pb[:, :], cA[0:1, 384:512], modrow[0:1, vi * D + hf * 512: vi * D + (hf + 1) * 512],
                    start=True, stop=True), reads=mr_all + ["cA"], writes=[("ps", 3 + hf)])
                if kind == "copy":
                    S.op("dve", lambda e, bt_=bt_, hf=hf, pb=pb: e.tensor_copy(out=bt_[:, hf * 512:(hf + 1) * 512], in_=pb[:, :]),
                         reads=[("ps", 3 + hf)], writes=[("bct", j % 2, hf)])
                else:
                    S.op("dve", lambda e, bt_=bt_, hf=hf, pb=pb: e.scalar_tensor_tensor(
                        out=bt_[:, hf * 512:(hf + 1) * 512], in0=pb[:, :], scalar=1.0,
                        in1=n2bt[:, hf * 512:(hf + 1) * 512], op0=ALU.add, op1=ALU.mult),
                        reads=[("ps", 3 + hf), "n2bt"], writes=[("bct", j % 2, hf)])
            S.dma("sp", lambda e, j=j, bt_=bt_: e.dma_start(out=scr_bc[j], in_=bt_[:]),
                  reads=[("bct", j % 2, 0), ("bct", j % 2, 1)], writes=[("scr", j)])
        if debug:
            S.dma("sp", lambda e: e.dma_start(out=dbg["mc"], in_=mc[:]), reads=[f"mc{j}" for j in range(6)])
        phase_end("p0")

    R = sb("R", [128, NT * D])
    x1 = R[:].rearrange("p (t d) -> p t d", d=D)
    hT = R[:, 0:9216].bitcast(BF16).rearrange("p (k t) -> p k t", k=8)
    vtok = R[:, 9216:13824].bitcast(BF16).rearrange("p (t d) -> p t d", d=512)
    with contextlib.ExitStack() as pm:
        mixT = sb("mixT", [128, 8, SEQ], BF16, stack=pm)

        with contextlib.ExitStack() as p1:
            xt = [sb(f"xt{j}", [128, D], stack=p1) for j in range(2)]
            xs = [sb(f"xs{j}", [128, D], BF16, stack=p1) for j in range(2)]
            junk = sb("junk1", [128, D], BF16, stack=p1)
            st = sb("st1", [128, 3 * NTT], stack=p1)
            for T in range(NTT):
                b = T % 2
                src = x[T * 128:(T + 1) * 128, :] if T < NT else ctx[(T - NT) * 128:(T - NT + 1) * 128, :]
                S.dma("sp" if b == 0 else "act", lambda e, b=b, src=src: e.dma_start(out=xt[b][:], in_=src),
                      writes=[("xt", b)])
                S.op("act", lambda e, b=b, T=T: e.activation(out=junk[:], in_=xt[b][:], func=AF.Square,
                                                             accum_out=st[:, T:T + 1]),
                     reads=[("xt", b)], writes=["junk", ("ssq", T)])
                S.op("dve", lambda e, T=T: e.tensor_scalar(out=st[:, NTT + T:NTT + T + 1], in0=st[:, T:T + 1],
                                                           scalar1=1.0 / D, scalar2=EPS, op0=ALU.mult, op1=ALU.add),
                     reads=[("ssq", T)], writes=[("ms", T)])
                S.op("act", lambda e, T=T: e.activation(out=st[:, NTT + T:NTT + T + 1], in_=st[:, NTT + T:NTT + T + 1],
                                                        func=AF.Sqrt), reads=[("ms", T)], writes=[("ms", T)])
                S.op("dve", lambda e, T=T: e.reciprocal(out=st[:, 2 * NTT + T:2 * NTT + T + 1],
                                                        in_=st[:, NTT + T:NTT + T + 1]),
                     reads=[("ms", T)], writes=[("rstd", T)])
                S.op("act", lambda e, b=b, T=T: e.activation(out=xs[b][:], in_=xt[b][:], func=AF.Copy,
                                                             scale=st[:, 2 * NTT + T:2 * NTT + T + 1]),
                     reads=[("xt", b), ("rstd", T)], writes=[("xs", b)])
                for k in range(8):
                    S.op("pe", lambda e, b=b, k=k: e.transpose(pT[b][:, k * 128:(k + 1) * 128],
                                                               xs[b][:, k * 128:(k + 1) * 128], identb[:]),
                         reads=[("xs", b), "identb"], writes=[("pT", b)])
                go, so = (0, 8) if T < NT else (16, 24)
                for k in range(8):
                    S.op("dve", lambda e, b=b, k=k, T=T, go=go, so=so: e.tensor_scalar(
                        out=hT[:, k, T * 128:(T + 1) * 128], in0=pT[b][:, k * 128:(k + 1) * 128],
                        scalar1=mc[:, go + k:go + k + 1], scalar2=mc[:, so + k:so + k + 1],
                        op0=ALU.mult, op1=ALU.add),
                        reads=[("pT", b)], writes=[("hT", T)])
            if debug:
                S.dma("sp", lambda e: e.dma_start(out=dbg["hT"], in_=R[:, 0:9216].bitcast(BF16)),
                      reads=[("hT", T) for T in range(NTT)])
            phase_end("p1")
        hT_all = [("hT", T) for T in range(NTT)]

        def load_w(tile_ap, dram_w, col0, ncols, key):
            for k0 in range(0, 8, 4):
                S.dma("pool", lambda e, k0=k0: e.dma_start(
                    out=tile_ap[:, k0:k0 + 4, :],
                    in_=dram_w[k0 * 128:(k0 + 4) * 128, col0:col0 + ncols].rearrange("(k p) n -> p k n", p=128)),
                    writes=[(key, k0)])
            return [(key, 0), (key, 4)]

        with contextlib.ExitStack() as p2:
            qT = sb("qT", [128, 4, SEQ], BF16, stack=p2)
            kT = sb("kT", [128, 4, TOK], BF16, stack=p2)
            vaug = sb("vaug", [128, NTT, 8, 65], BF16, stack=p2)
            p2a = contextlib.ExitStack()
            wna = sb("wna", [128, 8, 1536], BF16, stack=p2a)
            wk = load_w(wna, w_in, 0, 1536, "wna")
            S.op("pool", lambda e: e.memset(vaug[:, :, :, 64:65], 1.0), writes=["vones"])
            cnt = 0
            for which, dst, ntok, cbase in (("q", qT, SEQ, 0), ("k", kT, TOK, 512)):
                for hp in range(4):
                    for t0 in range(0, ntok, 512):
                        tw = min(512, ntok - t0)
                        pb = cnt % 4
                        for k in range(8):
                            S.op("pe", lambda e, k=k, hp=hp, t0=t0, tw=tw, pb=pb, cbase=cbase: e.matmul(
                                ps[pb][:, 0:tw], wna[:, k, cbase + hp * 128: cbase + (hp + 1) * 128],
                                hT[:, k, t0:t0 + tw], start=(k == 0), stop=(k == 7)),
                                reads=wk + hT_all, writes=[("ps", pb)])
                        eng = "act" if cnt % 2 == 0 else "dve"
                        if eng == "act":
                            S.op("act", lambda e, dst=dst, hp=hp, t0=t0, tw=tw, pb=pb: e.activation(
                                out=dst[:, hp, t0:t0 + tw], in_=ps[pb][:, 0:tw], func=AF.Copy),
                                reads=[("ps", pb)], writes=[(which, hp, t0)])
                        else:
                            S.op("dve", lambda e, dst=dst, hp=hp, t0=t0, tw=tw, pb=pb: e.tensor_copy(
                                out=dst[:, hp, t0:t0 + tw], in_=ps[pb][:, 0:tw]),
                                reads=[("ps", pb)], writes=[(which, hp, t0)])
                        cnt += 1
            for T in range(NTT):
                pb = cnt % 4
                for k in range(8):
                    S.op("pe", lambda e, k=k, T=T, pb=pb: e.matmul(
                        ps[pb][:, :], hT[:, k, T * 128:(T + 1) * 128], wna[:, k, 1024:1536],
                        start=(k == 0), stop=(k == 7)), reads=wk + hT_all, writes=[("ps", pb)])
                if cnt % 2 == 0:
                    S.op("act", lambda e, T=T, pb=pb: e.activation(
                        out=vaug[:, T, :, 0:64], in_=ps[pb][:, :].rearrange("p (h d) -> p h d", d=64), func=AF.Copy),
                        reads=[("ps", pb)], writes=[("v", T)])
                else:
                    S.op("dve", lambda e, T=T, pb=pb: e.tensor_copy(
                        out=vaug[:, T, :, 0:64], in_=ps[pb][:, :].rearrange("p (h d) -> p h d", d=64)),
                        reads=[("ps", pb)], writes=[("v", T)])
                cnt += 1
            phase_end("p2a")
            p2a.close()
            bt = sb("bt", [128, 8, 9, 128], stack=p2)
            Ssb = [sb(f"Ssb{j}", [128, 640], stack=p2) for j in range(2)]
            Pb = [sb(f"Pb{j}", [128, 896], BF16, stack=p2) for j in range(2)]
            rden = sb("rden", [128, 16], stack=p2)
            natok = [sb(f"natok{j}", [128, 512], BF16, stack=p2) for j in range(2)]
            S.dma("sp", lambda e: e.dma_start(out=bt[:].rearrange("p h t q -> p (h t q)"), in_=biasT), writes=["bt"])

            qk_all_r = []
            it = 0
            for i in range(NT):
                chunks = na_chunks(i)
                nw = len(chunks)
                nb = i % 2
                for h in range(8):
                    hp, po = h // 2, (h % 2) * 64
                    sbuf_i = it % 2
                    b0, b1 = ps[2 * sbuf_i], ps[2 * sbuf_i + 1]

                    def sloc(j):
                        return (b0, j * 128) if j < 4 else (b1, (j - 4) * 128)
                    for j, (c, t) in enumerate(chunks):
                        bk, co = sloc(j)
                        S.op("pe", lambda e, bk=bk, co=co, c=c, hp=hp, po=po, i=i: e.matmul(
                            bk[:, co:co + 128], kT[po:po + 64, hp, c * 128:(c + 1) * 128],
                            qT[po:po + 64, hp, i * 128:(i + 1) * 128], start=True, stop=True),
                            reads=[], writes=[("psS", sbuf_i, j // 4)])
                    for cc_ in range(2):
                        S.op("pe", lambda e, cc_=cc_, hp=hp, po=po, i=i, b1=b1: e.matmul(
                            b1[:, 128 + cc_ * 128: 256 + cc_ * 128],
                            kT[po:po + 64, hp, SEQ + cc_ * 128: SEQ + (cc_ + 1) * 128],
                            qT[po:po + 64, hp, i * 128:(i + 1) * 128], start=True, stop=True),
                            reads=[], writes=[("psS", sbuf_i, 1)])
                    for j, (c, t) in enumerate(chunks):
                        bk, co = sloc(j)
                        S.op("dve", lambda e, bk=bk, co=co, j=j, t=t, h=h, sbuf_i=sbuf_i: e.scalar_tensor_tensor(
                            out=Ssb[sbuf_i][:, j * 128:(j + 1) * 128], in0=bk[:, co:co + 128], scalar=0.125,
                            in1=bt[:, h, t, :], op0=ALU.mult, op1=ALU.add),
                            reads=[("psS", sbuf_i, j // 4), "bt"], writes=[("Ssb", sbuf_i)])
                    S.op("act", lambda e, nw=nw, sbuf_i=sbuf_i: e.activation(
                        out=Pb[sbuf_i][:, 0:nw * 128], in_=Ssb[sbuf_i][:, 0:nw * 128], func=AF.Exp),
                        reads=[("Ssb", sbuf_i)], writes=[("Pw", sbuf_i)])
                    S.op("act", lambda e, sbuf_i=sbuf_i, b1=b1: e.activation(
                        out=Pb[sbuf_i][:, 640:896], in_=b1[:, 128:384], func=AF.Exp, scale=0.125),
                        reads=[("psS", sbuf_i, 1)], writes=[("Pc", sbuf_i)])
                    ob = ps[4 + h // 4]
                    oc = (h % 4) * 128
                    nmm = nw + 2
                    for j, (c, t) in enumerate(chunks):
                        S.op("pe", lambda e, j=j, c=c, h=h, ob=ob, oc=oc, sbuf_i=sbuf_i, nmm=nmm: e.matmul(
                            ob[:, oc:oc + 65], Pb[sbuf_i][:, j * 128:(j + 1) * 128], vaug[:, c, h, :],
                            start=(j == 0), stop=False),
                            reads=[("Pw", sbuf_i)], writes=[("psO", h)])
                    for cc_ in range(2):
                        S.op("pe", lambda e, cc_=cc_, h=h, ob=ob, oc=oc, sbuf_i=sbuf_i: e.matmul(
                            ob[:, oc:oc + 65], Pb[sbuf_i][:, 640 + cc_ * 128: 768 + cc_ * 128], vaug[:, NT + cc_, h, :],
                            start=False, stop=(cc_ == 1)),
                            reads=[("Pc", sbuf_i)], writes=[("psO", h)])
                    S.op("dve", lambda e, h=h, ob=ob, oc=oc: e.reciprocal(out=rden[:, h:h + 1], in_=ob[:, oc + 64:oc + 65]),
                         reads=[("psO", h)], writes=[("rden", h)])
                    S.op("dve", lambda e, h=h, ob=ob, oc=oc, nb=nb: e.tensor_scalar(
                        out=natok[nb][:, h * 64:(h + 1) * 64], in0=ob[:, oc:oc + 64], scalar1=rden[:, h:h + 1],
                        scalar2=None, op0=ALU.mult),
                        reads=[("psO", h), ("rden", h)], writes=[("natok", nb)])
                    it += 1
                for j in range(4):
                    S.op("pe", lambda e, j=j, nb=nb: e.transpose(pT[nb][:, j * 128:(j + 1) * 128],
                                                                natok[nb][:, j * 128:(j + 1) * 128], identb[:]),
                         reads=[("natok", nb)], writes=[("pT", nb)])
                S.op("act", lambda e, i=i, nb=nb: e.activation(
                    out=mixT[:, 0:4, i * 128:(i + 1) * 128],
                    in_=pT[nb][:, 0:512].rearrange("p (j t) -> p j t", t=128), func=AF.Copy),
                    reads=[("pT", nb)], writes=[("mixna", i)])
            phase_end("p2")

        with contextlib.ExitStack() as p3:
            oacc = sb("oacc", [128, NT, 512], stack=p3)
            with contextlib.ExitStack() as p3a:
                wv = sb("wv", [128, 8, 512], BF16, stack=p3a)
                wk = load_w(wv, w_in, 3 * 512 + 3 * 512, 512, "wv")
                for T in range(NTT):
                    pb = T % 4
                    for k in range(8):
                        S.op("pe", lambda e, k=k, T=T, pb=pb: e.matmul(
                            ps[pb][:, :], hT[:, k, T * 128:(T + 1) * 128], wv[:, k, :],
                            start=(k == 0), stop=(k == 7)), reads=wk, writes=[("ps", pb)])
                    if T % 2 == 0:
                        S.op("act", lambda e, T=T, pb=pb: e.activation(out=vtok[:, T, :], in_=ps[pb][:, :], func=AF.Copy),
                             reads=[("ps", pb)], writes=[("vtok", T)])
                    else:
                        S.op("dve", lambda e, T=T, pb=pb: e.tensor_copy(out=vtok[:, T, :], in_=ps[pb][:, :]),
                             reads=[("ps", pb)], writes=[("vtok", T)])
                phase_end("p3a")
            with contextlib.ExitStack() as p3b:
                rmask = sb("rmask_sb", [128, TOK], stack=p3b)
                A_ = sb("hgA", [128, TOK], stack=p3b)
                B_ = sb("hgB", [128, TOK], stack=p3b)
                C_ = sb("hgC", [128, TOK], stack=p3b)
                qsb = sb("hgqs", [128, 512], stack=p3b)
                Qt = sb("hgQt", [128, SEQ], BF16, stack=p3b)
                Qs = sb("hgQs", [128, SEQ], BF16, stack=p3b)
                Ks = sb("hgKs", [128, TOK], BF16, stack=p3b)
                Kf = sb("hgKf", [128, TOK], BF16, stack=p3b)
                Ktok = sb("hgKtok", [128, NTT, 128], BF16, stack=p3b)
                wqf = sb("hgwqf", [128, 8, 256], BF16, stack=p3b)
                sc_ = sb("hgsc", [128, 5, NCH], stack=p3b)
                Sst = [sb(f"hgS{j}", [128, 128], stack=p3b) for j in range(2)]
                Usc = [sb(f"hgUsc{j}", [128, 128], stack=p3b) for j in range(4)]
                Smb = [sb(f"hgSmb{j}", [128, 128], BF16, stack=p3b) for j in range(2)]
                Qe = sb("hgQe", [128, SEQ], BF16, stack=p3b)
                Qo = sb("hgQo", [128, SEQ], BF16, stack=p3b)
                ATs = [sb(f"hgATs{j}", [128, 128], BF16, stack=p3b) for j in range(2)]

                S.dma("sp", lambda e: e.dma_start(out=rmask[:], in_=rmask_d), writes=["rmask"])

                def v3(t, n=TOK):
                    return t[:, 0:n].rearrange("p (t s) -> p t s", s=64)

                for dr in range(2):
                    for hh in range(4):
                        dh = dr * 4 + hh
                        S.dma("pool", lambda e, hh=hh: e.dma_start(
                            out=wqf[:, :, 0:128],
                            in_=w_in[:, 1536 + hh * 128:1536 + (hh + 1) * 128].rearrange("(k p) n -> p k n", p=128)),
                            writes=["wq_h"])
                        S.dma("pool", lambda e, hh=hh, dr=dr: e.dma_start(
                            out=wqf[:, :, 128:256],
                            in_=w_in[:, 2048 + dr * 512 + hh * 128:2048 + dr * 512 + (hh + 1) * 128].rearrange(
                                "(k p) n -> p k n", p=128)), writes=["wf_h"])
                        for ci, t0 in enumerate(range(0, TOK, 512)):
                            tw = min(512, TOK - t0)
                            pb = ci % 4
                            for k in range(8):
                                S.op("pe", lambda e, k=k, t0=t0, tw=tw, pb=pb: e.matmul(
                                    ps[pb][:, 0:tw], wqf[:, k, 128:256], hT[:, k, t0:t0 + tw],
                                    start=(k == 0), stop=(k == 7)), reads=["wf_h"], writes=[("ps", pb)])
                            S.op("act", lambda e, t0=t0, tw=tw, pb=pb: e.activation(
                                out=A_[:, t0:t0 + tw], in_=ps[pb][:, 0:tw], func=AF.Sigmoid),
                                reads=[("ps", pb)], writes=["A"])
                        for ci, t0 in enumerate(range(0, SEQ, 512)):
                            pb = ci % 4
                            for k in range(8):
                                S.op("pe", lambda e, k=k, t0=t0, pb=pb: e.matmul(
                                    ps[pb][:, :], wqf[:, k, 0:128], hT[:, k, t0:t0 + 512],
                                    start=(k == 0), stop=(k == 7)), reads=["wq_h"], writes=[("ps", pb)])
                        S.op("dve", lambda e, dh=dh: e.tensor_scalar(out=A_[:], in0=A_[:], scalar1=oml[:, dh:dh + 1],
                                                                     scalar2=lb[:, dh:dh + 1], op0=ALU.mult, op1=ALU.add),
                             reads=["A"], writes=["A"])
                        S.op("act", lambda e: e.activation(out=B_[:], in_=A_[:], func=AF.Ln), reads=["A"], writes=["B"])
                        S.op("dve", lambda e: e.tensor_scalar(out=A_[:], in0=A_[:], scalar1=-1.0, scalar2=1.0,
                                                              op0=ALU.mult, op1=ALU.add), reads=["A", "B"], writes=["A"])
                        S.op("dve", lambda e: e.tensor_tensor_scan(out=C_[:], data0=rmask[:], data1=B_[:], initial=0.0,
                                                                   op0=ALU.mult, op1=ALU.add),
                             reads=["B", "rmask"], writes=["C"])
                        if dr == 0:
                            gbuf, gkey = C_, "C"
                            refpos = 31
                        else:
                            S.op("dve", lambda e: e.tensor_tensor(out=B_[:], in0=B_[:], in1=C_[:], op=ALU.subtract),
                                 reads=["B", "C"], writes=["B"])
                            S.op("dve", lambda e: e.tensor_tensor(
                                out=v3(B_), in0=v3(B_), in1=v3(C_)[:, :, 63:64].to_broadcast([128, NCH, 64]), op=ALU.add),
                                reads=["B", "C"], writes=["B"])
                            gbuf, gkey = B_, "B"
                            refpos = 32
                        endpos = 63 if dr == 0 else 0
                        S.op("dve", lambda e, gbuf=gbuf, refpos=refpos: e.tensor_copy(
                            out=sc_[:, 0, :].unsqueeze(2), in_=v3(gbuf)[:, :, refpos:refpos + 1]), reads=[gkey], writes=["sc0"])
                        S.op("dve", lambda e, gbuf=gbuf, endpos=endpos: e.tensor_copy(
                            out=sc_[:, 1, :].unsqueeze(2), in_=v3(gbuf)[:, :, endpos:endpos + 1]), reads=[gkey], writes=["sc1"])
                        S.op("act", lambda e: e.activation(out=sc_[:, 2:4, :], in_=sc_[:, 0:2, :], func=AF.Exp),
                             reads=["sc0", "sc1"], writes=["sc23"])
                        S.op("dve", lambda e: e.tensor_tensor(out=sc_[:, 4, :], in0=sc_[:, 1, :], in1=sc_[:, 0, :],
                                                              op=ALU.subtract), reads=["sc0", "sc1"], writes=["sc4"])
                        S.op("act", lambda e: e.activation(out=sc_[:, 4, :], in_=sc_[:, 4, :], func=AF.Exp),
                             reads=["sc4"], writes=["sc4"])
                        obuf, okey = (B_, "B") if dr == 0 else (C_, "C")
                        S.op("dve", lambda e, gbuf=gbuf: e.tensor_tensor(
                            out=v3(gbuf), in0=v3(gbuf), in1=sc_[:, 0, :].unsqueeze(2).to_broadcast([128, NCH, 64]),
                            op=ALU.subtract), reads=[gkey, "sc0", "sc1"], writes=[gkey])
                        S.op("act", lambda e, gbuf=gbuf, obuf=obuf: e.activation(out=obuf[:, 0:SEQ], in_=gbuf[:, 0:SEQ], func=AF.Exp),
                             reads=[gkey, okey], writes=[okey])
                        S.op("act", lambda e, gbuf=gbuf: e.activation(out=gbuf[:], in_=gbuf[:], func=AF.Exp, scale=-1.0),
                             reads=[gkey, okey], writes=[gkey])
                        S.op("dve", lambda e, gbuf=gbuf: e.tensor_tensor(out=Kf[:], in0=A_[:], in1=gbuf[:], op=ALU.mult),
                             reads=["A", gkey], writes=["Kf"])
                        hk = 1 if dr == 0 else 0
                        hq = 1 - hk
                        def h32(t):
                            return t[:].rearrange("p (t two s) -> p t two s", two=2, s=32)

                        def h64(t):
                            return t[:].rearrange("p (t two s) -> p t two s", two=2, s=64)
                        if hh == 0:
                            S.op("pool", lambda e: e.memset(Ks[:], 0.0), reads=["Ks"], writes=["Ks"])
                            S.op("pool", lambda e: e.memset(Qs[:], 0.0), reads=["Qs"], writes=["Qs"])
                            if dr == 0:
                                S.op("pool", lambda e: e.memset(Qe[:], 0.0), reads=["Qe"], writes=["Qe"])
                                S.op("pool", lambda e: e.memset(Qo[:], 0.0), reads=["Qo"], writes=["Qo"])
                        S.op("dve", lambda e, hk=hk: e.tensor_copy(out=h32(Ks)[:, :, 1 - hk, :], in_=h32(Kf)[:, :, 1 - hk, :]),
                             reads=["Kf", "Ks"], writes=["Ks"])
                        for ci, t0 in enumerate(range(0, SEQ, 512)):
                            pb = ci % 4
                            S.op("act", lambda e, pb=pb: e.activation(out=qsb[:], in_=ps[pb][:, :], func=AF.Silu),
                                 reads=[("ps", pb)], writes=["qsb"])
                            S.op("dve", lambda e, t0=t0, obuf=obuf: e.tensor_tensor(out=Qt[:, t0:t0 + 512], in0=qsb[:],
                                                                                    in1=obuf[:, t0:t0 + 512], op=ALU.mult),
                                 reads=["qsb", okey], writes=["Qt"])
                        S.op("dve", lambda e, hq=hq: e.tensor_copy(out=h32(Qs)[:, :, 1 - hq, :], in_=h32(Qt)[:, :, 1 - hq, :]),
                             reads=["Qt", "Qs"], writes=["Qs"])
                        S.op("act", lambda e: e.activation(out=h64(Qe)[:, :, 0, :], in_=h64(Qt)[:, :, 0, :], func=AF.Copy),
                             reads=["Qt", "Qe"], writes=["Qe"])
                        S.op("act", lambda e: e.activation(out=h64(Qo)[:, :, 1, :], in_=h64(Qt)[:, :, 1, :], func=AF.Copy),
                             reads=["Qt", "Qo"], writes=["Qo"])
                        for g0 in range(0, NTT, 8):
                            nb = (g0 // 8) % 2
                            gn = min(8, NTT - g0)
                            for j in range(gn):
                                S.op("pe", lambda e, T=g0 + j, j=j, nb=nb: e.transpose(
                                    pT[nb][:, j * 128:(j + 1) * 128], Kf[:, T * 128:(T + 1) * 128], identb[:]),
                                    reads=["Kf"], writes=[("pT", nb)])
                            S.op("act", lambda e, g0=g0, gn=gn, nb=nb: e.activation(
                                out=Ktok[:, g0:g0 + gn, :],
                                in_=pT[nb][:, 0:gn * 128].rearrange("p (j t) -> p j t", t=128), func=AF.Copy),
                                reads=[("pT", nb)], writes=[("Ktok", T) for T in range(g0, g0 + gn)])
                        S.op("pool", lambda e, hk=hk: e.memset(
                            Kf[:].rearrange("p (t two s) -> p t two s", two=2, s=32)[:, :, 1 - hk, :], 0.0),
                            reads=["Kf"], writes=["Kf"])
                        order = [16, 17] + list(range(NT)) if dr == 0 else [17, 16] + list(range(NT - 1, -1, -1))
                        tri = TRIF if dr == 0 else TRIB
                        kcnt = [0]
                        S.op("dve", lambda e: e.memset(Sst[0][:], 0.0), reads=[("S", 0)], writes=[("S", 0)])

                        def chunks_of(T):
                            return [2 * T, 2 * T + 1] if dr == 0 else [2 * T + 1, 2 * T]

                        def stage_a(n):
                            T = order[n]
                            q = n % 2
                            for ci, c in enumerate(chunks_of(T)):
                                par = c % 2
                                S.op("pe", lambda e, T=T, par=par, ci=ci, hh=hh: e.matmul(
                                    ps[2 + ci][:, 0:128], Ktok[par * 64:(par + 1) * 64, T, :],
                                    vtok[par * 64:(par + 1) * 64, T, hh * 128:(hh + 1) * 128], start=True, stop=True),
                                    reads=[("Ktok", T)], writes=[("ps", 2 + ci)])
                                S.op("dve", lambda e, c=c, ci=ci, q=q: e.tensor_scalar(
                                    out=Usc[2 * q + ci][:], in0=ps[2 + ci][:, 0:128], scalar1=sc_[:, 4, c:c + 1], scalar2=None,
                                    op0=ALU.mult), reads=[("ps", 2 + ci), "sc4"], writes=[("Usc", 2 * q + ci)])
                            if T < NT:
                                S.op("pe", lambda e, T=T: e.matmul(ps[4][:, 0:128], Ks[:, T * 128:(T + 1) * 128],
                                                                   Qt[:, T * 128:(T + 1) * 128], start=True, stop=False),
                                     reads=["Ks", "Qt"], writes=[("ps", 4)])
                                S.op("pe", lambda e, T=T: e.matmul(ps[4][:, 0:128], Kf[:, T * 128:(T + 1) * 128],
                                                                   Qs[:, T * 128:(T + 1) * 128], start=False, stop=True),
                                     reads=["Kf", "Qs"], writes=[("ps", 4)])

                        def stage_a2(n):
                            T = order[n]
                            q = n % 2
                            if T < NT:
                                S.op("dve", lambda e, q=q, tri=tri: e.tensor_tensor(out=ATs[q][:], in0=ps[4][:, 0:128], in1=tri, op=ALU.mult),
                                     reads=[("ps", 4), "cA"], writes=[("ATs", q)])
                                ob = ps[5] if q == 0 else ps[1]
                                S.op("pe", lambda e, T=T, q=q, ob=ob, hh=hh: e.matmul(ob[:, 0:128], ATs[q][:], vtok[:, T, hh * 128:(hh + 1) * 128],
                                                                               start=True, stop=False),
                                     reads=[("ATs", q)], writes=[("ps", 5 if q == 0 else 1)])

                        def stage_b(n):
                            T = order[n]
                            q = n % 2
                            lat = T < NT
                            ob = ps[5] if q == 0 else ps[1]
                            for ci, c in enumerate(chunks_of(T)):
                                par = c % 2
                                k = kcnt[0]
                                kcnt[0] += 1
                                si, so = k % 2, (k + 1) % 2
                                if lat:
                                    S.op("act", lambda e, c=c, ci=ci, si=si: e.activation(
                                        out=Smb[ci][:], in_=Sst[si][:], func=AF.Copy, scale=sc_[:, 2, c:c + 1]),
                                        reads=[("S", si), "sc23"], writes=[("Smb", ci)])
                                    Qz, qzk = (Qe, "Qe") if par == 0 else (Qo, "Qo")
                                    S.op("pe", lambda e, T=T, ci=ci, Qz=Qz, ob=ob: e.matmul(
                                        ob[:, 0:128], Qz[:, T * 128:(T + 1) * 128], Smb[ci][:], start=False, stop=(ci == 1)),
                                        reads=[("Smb", ci), qzk], writes=[("ps", 5 if q == 0 else 1)])
                                S.op("dve", lambda e, c=c, ci=ci, q=q, si=si, so=so: e.scalar_tensor_tensor(
                                    out=Sst[so][:], in0=Sst[si][:], scalar=sc_[:, 3, c:c + 1], in1=Usc[2 * q + ci][:],
                                    op0=ALU.mult, op1=ALU.add), reads=[("Usc", 2 * q + ci), ("S", si), "sc23"], writes=[("S", so)])
                            if lat:
                                if dr == 0:
                                    S.op("act", lambda e, T=T, ob=ob, hh=hh: e.activation(
                                        out=oacc[:, T, hh * 128:(hh + 1) * 128], in_=ob[:, 0:128], func=AF.Copy),
                                        reads=[("ps", 5 if q == 0 else 1)], writes=[("oacc", T, hh)])
                                else:
                                    S.op("dve", lambda e, T=T, ob=ob, hh=hh: e.tensor_tensor(
                                        out=oacc[:, T, hh * 128:(hh + 1) * 128], in0=ob[:, 0:128],
                                        in1=oacc[:, T, hh * 128:(hh + 1) * 128], op=ALU.add),
                                        reads=[("ps", 5 if q == 0 else 1), ("oacc", T, hh)], writes=[("oacc", T, hh)])

                        stage_a(0)
                        stage_a2(0)
                        for n in range(len(order)):
                            if n + 1 < len(order):
                                stage_a(n + 1)
                            stage_b(n)
                            if n + 1 < len(order):
                                stage_a2(n + 1)
                        if kcnt[0] % 2 == 1:
                            pass
                phase_end("p3b")
            with contextlib.ExitStack() as p3c:
                wg = sb("wg", [128, 8, 512], BF16, stack=p3c)
                hgnb = sb("hgnb", [128, 512], stack=p3c)
                sg = [sb(f"sg{j}", [128, 512], stack=p3c) for j in range(2)]
                yb = [sb(f"yb{j}", [128, 512], stack=p3c) for j in range(2)]
                yt = [sb(f"yt{j}", [128, 512], BF16, stack=p3c) for j in range(2)]
                jk = sb("jk3", [128, 128], BF16, stack=p3c)
                st3 = sb("st3", [128, NT, 8], stack=p3c)
                wk = load_w(wg, w_in, 1536 + 4 * 512, 512, "wg")
                S.dma("sp", lambda e: e.dma_start(out=hgnb[:], in_=hgn), writes=["hgnb"])
                for T in range(NT):
                    for hh in range(4):
                        S.op("act", lambda e, T=T, hh=hh: e.activation(
                            out=jk[:], in_=oacc[:, T, hh * 128:(hh + 1) * 128], func=AF.Square,
                            accum_out=st3[:, T, hh:hh + 1]), reads=[], writes=["jk3", ("ss3", T)])
                ss_all = [("ss3", T) for T in range(NT)]
                S.op("dve", lambda e: e.tensor_scalar(out=st3[:, :, 4:8], in0=st3[:, :, 0:4], scalar1=1.0 / 128,
                                                      scalar2=EPS, op0=ALU.mult, op1=ALU.add),
                     reads=ss_all, writes=["ms3"])
                S.op("act", lambda e: e.activation(out=st3[:, :, 4:8], in_=st3[:, :, 4:8], func=AF.Sqrt),
                     reads=["ms3"], writes=["ms3"])
                S.op("dve", lambda e: e.reciprocal(out=st3[:, :, 0:4], in_=st3[:, :, 4:8]),
                     reads=["ms3"] + ss_all, writes=["rs3"])
                for T in range(NT):
                    b = T % 2
                    for k in range(8):
                        S.op("pe", lambda e, k=k, T=T, b=b: e.matmul(
                            ps[b][:, :], hT[:, k, T * 128:(T + 1) * 128], wg[:, k, :],
                            start=(k == 0), stop=(k == 7)), reads=wk, writes=[("ps", b)])
                    S.op("act", lambda e, b=b: e.activation(out=sg[b][:], in_=ps[b][:, :], func=AF.Silu),
                         reads=[("ps", b)], writes=[("sg", b)])
                    S.op("dve", lambda e, T=T, b=b: e.tensor_tensor(
                        out=yb[b][:].rearrange("p (h d) -> p h d", d=128),
                        in0=oacc[:, T, :].rearrange("p (h d) -> p h d", d=128),
                        in1=st3[:, T, 0:4].unsqueeze(2).to_broadcast([128, 4, 128]), op=ALU.mult),
                        reads=["rs3"], writes=[("yb", b)])
                    S.op("dve", lambda e, b=b: e.tensor_tensor(out=yb[b][:], in0=yb[b][:], in1=hgnb[:], op=ALU.mult),
                         reads=[("yb", b), "hgnb"], writes=[("yb", b)])
                    S.op("dve", lambda e, b=b: e.tensor_tensor(out=yt[b][:], in0=yb[b][:], in1=sg[b][:], op=ALU.mult),
                         reads=[("yb", b), ("sg", b)], writes=[("yt", b)])
                    for j in range(4):
                        S.op("pe", lambda e, j=j, b=b: e.transpose(pT[b][:, j * 128:(j + 1) * 128],
                                                                   yt[b][:, j * 128:(j + 1) * 128], identb[:]),
                             reads=[("yt", b)], writes=[("pT", b)])
                    S.op("act", lambda e, T=T, b=b: e.activation(
                        out=mixT[:, 4:8, T * 128:(T + 1) * 128],
                        in_=pT[b][:, 0:512].rearrange("p (j t) -> p j t", t=128), func=AF.Copy),
                        reads=[("pT", b)], writes=[("mixhg", T)])
                if debug:
                    S.dma("sp", lambda e: e.dma_start(out=dbg["mixT"], in_=mixT[:].rearrange("p k t -> p (k t)")),
                          reads=[("mixhg", T) for T in range(NT)])
                phase_end("p3")

        with contextlib.ExitStack() as p4:
            wo32 = sb("wo32", [128, 8, D], stack=p4)
            wob = sb("wob", [128, 8, D], BF16, stack=p4)
            g1b = sb("g1b", [128, D], stack=p4)
            S.dma("sp", lambda e: e.dma_start(out=g1b[:], in_=scr_bc[0]), writes=["g1b"])
            for k in range(8):
                S.dma("sp" if k % 2 == 0 else "act", lambda e, k=k: e.dma_start(
                    out=wo32[:, k, :], in_=w_out[k * 128:(k + 1) * 128, :]), writes=[("wo32", k)])
                S.op("dve" if k % 2 == 0 else "pool", lambda e, k=k: e.tensor_tensor(
                    out=wob[:, k, :], in0=wo32[:, k, :], in1=g1b[:], op=ALU.mult),
                    reads=[("wo32", k), "g1b"], writes=[("wob", k)])
            wob_all = [("wob", k) for k in range(8)]
            for T in range(NT):
                S.dma("sp" if T % 2 == 0 else "act", lambda e, T=T: e.dma_start(
                    out=x1[:, T, :], in_=x[T * 128:(T + 1) * 128, :]), writes=[("x1", T)])
                for hf in range(2):
                    pb = (2 * T + hf) % 4
                    for k in range(8):
                        S.op("pe", lambda e, k=k, T=T, hf=hf, pb=pb: e.matmul(
                            ps[pb][:, :], mixT[:, k, T * 128:(T + 1) * 128], wob[:, k, hf * 512:(hf + 1) * 512],
                            start=(k == 0), stop=(k == 7)), reads=wob_all, writes=[("ps", pb)])
                    S.op("dve", lambda e, T=T, hf=hf, pb=pb: e.tensor_tensor(
                        out=x1[:, T, hf * 512:(hf + 1) * 512], in0=ps[pb][:, :], in1=x1[:, T, hf * 512:(hf + 1) * 512],
                        op=ALU.add), reads=[("ps", pb), ("x1", T)], writes=[("x1", T)])
            if debug:
                S.dma("sp", lambda e: e.dma_start(out=dbg["x1"], in_=R[:]),
                      reads=[("x1", T) for T in range(NT)])
            phase_end("p4")

    eidx = sb("eidx", [128, NT, 128], I32)
    gw = sb("gw", [128, NT, 128])
    rs2 = sb("rs2", [128, NT])
    with contextlib.ExitStack() as p5:
        wqb = sb("wqb", [128, 8, 2048], BF16, stack=p5)
        kTb = sb("kTb", [128, 16, 128], BF16, stack=p5)
        junk = sb("junk5", [128, D], BF16, stack=p5)
        xs = [sb(f"xs5{j}", [128, D], BF16, stack=p5) for j in range(2)]
        h2T = [sb(f"h2T{j}", [128, 8, 128], BF16, stack=p5) for j in range(2)]
        qTp = [sb(f"qTp{j}", [128, 16, 128], BF16, stack=p5) for j in range(2)]
        ssb = sb("ssb", [128, 16, 128], stack=p5)
        s2 = sb("s2", [128, 16, 128], stack=p5)
        top = sb("top", [128, 16, 16], stack=p5)
        itop = sb("itop", [128, 16, 16], U32, stack=p5)
        itf = sb("itf", [128, 16, 16], stack=p5)
        cand = sb("cand", [128, 8, 256], stack=p5)
        cand2 = sb("cand2", [128, 8, 256], stack=p5)
        ctop = sb("ctop", [128, 8, 16], stack=p5)
        cpos = sb("cpos", [128, 8, 16], U32, stack=p5)
        paf = sb("paf", [128, 128], stack=p5)
        pai = sb("pai", [128, 128], I32, stack=p5)
        pbf = sb("pbf", [128, 128], stack=p5)
        oh = sb("oh", [128, 128, 16], stack=p5)
        selA = sb("selA", [128, 128], stack=p5)
        selB = sb("selB", [128, 128], stack=p5)
        ef = sb("ef", [128, 128], stack=p5)
        ee = sb("ee", [128, 8, 16], stack=p5)
        zz = sb("zz", [128, 16], stack=p5)
        st5 = sb("st5", [128, 2 * NT], stack=p5)

        stg = [sb(f"stg{j}", [128, 4, D], BF16, stack=p5) for j in range(2)]
        conv_steps = [(tab, c) for tab in range(2) for c in range(32)]

        def emit_conv(si):
            tab, c = conv_steps[si]
            src = (u_t, v_t)[tab].rearrange("(p j c) d -> c p j d", p=128, j=4, c=32)[c]
            dst = uv_bf.rearrange("(p j c) d -> c p j d", p=128, j=4, c=32)[c][:, :, tab * D:(tab + 1) * D]
            b = si % 2
            S.dma("pool", lambda e: e.dma_start(out=stg[b][:], in_=src), writes=[("stg", b)])
            S.dma("sp", lambda e: e.dma_start(out=dst, in_=stg[b][:]), reads=[("stg", b)], writes=[("tab", tab, c)])

        wkq = load_w(wqb[:, :, 0:1024], wq, 0, 1024, "wqa") + load_w(wqb[:, :, 1024:2048], wq, 1024, 1024, "wqb")
        S.dma("pool", lambda e: e.dma_start(out=kTb[:].rearrange("p c k -> p (c k)"), in_=keysT), writes=["kTb"])
        for T in range(NT):
            S.op("act", lambda e, T=T: e.activation(out=junk[:], in_=x1[:, T, :], func=AF.Square,
                                                    accum_out=st5[:, T:T + 1]), reads=[], writes=["junk5", ("ssq5", T)])
        S.op("dve", lambda e: e.tensor_scalar(out=st5[:, NT:2 * NT], in0=st5[:, 0:NT],
                                              scalar1=1.0 / D, scalar2=EPS, op0=ALU.mult, op1=ALU.add),
             reads=[("ssq5", T) for T in range(NT)], writes=["ms5"])
        S.op("act", lambda e: e.activation(out=st5[:, NT:2 * NT], in_=st5[:, NT:2 * NT], func=AF.Sqrt),
             reads=["ms5"], writes=["ms5"])
        S.op("dve", lambda e: e.reciprocal(out=rs2[:, :], in_=st5[:, NT:2 * NT]), reads=["ms5"], writes=["rs2"])
        for T in range(NT):
            b = T % 2
            if not _NOCONV:
                for q_ in range(4):
                    emit_conv(4 * T + q_)
            S.op("act", lambda e, b=b, T=T: e.activation(out=xs[b][:], in_=x1[:, T, :], func=AF.Copy,
                                                         scale=rs2[:, T:T + 1]), reads=["rs2"], writes=[("xs5", b)])
            for k in range(8):
                S.op("pe", lambda e, b=b, k=k: e.transpose(pT[b][:, k * 128:(k + 1) * 128],
                                                           xs[b][:, k * 128:(k + 1) * 128], identb[:]),
                     reads=[("xs5", b)], writes=[("pT", b)])
            for k in range(8):
                S.op("dve" if k % 2 == 0 else "act", (lambda e, b=b, k=k: e.tensor_scalar(
                    out=h2T[b][:, k, :], in0=pT[b][:, k * 128:(k + 1) * 128],
                    scalar1=mc[:, 32 + k:33 + k], scalar2=mc[:, 40 + k:41 + k], op0=ALU.mult, op1=ALU.add))
                    if k % 2 == 0 else (lambda e, b=b, k=k: e.activation(
                        out=h2T[b][:, k, :], in_=pT[b][:, k * 128:(k + 1) * 128], func=AF.Identity,
                        scale=mc[:, 32 + k:33 + k], bias=mc[:, 40 + k:41 + k])),
                    reads=[("pT", b)], writes=[("h2T", b)])
            for g4 in range(4):
                pb = g4
                for j in range(4):
                    pc = g4 * 4 + j
                    for k in range(8):
                        S.op("pe", lambda e, b=b, k=k, pc=pc, j=j, pb=pb: e.matmul(
                            ps[pb][:, j * 128:(j + 1) * 128], wqb[:, k, pc * 128:(pc + 1) * 128], h2T[b][:, k, :],
                            start=(k == 0), stop=(k == 7)), reads=wkq + [("h2T", b)], writes=[("ps", pb)])
                if g4 % 2 == 0:
                    S.op("act", lambda e, b=b, g4=g4, pb=pb: e.activation(
                        out=qTp[b][:, g4 * 4:(g4 + 1) * 4, :], in_=ps[pb][:, :].rearrange("p (j t) -> p j t", t=128),
                        func=AF.Copy), reads=[("ps", pb)], writes=[("qTp", b, g4)])
                else:
                    S.op("dve", lambda e, b=b, g4=g4, pb=pb: e.tensor_copy(
                        out=qTp[b][:, g4 * 4:(g4 + 1) * 4, :], in_=ps[pb][:, :].rearrange("p (j t) -> p j t", t=128)),
                        reads=[("ps", pb)], writes=[("qTp", b, g4)])
            for g4 in range(4):
                pb = 4 + (g4 % 2)
                for j in range(4):
                    pc = g4 * 4 + j
                    S.op("pe", lambda e, b=b, pc=pc, j=j, pb=pb: e.matmul(
                        ps[pb][:, j * 128:(j + 1) * 128], qTp[b][:, pc, :], kTb[:, pc, :], start=True, stop=True),
                        reads=[("qTp", b, g4), "kTb"], writes=[("ps", pb)])
                S.op("act", lambda e, g4=g4, pb=pb: e.activation(
                    out=ssb[:, g4 * 4:(g4 + 1) * 4, :], in_=ps[pb][:, :].rearrange("p (j t) -> p j t", t=128),
                    func=AF.Copy), reads=[("ps", pb)], writes=[("ssb", g4)])
            for pc in range(16):
                S.op("dve", lambda e, pc=pc: e.max(out=top[:, pc, 0:8], in_=ssb[:, pc, :]),
                     reads=[("ssb", pc // 4)], writes=[("top", pc, 0)])
            for pc in range(16):
                S.op("dve", lambda e, pc=pc: e.match_replace(out=s2[:, pc, :], in_to_replace=top[:, pc, 0:8],
                                                             in_values=ssb[:, pc, :], imm_value=NEG),
                     reads=[("ssb", pc // 4), ("top", pc, 0)], writes=[("s2", pc)])
            for pc in range(16):
                S.op("dve", lambda e, pc=pc: e.max(out=top[:, pc, 8:16], in_=s2[:, pc, :]),
                     reads=[("s2", pc)], writes=[("top", pc, 1)])
            for pc in range(16):
                S.op("dve", lambda e, pc=pc: e.max_index(out=itop[:, pc, 0:8], in_max=top[:, pc, 0:8], in_values=ssb[:, pc, :]),
                     reads=[("ssb", pc // 4), ("top", pc, 0)], writes=[("itop", pc, 0)])
            for pc in range(16):
                S.op("dve", lambda e, pc=pc: e.max_index(out=itop[:, pc, 8:16], in_max=top[:, pc, 8:16], in_values=ssb[:, pc, :]),
                     reads=[("ssb", pc // 4), ("top", pc, 1)], writes=[("itop", pc, 1)])
            tops = [("top", pc, j) for pc in range(16) for j in range(2)]
            itops = [("itop", pc, j) for pc in range(16) for j in range(2)]
            S.op("dve", lambda e: e.tensor_copy(out=itf[:], in_=itop[:]), reads=itops, writes=["itf"])
            topv = top[:].rearrange("p (h c) a -> p h c a", c=2)
            S.op("dve", lambda e: e.tensor_tensor(
                out=cand[:].rearrange("p h (a b) -> p h a b", b=16),
                in0=topv[:, :, 0, :].unsqueeze(3).to_broadcast([128, 8, 16, 16]),
                in1=topv[:, :, 1, :].unsqueeze(2).to_broadcast([128, 8, 16, 16]), op=ALU.add),
                reads=tops, writes=["cand"])
            for p in range(8):
                S.op("dve", lambda e, p=p: e.max(out=ctop[:, p, 0:8], in_=cand[:, p, :]), reads=["cand"], writes=[("ctop", p, 0)])
            for p in range(8):
                S.op("dve", lambda e, p=p: e.match_replace(out=cand2[:, p, :], in_to_replace=ctop[:, p, 0:8],
                                                           in_values=cand[:, p, :], imm_value=NEG),
                     reads=["cand", ("ctop", p, 0)], writes=[("cand2", p)])
            for p in range(8):
                S.op("dve", lambda e, p=p: e.max(out=ctop[:, p, 8:16], in_=cand2[:, p, :]), reads=[("cand2", p)], writes=[("ctop", p, 1)])
            for p in range(8):
                S.op("dve", lambda e, p=p: e.max_index(out=cpos[:, p, 0:8], in_max=ctop[:, p, 0:8], in_values=cand[:, p, :]),
                     reads=["cand", ("ctop", p, 0)], writes=[("cpos", p, 0)])
            for p in range(8):
                S.op("dve", lambda e, p=p: e.max_index(out=cpos[:, p, 8:16], in_max=ctop[:, p, 8:16], in_values=cand[:, p, :]),
                     reads=["cand", ("ctop", p, 1)], writes=[("cpos", p, 1)])
            ctops = [("ctop", p, j) for p in range(8) for j in range(2)]
            cposs = [("cpos", p, j) for p in range(8) for j in range(2)]
            cposf = cpos[:].rearrange("p h j -> p (h j)")
            S.op("dve", lambda e: e.tensor_copy(out=selA[:], in_=cposf), reads=cposs + [("sel", 0)], writes=["posf"])
            S.op("dve", lambda e: e.tensor_scalar(out=pbf[:], in0=selA[:], scalar1=0.0625, scalar2=None, op0=ALU.mult),
                 reads=["posf"], writes=["pbf"])
            S.op("dve", lambda e: e.tensor_copy(out=pai[:], in_=pbf[:]), reads=["pbf"], writes=["pai"])
            S.op("dve", lambda e: e.tensor_copy(out=paf[:], in_=pai[:]), reads=["pai"], writes=["paf"])
            S.op("dve", lambda e: e.scalar_tensor_tensor(out=pbf[:], in0=paf[:], scalar=16.0, in1=selA[:],
                                                         op0=ALU.mult, op1=ALU.is_gt), reads=["paf", "posf", "pai"], writes=["pbf"])
            S.op("dve", lambda e: e.tensor_tensor(out=paf[:], in0=paf[:], in1=pbf[:], op=ALU.subtract),
                 reads=["paf", "pbf"], writes=["paf"])
            S.op("dve", lambda e: e.scalar_tensor_tensor(out=pbf[:], in0=paf[:], scalar=-16.0, in1=selA[:],
                                                         op0=ALU.mult, op1=ALU.add), reads=["paf", "posf"], writes=["pbf"])
            itv = itf[:].rearrange("p (h c) a -> p h c a", c=2)
            for which, pf, sel in ((0, paf, selA), (1, pbf, selB)):
                S.op("dve", lambda e, pf=pf: e.tensor_tensor(
                    out=oh[:], in0=pf[:].unsqueeze(2).to_broadcast([128, 128, 16]),
                    in1=IOTA16.unsqueeze(1).to_broadcast([128, 128, 16]), op=ALU.is_equal),
                    reads=["paf", "pbf", "cA", "oh"], writes=["oh"])
                S.op("dve", lambda e, which=which: e.tensor_tensor(
                    out=oh[:].rearrange("p (h j) a -> p h j a", j=16),
                    in0=oh[:].rearrange("p (h j) a -> p h j a", j=16),
                    in1=itv[:, :, which, :].unsqueeze(2).to_broadcast([128, 8, 16, 16]), op=ALU.mult),
                    reads=["oh", "itf"], writes=["oh"])
                S.op("dve", lambda e, sel=sel: e.tensor_reduce(out=sel[:], in_=oh[:], axis=AX.X, op=ALU.add),
                     reads=["oh", "posf"], writes=[("sel", which)])
            S.op("dve", lambda e: e.scalar_tensor_tensor(out=ef[:], in0=selA[:], scalar=128.0, in1=selB[:],
                                                         op0=ALU.mult, op1=ALU.add),
                 reads=[("sel", 0), ("sel", 1)], writes=["ef"])
            S.op("dve", lambda e, T=T: e.tensor_copy(out=eidx[:, T, :], in_=ef[:]), reads=["ef"], writes=[("eidx", T)])
            S.op("dve", lambda e: e.tensor_tensor(out=ee[:], in0=ctop[:], in1=ctop[:, :, 0:1].to_broadcast([128, 8, 16]),
                                                  op=ALU.subtract), reads=ctops, writes=["ee"])
            S.op("act", lambda e: e.activation(out=ee[:], in_=ee[:], func=AF.Exp), reads=["ee"], writes=["ee"])
            S.op("dve", lambda e: e.tensor_reduce(out=zz[:, 0:8], in_=ee[:], axis=AX.X, op=ALU.add),
                 reads=["ee"], writes=["zz"])
            S.op("dve", lambda e: e.reciprocal(out=zz[:, 8:16], in_=zz[:, 0:8]), reads=["zz"], writes=["zz2"])
            S.op("dve", lambda e, T=T: e.tensor_tensor(
                out=gw[:, T, :].rearrange("p (h j) -> p h j", j=16), in0=ee[:],
                in1=zz[:, 8:16].unsqueeze(2).to_broadcast([128, 8, 16]), op=ALU.mult),
                reads=["ee", "zz2"], writes=[("gw", T)])
        if debug:
            S.dma("sp", lambda e: e.dma_start(out=dbg["eidx"], in_=eidx[:].rearrange("p t s -> p (t s)")),
                  reads=[("eidx", T) for T in range(NT)])
            S.dma("sp", lambda e: e.dma_start(out=dbg["gw"], in_=gw[:].rearrange("p t s -> p (t s)")),
                  reads=[("gw", T) for T in range(NT)])
        phase_end("p5a")

    with contextlib.ExitStack() as p6:
        NB = 12
        ring = [sb(f"ring{j}", [128, 2 * D], BF16, stack=p6) for j in range(NB)]
        NDG = 6
        dg = [sb(f"dg{j}", [128, 128], BF16, stack=p6) for j in range(NDG)]
        bc = sb("bc5", [128, 4, D], stack=p6)
        h2 = [sb(f"h2_{j}", [128, D], stack=p6) for j in range(2)]
        junk = sb("junk6", [128, D], BF16, stack=p6)
        accs = sb("accs", [128, D], stack=p6)
        aa = [sb(f"aa{j}", [128, 128], stack=p6) for j in range(2)]
        gl = [sb(f"gl{j}", [128, 128], stack=p6) for j in range(2)]
        ww = [sb(f"ww{j}", [128, 128], stack=p6) for j in range(2)]
        st6 = sb("st6", [128, 2 * NT], stack=p6)
        S.dma("sp", lambda e: e.dma_start(out=bc[:, 0, :], in_=scr_bc[1]), writes=[("bc", 0)])
        S.dma("sp", lambda e: e.dma_start(out=bc[:, 1, :], in_=scr_bc[2]), writes=[("bc", 1)])
        S.dma("sp", lambda e: e.dma_start(out=bc[:, 2, :], in_=scr_bc[3]), writes=[("bc", 2)])
        S.dma("sp", lambda e: e.dma_start(out=bc[:, 3, :], in_=nfb), writes=[("bc", 3)])
        gi = gd = 0
        for T in range(NT):
            pu = T % 2
            S.op("dve", lambda e, T=T, pu=pu: e.scalar_tensor_tensor(out=h2[pu][:], in0=x1[:, T, :], scalar=rs2[:, T:T + 1],
                                                                     in1=bc[:, 1, :], op0=ALU.mult, op1=ALU.mult),
                 reads=[("bc", 1)], writes=[("h2", pu)])
            S.op("dve", lambda e, pu=pu: e.tensor_tensor(out=h2[pu][:], in0=h2[pu][:], in1=bc[:, 2, :], op=ALU.add),
                 reads=[("h2", pu), ("bc", 2)], writes=[("h2", pu)])
            S.op("dve", lambda e, pu=pu: e.memset(aa[pu][:], 0.0), writes=[("aa", pu)])
            for s_ in range(128):
                r = gi % NB
                gi += 1
                S.dma("pool", lambda e, T=T, s_=s_, r=r: e.indirect_dma_start(
                    out=ring[r][:], out_offset=None, in_=uv_bf,
                    in_offset=bass.IndirectOffsetOnAxis(ap=eidx[:, T, s_:s_ + 1], axis=0)),
                    reads=[], writes=[("ring", r)])
                S.op("dve", lambda e, s_=s_, r=r, pu=pu: e.scalar_tensor_tensor(
                    out=junk[:], in0=ring[r][:, 0:D], scalar=1.0, in1=h2[pu][:], op0=ALU.mult, op1=ALU.mult,
                    accum_out=aa[pu][:, s_:s_ + 1]), reads=[("ring", r), ("h2", pu), ("aa", pu)],
                    writes=["junk6", ("aas", pu, s_)])
                S.op("act", lambda e, s_=s_, pu=pu: e.activation(out=gl[pu][:, s_:s_ + 1], in_=aa[pu][:, s_:s_ + 1], func=AF.Gelu),
                     reads=[("aas", pu, s_)], writes=[("gl", pu, s_)])
                S.op("act", lambda e, T=T, s_=s_, pu=pu: e.activation(out=ww[pu][:, s_:s_ + 1], in_=gl[pu][:, s_:s_ + 1],
                                                                     func=AF.Copy, scale=gw[:, T, s_:s_ + 1]),
                     reads=[("gl", pu, s_)], writes=[("ww", pu, s_)])
                dj = gd % NDG
                gd += 1
                S.op("act", lambda e, s_=s_, dj=dj, pu=pu: e.activation(
                    out=dg[dj][:], in_=identb[:], func=AF.Copy, scale=ww[pu][:, s_:s_ + 1]),
                    reads=[("ww", pu, s_)], writes=[("dg", dj)])
                for hf in range(2):
                    S.op("pe", lambda e, s_=s_, dj=dj, r=r, pu=pu, hf=hf: e.matmul(
                        ps[2 * pu + hf][:, :], dg[dj][:], ring[r][:, D + hf * 512: D + (hf + 1) * 512],
                        start=(s_ == 0), stop=(s_ == 127)),
                        reads=[("dg", dj), ("ring", r)], writes=[("accP", pu, hf)])
            for hf in range(2):
                S.op("dve", lambda e, pu=pu, hf=hf: e.tensor_tensor(
                    out=accs[:, hf * 512:(hf + 1) * 512], in0=ps[2 * pu + hf][:, :], in1=bc[:, 0, hf * 512:(hf + 1) * 512],
                    op=ALU.mult), reads=[("accP", pu, hf), ("bc", 0)], writes=[("accs", hf)])
            S.op("dve", lambda e, T=T: e.tensor_tensor(out=accs[:], in0=accs[:], in1=x1[:, T, :], op=ALU.add),
                 reads=[("accs", 0), ("accs", 1)], writes=["accsum"])
            S.op("act", lambda e, T=T: e.activation(out=junk[:], in_=accs[:], func=AF.Square, accum_out=st6[:, T:T + 1]),
                 reads=["accsum"], writes=["junk6", ("ssq6", T)])
            S.op("dve", lambda e, T=T: e.tensor_scalar(out=st6[:, NT + T:NT + T + 1], in0=st6[:, T:T + 1],
                                                       scalar1=1.0 / D, scalar2=EPS, op0=ALU.mult, op1=ALU.add),
                 reads=[("ssq6", T)], writes=[("ms6", T)])
            S.op("act", lambda e, T=T: e.activation(out=st6[:, NT + T:NT + T + 1], in_=st6[:, NT + T:NT + T + 1],
                                                    func=AF.Sqrt), reads=[("ms6", T)], writes=[("ms6", T)])
            S.op("dve", lambda e, T=T: e.reciprocal(out=st6[:, T:T + 1], in_=st6[:, NT + T:NT + T + 1]),
                 reads=[("ms6", T)], writes=[("rs6", T)])
            S.op("dve", lambda e, T=T: e.scalar_tensor_tensor(out=x1[:, T, :], in0=accs[:], scalar=st6[:, T:T + 1],
                                                              in1=bc[:, 3, :], op0=ALU.mult, op1=ALU.mult),
                 reads=["accsum", ("rs6", T), ("bc", 3)], writes=[("xo", T), ("accs", 0), ("accs", 1)])
            S.dma("sp", lambda e, T=T: e.dma_start(out=out[T * 128:(T + 1) * 128, :], in_=x1[:, T, :]),
                  reads=[("xo", T)], writes=[("out", T)])
        phase_end("p5b")
    S.finish()
    es.close()
    return nc


def _col(v):
    return np.ascontiguousarray(np.asarray(v, np.float32).reshape(8, 128).T)


def make_inputs(inp):
    global _BIAS_IDX
    f = lambda a: np.ascontiguousarray(np.asarray(a, dtype=np.float32))
    if _BIAS_IDX is None:
        _BIAS_IDX = build_bias_index()
    rpb = f(inp["na_rpb"])[0]
    ext = np.concatenate([rpb.reshape(8, -1), np.full((8, 1), MASKV, np.float32)], axis=1)
    biasT = np.stack([ext[h][_BIAS_IDX] for h in range(8)], axis=1)
    cstm = np.zeros((128, 528), np.float32)
    cstm[:, 0:128] = np.eye(128, dtype=np.float32)
    sidx = np.arange(128)
    blk = (sidx[:, None] // 64) == (sidx[None, :] // 64)
    cstm[:, 128:256] = (blk & (sidx[:, None] <= sidx[None, :])).astype(np.float32)
    cstm[:, 256:384] = (blk & (sidx[:, None] >= sidx[None, :])).astype(np.float32)
    cstm[:, 384:512] = 1.0
    cstm[:, 512:528] = np.arange(16, dtype=np.float32)[None, :]
    rmask = np.ones((128, TOK), np.float32)
    rmask[:, ::64] = 0.0
    c_ctx = f(inp["c_ctx"])
    hg_lb = f(inp["hg_lb"])
    lbraw = np.ascontiguousarray(hg_lb.reshape(2, 2, 4, 128).transpose(3, 0, 1, 2).reshape(128, 16))
    keys = f(inp["peer_keys"])[0]
    keysT = np.ascontiguousarray(keys.transpose(3, 0, 1, 2).reshape(128, 16 * 128))
    shared = dict(
        w_mod=f(inp["w_mod"])[0], b_mod=f(inp["b_mod"])[0].reshape(1, -1),
        n1c=_col(f(inp["norm1"])[0]), n2c=_col(f(inp["norm2"])[0]),
        n2b=np.ascontiguousarray(np.broadcast_to(f(inp["norm2"])[0][None, :], (128, D))),
        nfb=np.ascontiguousarray(np.broadcast_to(f(inp["norm_f"])[None, :], (128, D))),
        w_in=f(inp["w_in"])[0], w_out=f(inp["w_out"])[0], wq=f(inp["peer_wq"])[0],
        keysT=keysT, u=f(inp["peer_u"])[0], v=f(inp["peer_v"])[0], lbraw=lbraw,
        hgn=np.ascontiguousarray(np.broadcast_to(f(inp["hg_norm"])[0][None, :], (128, 512))),
        biasT=np.ascontiguousarray(biasT.reshape(128, -1)), cst=cstm, rmask=rmask,
    )
    xs = f(inp["x"]); cs = f(inp["c"]); ctxs = f(inp["ctx"])
    maps = []
    for b in range(xs.shape[0]):
        cc = np.stack([_col(cs[b]), _col(c_ctx)], axis=2).reshape(128, 16)
        m = dict(shared)
        m.update(x=xs[b], ctx=ctxs[b], ccol=np.ascontiguousarray(cc))
        maps.append(m)
    return maps


def kernel(**inputs):
    maps = make_inputs(inputs)
    nc = build()
    res = run_bass_kernel_spmd(nc, maps, core_ids=list(range(len(maps))))
    return np.stack([np.asarray(r["out"], dtype=np.float32) for r in res.results], axis=0)
```

```python
import contextlib
import numpy as np
import concourse.bass as bass
import concourse.mybir as mybir
from concourse.bass_utils import run_bass_kernel_spmd

F32 = mybir.dt.float32
BF16 = mybir.dt.bfloat16
I32 = mybir.dt.int32
U32 = mybir.dt.uint32
ALU = mybir.AluOpType
AF = mybir.ActivationFunctionType
AX = mybir.AxisListType

D = 1024
SEQ = 2048
CTX = 256
NT = 16
NTT = 18
TOK = SEQ + CTX
NCH = TOK // 64
EPS = 1e-6
MASKV = -30000.0
NEG = -1.0e30


class Sched:
    COMPUTE = ("pe", "dve", "act", "pool")

    def __init__(self, nc, n_dsem=None):
        self.nc = nc
        self.engs = {"pe": nc.tensor, "dve": nc.vector, "act": nc.scalar,
                     "pool": nc.gpsimd, "sp": nc.sync}
        self.n_dsem = n_dsem or {"sp": 8, "act": 4, "pool": 16}
        self.es = contextlib.ExitStack()
        self.csem = {e: self.es.enter_context(nc.semaphore("cs_" + e)) for e in self.COMPUTE}
        self.dsem = {q: [self.es.enter_context(nc.semaphore(f"ds_{q}{j}")) for j in range(n)]
                     for q, n in self.n_dsem.items()}
        self.ccount = {e: 0 for e in self.COMPUTE}
        self.dcount = {q: 0 for q in self.n_dsem}
        self.clock = {e: {} for e in self.engs}
        self.bar_sig = 0
        self.bar_clock = {}
        self.bar_tile = None
        self.ops = []
        self.last_writer = {}
        self.readers = {}
        self.total_ops = 0

    def op(self, eng, fn, reads=(), writes=(), dma=False):
        deps = set()
        for r in reads:
            w = self.last_writer.get(r)
            if w is not None:
                deps.add(w)
        for w_ in writes:
            w = self.last_writer.get(w_)
            if w is not None:
                deps.add(w)
            for rd in self.readers.get(w_, ()):
                deps.add(rd)
        i = len(self.ops)
        deps.discard(i)
        self.ops.append(dict(eng=eng, fn=fn, deps=deps, dma=dma))
        for r in reads:
            self.readers.setdefault(r, []).append(i)
        for w_ in writes:
            self.last_writer[w_] = i
            self.readers[w_] = []
        return i

    def dma(self, q, fn, reads=(), writes=()):
        return self.op(q, fn, reads, writes, dma=True)

    def _wait(self, E, key, sem, val):
        ck = self.clock[E]
        if ck.get(key, 0) < val:
            self.engs[E].wait_ge(sem, val)
            ck[key] = val

    def _merge(self, E, clk):
        ck = self.clock[E]
        for k, v in clk.items():
            if ck.get(k, 0) < v:
                ck[k] = v

    def flush(self, barrier=True):
        ops = self.ops
        need_sig = [False] * len(ops)
        for i, o in enumerate(ops):
            for d in o["deps"]:
                od = ops[d]
                if od["dma"]:
                    continue
                if od["eng"] == "pe" and o["eng"] == "pe" and not o["dma"]:
                    continue
                need_sig[d] = True
        if barrier:
            last = {}
            for i, o in enumerate(ops):
                if not o["dma"]:
                    last[o["eng"]] = i
            for e, i in last.items():
                need_sig[i] = True
        for i, o in enumerate(ops):
            E = o["eng"]
            eng = self.engs[E]
            if self.bar_sig:
                self._wait(E, ("c", "dve"), self.csem["dve"], self.bar_sig)
                self._merge(E, self.bar_clock)
            for d in sorted(o["deps"]):
                od = ops[d]
                if od["dma"]:
                    q = od["eng"]
                    self._wait(E, ("d", q, od["dsem_idx"]), self.dsem[q][od["dsem_idx"]], od["dval"])
                else:
                    F = od["eng"]
                    if F == "pe" and E == "pe" and not o["dma"]:
                        continue
                    self._wait(E, ("c", F), self.csem[F], od["sig"])
                self._merge(E, od["clk"])
            if o["dma"]:
                n = self.n_dsem[E]
                j = self.dcount[E] % n
                prev = self.dcount[E] // n
                if prev > 0:
                    self._wait(E, ("d", E, j), self.dsem[E][j], 16 * prev)
                ins = o["fn"](eng)
                ins.then_inc(self.dsem[E][j], 16)
                o["dsem_idx"] = j
                o["dval"] = 16 * (prev + 1)
                self.dcount[E] += 1
                o["clk"] = dict(self.clock[E])
            else:
                ins = o["fn"](eng)
                if need_sig[i]:
                    self.ccount[E] += 1
                    ins.then_inc(self.csem[E], 1)
                    o["sig"] = self.ccount[E]
                else:
                    o["sig"] = None
                o["clk"] = dict(self.clock[E])
            o["fn"] = None
        self.total_ops += len(ops)
        if barrier:
            self._barrier()
        self.ops = []
        self.last_writer = {}
        self.readers = {}

    def _wait_all(self, E):
        for q, n in self.n_dsem.items():
            for j in range(n):
                uses = (self.dcount[q] - j + n - 1) // n if self.dcount[q] > j else 0
                if uses > 0:
                    self._wait(E, ("d", q, j), self.dsem[q][j], 16 * uses)
        for e in self.COMPUTE:
            if self.ccount[e] > 0:
                self._wait(E, ("c", e), self.csem[e], self.ccount[e])

    def _barrier(self):
        self._wait_all("dve")
        ins = self.engs["dve"].memset(self.bar_tile, 0.0)
        self.ccount["dve"] += 1
        ins.then_inc(self.csem["dve"], 1)
        self.bar_sig = self.ccount["dve"]
        self.bar_clock = dict(self.clock["dve"])

    def finish(self, eng="sp"):
        self.flush(barrier=True)
        self._wait_all(eng)
        self.es.close()


NA_VARIANTS = [(-2, True), (-1, False), (0, False), (1, False), (2, True),
               (-3, False), (-2, False), (2, False), (3, False)]


def na_chunks(i):
    if i in (0, 1, 14, 15):
        cs = range(0, 4) if i < 2 else range(12, 16)
        out = []
        for c in cs:
            d = c - i
            t = {(-3): 5, (-2): 6, (-1): 1, 0: 2, 1: 3, 2: 7, 3: 8}[d]
            out.append((c, t))
        return out
    return [(i + d, d + 2) for d in range(-2, 3)]


def build_bias_index():
    idx = np.full((128, 9, 128), 15 * 31, dtype=np.int64)
    for t, (d, partial) in enumerate(NA_VARIANTS):
        for j in range(2):
            for jq in range(2):
                dr = 2 * d + j - jq
                if abs(dr) > 7:
                    continue
                if partial:
                    if d == -2 and not (j >= jq):
                        continue
                    if d == 2 and not (j == 0 and jq == 1):
                        continue
                for cq in range(64):
                    cstart = min(max(cq - 8, 0), 48)
                    for ck in range(cstart, cstart + 16):
                        idx[j * 64 + ck, t, jq * 64 + cq] = (dr + 7) * 31 + (ck - cq + 15)
    return idx


_BIAS_IDX = None


import os as _os
_NOCONV = bool(_os.environ.get('NOCONV'))


class _Stop(Exception):
    pass


def build(debug=False, stop=None):
    nc = bass.Bass("TRN2", target_bir_lowering=False)
    try:
        return _build(nc, debug, stop)
    except _Stop:
        return nc


def _build(nc, debug, stop):

    def din(name, shape, dt=F32):
        return nc.dram_tensor(name, shape, dt, kind="ExternalInput").ap()

    x = din("x", [SEQ, D])
    ctx = din("ctx", [CTX, D])
    ccol = din("ccol", [128, 16])
    w_mod = din("w_mod", [D, 6 * D])
    b_mod = din("b_mod", [1, 6 * D])
    n1c = din("n1c", [128, 8])
    n2c = din("n2c", [128, 8])
    n2b = din("n2b", [128, D])
    nfb = din("nfb", [128, D])
    w_in = din("w_in", [D, 4096])
    w_out = din("w_out", [D, D])
    wq = din("wq", [D, 2048])
    keysT = din("keysT", [128, 2048])
    u_t = din("u", [16384, D])
    v_t = din("v", [16384, D])
    lbraw = din("lbraw", [128, 16])
    hgn = din("hgn", [128, 512])
    biasT = din("biasT", [128, 8 * 9 * 128])
    cst = din("cst", [128, 528])
    rmask_d = din("rmask", [128, TOK])
    icst = din("icst", [128, 388], I32)
    out = nc.dram_tensor("out", [SEQ, D], F32, kind="ExternalOutput").ap()
    scr_bc = nc.dram_tensor("scr_bc", [4, 128, D], F32, kind="Internal").ap()
    uv_bf = nc.dram_tensor("uv_bf", [16384, 2 * D], BF16, kind="Internal").ap()
    dbg = {}
    if debug:
        dbg["hT"] = nc.dram_tensor("d_hT", [128, 8 * TOK], BF16, kind="ExternalOutput").ap()
        dbg["mixT"] = nc.dram_tensor("d_mixT", [128, 8 * SEQ], BF16, kind="ExternalOutput").ap()
        dbg["x1"] = nc.dram_tensor("d_x1", [128, NT * D], F32, kind="ExternalOutput").ap()
        dbg["mc"] = nc.dram_tensor("d_mc", [128, 48], F32, kind="ExternalOutput").ap()
        dbg["eidx"] = nc.dram_tensor("d_eidx", [128, NT * 128], I32, kind="ExternalOutput").ap()
        dbg["gw"] = nc.dram_tensor("d_gw", [128, NT * 128], F32, kind="ExternalOutput").ap()

    es = contextlib.ExitStack()

    def sb(name, shape, dt=F32, stack=None):
        return (stack or es).enter_context(nc.sbuf_tensor(name, shape, dt))

    bar = sb("bar", [128, 1])
    cA = sb("cA", [128, 528])
    identb = sb("identb", [128, 128], BF16)
    mc = sb("mc", [128, 48])
    lb = sb("lb", [128, 8])
    oml = sb("oml", [128, 8])
    ps = [es.enter_context(nc.psum_tensor(f"ps{j}", [128, 512], F32)) for j in range(6)]
    pT = [es.enter_context(nc.psum_tensor(f"pT{j}", [128, 1024], BF16)) for j in range(2)]

    S = Sched(nc)
    S.bar_tile = bar[:]

    def phase_end(name):
        S.flush()
        if stop == name:
            S.finish()
            raise _Stop()

    IDENT = cA[:, 0:128]
    TRIF = cA[:, 128:256]
    TRIB = cA[:, 256:384]
    ONES = cA[:, 384:512]
    IOTA16 = cA[:, 512:528]

    with contextlib.ExitStack() as p0:
        cc = sb("cc", [128, 16], stack=p0)
        scl = sb("scl", [128, 8, 33], BF16, stack=p0)
        wm = [sb(f"wm{j}", [128, 8, 512], stack=p0) for j in range(3)]
        wmb = [sb(f"wmb{j}", [128, 8, 512], BF16, stack=p0) for j in range(2)]
        bm = sb("bm", [33, 6 * D], stack=p0)
        modrow = sb("modrow", [33, 6 * D], stack=p0)
        mcol = sb("mcol", [128, 48], stack=p0)
        n1 = sb("n1", [128, 8], stack=p0)
        n2 = sb("n2", [128, 8], stack=p0)
        n2bt = sb("n2bt", [128, D], stack=p0)
        bct = [sb(f"bct{j}", [128, D], stack=p0) for j in range(2)]
        lbr = sb("lbr", [128, 16], stack=p0)

        S.dma("sp", lambda e: e.dma_start(out=cA[:], in_=cst), writes=["cA"])
        S.dma("sp", lambda e: e.dma_start(out=cc[:], in_=ccol), writes=["cc"])
        S.dma("sp", lambda e: e.dma_start(out=n1[:], in_=n1c), writes=["n1"])
        S.dma("sp", lambda e: e.dma_start(out=n2[:], in_=n2c), writes=["n2"])
        S.dma("sp", lambda e: e.dma_start(out=lbr[:], in_=lbraw), writes=["lbr"])
        S.dma("sp", lambda e: e.dma_start(out=n2bt[:], in_=n2b), writes=["n2bt"])
        S.op("dve", lambda e: e.memset(bm[:], 0.0), writes=["bm"])
        S.dma("sp", lambda e: e.dma_start(out=bm[0:1, :], in_=b_mod), reads=["bm"], writes=["bm0"])
        S.dma("sp", lambda e: e.dma_start(out=bm[32:33, :], in_=b_mod), reads=["bm"], writes=["bm32"])
        S.op("dve", lambda e: e.tensor_copy(out=identb[:], in_=IDENT), reads=["cA"], writes=["identb"])
        S.op("dve", lambda e: e.memset(scl[:], 0.0), writes=["scl"])
        ccv = cc[:].rearrange("p (k t) -> p k t", t=2)
        S.op("act", lambda e: e.activation(out=scl[:, :, 0:1], in_=ccv[:, :, 0:1], func=AF.Silu),
             reads=["cc", "scl"], writes=["scl"])
        S.op("act", lambda e: e.activation(out=scl[:, :, 32:33], in_=ccv[:, :, 1:2], func=AF.Silu),
             reads=["cc", "scl"], writes=["scl"])
        S.op("dve", lambda e: e.tensor_tensor(out=lb[:], in0=lbr[:, 0:8], in1=lbr[:, 8:16], op=ALU.subtract),
             reads=["lbr"], writes=["lb"])
        S.op("act", lambda e: e.activation(out=lb[:], in_=lb[:], func=AF.Sigmoid), reads=["lb"], writes=["lb"])
        S.op("dve", lambda e: e.tensor_scalar(out=oml[:], in0=lb[:], scalar1=-1.0, scalar2=1.0,
                                              op0=ALU.mult, op1=ALU.add), reads=["lb"], writes=["oml"])
        for n in range(12):
            wb = wm[n % 3]
            S.dma("sp" if n % 2 == 0 else "act",
                  lambda e, n=n, wb=wb: e.dma_start(
                      out=wb[:], in_=w_mod[:, n * 512:(n + 1) * 512].rearrange("(k p) n -> p k n", p=128)),
                  writes=[("wm", n % 3)])
            wbb = wmb[n % 2]
            if n % 2 == 0:
                S.op("dve", lambda e, wb=wb, wbb=wbb: e.tensor_copy(out=wbb[:], in_=wb[:]),
                     reads=[("wm", n % 3)], writes=[("wmb", n % 2)])
            else:
                S.op("act", lambda e, wb=wb, wbb=wbb: e.activation(out=wbb[:], in_=wb[:], func=AF.Copy),
                     reads=[("wm", n % 3)], writes=[("wmb", n % 2)])
            pb = ps[n % 2]
            for k in range(8):
                S.op("pe", lambda e, k=k, wbb=wbb, pb=pb: e.matmul(pb[0:33, :], scl[:, k, :], wbb[:, k, :],
                                                                  start=(k == 0), stop=(k == 7)),
                     reads=["scl", ("wmb", n % 2)], writes=[("ps", n % 2)])
            S.op("dve", lambda e, n=n, pb=pb: e.tensor_tensor(out=modrow[:, n * 512:(n + 1) * 512], in0=pb[0:33, :],
                                                             in1=bm[:, n * 512:(n + 1) * 512], op=ALU.add),
                 reads=[("ps", n % 2), "bm", "bm0", "bm32"], writes=[("modrow", n)])
        mr_all = [("modrow", n) for n in range(12)]
        col_specs = [(0, 0), (0, 1), (0, 3), (0, 4), (32, 0), (32, 1)]
        for si, (r, vi) in enumerate(col_specs):
            for k in range(8):
                c0 = 2 * (si * 8 + k)
                S.op("pe", lambda e, r=r, vi=vi, k=k, c0=c0: e.matmul(
                    ps[2][:, c0:c0 + 2], modrow[r:r + 1, vi * D + k * 128: vi * D + (k + 1) * 128],
                    cA[r:r + 1, 384:386], start=True, stop=True),
                    reads=mr_all + ["cA"], writes=[("ps", 2)])
        S.op("dve", lambda e: e.tensor_copy(out=mcol[:].unsqueeze(2), in_=ps[2][:, 0:96].rearrange("p (c two) -> p c two", two=2)[:, :, 0:1]), reads=[("ps", 2)], writes=["mcol"])
        S.op("dve", lambda e: e.scalar_tensor_tensor(out=mc[:, 0:8], in0=mcol[:, 8:16], scalar=1.0, in1=n1[:],
                                                     op0=ALU.add, op1=ALU.mult), reads=["mcol", "n1"], writes=["mc0"])
        S.op("dve", lambda e: e.tensor_copy(out=mc[:, 8:16], in_=mcol[:, 0:8]), reads=["mcol"], writes=["mc1"])
        S.op("dve", lambda e: e.scalar_tensor_tensor(out=mc[:, 16:24], in0=mcol[:, 40:48], scalar=1.0, in1=n1[:],
                                                     op0=ALU.add, op1=ALU.mult), reads=["mcol", "n1"], writes=["mc2"])
        S.op("dve", lambda e: e.tensor_copy(out=mc[:, 24:32], in_=mcol[:, 32:40]), reads=["mcol"], writes=["mc3"])
        S.op("dve", lambda e: e.scalar_tensor_tensor(out=mc[:, 32:40], in0=mcol[:, 24:32], scalar=1.0, in1=n2[:],
                                                     op0=ALU.add, op1=ALU.mult), reads=["mcol", "n2"], writes=["mc4"])
        S.op("dve", lambda e: e.tensor_copy(out=mc[:, 40:48], in_=mcol[:, 16:24]), reads=["mcol"], writes=["mc5"])
        for j, (vi, kind) in enumerate([(2, "copy"), (5, "copy"), (4, "g2"), (3, "copy")]):
            bt_ = bct[j % 2]
            for hf in range(2):
                pb = ps[3 + hf]
                S.op("pe", lambda e, vi=vi, hf=hf, pb=pb: e.matmul(
                    pb[:, :], cA[0:1, 384:512], modrow[0:1, vi * D + hf * 512: vi * D + (hf + 1) * 512],
                    start=True, stop=True), reads=mr_all + ["cA"], writes=[("ps", 3 + hf)])
                if kind == "copy":
                    S.op("dve", lambda e, bt_=bt_, hf=hf, pb=pb: e.tensor_copy(out=bt_[:, hf * 512:(hf + 1) * 512], in_=pb[:, :]),
                         reads=[("ps", 3 + hf)], writes=[("bct", j % 2, hf)])
                else:
                    S.op("dve", lambda e, bt_=bt_, hf=hf, pb=pb: e.scalar_tensor_tensor(
                        out=bt_[:, hf * 512:(hf + 1) * 512], in0=pb[:, :], scalar=1.0,
                        in1=n2bt[:, hf * 512:(hf + 1) * 512], op0=ALU.add, op1=ALU.mult),
                        reads=[("ps", 3 + hf), "n2bt"], writes=[("bct", j % 2, hf)])
            S.dma("sp", lambda e, j=j, bt_=bt_: e.dma_start(out=scr_bc[j], in_=bt_[:]),
                  reads=[("bct", j % 2, 0), ("bct", j % 2, 1)], writes=[("scr", j)])
        if debug:
            S.dma("sp", lambda e: e.dma_start(out=dbg["mc"], in_=mc[:]), reads=[f"mc{j}" for j in range(6)])
        phase_end("p0")

    R = sb("R", [128, NT * D])
    x1 = R[:].rearrange("p (t d) -> p t d", d=D)
    hT = R[:, 0:9216].bitcast(BF16).rearrange("p (k t) -> p k t", k=8)
    vtok = R[:, 9216:13824].bitcast(BF16).rearrange("p (t d) -> p t d", d=512)
    with contextlib.ExitStack() as pm:
        mixT = sb("mixT", [128, 8, SEQ], BF16, stack=pm)

        with contextlib.ExitStack() as p1:
            xt = [sb(f"xt{j}", [128, D], stack=p1) for j in range(2)]
            xs = [sb(f"xs{j}", [128, D], BF16, stack=p1) for j in range(2)]
            junk = sb("junk1", [128, D], BF16, stack=p1)
            st = sb("st1", [128, 3 * NTT], stack=p1)
            for T in range(NTT):
                b = T % 2
                src = x[T * 128:(T + 1) * 128, :] if T < NT else ctx[(T - NT) * 128:(T - NT + 1) * 128, :]
                S.dma("sp" if b == 0 else "act", lambda e, b=b, src=src: e.dma_start(out=xt[b][:], in_=src),
                      writes=[("xt", b)])
                S.op("act", lambda e, b=b, T=T: e.activation(out=junk[:], in_=xt[b][:], func=AF.Square,
                                                             accum_out=st[:, T:T + 1]),
                     reads=[("xt", b)], writes=["junk", ("ssq", T)])
                S.op("dve", lambda e, T=T: e.tensor_scalar(out=st[:, NTT + T:NTT + T + 1], in0=st[:, T:T + 1],
                                                           scalar1=1.0 / D, scalar2=EPS, op0=ALU.mult, op1=ALU.add),
                     reads=[("ssq", T)], writes=[("ms", T)])
                S.op("act", lambda e, T=T: e.activation(out=st[:, NTT + T:NTT + T + 1], in_=st[:, NTT + T:NTT + T + 1],
                                                        func=AF.Sqrt), reads=[("ms", T)], writes=[("ms", T)])
                S.op("dve", lambda e, T=T: e.reciprocal(out=st[:, 2 * NTT + T:2 * NTT + T + 1],
                                                        in_=st[:, NTT + T:NTT + T + 1]),
                     reads=[("ms", T)], writes=[("rstd", T)])
                S.op("act", lambda e, b=b, T=T: e.activation(out=xs[b][:], in_=xt[b][:], func=AF.Copy,
                                                             scale=st[:, 2 * NTT + T:2 * NTT + T + 1]),
                     reads=[("xt", b), ("rstd", T)], writes=[("xs", b)])
                for k in range(8):
                    S.op("pe", lambda e, b=b, k=k: e.transpose(pT[b][:, k * 128:(k + 1) * 128],
                                                               xs[b][:, k * 128:(k + 1) * 128], identb[:]),
                         reads=[("xs", b), "identb"], writes=[("pT", b)])
                go, so = (0, 8) if T < NT else (16, 24)
                for k in range(8):
                    S.op("dve", lambda e, b=b, k=k, T=T, go=go, so=so: e.tensor_scalar(
                        out=hT[:, k, T * 128:(T + 1) * 128], in0=pT[b][:, k * 128:(k + 1) * 128],
                        scalar1=mc[:, go + k:go + k + 1], scalar2=mc[:, so + k:so + k + 1],
                        op0=ALU.mult, op1=ALU.add),
                        reads=[("pT", b)], writes=[("hT", T)])
            if debug:
                S.dma("sp", lambda e: e.dma_start(out=dbg["hT"], in_=R[:, 0:9216].bitcast(BF16)),
                      reads=[("hT", T) for T in range(NTT)])
            phase_end("p1")
        hT_all = [("hT", T) for T in range(NTT)]

        def load_w(tile_ap, dram_w, col0, ncols, key):
            for k0 in range(0, 8, 4):
                S.dma("pool", lambda e, k0=k0: e.dma_start(
                    out=tile_ap[:, k0:k0 + 4, :],
                    in_=dram_w[k0 * 128:(k0 + 4) * 128, col0:col0 + ncols].rearrange("(k p) n -> p k n", p=128)),
                    writes=[(key, k0)])
            return [(key, 0), (key, 4)]

        with contextlib.ExitStack() as p2:
            qT = sb("qT", [128, 4, SEQ], BF16, stack=p2)
            kT = sb("kT", [128, 4, TOK], BF16, stack=p2)
            vaug = sb("vaug", [128, NTT, 8, 65], BF16, stack=p2)
            p2a = contextlib.ExitStack()
            wna = sb("wna", [128, 8, 1536], BF16, stack=p2a)
            wk = load_w(wna, w_in, 0, 1536, "wna")
            S.op("pool", lambda e: e.memset(vaug[:, :, :, 64:65], 1.0), writes=["vones"])
            cnt = 0
            for which, dst, ntok, cbase in (("q", qT, SEQ, 0), ("k", kT, TOK, 512)):
                for hp in range(4):
                    for t0 in range(0, ntok, 512):
                        tw = min(512, ntok - t0)
                        pb = cnt % 4
                        for k in range(8):
                            S.op("pe", lambda e, k=k, hp=hp, t0=t0, tw=tw, pb=pb, cbase=cbase: e.matmul(
                                ps[pb][:, 0:tw], wna[:, k, cbase + hp * 128: cbase + (hp + 1) * 128],
                                hT[:, k, t0:t0 + tw], start=(k == 0), stop=(k == 7)),
                                reads=wk + hT_all, writes=[("ps", pb)])
                        eng = "act" if cnt % 2 == 0 else "dve"
                        if eng == "act":
                            S.op("act", lambda e, dst=dst, hp=hp, t0=t0, tw=tw, pb=pb: e.activation(
                                out=dst[:, hp, t0:t0 + tw], in_=ps[pb][:, 0:tw], func=AF.Copy),
                                reads=[("ps", pb)], writes=[(which, hp, t0)])
                        else:
                            S.op("dve", lambda e, dst=dst, hp=hp, t0=t0, tw=tw, pb=pb: e.tensor_copy(
                                out=dst[:, hp, t0:t0 + tw], in_=ps[pb][:, 0:tw]),
                                reads=[("ps", pb)], writes=[(which, hp, t0)])
                        cnt += 1
            for T in range(NTT):
                pb = cnt % 4
                for k in range(8):
                    S.op("pe", lambda e, k=k, T=T, pb=pb: e.matmul(
                        ps[pb][:, :], hT[:, k, T * 128:(T + 1) * 128], wna[:, k, 1024:1536],
                        start=(k == 0), stop=(k == 7)), reads=wk + hT_all, writes=[("ps", pb)])
                if cnt % 2 == 0:
                    S.op("act", lambda e, T=T, pb=pb: e.activation(
                        out=vaug[:, T, :, 0:64], in_=ps[pb][:, :].rearrange("p (h d) -> p h d", d=64), func=AF.Copy),
                        reads=[("ps", pb)], writes=[("v", T)])
                else:
                    S.op("dve", lambda e, T=T, pb=pb: e.tensor_copy(
                        out=vaug[:, T, :, 0:64], in_=ps[pb][:, :].rearrange("p (h d) -> p h d", d=64)),
                        reads=[("ps", pb)], writes=[("v", T)])
                cnt += 1
            phase_end("p2a")
            p2a.close()
            bt = sb("bt", [128, 8, 9, 128], stack=p2)
            Ssb = [sb(f"Ssb{j}", [128, 640], stack=p2) for j in range(2)]
            Pb = [sb(f"Pb{j}", [128, 896], BF16, stack=p2) for j in range(2)]
            rden = sb("rden", [128, 16], stack=p2)
            natok = [sb(f"natok{j}", [128, 512], BF16, stack=p2) for j in range(2)]
            S.dma("sp", lambda e: e.dma_start(out=bt[:].rearrange("p h t q -> p (h t q)"), in_=biasT), writes=["bt"])

            qk_all_r = []
            it = 0
            for i in range(NT):
                chunks = na_chunks(i)
                nw = len(chunks)
                nb = i % 2
                for h in range(8):
                    hp, po = h // 2, (h % 2) * 64
                    sbuf_i = it % 2
                    b0, b1 = ps[2 * sbuf_i], ps[2 * sbuf_i + 1]

                    def sloc(j):
                        return (b0, j * 128) if j < 4 else (b1, (j - 4) * 128)
                    for j, (c, t) in enumerate(chunks):
                        bk, co = sloc(j)
                        S.op("pe", lambda e, bk=bk, co=co, c=c, hp=hp, po=po, i=i: e.matmul(
                            bk[:, co:co + 128], kT[po:po + 64, hp, c * 128:(c + 1) * 128],
                            qT[po:po + 64, hp, i * 128:(i + 1) * 128], start=True, stop=True),
                            reads=[], writes=[("psS", sbuf_i, j // 4)])
                    for cc_ in range(2):
                        S.op("pe", lambda e, cc_=cc_, hp=hp, po=po, i=i, b1=b1: e.matmul(
                            b1[:, 128 + cc_ * 128: 256 + cc_ * 128],
                            kT[po:po + 64, hp, SEQ + cc_ * 128: SEQ + (cc_ + 1) * 128],
                            qT[po:po + 64, hp, i * 128:(i + 1) * 128], start=True, stop=True),
                            reads=[], writes=[("psS", sbuf_i, 1)])
                    for j, (c, t) in enumerate(chunks):
                        bk, co = sloc(j)
                        S.op("dve", lambda e, bk=bk, co=co, j=j, t=t, h=h, sbuf_i=sbuf_i: e.scalar_tensor_tensor(
                            out=Ssb[sbuf_i][:, j * 128:(j + 1) * 128], in0=bk[:, co:co + 128], scalar=0.125,
                            in1=bt[:, h, t, :], op0=ALU.mult, op1=ALU.add),
                            reads=[("psS", sbuf_i, j // 4), "bt"], writes=[("Ssb", sbuf_i)])
                    S.op("act", lambda e, nw=nw, sbuf_i=sbuf_i: e.activation(
                        out=Pb[sbuf_i][:, 0:nw * 128], in_=Ssb[sbuf_i][:, 0:nw * 128], func=AF.Exp),
                        reads=[("Ssb", sbuf_i)], writes=[("Pw", sbuf_i)])
                    S.op("act", lambda e, sbuf_i=sbuf_i, b1=b1: e.activation(
                        out=Pb[sbuf_i][:, 640:896], in_=b1[:, 128:384], func=AF.Exp, scale=0.125),
                        reads=[("psS", sbuf_i, 1)], writes=[("Pc", sbuf_i)])
                    ob = ps[4 + h // 4]
                    oc = (h % 4) * 128
                    nmm = nw + 2
                    for j, (c, t) in enumerate(chunks):
                        S.op("pe", lambda e, j=j, c=c, h=h, ob=ob, oc=oc, sbuf_i=sbuf_i, nmm=nmm: e.matmul(
                            ob[:, oc:oc + 65], Pb[sbuf_i][:, j * 128:(j + 1) * 128], vaug[:, c, h, :],
                            start=(j == 0), stop=False),
                            reads=[("Pw", sbuf_i)], writes=[("psO", h)])
                    for cc_ in range(2):
                        S.op("pe", lambda e, cc_=cc_, h=h, ob=ob, oc=oc, sbuf_i=sbuf_i: e.matmul(
                            ob[:, oc:oc + 65], Pb[sbuf_i][:, 640 + cc_ * 128: 768 + cc_ * 128], vaug[:, NT + cc_, h, :],
                            start=False, stop=(cc_ == 1)),
                            reads=[("Pc", sbuf_i)], writes=[("psO", h)])
                    S.op("dve", lambda e, h=h, ob=ob, oc=oc: e.reciprocal(out=rden[:, h:h + 1], in_=ob[:, oc + 64:oc + 65]),
                         reads=[("psO", h)], writes=[("rden", h)])
                    S.op("dve", lambda e, h=h, ob=ob, oc=oc, nb=nb: e.tensor_scalar(
                        out=natok[nb][:, h * 64:(h + 1) * 64], in0=ob[:, oc:oc + 64], scalar1=rden[:, h:h + 1],
                        scalar2=None, op0=ALU.mult),
                        reads=[("psO", h), ("rden", h)], writes=[("natok", nb)])
                    it += 1
                for j in range(4):
                    S.op("pe", lambda e, j=j, nb=nb: e.transpose(pT[nb][:, j * 128:(j + 1) * 128],
                                                                natok[nb][:, j * 128:(j + 1) * 128], identb[:]),
                         reads=[("natok", nb)], writes=[("pT", nb)])
                S.op("act", lambda e, i=i, nb=nb: e.activation(
                    out=mixT[:, 0:4, i * 128:(i + 1) * 128],
                    in_=pT[nb][:, 0:512].rearrange("p (j t) -> p j t", t=128), func=AF.Copy),
                    reads=[("pT", nb)], writes=[("mixna", i)])
            phase_end("p2")

        with contextlib.ExitStack() as p3:
            oacc = sb("oacc", [128, NT, 512], stack=p3)
            with contextlib.ExitStack() as p3a:
                wv = sb("wv", [128, 8, 512], BF16, stack=p3a)
                wk = load_w(wv, w_in, 3 * 512 + 3 * 512, 512, "wv")
                for T in range(NTT):
                    pb = T % 4
                    for k in range(8):
                        S.op("pe", lambda e, k=k, T=T, pb=pb: e.matmul(
                            ps[pb][:, :], hT[:, k, T * 128:(T + 1) * 128], wv[:, k, :],
                            start=(k == 0), stop=(k == 7)), reads=wk, writes=[("ps", pb)])
                    if T % 2 == 0:
                        S.op("act", lambda e, T=T, pb=pb: e.activation(out=vtok[:, T, :], in_=ps[pb][:, :], func=AF.Copy),
                             reads=[("ps", pb)], writes=[("vtok", T)])
                    else:
                        S.op("dve", lambda e, T=T, pb=pb: e.tensor_copy(out=vtok[:, T, :], in_=ps[pb][:, :]),
                             reads=[("ps", pb)], writes=[("vtok", T)])
                phase_end("p3a")
            with contextlib.ExitStack() as p3b:
                rmask = sb("rmask_sb", [128, TOK], stack=p3b)
                A_ = sb("hgA", [128, TOK], stack=p3b)
                B_ = sb("hgB", [128, TOK], stack=p3b)
                C_ = sb("hgC", [128, TOK], stack=p3b)
                qsb = sb("hgqs", [128, 512], stack=p3b)
                Qt = sb("hgQt", [128, SEQ], BF16, stack=p3b)
                Qs = sb("hgQs", [128, SEQ], BF16, stack=p3b)
                Ks = sb("hgKs", [128, TOK], BF16, stack=p3b)
                Kf = sb("hgKf", [128, TOK], BF16, stack=p3b)
                Ktok = sb("hgKtok", [128, NTT, 128], BF16, stack=p3b)
                wqf = sb("hgwqf", [128, 8, 256], BF16, stack=p3b)
                sc_ = sb("hgsc", [128, 5, NCH], stack=p3b)
                Sst = [sb(f"hgS{j}", [128, 128], stack=p3b) for j in range(2)]
                Usc = [sb(f"hgUsc{j}", [128, 128], stack=p3b) for j in range(4)]
                Smb = [sb(f"hgSmb{j}", [128, 128], BF16, stack=p3b) for j in range(2)]
                Qe = sb("hgQe", [128, SEQ], BF16, stack=p3b)
                Qo = sb("hgQo", [128, SEQ], BF16, stack=p3b)
                ATs = [sb(f"hgATs{j}", [128, 128], BF16, stack=p3b) for j in range(2)]

                S.dma("sp", lambda e: e.dma_start(out=rmask[:], in_=rmask_d), writes=["rmask"])

                def v3(t, n=TOK):
                    return t[:, 0:n].rearrange("p (t s) -> p t s", s=64)

                for dr in range(2):
                    for hh in range(4):
                        dh = dr * 4 + hh
                        S.dma("pool", lambda e, hh=hh: e.dma_start(
                            out=wqf[:, :, 0:128],
                            in_=w_in[:, 1536 + hh * 128:1536 + (hh + 1) * 128].rearrange("(k p) n -> p k n", p=128)),
                            writes=["wq_h"])
                        S.dma("pool", lambda e, hh=hh, dr=dr: e.dma_start(
                            out=wqf[:, :, 128:256],
                            in_=w_in[:, 2048 + dr * 512 + hh * 128:2048 + dr * 512 + (hh + 1) * 128].rearrange(
                                "(k p) n -> p k n", p=128)), writes=["wf_h"])
                        for ci, t0 in enumerate(range(0, TOK, 512)):
                            tw = min(512, TOK - t0)
                            pb = ci % 4
                            for k in range(8):
                                S.op("pe", lambda e, k=k, t0=t0, tw=tw, pb=pb: e.matmul(
                                    ps[pb][:, 0:tw], wqf[:, k, 128:256], hT[:, k, t0:t0 + tw],
                                    start=(k == 0), stop=(k == 7)), reads=["wf_h"], writes=[("ps", pb)])
                            S.op("act", lambda e, t0=t0, tw=tw, pb=pb: e.activation(
                                out=A_[:, t0:t0 + tw], in_=ps[pb][:, 0:tw], func=AF.Sigmoid),
                                reads=[("ps", pb)], writes=["A"])
                        for ci, t0 in enumerate(range(0, SEQ, 512)):
                            pb = ci % 4
                            for k in range(8):
                                S.op("pe", lambda e, k=k, t0=t0, pb=pb: e.matmul(
                                    ps[pb][:, :], wqf[:, k, 0:128], hT[:, k, t0:t0 + 512],
                                    start=(k == 0), stop=(k == 7)), reads=["wq_h"], writes=[("ps", pb)])
                        S.op("dve", lambda e, dh=dh: e.tensor_scalar(out=A_[:], in0=A_[:], scalar1=oml[:, dh:dh + 1],
                                                                     scalar2=lb[:, dh:dh + 1], op0=ALU.mult, op1=ALU.add),
                             reads=["A"], writes=["A"])
                        S.op("act", lambda e: e.activation(out=B_[:], in_=A_[:], func=AF.Ln), reads=["A"], writes=["B"])
                        S.op("dve", lambda e: e.tensor_scalar(out=A_[:], in0=A_[:], scalar1=-1.0, scalar2=1.0,
                                                              op0=ALU.mult, op1=ALU.add), reads=["A", "B"], writes=["A"])
                        S.op("dve", lambda e: e.tensor_tensor_scan(out=C_[:], data0=rmask[:], data1=B_[:], initial=0.0,
                                                                   op0=ALU.mult, op1=ALU.add),
                             reads=["B", "rmask"], writes=["C"])
                        if dr == 0:
                            gbuf, gkey = C_, "C"
                            refpos = 31
                        else:
                            S.op("dve", lambda e: e.tensor_tensor(out=B_[:], in0=B_[:], in1=C_[:], op=ALU.subtract),
                                 reads=["B", "C"], writes=["B"])
                            S.op("dve", lambda e: e.tensor_tensor(
                                out=v3(B_), in0=v3(B_), in1=v3(C_)[:, :, 63:64].to_broadcast([128, NCH, 64]), op=ALU.add),
                                reads=["B", "C"], writes=["B"])
                            gbuf, gkey = B_, "B"
                            refpos = 32
                        endpos = 63 if dr == 0 else 0
                        S.op("dve", lambda e, gbuf=gbuf, refpos=refpos: e.tensor_copy(
                            out=sc_[:, 0, :].unsqueeze(2), in_=v3(gbuf)[:, :, refpos:refpos + 1]), reads=[gkey], writes=["sc0"])
                        S.op("dve", lambda e, gbuf=gbuf, endpos=endpos: e.tensor_copy(
                            out=sc_[:, 1, :].unsqueeze(2), in_=v3(gbuf)[:, :, endpos:endpos + 1]), reads=[gkey], writes=["sc1"])
                        S.op("act", lambda e: e.activation(out=sc_[:, 2:4, :], in_=sc_[:, 0:2, :], func=AF.Exp),
                             reads=["sc0", "sc1"], writes=["sc23"])
                        S.op("dve", lambda e: e.tensor_tensor(out=sc_[:, 4, :], in0=sc_[:, 1, :], in1=sc_[:, 0, :],
                                                              op=ALU.subtract), reads=["sc0", "sc1"], writes=["sc4"])
                        S.op("act", lambda e: e.activation(out=sc_[:, 4, :], in_=sc_[:, 4, :], func=AF.Exp),
                             reads=["sc4"], writes=["sc4"])
                        obuf, okey = (B_, "B") if dr == 0 else (C_, "C")
                        S.op("dve", lambda e, gbuf=gbuf: e.tensor_tensor(
                            out=v3(gbuf), in0=v3(gbuf), in1=sc_[:, 0, :].unsqueeze(2).to_broadcast([128, NCH, 64]),
                            op=ALU.subtract), reads=[gkey, "sc0", "sc1"], writes=[gkey])
                        S.op("act", lambda e, gbuf=gbuf, obuf=obuf: e.activation(out=obuf[:, 0:SEQ], in_=gbuf[:, 0:SEQ], func=AF.Exp),
                             reads=[gkey, okey], writes=[okey])
                        S.op("act", lambda e, gbuf=gbuf: e.activation(out=gbuf[:], in_=gbuf[:], func=AF.Exp, scale=-1.0),
                             reads=[gkey, okey], writes=[gkey])
                        S.op("dve", lambda e, gbuf=gbuf: e.tensor_tensor(out=Kf[:], in0=A_[:], in1=gbuf[:], op=ALU.mult),
                             reads=["A", gkey], writes=["Kf"])
                        hk = 1 if dr == 0 else 0
                        hq = 1 - hk
                        def h32(t):
                            return t[:].rearrange("p (t two s) -> p t two s", two=2, s=32)

                        def h64(t):
                            return t[:].rearrange("p (t two s) -> p t two s", two=2, s=64)
                        if hh == 0:
                            S.op("pool", lambda e: e.memset(Ks[:], 0.0), reads=["Ks"], writes=["Ks"])
                            S.op("pool", lambda e: e.memset(Qs[:], 0.0), reads=["Qs"], writes=["Qs"])
                            if dr == 0:
                                S.op("pool", lambda e: e.memset(Qe[:], 0.0), reads=["Qe"], writes=["Qe"])
                                S.op("pool", lambda e: e.memset(Qo[:], 0.0), reads=["Qo"], writes=["Qo"])
                        S.op("dve", lambda e, hk=hk: e.tensor_copy(out=h32(Ks)[:, :, 1 - hk, :], in_=h32(Kf)[:, :, 1 - hk, :]),
                             reads=["Kf", "Ks"], writes=["Ks"])
                        for ci, t0 in enumerate(range(0, SEQ, 512)):
                            pb = ci % 4
                            S.op("act", lambda e, pb=pb: e.activation(out=qsb[:], in_=ps[pb][:, :], func=AF.Silu),
                                 reads=[("ps", pb)], writes=["qsb"])
                            S.op("dve", lambda e, t0=t0, obuf=obuf: e.tensor_tensor(out=Qt[:, t0:t0 + 512], in0=qsb[:],
                                                                                    in1=obuf[:, t0:t0 + 512], op=ALU.mult),
                                 reads=["qsb", okey], writes=["Qt"])
                        S.op("dve", lambda e, hq=hq: e.tensor_copy(out=h32(Qs)[:, :, 1 - hq, :], in_=h32(Qt)[:, :, 1 - hq, :]),
                             reads=["Qt", "Qs"], writes=["Qs"])
                        S.op("act", lambda e: e.activation(out=h64(Qe)[:, :, 0, :], in_=h64(Qt)[:, :, 0, :], func=AF.Copy),
                             reads=["Qt", "Qe"], writes=["Qe"])
                        S.op("act", lambda e: e.activation(out=h64(Qo)[:, :, 1, :], in_=h64(Qt)[:, :, 1, :], func=AF.Copy),
                             reads=["Qt", "Qo"], writes=["Qo"])
                        for g0 in range(0, NTT, 8):
                            nb = (g0 // 8) % 2
                            gn = min(8, NTT - g0)
                            for j in range(gn):
                                S.op("pe", lambda e, T=g0 + j, j=j, nb=nb: e.transpose(
                                    pT[nb][:, j * 128:(j + 1) * 128], Kf[:, T * 128:(T + 1) * 128], identb[:]),
                                    reads=["Kf"], writes=[("pT", nb)])
                            S.op("act", lambda e, g0=g0, gn=gn, nb=nb: e.activation(
                                out=Ktok[:, g0:g0 + gn, :],
                                in_=pT[nb][:, 0:gn * 128].rearrange("p (j t) -> p j t", t=128), func=AF.Copy),
                                reads=[("pT", nb)], writes=[("Ktok", T) for T in range(g0, g0 + gn)])
                        S.op("pool", lambda e, hk=hk: e.memset(
                            Kf[:].rearrange("p (t two s) -> p t two s", two=2, s=32)[:, :, 1 - hk, :], 0.0),
                            reads=["Kf"], writes=["Kf"])
                        order = [16, 17] + list(range(NT)) if dr == 0 else [17, 16] + list(range(NT - 1, -1, -1))
                        tri = TRIF if dr == 0 else TRIB
                        kcnt = [0]
                        S.op("dve", lambda e: e.memset(Sst[0][:], 0.0), reads=[("S", 0)], writes=[("S", 0)])

                        def chunks_of(T):
                            return [2 * T, 2 * T + 1] if dr == 0 else [2 * T + 1, 2 * T]

                        def stage_a(n):
                            T = order[n]
                            q = n % 2
                            for ci, c in enumerate(chunks_of(T)):
                                par = c % 2
                                S.op("pe", lambda e, T=T, par=par, ci=ci, hh=hh: e.matmul(
                                    ps[2 + ci][:, 0:128], Ktok[par * 64:(par + 1) * 64, T, :],
                                    vtok[par * 64:(par + 1) * 64, T, hh * 128:(hh + 1) * 128], start=True, stop=True),
                                    reads=[("Ktok", T)], writes=[("ps", 2 + ci)])
                                S.op("dve", lambda e, c=c, ci=ci, q=q: e.tensor_scalar(
                                    out=Usc[2 * q + ci][:], in0=ps[2 + ci][:, 0:128], scalar1=sc_[:, 4, c:c + 1], scalar2=None,
                                    op0=ALU.mult), reads=[("ps", 2 + ci), "sc4"], writes=[("Usc", 2 * q + ci)])
                            if T < NT:
                                S.op("pe", lambda e, T=T: e.matmul(ps[4][:, 0:128], Ks[:, T * 128:(T + 1) * 128],
                                                                   Qt[:, T * 128:(T + 1) * 128], start=True, stop=False),
                                     reads=["Ks", "Qt"], writes=[("ps", 4)])
                                S.op("pe", lambda e, T=T: e.matmul(ps[4][:, 0:128], Kf[:, T * 128:(T + 1) * 128],
                                                                   Qs[:, T * 128:(T + 1) * 128], start=False, stop=True),
                                     reads=["Kf", "Qs"], writes=[("ps", 4)])

                        def stage_a2(n):
                            T = order[n]
                            q = n % 2
                            if T < NT:
                                S.op("dve", lambda e, q=q, tri=tri: e.tensor_tensor(out=ATs[q][:], in0=ps[4][:, 0:128], in1=tri, op=ALU.mult),
                                     reads=[("ps", 4), "cA"], writes=[("ATs", q)])
                                ob = ps[5] if q == 0 else ps[1]
                                S.op("pe", lambda e, T=T, q=q, ob=ob, hh=hh: e.matmul(ob[:, 0:128], ATs[q][:], vtok[:, T, hh * 128:(hh + 1) * 128],
                                                                               start=True, stop=False),
                                     reads=[("ATs", q)], writes=[("ps", 5 if q == 0 else 1)])

                        def stage_b(n):
                            T = order[n]
                            q = n % 2
                            lat = T < NT
                            ob = ps[5] if q == 0 else ps[1]
                            for ci, c in enumerate(chunks_of(T)):
                                par = c % 2
                                k = kcnt[0]
                                kcnt[0] += 1
                                si, so = k % 2, (k + 1) % 2
                                if lat:
                                    S.op("act", lambda e, c=c, ci=ci, si=si: e.activation(
                                        out=Smb[ci][:], in_=Sst[si][:], func=AF.Copy, scale=sc_[:, 2, c:c + 1]),
                                        reads=[("S", si), "sc23"], writes=[("Smb", ci)])
                                    Qz, qzk = (Qe, "Qe") if par == 0 else (Qo, "Qo")
                                    S.op("pe", lambda e, T=T, ci=ci, Qz=Qz, ob=ob: e.matmul(
                                        ob[:, 0:128], Qz[:, T * 128:(T + 1) * 128], Smb[ci][:], start=False, stop=(ci == 1)),
                                        reads=[("Smb", ci), qzk], writes=[("ps", 5 if q == 0 else 1)])
                                S.op("dve", lambda e, c=c, ci=ci, q=q, si=si, so=so: e.scalar_tensor_tensor(
                                    out=Sst[so][:], in0=Sst[si][:], scalar=sc_[:, 3, c:c + 1], in1=Usc[2 * q + ci][:],
                                    op0=ALU.mult, op1=ALU.add), reads=[("Usc", 2 * q + ci), ("S", si), "sc23"], writes=[("S", so)])
                            if lat:
                                if dr == 0:
                                    S.op("act", lambda e, T=T, ob=ob, hh=hh: e.activation(
                                        out=oacc[:, T, hh * 128:(hh + 1) * 128], in_=ob[:, 0:128], func=AF.Copy),
                                        reads=[("ps", 5 if q == 0 else 1)], writes=[("oacc", T, hh)])
                                else:
                                    S.op("dve", lambda e, T=T, ob=ob, hh=hh: e.tensor_tensor(
                                        out=oacc[:, T, hh * 128:(hh + 1) * 128], in0=ob[:, 0:128],
                                        in1=oacc[:, T, hh * 128:(hh + 1) * 128], op=ALU.add),
                                        reads=[("ps", 5 if q == 0 else 1), ("oacc", T, hh)], writes=[("oacc", T, hh)])

                        stage_a(0)
                        stage_a2(0)
                        for n in range(len(order)):
                            if n + 1 < len(order):
                                stage_a(n + 1)
                            stage_b(n)
                            if n + 1 < len(order):
                                stage_a2(n + 1)
                        if kcnt[0] % 2 == 1:
                            pass
                phase_end("p3b")
            with contextlib.ExitStack() as p3c:
                wg = sb("wg", [128, 8, 512], BF16, stack=p3c)
                hgnb = sb("hgnb", [128, 512], stack=p3c)
                sg = [sb(f"sg{j}", [128, 512], stack=p3c) for j in range(2)]
                yb = [sb(f"yb{j}", [128, 512], stack=p3c) for j in range(2)]
                yt = [sb(f"yt{j}", [128, 512], BF16, stack=p3c) for j in range(2)]
                jk = sb("jk3", [128, 128], BF16, stack=p3c)
                st3 = sb("st3", [128, NT, 8], stack=p3c)
                wk = load_w(wg, w_in, 1536 + 4 * 512, 512, "wg")
                S.dma("sp", lambda e: e.dma_start(out=hgnb[:], in_=hgn), writes=["hgnb"])
                for T in range(NT):
                    for hh in range(4):
                        S.op("act", lambda e, T=T, hh=hh: e.activation(
                            out=jk[:], in_=oacc[:, T, hh * 128:(hh + 1) * 128], func=AF.Square,
                            accum_out=st3[:, T, hh:hh + 1]), reads=[], writes=["jk3", ("ss3", T)])
                ss_all = [("ss3", T) for T in range(NT)]
                S.op("dve", lambda e: e.tensor_scalar(out=st3[:, :, 4:8], in0=st3[:, :, 0:4], scalar1=1.0 / 128,
                                                      scalar2=EPS, op0=ALU.mult, op1=ALU.add),
                     reads=ss_all, writes=["ms3"])
                S.op("act", lambda e: e.activation(out=st3[:, :, 4:8], in_=st3[:, :, 4:8], func=AF.Sqrt),
                     reads=["ms3"], writes=["ms3"])
                S.op("dve", lambda e: e.reciprocal(out=st3[:, :, 0:4], in_=st3[:, :, 4:8]),
                     reads=["ms3"] + ss_all, writes=["rs3"])
                for T in range(NT):
                    b = T % 2
                    for k in range(8):
                        S.op("pe", lambda e, k=k, T=T, b=b: e.matmul(
                            ps[b][:, :], hT[:, k, T * 128:(T + 1) * 128], wg[:, k, :],
                            start=(k == 0), stop=(k == 7)), reads=wk, writes=[("ps", b)])
                    S.op("act", lambda e, b=b: e.activation(out=sg[b][:], in_=ps[b][:, :], func=AF.Silu),
                         reads=[("ps", b)], writes=[("sg", b)])
                    S.op("dve", lambda e, T=T, b=b: e.tensor_tensor(
                        out=yb[b][:].rearrange("p (h d) -> p h d", d=128),
                        in0=oacc[:, T, :].rearrange("p (h d) -> p h d", d=128),
                        in1=st3[:, T, 0:4].unsqueeze(2).to_broadcast([128, 4, 128]), op=ALU.mult),
                        reads=["rs3"], writes=[("yb", b)])
                    S.op("dve", lambda e, b=b: e.tensor_tensor(out=yb[b][:], in0=yb[b][:], in1=hgnb[:], op=ALU.mult),
                         reads=[("yb", b), "hgnb"], writes=[("yb", b)])
                    S.op("dve", lambda e, b=b: e.tensor_tensor(out=yt[b][:], in0=yb[b][:], in1=sg[b][:], op=ALU.mult),
                         reads=[("yb", b), ("sg", b)], writes=[("yt", b)])
                    for j in range(4):
                        S.op("pe", lambda e, j=j, b=b: e.transpose(pT[b][:, j * 128:(j + 1) * 128],
                                                                   yt[b][:, j * 128:(j + 1) * 128], identb[:]),
                             reads=[("yt", b)], writes=[("pT", b)])
                    S.op("act", lambda e, T=T, b=b: e.activation(
                        out=mixT[:, 4:8, T * 128:(T + 1) * 128],
                        in_=pT[b][:, 0:512].rearrange("p (j t) -> p j t", t=128), func=AF.Copy),
                        reads=[("pT", b)], writes=[("mixhg", T)])
                if debug:
                    S.dma("sp", lambda e: e.dma_start(out=dbg["mixT"], in_=mixT[:].rearrange("p k t -> p (k t)")),
                          reads=[("mixhg", T) for T in range(NT)])
                phase_end("p3")

        with contextlib.ExitStack() as p4:
            wo32 = sb("wo32", [128, 8, D], stack=p4)
            wob = sb("wob", [128, 8, D], BF16, stack=p4)
            g1b = sb("g1b", [128, D], stack=p4)
            S.dma("sp", lambda e: e.dma_start(out=g1b[:], in_=scr_bc[0]), writes=["g1b"])
            for k in range(8):
                S.dma("sp" if k % 2 == 0 else "act", lambda e, k=k: e.dma_start(
                    out=wo32[:, k, :], in_=w_out[k * 128:(k + 1) * 128, :]), writes=[("wo32", k)])
                S.op("dve" if k % 2 == 0 else "pool", lambda e, k=k: e.tensor_tensor(
                    out=wob[:, k, :], in0=wo32[:, k, :], in1=g1b[:], op=ALU.mult),
                    reads=[("wo32", k), "g1b"], writes=[("wob", k)])
            wob_all = [("wob", k) for k in range(8)]
            for T in range(NT):
                S.dma("sp" if T % 2 == 0 else "act", lambda e, T=T: e.dma_start(
                    out=x1[:, T, :], in_=x[T * 128:(T + 1) * 128, :]), writes=[("x1", T)])
                for hf in range(2):
                    pb = (2 * T + hf) % 4
                    for k in range(8):
                        S.op("pe", lambda e, k=k, T=T, hf=hf, pb=pb: e.matmul(
                            ps[pb][:, :], mixT[:, k, T * 128:(T + 1) * 128], wob[:, k, hf * 512:(hf + 1) * 512],
                            start=(k == 0), stop=(k == 7)), reads=wob_all, writes=[("ps", pb)])
                    S.op("dve", lambda e, T=T, hf=hf, pb=pb: e.tensor_tensor(
                        out=x1[:, T, hf * 512:(hf + 1) * 512], in0=ps[pb][:, :], in1=x1[:, T, hf * 512:(hf + 1) * 512],
                        op=ALU.add), reads=[("ps", pb), ("x1", T)], writes=[("x1", T)])
            if debug:
                S.dma("sp", lambda e: e.dma_start(out=dbg["x1"], in_=R[:]),
                      reads=[("x1", T) for T in range(NT)])
            phase_end("p4")

    eidx = sb("eidx", [128, NT, 128], I32)
    gw = sb("gw", [128, NT, 128])
    rs2 = sb("rs2", [128, NT])
    with contextlib.ExitStack() as p5:
        wqb = sb("wqb", [128, 8, 2048], BF16, stack=p5)
        kTb = sb("kTb", [128, 16, 128], BF16, stack=p5)
        junk = sb("junk5", [128, D], BF16, stack=p5)
        xs = [sb(f"xs5{j}", [128, D], BF16, stack=p5) for j in range(2)]
        h2T = [sb(f"h2T{j}", [128, 8, 128], BF16, stack=p5) for j in range(2)]
        qTp = [sb(f"qTp{j}", [128, 16, 128], BF16, stack=p5) for j in range(2)]
        ssb = sb("ssb", [128, 16, 128], stack=p5)
        s2 = sb("s2", [128, 16, 128], stack=p5)
        top = sb("top", [128, 16, 16], stack=p5)
        itop = sb("itop", [128, 16, 16], I32, stack=p5)
        ic = sb("ic", [128, 388], I32, stack=p5)
        itf = sb("itf", [128, 16, 16], stack=p5)
        cand = sb("cand", [128, 8, 256], stack=p5)
        cand2 = sb("cand2", [128, 8, 256], stack=p5)
        ctop = sb("ctop", [128, 8, 16], stack=p5)
        cpos = sb("cpos", [128, 8, 16], I32, stack=p5)
        paf = sb("paf", [128, 128], stack=p5)
        pai = sb("pai", [128, 128], I32, stack=p5)
        pbf = sb("pbf", [128, 128], stack=p5)
        oh = sb("oh", [128, 128, 16], stack=p5)
        selA = sb("selA", [128, 128], stack=p5)
        selB = sb("selB", [128, 128], stack=p5)
        ef = sb("ef", [128, 128], stack=p5)
        ee = sb("ee", [128, 8, 16], stack=p5)
        zz = sb("zz", [128, 16], stack=p5)
        st5 = sb("st5", [128, 2 * NT], stack=p5)

        stg = [sb(f"stg{j}", [128, 4, D], BF16, stack=p5) for j in range(2)]
        conv_steps = [(tab, c) for tab in range(2) for c in range(32)]

        def emit_conv(si):
            tab, c = conv_steps[si]
            src = (u_t, v_t)[tab].rearrange("(p j c) d -> c p j d", p=128, j=4, c=32)[c]
            dst = uv_bf.rearrange("(p j c) d -> c p j d", p=128, j=4, c=32)[c][:, :, tab * D:(tab + 1) * D]
            b = si % 2
            S.dma("pool", lambda e: e.dma_start(out=stg[b][:], in_=src), writes=[("stg", b)])
            S.dma("sp", lambda e: e.dma_start(out=dst, in_=stg[b][:]), reads=[("stg", b)], writes=[("tab", tab, c)])

        wkq = load_w(wqb[:, :, 0:1024], wq, 0, 1024, "wqa") + load_w(wqb[:, :, 1024:2048], wq, 1024, 1024, "wqb")
        S.dma("pool", lambda e: e.dma_start(out=kTb[:].rearrange("p c k -> p (c k)"), in_=keysT), writes=["kTb"])
        S.dma("sp", lambda e: e.dma_start(out=ic[:], in_=icst), writes=["ic"])
        for T in range(NT):
            S.op("act", lambda e, T=T: e.activation(out=junk[:], in_=x1[:, T, :], func=AF.Square,
                                                    accum_out=st5[:, T:T + 1]), reads=[], writes=["junk5", ("ssq5", T)])
        S.op("dve", lambda e: e.tensor_scalar(out=st5[:, NT:2 * NT], in0=st5[:, 0:NT],
                                              scalar1=1.0 / D, scalar2=EPS, op0=ALU.mult, op1=ALU.add),
             reads=[("ssq5", T) for T in range(NT)], writes=["ms5"])
        S.op("act", lambda e: e.activation(out=st5[:, NT:2 * NT], in_=st5[:, NT:2 * NT], func=AF.Sqrt),
             reads=["ms5"], writes=["ms5"])
        S.op("dve", lambda e: e.reciprocal(out=rs2[:, :], in_=st5[:, NT:2 * NT]), reads=["ms5"], writes=["rs2"])
        for T in range(NT):
            b = T % 2
            if not _NOCONV:
                for q_ in range(4):
                    emit_conv(4 * T + q_)
            S.op("act", lambda e, b=b, T=T: e.activation(out=xs[b][:], in_=x1[:, T, :], func=AF.Copy,
                                                         scale=rs2[:, T:T + 1]), reads=["rs2"], writes=[("xs5", b)])
            for k in range(8):
                S.op("pe", lambda e, b=b, k=k: e.transpose(pT[b][:, k * 128:(k + 1) * 128],
                                                           xs[b][:, k * 128:(k + 1) * 128], identb[:]),
                     reads=[("xs5", b)], writes=[("pT", b)])
            for k in range(8):
                S.op("dve" if k % 2 == 0 else "act", (lambda e, b=b, k=k: e.tensor_scalar(
                    out=h2T[b][:, k, :], in0=pT[b][:, k * 128:(k + 1) * 128],
                    scalar1=mc[:, 32 + k:33 + k], scalar2=mc[:, 40 + k:41 + k], op0=ALU.mult, op1=ALU.add))
                    if k % 2 == 0 else (lambda e, b=b, k=k: e.activation(
                        out=h2T[b][:, k, :], in_=pT[b][:, k * 128:(k + 1) * 128], func=AF.Identity,
                        scale=mc[:, 32 + k:33 + k], bias=mc[:, 40 + k:41 + k])),
                    reads=[("pT", b)], writes=[("h2T", b)])
            for g4 in range(4):
                pb = g4
                for j in range(4):
                    pc = g4 * 4 + j
                    for k in range(8):
                        S.op("pe", lambda e, b=b, k=k, pc=pc, j=j, pb=pb: e.matmul(
                            ps[pb][:, j * 128:(j + 1) * 128], wqb[:, k, pc * 128:(pc + 1) * 128], h2T[b][:, k, :],
                            start=(k == 0), stop=(k == 7)), reads=wkq + [("h2T", b)], writes=[("ps", pb)])
                if g4 % 2 == 0:
                    S.op("act", lambda e, b=b, g4=g4, pb=pb: e.activation(
                        out=qTp[b][:, g4 * 4:(g4 + 1) * 4, :], in_=ps[pb][:, :].rearrange("p (j t) -> p j t", t=128),
                        func=AF.Copy), reads=[("ps", pb)], writes=[("qTp", b, g4)])
                else:
                    S.op("dve", lambda e, b=b, g4=g4, pb=pb: e.tensor_copy(
                        out=qTp[b][:, g4 * 4:(g4 + 1) * 4, :], in_=ps[pb][:, :].rearrange("p (j t) -> p j t", t=128)),
                        reads=[("ps", pb)], writes=[("qTp", b, g4)])
            for g4 in range(4):
                pb = 4 + (g4 % 2)
                for j in range(4):
                    pc = g4 * 4 + j
                    S.op("pe", lambda e, b=b, pc=pc, j=j, pb=pb: e.matmul(
                        ps[pb][:, j * 128:(j + 1) * 128], qTp[b][:, pc, :], kTb[:, pc, :], start=True, stop=True),
                        reads=[("qTp", b, g4), "kTb"], writes=[("ps", pb)])
                S.op("act", lambda e, g4=g4, pb=pb: e.activation(
                    out=ssb[:, g4 * 4:(g4 + 1) * 4, :], in_=ps[pb][:, :].rearrange("p (j t) -> p j t", t=128),
                    func=AF.Copy), reads=[("ps", pb)], writes=[("ssb", g4)])
            ssb_keys = [("ssb", g) for g in range(4)]
            ssbi = ssb[:].bitcast(I32)
            S.op("dve", lambda e: e.tensor_tensor(out=ssbi, in0=ssbi, in1=ic[:, 384:385].unsqueeze(2).to_broadcast([128, 16, 128]),
                                                  op=ALU.bitwise_and), reads=ssb_keys + ["ic"], writes=ssb_keys)
            S.op("dve", lambda e: e.tensor_tensor(out=ssbi, in0=ssbi, in1=ic[:, 0:128].unsqueeze(1).to_broadcast([128, 16, 128]),
                                                  op=ALU.bitwise_or), reads=ssb_keys + ["ic"], writes=ssb_keys)
            for pc in range(16):
                S.op("dve", lambda e, pc=pc: e.max(out=top[:, pc, 0:8], in_=ssb[:, pc, :]),
                     reads=[("ssb", pc // 4)], writes=[("top", pc, 0)])
            for pc in range(16):
                S.op("dve", lambda e, pc=pc: e.match_replace(out=s2[:, pc, :], in_to_replace=top[:, pc, 0:8],
                                                             in_values=ssb[:, pc, :], imm_value=NEG),
                     reads=[("ssb", pc // 4), ("top", pc, 0)], writes=[("s2", pc)])
            for pc in range(16):
                S.op("dve", lambda e, pc=pc: e.max(out=top[:, pc, 8:16], in_=s2[:, pc, :]),
                     reads=[("s2", pc)], writes=[("top", pc, 1)])
            tops = [("top", pc, j) for pc in range(16) for j in range(2)]
            S.op("dve", lambda e: e.tensor_tensor(out=itop[:], in0=top[:].bitcast(I32),
                                                  in1=ic[:, 386:387].unsqueeze(2).to_broadcast([128, 16, 16]),
                                                  op=ALU.bitwise_and), reads=tops + ["ic"], writes=["itop"])
            S.op("dve", lambda e: e.tensor_copy(out=itf[:], in_=itop[:]), reads=["itop"], writes=["itf"])
            topv = top[:].rearrange("p (h c) a -> p h c a", c=2)
            S.op("dve", lambda e: e.tensor_tensor(
                out=cand[:].rearrange("p h (a b) -> p h a b", b=16),
                in0=topv[:, :, 0, :].unsqueeze(3).to_broadcast([128, 8, 16, 16]),
                in1=topv[:, :, 1, :].unsqueeze(2).to_broadcast([128, 8, 16, 16]), op=ALU.add),
                reads=tops, writes=["cand"])
            candi = cand[:].bitcast(I32)
            S.op("dve", lambda e: e.tensor_tensor(out=candi, in0=candi, in1=ic[:, 385:386].unsqueeze(2).to_broadcast([128, 8, 256]),
                                                  op=ALU.bitwise_and), reads=["cand", "ic"], writes=["cand"])
            S.op("dve", lambda e: e.tensor_tensor(out=candi, in0=candi, in1=ic[:, 128:384].unsqueeze(1).to_broadcast([128, 8, 256]),
                                                  op=ALU.bitwise_or), reads=["cand", "ic"], writes=["cand"])
            for p in range(8):
                S.op("dve", lambda e, p=p: e.max(out=ctop[:, p, 0:8], in_=cand[:, p, :]), reads=["cand"], writes=[("ctop", p, 0)])
            for p in range(8):
                S.op("dve", lambda e, p=p: e.match_replace(out=cand2[:, p, :], in_to_replace=ctop[:, p, 0:8],
                                                           in_values=cand[:, p, :], imm_value=NEG),
                     reads=["cand", ("ctop", p, 0)], writes=[("cand2", p)])
            for p in range(8):
                S.op("dve", lambda e, p=p: e.max(out=ctop[:, p, 8:16], in_=cand2[:, p, :]), reads=[("cand2", p)], writes=[("ctop", p, 1)])
            ctops = [("ctop", p, j) for p in range(8) for j in range(2)]
            S.op("dve", lambda e: e.tensor_tensor(out=cpos[:], in0=ctop[:].bitcast(I32),
                                                  in1=ic[:, 387:388].unsqueeze(2).to_broadcast([128, 8, 16]),
                                                  op=ALU.bitwise_and), reads=ctops + ["ic"], writes=["cpos"])
            cposs = ["cpos"]
            cposf = cpos[:].rearrange("p h j -> p (h j)")
            S.op("dve", lambda e: e.tensor_copy(out=selA[:], in_=cposf), reads=cposs + [("sel", 0)], writes=["posf"])
            S.op("dve", lambda e: e.tensor_scalar(out=pbf[:], in0=selA[:], scalar1=0.0625, scalar2=None, op0=ALU.mult),
                 reads=["posf"], writes=["pbf"])
            S.op("dve", lambda e: e.tensor_copy(out=pai[:], in_=pbf[:]), reads=["pbf"], writes=["pai"])
            S.op("dve", lambda e: e.tensor_copy(out=paf[:], in_=pai[:]), reads=["pai"], writes=["paf"])
            S.op("dve", lambda e: e.scalar_tensor_tensor(out=pbf[:], in0=paf[:], scalar=16.0, in1=selA[:],
                                                         op0=ALU.mult, op1=ALU.is_gt), reads=["paf", "posf", "pai"], writes=["pbf"])
            S.op("dve", lambda e: e.tensor_tensor(out=paf[:], in0=paf[:], in1=pbf[:], op=ALU.subtract),
                 reads=["paf", "pbf"], writes=["paf"])
            S.op("dve", lambda e: e.scalar_tensor_tensor(out=pbf[:], in0=paf[:], scalar=-16.0, in1=selA[:],
                                                         op0=ALU.mult, op1=ALU.add), reads=["paf", "posf"], writes=["pbf"])
            itv = itf[:].rearrange("p (h c) a -> p h c a", c=2)
            for which, pf, sel in ((0, paf, selA), (1, pbf, selB)):
                S.op("dve", lambda e, pf=pf: e.tensor_tensor(
                    out=oh[:], in0=pf[:].unsqueeze(2).to_broadcast([128, 128, 16]),
                    in1=IOTA16.unsqueeze(1).to_broadcast([128, 128, 16]), op=ALU.is_equal),
                    reads=["paf", "pbf", "cA", "oh"], writes=["oh"])
                S.op("dve", lambda e, which=which: e.tensor_tensor(
                    out=oh[:].rearrange("p (h j) a -> p h j a", j=16),
                    in0=oh[:].rearrange("p (h j) a -> p h j a", j=16),
                    in1=itv[:, :, which, :].unsqueeze(2).to_broadcast([128, 8, 16, 16]), op=ALU.mult),
                    reads=["oh", "itf"], writes=["oh"])
                S.op("dve", lambda e, sel=sel: e.tensor_reduce(out=sel[:], in_=oh[:], axis=AX.X, op=ALU.add),
                     reads=["oh", "posf"], writes=[("sel", which)])
            S.op("dve", lambda e: e.scalar_tensor_tensor(out=ef[:], in0=selA[:], scalar=128.0, in1=selB[:],
                                                         op0=ALU.mult, op1=ALU.add),
                 reads=[("sel", 0), ("sel", 1)], writes=["ef"])
            S.op("dve", lambda e, T=T: e.tensor_copy(out=eidx[:, T, :], in_=ef[:]), reads=["ef"], writes=[("eidx", T)])
            S.op("dve", lambda e: e.tensor_tensor(out=ee[:], in0=ctop[:], in1=ctop[:, :, 0:1].to_broadcast([128, 8, 16]),
                                                  op=ALU.subtract), reads=ctops, writes=["ee"])
            S.op("act", lambda e: e.activation(out=ee[:], in_=ee[:], func=AF.Exp), reads=["ee"], writes=["ee"])
            S.op("dve", lambda e: e.tensor_reduce(out=zz[:, 0:8], in_=ee[:], axis=AX.X, op=ALU.add),
                 reads=["ee"], writes=["zz"])
            S.op("dve", lambda e: e.reciprocal(out=zz[:, 8:16], in_=zz[:, 0:8]), reads=["zz"], writes=["zz2"])
            S.op("dve", lambda e, T=T: e.tensor_tensor(
                out=gw[:, T, :].rearrange("p (h j) -> p h j", j=16), in0=ee[:],
                in1=zz[:, 8:16].unsqueeze(2).to_broadcast([128, 8, 16]), op=ALU.mult),
                reads=["ee", "zz2"], writes=[("gw", T)])
        if debug:
            S.dma("sp", lambda e: e.dma_start(out=dbg["eidx"], in_=eidx[:].rearrange("p t s -> p (t s)")),
                  reads=[("eidx", T) for T in range(NT)])
            S.dma("sp", lambda e: e.dma_start(out=dbg["gw"], in_=gw[:].rearrange("p t s -> p (t s)")),
                  reads=[("gw", T) for T in range(NT)])
        phase_end("p5a")

    with contextlib.ExitStack() as p6:
        NB = 12
        ring = [sb(f"ring{j}", [128, 2 * D], BF16, stack=p6) for j in range(NB)]
        NDG = 6
        dg = [sb(f"dg{j}", [128, 128], BF16, stack=p6) for j in range(NDG)]
        bc = sb("bc5", [128, 4, D], stack=p6)
        h2 = [sb(f"h2_{j}", [128, D], stack=p6) for j in range(2)]
        junk = sb("junk6", [128, D], BF16, stack=p6)
        accs = sb("accs", [128, D], stack=p6)
        aa = [sb(f"aa{j}", [128, 128], stack=p6) for j in range(2)]
        gl = [sb(f"gl{j}", [128, 128], stack=p6) for j in range(2)]
        ww = [sb(f"ww{j}", [128, 128], stack=p6) for j in range(2)]
        st6 = sb("st6", [128, 2 * NT], stack=p6)
        S.dma("sp", lambda e: e.dma_start(out=bc[:, 0, :], in_=scr_bc[1]), writes=[("bc", 0)])
        S.dma("sp", lambda e: e.dma_start(out=bc[:, 1, :], in_=scr_bc[2]), writes=[("bc", 1)])
        S.dma("sp", lambda e: e.dma_start(out=bc[:, 2, :], in_=scr_bc[3]), writes=[("bc", 2)])
        S.dma("sp", lambda e: e.dma_start(out=bc[:, 3, :], in_=nfb), writes=[("bc", 3)])
        gi = gd = 0
        for T in range(NT):
            pu = T % 2
            S.op("dve", lambda e, T=T, pu=pu: e.scalar_tensor_tensor(out=h2[pu][:], in0=x1[:, T, :], scalar=rs2[:, T:T + 1],
                                                                     in1=bc[:, 1, :], op0=ALU.mult, op1=ALU.mult),
                 reads=[("bc", 1)], writes=[("h2", pu)])
            S.op("dve", lambda e, pu=pu: e.tensor_tensor(out=h2[pu][:], in0=h2[pu][:], in1=bc[:, 2, :], op=ALU.add),
                 reads=[("h2", pu), ("bc", 2)], writes=[("h2", pu)])
            S.op("dve", lambda e, pu=pu: e.memset(aa[pu][:], 0.0), writes=[("aa", pu)])
            for s_ in range(128):
                r = gi % NB
                gi += 1
                S.dma("pool", lambda e, T=T, s_=s_, r=r: e.indirect_dma_start(
                    out=ring[r][:], out_offset=None, in_=uv_bf,
                    in_offset=bass.IndirectOffsetOnAxis(ap=eidx[:, T, s_:s_ + 1], axis=0)),
                    reads=[], writes=[("ring", r)])
                S.op("dve", lambda e, s_=s_, r=r, pu=pu: e.scalar_tensor_tensor(
                    out=junk[:], in0=ring[r][:, 0:D], scalar=1.0, in1=h2[pu][:], op0=ALU.mult, op1=ALU.mult,
                    accum_out=aa[pu][:, s_:s_ + 1]), reads=[("ring", r), ("h2", pu), ("aa", pu)],
                    writes=["junk6", ("aas", pu, s_)])
                S.op("act", lambda e, s_=s_, pu=pu: e.activation(out=gl[pu][:, s_:s_ + 1], in_=aa[pu][:, s_:s_ + 1], func=AF.Gelu),
                     reads=[("aas", pu, s_)], writes=[("gl", pu, s_)])
                S.op("act", lambda e, T=T, s_=s_, pu=pu: e.activation(out=ww[pu][:, s_:s_ + 1], in_=gl[pu][:, s_:s_ + 1],
                                                                     func=AF.Copy, scale=gw[:, T, s_:s_ + 1]),
                     reads=[("gl", pu, s_)], writes=[("ww", pu, s_)])
                dj = gd % NDG
                gd += 1
                S.op("act", lambda e, s_=s_, dj=dj, pu=pu: e.activation(
                    out=dg[dj][:], in_=identb[:], func=AF.Copy, scale=ww[pu][:, s_:s_ + 1]),
                    reads=[("ww", pu, s_)], writes=[("dg", dj)])
                for hf in range(2):
                    S.op("pe", lambda e, s_=s_, dj=dj, r=r, pu=pu, hf=hf: e.matmul(
                        ps[2 * pu + hf][:, :], dg[dj][:], ring[r][:, D + hf * 512: D + (hf + 1) * 512],
                        start=(s_ == 0), stop=(s_ == 127)),
                        reads=[("dg", dj), ("ring", r)], writes=[("accP", pu, hf)])
            for hf in range(2):
                S.op("dve", lambda e, pu=pu, hf=hf: e.tensor_tensor(
                    out=accs[:, hf * 512:(hf + 1) * 512], in0=ps[2 * pu + hf][:, :], in1=bc[:, 0, hf * 512:(hf + 1) * 512],
                    op=ALU.mult), reads=[("accP", pu, hf), ("bc", 0)], writes=[("accs", hf)])
            S.op("dve", lambda e, T=T: e.tensor_tensor(out=accs[:], in0=accs[:], in1=x1[:, T, :], op=ALU.add),
                 reads=[("accs", 0), ("accs", 1)], writes=["accsum"])
            S.op("act", lambda e, T=T: e.activation(out=junk[:], in_=accs[:], func=AF.Square, accum_out=st6[:, T:T + 1]),
                 reads=["accsum"], writes=["junk6", ("ssq6", T)])
            S.op("dve", lambda e, T=T: e.tensor_scalar(out=st6[:, NT + T:NT + T + 1], in0=st6[:, T:T + 1],
                                                       scalar1=1.0 / D, scalar2=EPS, op0=ALU.mult, op1=ALU.add),
                 reads=[("ssq6", T)], writes=[("ms6", T)])
            S.op("act", lambda e, T=T: e.activation(out=st6[:, NT + T:NT + T + 1], in_=st6[:, NT + T:NT + T + 1],
                                                    func=AF.Sqrt), reads=[("ms6", T)], writes=[("ms6", T)])
            S.op("dve", lambda e, T=T: e.reciprocal(out=st6[:, T:T + 1], in_=st6[:, NT + T:NT + T + 1]),
                 reads=[("ms6", T)], writes=[("rs6", T)])
            S.op("dve", lambda e, T=T: e.scalar_tensor_tensor(out=x1[:, T, :], in0=accs[:], scalar=st6[:, T:T + 1],
                                                              in1=bc[:, 3, :], op0=ALU.mult, op1=ALU.mult),
                 reads=["accsum", ("rs6", T), ("bc", 3)], writes=[("xo", T), ("accs", 0), ("accs", 1)])
            S.dma("sp", lambda e, T=T: e.dma_start(out=out[T * 128:(T + 1) * 128, :], in_=x1[:, T, :]),
                  reads=[("xo", T)], writes=[("out", T)])
        phase_end("p5b")
    S.finish()
    es.close()
    return nc


def _col(v):
    return np.ascontiguousarray(np.asarray(v, np.float32).reshape(8, 128).T)


def make_inputs(inp):
    global _BIAS_IDX
    f = lambda a: np.ascontiguousarray(np.asarray(a, dtype=np.float32))
    if _BIAS_IDX is None:
        _BIAS_IDX = build_bias_index()
    rpb = f(inp["na_rpb"])[0]
    ext = np.concatenate([rpb.reshape(8, -1), np.full((8, 1), MASKV, np.float32)], axis=1)
    biasT = np.stack([ext[h][_BIAS_IDX] for h in range(8)], axis=1)
    cstm = np.zeros((128, 528), np.float32)
    cstm[:, 0:128] = np.eye(128, dtype=np.float32)
    sidx = np.arange(128)
    blk = (sidx[:, None] // 64) == (sidx[None, :] // 64)
    cstm[:, 128:256] = (blk & (sidx[:, None] <= sidx[None, :])).astype(np.float32)
    cstm[:, 256:384] = (blk & (sidx[:, None] >= sidx[None, :])).astype(np.float32)
    cstm[:, 384:512] = 1.0
    cstm[:, 512:528] = np.arange(16, dtype=np.float32)[None, :]
    icm = np.zeros((128, 388), np.int32)
    icm[:, 0:128] = np.arange(128, dtype=np.int32)[None, :]
    icm[:, 128:384] = np.arange(256, dtype=np.int32)[None, :]
    icm[:, 384] = -128
    icm[:, 385] = -256
    icm[:, 386] = 127
    icm[:, 387] = 255
    rmask = np.ones((128, TOK), np.float32)
    rmask[:, ::64] = 0.0
    c_ctx = f(inp["c_ctx"])
    hg_lb = f(inp["hg_lb"])
    lbraw = np.ascontiguousarray(hg_lb.reshape(2, 2, 4, 128).transpose(3, 0, 1, 2).reshape(128, 16))
    keys = f(inp["peer_keys"])[0]
    keysT = np.ascontiguousarray(keys.transpose(3, 0, 1, 2).reshape(128, 16 * 128))
    shared = dict(
        w_mod=f(inp["w_mod"])[0], b_mod=f(inp["b_mod"])[0].reshape(1, -1),
        n1c=_col(f(inp["norm1"])[0]), n2c=_col(f(inp["norm2"])[0]),
        n2b=np.ascontiguousarray(np.broadcast_to(f(inp["norm2"])[0][None, :], (128, D))),
        nfb=np.ascontiguousarray(np.broadcast_to(f(inp["norm_f"])[None, :], (128, D))),
        w_in=f(inp["w_in"])[0], w_out=f(inp["w_out"])[0], wq=f(inp["peer_wq"])[0],
        keysT=keysT, u=f(inp["peer_u"])[0], v=f(inp["peer_v"])[0], lbraw=lbraw,
        hgn=np.ascontiguousarray(np.broadcast_to(f(inp["hg_norm"])[0][None, :], (128, 512))),
        biasT=np.ascontiguousarray(biasT.reshape(128, -1)), cst=cstm, rmask=rmask, icst=icm,
    )
    xs = f(inp["x"]); cs = f(inp["c"]); ctxs = f(inp["ctx"])
    maps = []
    for b in range(xs.shape[0]):
        cc = np.stack([_col(cs[b]), _col(c_ctx)], axis=2).reshape(128, 16)
        m = dict(shared)
        m.update(x=xs[b], ctx=ctxs[b], ccol=np.ascontiguousarray(cc))
        maps.append(m)
    return maps


def kernel(**inputs):
    maps = make_inputs(inputs)
    nc = build()
    res = run_bass_kernel_spmd(nc, maps, core_ids=list(range(len(maps))))
    return np.stack([np.asarray(r["out"], dtype=np.float32) for r in res.results], axis=0)
```

```python
import contextlib
import numpy as np
import concourse.bass as bass
import concourse.mybir as mybir
from concourse.bass_utils import run_bass_kernel_spmd

F32 = mybir.dt.float32
BF16 = mybir.dt.bfloat16
I32 = mybir.dt.int32
U32 = mybir.dt.uint32
ALU = mybir.AluOpType
AF = mybir.ActivationFunctionType
AX = mybir.AxisListType

D = 1024
SEQ = 2048
CTX = 256
NT = 16
NTT = 18
TOK = SEQ + CTX
NCH = TOK // 64
EPS = 1e-6
MASKV = -30000.0
NEG = -1.0e30


class Sched:
    COMPUTE = ("pe", "dve", "act", "pool")

    def __init__(self, nc, n_dsem=None):
        self.nc = nc
        self.engs = {"pe": nc.tensor, "dve": nc.vector, "act": nc.scalar,
                     "pool": nc.gpsimd, "sp": nc.sync}
        self.n_dsem = n_dsem or {"sp": 8, "act": 4, "pool": 16}
        self.es = contextlib.ExitStack()
        self.csem = {e: self.es.enter_context(nc.semaphore("cs_" + e)) for e in self.COMPUTE}
        self.dsem = {q: [self.es.enter_context(nc.semaphore(f"ds_{q}{j}")) for j in range(n)]
                     for q, n in self.n_dsem.items()}
        self.ccount = {e: 0 for e in self.COMPUTE}
        self.dcount = {q: 0 for q in self.n_dsem}
        self.clock = {e: {} for e in self.engs}
        self.bar_sig = 0
        self.bar_clock = {}
        self.bar_tile = None
        self.ops = []
        self.last_writer = {}
        self.readers = {}
        self.total_ops = 0

    def op(self, eng, fn, reads=(), writes=(), dma=False):
        deps = set()
        for r in reads:
            w = self.last_writer.get(r)
            if w is not None:
                deps.add(w)
        for w_ in writes:
            w = self.last_writer.get(w_)
            if w is not None:
                deps.add(w)
            for rd in self.readers.get(w_, ()):
                deps.add(rd)
        i = len(self.ops)
        deps.discard(i)
        self.ops.append(dict(eng=eng, fn=fn, deps=deps, dma=dma))
        for r in reads:
            self.readers.setdefault(r, []).append(i)
        for w_ in writes:
            self.last_writer[w_] = i
            self.readers[w_] = []
        return i

    def dma(self, q, fn, reads=(), writes=()):
        return self.op(q, fn, reads, writes, dma=True)

    def _wait(self, E, key, sem, val):
        ck = self.clock[E]
        if ck.get(key, 0) < val:
            self.engs[E].wait_ge(sem, val)
            ck[key] = val

    def _merge(self, E, clk):
        ck = self.clock[E]
        for k, v in clk.items():
            if ck.get(k, 0) < v:
                ck[k] = v

    def flush(self, barrier=True):
        ops = self.ops
        need_sig = [False] * len(ops)
        for i, o in enumerate(ops):
            for d in o["deps"]:
                od = ops[d]
                if od["dma"]:
                    continue
                if od["eng"] == "pe" and o["eng"] == "pe" and not o["dma"]:
                    continue
                need_sig[d] = True
        if barrier:
            last = {}
            for i, o in enumerate(ops):
                if not o["dma"]:
                    last[o["eng"]] = i
            for e, i in last.items():
                need_sig[i] = True
        for i, o in enumerate(ops):
            E = o["eng"]
            eng = self.engs[E]
            if self.bar_sig:
                self._wait(E, ("c", "dve"), self.csem["dve"], self.bar_sig)
                self._merge(E, self.bar_clock)
            for d in sorted(o["deps"]):
                od = ops[d]
                if od["dma"]:
                    q = od["eng"]
                    self._wait(E, ("d", q, od["dsem_idx"]), self.dsem[q][od["dsem_idx"]], od["dval"])
                else:
                    F = od["eng"]
                    if F == "pe" and E == "pe" and not o["dma"]:
                        continue
                    self._wait(E, ("c", F), self.csem[F], od["sig"])
                self._merge(E, od["clk"])
            if o["dma"]:
                n = self.n_dsem[E]
                j = self.dcount[E] % n
                prev = self.dcount[E] // n
                if prev > 0:
                    self._wait(E, ("d", E, j), self.dsem[E][j], 16 * prev)
                ins = o["fn"](eng)
                ins.then_inc(self.dsem[E][j], 16)
                o["dsem_idx"] = j
                o["dval"] = 16 * (prev + 1)
                self.dcount[E] += 1
                o["clk"] = dict(self.clock[E])
            else:
                ins = o["fn"](eng)
                if need_sig[i]:
                    self.ccount[E] += 1
                    ins.then_inc(self.csem[E], 1)
                    o["sig"] = self.ccount[E]
                else:
                    o["sig"] = None
                o["clk"] = dict(self.clock[E])
            o["fn"] = None
        self.total_ops += len(ops)
        if barrier:
            self._barrier()
        self.ops = []
        self.last_writer = {}
        self.readers = {}

    def _wait_all(self, E):
        for q, n in self.n_dsem.items():
            for j in range(n):
                uses = (self.dcount[q] - j + n - 1) // n if self.dcount[q] > j else 0
                if uses > 0:
                    self._wait(E, ("d", q, j), self.dsem[q][j], 16 * uses)
        for e in self.COMPUTE:
            if self.ccount[e] > 0:
                self._wait(E, ("c", e), self.csem[e], self.ccount[e])

    def _barrier(self):
        self._wait_all("dve")
        ins = self.engs["dve"].memset(self.bar_tile, 0.0)
        self.ccount["dve"] += 1
        ins.then_inc(self.csem["dve"], 1)
        self.bar_sig = self.ccount["dve"]
        self.bar_clock = dict(self.clock["dve"])

    def finish(self, eng="sp"):
        self.flush(barrier=True)
        self._wait_all(eng)
        self.es.close()


NA_VARIANTS = [(-2, True), (-1, False), (0, False), (1, False), (2, True),
               (-3, False), (-2, False), (2, False), (3, False)]


def na_chunks(i):
    if i in (0, 1, 14, 15):
        cs = range(0, 4) if i < 2 else range(12, 16)
        out = []
        for c in cs:
            d = c - i
            t = {(-3): 5, (-2): 6, (-1): 1, 0: 2, 1: 3, 2: 7, 3: 8}[d]
            out.append((c, t))
        return out
    return [(i + d, d + 2) for d in range(-2, 3)]


def build_bias_index():
    idx = np.full((128, 9, 128), 15 * 31, dtype=np.int64)
    for t, (d, partial) in enumerate(NA_VARIANTS):
        for j in range(2):
            for jq in range(2):
                dr = 2 * d + j - jq
                if abs(dr) > 7:
                    continue
                if partial:
                    if d == -2 and not (j >= jq):
                        continue
                    if d == 2 and not (j == 0 and jq == 1):
                        continue
                for cq in range(64):
                    cstart = min(max(cq - 8, 0), 48)
                    for ck in range(cstart, cstart + 16):
                        idx[j * 64 + ck, t, jq * 64 + cq] = (dr + 7) * 31 + (ck - cq + 15)
    return idx


_BIAS_IDX = None


import os as _os
_NOCONV = bool(_os.environ.get('NOCONV'))


class _Stop(Exception):
    pass


def build(debug=False, stop=None):
    nc = bass.Bass("TRN2", target_bir_lowering=False)
    try:
        return _build(nc, debug, stop)
    except _Stop:
        return nc


def _build(nc, debug, stop):

    def din(name, shape, dt=F32):
        return nc.dram_tensor(name, shape, dt, kind="ExternalInput").ap()

    x = din("x", [SEQ, D])
    ctx = din("ctx", [CTX, D])
    ccol = din("ccol", [128, 16])
    w_mod = din("w_mod", [D, 6 * D])
    b_mod = din("b_mod", [1, 6 * D])
    n1c = din("n1c", [128, 8])
    n2c = din("n2c", [128, 8])
    n2b = din("n2b", [128, D])
    nfb = din("nfb", [128, D])
    w_in = din("w_in", [D, 4096])
    w_out = din("w_out", [D, D])
    wq = din("wq", [D, 2048])
    keysT = din("keysT", [128, 2048])
    u_t = din("u", [16384, D])
    v_t = din("v", [16384, D])
    lbraw = din("lbraw", [128, 16])
    hgn = din("hgn", [128, 512])
    biasT = din("biasT", [128, 8 * 9 * 128])
    cst = din("cst", [128, 528])
    rmask_d = din("rmask", [128, TOK])
    icst = din("icst", [128, 388], I32)
    out = nc.dram_tensor("out", [SEQ, D], F32, kind="ExternalOutput").ap()
    scr_bc = nc.dram_tensor("scr_bc", [4, 128, D], F32, kind="Internal").ap()
    uv_bf = nc.dram_tensor("uv_bf", [16384, 2 * D], BF16, kind="Internal").ap()
    dbg = {}
    if debug:
        dbg["hT"] = nc.dram_tensor("d_hT", [128, 8 * TOK], BF16, kind="ExternalOutput").ap()
        dbg["mixT"] = nc.dram_tensor("d_mixT", [128, 8 * SEQ], BF16, kind="ExternalOutput").ap()
        dbg["x1"] = nc.dram_tensor("d_x1", [128, NT * D], F32, kind="ExternalOutput").ap()
        dbg["mc"] = nc.dram_tensor("d_mc", [128, 48], F32, kind="ExternalOutput").ap()
        dbg["eidx"] = nc.dram_tensor("d_eidx", [128, NT * 128], I32, kind="ExternalOutput").ap()
        dbg["gw"] = nc.dram_tensor("d_gw", [128, NT * 128], F32, kind="ExternalOutput").ap()

    es = contextlib.ExitStack()

    def sb(name, shape, dt=F32, stack=None):
        return (stack or es).enter_context(nc.sbuf_tensor(name, shape, dt))

    bar = sb("bar", [128, 1])
    cA = sb("cA", [128, 528])
    identb = sb("identb", [128, 128], BF16)
    mc = sb("mc", [128, 48])
    lb = sb("lb", [128, 8])
    oml = sb("oml", [128, 8])
    ps = [es.enter_context(nc.psum_tensor(f"ps{j}", [128, 512], F32)) for j in range(6)]
    pT = [es.enter_context(nc.psum_tensor(f"pT{j}", [128, 1024], BF16)) for j in range(2)]

    S = Sched(nc)
    S.bar_tile = bar[:]

    def phase_end(name):
        S.flush()
        if stop == name:
            S.finish()
            raise _Stop()

    IDENT = cA[:, 0:128]
    TRIF = cA[:, 128:256]
    TRIB = cA[:, 256:384]
    ONES = cA[:, 384:512]
    IOTA16 = cA[:, 512:528]

    with contextlib.ExitStack() as p0:
        cc = sb("cc", [128, 16], stack=p0)
        scl = sb("scl", [128, 8, 33], BF16, stack=p0)
        wm = [sb(f"wm{j}", [128, 8, 512], stack=p0) for j in range(3)]
        wmb = [sb(f"wmb{j}", [128, 8, 512], BF16, stack=p0) for j in range(2)]
        bm = sb("bm", [33, 6 * D], stack=p0)
        modrow = sb("modrow", [33, 6 * D], stack=p0)
        mcol = sb("mcol", [128, 48], stack=p0)
        n1 = sb("n1", [128, 8], stack=p0)
        n2 = sb("n2", [128, 8], stack=p0)
        n2bt = sb("n2bt", [128, D], stack=p0)
        bct = [sb(f"bct{j}", [128, D], stack=p0) for j in range(2)]
        lbr = sb("lbr", [128, 16], stack=p0)

        S.dma("sp", lambda e: e.dma_start(out=cA[:], in_=cst), writes=["cA"])
        S.dma("sp", lambda e: e.dma_start(out=cc[:], in_=ccol), writes=["cc"])
        S.dma("sp", lambda e: e.dma_start(out=n1[:], in_=n1c), writes=["n1"])
        S.dma("sp", lambda e: e.dma_start(out=n2[:], in_=n2c), writes=["n2"])
        S.dma("sp", lambda e: e.dma_start(out=lbr[:], in_=lbraw), writes=["lbr"])
        S.dma("sp", lambda e: e.dma_start(out=n2bt[:], in_=n2b), writes=["n2bt"])
        S.op("dve", lambda e: e.memset(bm[:], 0.0), writes=["bm"])
        S.dma("sp", lambda e: e.dma_start(out=bm[0:1, :], in_=b_mod), reads=["bm"], writes=["bm0"])
        S.dma("sp", lambda e: e.dma_start(out=bm[32:33, :], in_=b_mod), reads=["bm"], writes=["bm32"])
        S.op("dve", lambda e: e.tensor_copy(out=identb[:], in_=IDENT), reads=["cA"], writes=["identb"])
        S.op("dve", lambda e: e.memset(scl[:], 0.0), writes=["scl"])
        ccv = cc[:].rearrange("p (k t) -> p k t", t=2)
        S.op("act", lambda e: e.activation(out=scl[:, :, 0:1], in_=ccv[:, :, 0:1], func=AF.Silu),
             reads=["cc", "scl"], writes=["scl"])
        S.op("act", lambda e: e.activation(out=scl[:, :, 32:33], in_=ccv[:, :, 1:2], func=AF.Silu),
             reads=["cc", "scl"], writes=["scl"])
        S.op("dve", lambda e: e.tensor_tensor(out=lb[:], in0=lbr[:, 0:8], in1=lbr[:, 8:16], op=ALU.subtract),
             reads=["lbr"], writes=["lb"])
        S.op("act", lambda e: e.activation(out=lb[:], in_=lb[:], func=AF.Sigmoid), reads=["lb"], writes=["lb"])
        S.op("dve", lambda e: e.tensor_scalar(out=oml[:], in0=lb[:], scalar1=-1.0, scalar2=1.0,
                                              op0=ALU.mult, op1=ALU.add), reads=["lb"], writes=["oml"])
        for n in range(12):
            wb = wm[n % 3]
            S.dma("sp" if n % 2 == 0 else "act",
                  lambda e, n=n, wb=wb: e.dma_start(
                      out=wb[:], in_=w_mod[:, n * 512:(n + 1) * 512].rearrange("(k p) n -> p k n", p=128)),
                  writes=[("wm", n % 3)])
            wbb = wmb[n % 2]
            if n % 2 == 0:
                S.op("dve", lambda e, wb=wb, wbb=wbb: e.tensor_copy(out=wbb[:], in_=wb[:]),
                     reads=[("wm", n % 3)], writes=[("wmb", n % 2)])
            else:
                S.op("act", lambda e, wb=wb, wbb=wbb: e.activation(out=wbb[:], in_=wb[:], func=AF.Copy),
                     reads=[("wm", n % 3)], writes=[("wmb", n % 2)])
            pb = ps[n % 2]
            for k in range(8):
                S.op("pe", lambda e, k=k, wbb=wbb, pb=pb: e.matmul(pb[0:33, :], scl[:, k, :], wbb[:, k, :],
                                                                  start=(k == 0), stop=(k == 7)),
                     reads=["scl", ("wmb", n % 2)], writes=[("ps", n % 2)])
            S.op("dve", lambda e, n=n, pb=pb: e.tensor_tensor(out=modrow[:, n * 512:(n + 1) * 512], in0=pb[0:33, :],
                                                             in1=bm[:, n * 512:(n + 1) * 512], op=ALU.add),
                 reads=[("ps", n % 2), "bm", "bm0", "bm32"], writes=[("modrow", n)])
        mr_all = [("modrow", n) for n in range(12)]
        col_specs = [(0, 0), (0, 1), (0, 3), (0, 4), (32, 0), (32, 1)]
        for si, (r, vi) in enumerate(col_specs):
            for k in range(8):
                c0 = 2 * (si * 8 + k)
                S.op("pe", lambda e, r=r, vi=vi, k=k, c0=c0: e.matmul(
                    ps[2][:, c0:c0 + 2], modrow[r:r + 1, vi * D + k * 128: vi * D + (k + 1) * 128],
                    cA[r:r + 1, 384:386], start=True, stop=True),
                    reads=mr_all + ["cA"], writes=[("ps", 2)])
        S.op("dve", lambda e: e.tensor_copy(out=mcol[:].unsqueeze(2), in_=ps[2][:, 0:96].rearrange("p (c two) -> p c two", two=2)[:, :, 0:1]), reads=[("ps", 2)], writes=["mcol"])
        S.op("dve", lambda e: e.scalar_tensor_tensor(out=mc[:, 0:8], in0=mcol[:, 8:16], scalar=1.0, in1=n1[:],
                                                     op0=ALU.add, op1=ALU.mult), reads=["mcol", "n1"], writes=["mc0"])
        S.op("dve", lambda e: e.tensor_copy(out=mc[:, 8:16], in_=mcol[:, 0:8]), reads=["mcol"], writes=["mc1"])
        S.op("dve", lambda e: e.scalar_tensor_tensor(out=mc[:, 16:24], in0=mcol[:, 40:48], scalar=1.0, in1=n1[:],
                                                     op0=ALU.add, op1=ALU.mult), reads=["mcol", "n1"], writes=["mc2"])
        S.op("dve", lambda e: e.tensor_copy(out=mc[:, 24:32], in_=mcol[:, 32:40]), reads=["mcol"], writes=["mc3"])
        S.op("dve", lambda e: e.scalar_tensor_tensor(out=mc[:, 32:40], in0=mcol[:, 24:32], scalar=1.0, in1=n2[:],
                                                     op0=ALU.add, op1=ALU.mult), reads=["mcol", "n2"], writes=["mc4"])
        S.op("dve", lambda e: e.tensor_copy(out=mc[:, 40:48], in_=mcol[:, 16:24]), reads=["mcol"], writes=["mc5"])
        for j, (vi, kind) in enumerate([(2, "copy"), (5, "copy"), (4, "g2"), (3, "copy")]):
            bt_ = bct[j % 2]
            for hf in range(2):
                pb = ps[3 + hf]
                S.op("pe", lambda e, vi=vi, hf=hf, pb=pb: e.matmul(
                    pb[:, :], cA[0:1, 384:512], modrow[0:1, vi * D + hf * 512: vi * D + (hf + 1) * 512],
                    start=True, stop=True), reads=mr_all + ["cA"], writes=[("ps", 3 + hf)])
                if kind == "copy":
                    S.op("dve", lambda e, bt_=bt_, hf=hf, pb=pb: e.tensor_copy(out=bt_[:, hf * 512:(hf + 1) * 512], in_=pb[:, :]),
                         reads=[("ps", 3 + hf)], writes=[("bct", j % 2, hf)])
                else:
                    S.op("dve", lambda e, bt_=bt_, hf=hf, pb=pb: e.scalar_tensor_tensor(
                        out=bt_[:, hf * 512:(hf + 1) * 512], in0=pb[:, :], scalar=1.0,
                        in1=n2bt[:, hf * 512:(hf + 1) * 512], op0=ALU.add, op1=ALU.mult),
                        reads=[("ps", 3 + hf), "n2bt"], writes=[("bct", j % 2, hf)])
            S.dma("sp", lambda e, j=j, bt_=bt_: e.dma_start(out=scr_bc[j], in_=bt_[:]),
                  reads=[("bct", j % 2, 0), ("bct", j % 2, 1)], writes=[("scr", j)])
        if debug:
            S.dma("sp", lambda e: e.dma_start(out=dbg["mc"], in_=mc[:]), reads=[f"mc{j}" for j in range(6)])
        phase_end("p0")

    R = sb("R", [128, NT * D])
    x1 = R[:].rearrange("p (t d) -> p t d", d=D)
    hT = R[:, 0:9216].bitcast(BF16).rearrange("p (k t) -> p k t", k=8)
    vtok = R[:, 9216:13824].bitcast(BF16).rearrange("p (t d) -> p t d", d=512)
    with contextlib.ExitStack() as pm:
        mixT = sb("mixT", [128, 8, SEQ], BF16, stack=pm)

        with contextlib.ExitStack() as p1:
            xt = [sb(f"xt{j}", [128, D], stack=p1) for j in range(2)]
            xs = [sb(f"xs{j}", [128, D], BF16, stack=p1) for j in range(2)]
            junk = sb("junk1", [128, D], BF16, stack=p1)
            st = sb("st1", [128, 3 * NTT], stack=p1)
            for T in range(NTT):
                b = T % 2
                src = x[T * 128:(T + 1) * 128, :] if T < NT else ctx[(T - NT) * 128:(T - NT + 1) * 128, :]
                S.dma("sp" if b == 0 else "act", lambda e, b=b, src=src: e.dma_start(out=xt[b][:], in_=src),
                      writes=[("xt", b)])
                S.op("act", lambda e, b=b, T=T: e.activation(out=junk[:], in_=xt[b][:], func=AF.Square,
                                                             accum_out=st[:, T:T + 1]),
                     reads=[("xt", b)], writes=["junk", ("ssq", T)])
                S.op("dve", lambda e, T=T: e.tensor_scalar(out=st[:, NTT + T:NTT + T + 1], in0=st[:, T:T + 1],
                                                           scalar1=1.0 / D, scalar2=EPS, op0=ALU.mult, op1=ALU.add),
                     reads=[("ssq", T)], writes=[("ms", T)])
                S.op("act", lambda e, T=T: e.activation(out=st[:, NTT + T:NTT + T + 1], in_=st[:, NTT + T:NTT + T + 1],
                                                        func=AF.Sqrt), reads=[("ms", T)], writes=[("ms", T)])
                S.op("dve", lambda e, T=T: e.reciprocal(out=st[:, 2 * NTT + T:2 * NTT + T + 1],
                                                        in_=st[:, NTT + T:NTT + T + 1]),
                     reads=[("ms", T)], writes=[("rstd", T)])
                S.op("act", lambda e, b=b, T=T: e.activation(out=xs[b][:], in_=xt[b][:], func=AF.Copy,
                                                             scale=st[:, 2 * NTT + T:2 * NTT + T + 1]),
                     reads=[("xt", b), ("rstd", T)], writes=[("xs", b)])
                for k in range(8):
                    S.op("pe", lambda e, b=b, k=k: e.transpose(pT[b][:, k * 128:(k + 1) * 128],
                                                               xs[b][:, k * 128:(k + 1) * 128], identb[:]),
                         reads=[("xs", b), "identb"], writes=[("pT", b)])
                go, so = (0, 8) if T < NT else (16, 24)
                for k in range(8):
                    S.op("dve", lambda e, b=b, k=k, T=T, go=go, so=so: e.tensor_scalar(
                        out=hT[:, k, T * 128:(T + 1) * 128], in0=pT[b][:, k * 128:(k + 1) * 128],
                        scalar1=mc[:, go + k:go + k + 1], scalar2=mc[:, so + k:so + k + 1],
                        op0=ALU.mult, op1=ALU.add),
                        reads=[("pT", b)], writes=[("hT", T)])
            if debug:
                S.dma("sp", lambda e: e.dma_start(out=dbg["hT"], in_=R[:, 0:9216].bitcast(BF16)),
                      reads=[("hT", T) for T in range(NTT)])
            phase_end("p1")
        hT_all = [("hT", T) for T in range(NTT)]

        def load_w(tile_ap, dram_w, col0, ncols, key):
            for k0 in range(0, 8, 4):
                S.dma("pool", lambda e, k0=k0: e.dma_start(
                    out=tile_ap[:, k0:k0 + 4, :],
                    in_=dram_w[k0 * 128:(k0 + 4) * 128, col0:col0 + ncols].rearrange("(k p) n -> p k n", p=128)),
                    writes=[(key, k0)])
            return [(key, 0), (key, 4)]

        with contextlib.ExitStack() as p2:
            qT = sb("qT", [128, 4, SEQ], BF16, stack=p2)
            kT = sb("kT", [128, 4, TOK], BF16, stack=p2)
            vaug = sb("vaug", [128, NTT, 8, 65], BF16, stack=p2)
            p2a = contextlib.ExitStack()
            wna = sb("wna", [128, 8, 1536], BF16, stack=p2a)
            wk = load_w(wna, w_in, 0, 1536, "wna")
            S.op("pool", lambda e: e.memset(vaug[:, :, :, 64:65], 1.0), writes=["vones"])
            cnt = 0
            for which, dst, ntok, cbase in (("q", qT, SEQ, 0), ("k", kT, TOK, 512)):
                for hp in range(4):
                    for t0 in range(0, ntok, 512):
                        tw = min(512, ntok - t0)
                        pb = cnt % 4
                        for k in range(8):
                            S.op("pe", lambda e, k=k, hp=hp, t0=t0, tw=tw, pb=pb, cbase=cbase: e.matmul(
                                ps[pb][:, 0:tw], wna[:, k, cbase + hp * 128: cbase + (hp + 1) * 128],
                                hT[:, k, t0:t0 + tw], start=(k == 0), stop=(k == 7)),
                                reads=wk + hT_all, writes=[("ps", pb)])
                        eng = "act" if cnt % 2 == 0 else "dve"
                        if eng == "act":
                            S.op("act", lambda e, dst=dst, hp=hp, t0=t0, tw=tw, pb=pb: e.activation(
                                out=dst[:, hp, t0:t0 + tw], in_=ps[pb][:, 0:tw], func=AF.Copy),
                                reads=[("ps", pb)], writes=[(which, hp, t0)])
                        else:
                            S.op("dve", lambda e, dst=dst, hp=hp, t0=t0, tw=tw, pb=pb: e.tensor_copy(
                                out=dst[:, hp, t0:t0 + tw], in_=ps[pb][:, 0:tw]),
                                reads=[("ps", pb)], writes=[(which, hp, t0)])
                        cnt += 1
            for T in range(NTT):
                pb = cnt % 4
                for k in range(8):
                    S.op("pe", lambda e, k=k, T=T, pb=pb: e.matmul(
                        ps[pb][:, :], hT[:, k, T * 128:(T + 1) * 128], wna[:, k, 1024:1536],
                        start=(k == 0), stop=(k == 7)), reads=wk + hT_all, writes=[("ps", pb)])
                if cnt % 2 == 0:
                    S.op("act", lambda e, T=T, pb=pb: e.activation(
                        out=vaug[:, T, :, 0:64], in_=ps[pb][:, :].rearrange("p (h d) -> p h d", d=64), func=AF.Copy),
                        reads=[("ps", pb)], writes=[("v", T)])
                else:
                    S.op("dve", lambda e, T=T, pb=pb: e.tensor_copy(
                        out=vaug[:, T, :, 0:64], in_=ps[pb][:, :].rearrange("p (h d) -> p h d", d=64)),
                        reads=[("ps", pb)], writes=[("v", T)])
                cnt += 1
            phase_end("p2a")
            p2a.close()
            bt = sb("bt", [128, 8, 9, 128], stack=p2)
            Ssb = [sb(f"Ssb{j}", [128, 640], stack=p2) for j in range(2)]
            Pb = [sb(f"Pb{j}", [128, 896], BF16, stack=p2) for j in range(2)]
            rden = sb("rden", [128, 16], stack=p2)
            natok = [sb(f"natok{j}", [128, 512], BF16, stack=p2) for j in range(2)]
            S.dma("sp", lambda e: e.dma_start(out=bt[:].rearrange("p h t q -> p (h t q)"), in_=biasT), writes=["bt"])

            qk_all_r = []
            it = 0
            for i in range(NT):
                chunks = na_chunks(i)
                nw = len(chunks)
                nb = i % 2
                for h in range(8):
                    hp, po = h // 2, (h % 2) * 64
                    sbuf_i = it % 2
                    b0, b1 = ps[2 * sbuf_i], ps[2 * sbuf_i + 1]

                    def sloc(j):
                        return (b0, j * 128) if j < 4 else (b1, (j - 4) * 128)
                    for j, (c, t) in enumerate(chunks):
                        bk, co = sloc(j)
                        S.op("pe", lambda e, bk=bk, co=co, c=c, hp=hp, po=po, i=i: e.matmul(
                            bk[:, co:co + 128], kT[po:po + 64, hp, c * 128:(c + 1) * 128],
                            qT[po:po + 64, hp, i * 128:(i + 1) * 128], start=True, stop=True),
                            reads=[], writes=[("psS", sbuf_i, j // 4)])
                    for cc_ in range(2):
                        S.op("pe", lambda e, cc_=cc_, hp=hp, po=po, i=i, b1=b1: e.matmul(
                            b1[:, 128 + cc_ * 128: 256 + cc_ * 128],
                            kT[po:po + 64, hp, SEQ + cc_ * 128: SEQ + (cc_ + 1) * 128],
                            qT[po:po + 64, hp, i * 128:(i + 1) * 128], start=True, stop=True),
                            reads=[], writes=[("psS", sbuf_i, 1)])
                    for j, (c, t) in enumerate(chunks):
                        bk, co = sloc(j)
                        S.op("dve", lambda e, bk=bk, co=co, j=j, t=t, h=h, sbuf_i=sbuf_i: e.scalar_tensor_tensor(
                            out=Ssb[sbuf_i][:, j * 128:(j + 1) * 128], in0=bk[:, co:co + 128], scalar=0.125,
                            in1=bt[:, h, t, :], op0=ALU.mult, op1=ALU.add),
                            reads=[("psS", sbuf_i, j // 4), "bt"], writes=[("Ssb", sbuf_i)])
                    S.op("act", lambda e, nw=nw, sbuf_i=sbuf_i: e.activation(
                        out=Pb[sbuf_i][:, 0:nw * 128], in_=Ssb[sbuf_i][:, 0:nw * 128], func=AF.Exp),
                        reads=[("Ssb", sbuf_i)], writes=[("Pw", sbuf_i)])
                    S.op("act", lambda e, sbuf_i=sbuf_i, b1=b1: e.activation(
                        out=Pb[sbuf_i][:, 640:896], in_=b1[:, 128:384], func=AF.Exp, scale=0.125),
                        reads=[("psS", sbuf_i, 1)], writes=[("Pc", sbuf_i)])
                    ob = ps[4 + h // 4]
                    oc = (h % 4) * 128
                    nmm = nw + 2
                    for j, (c, t) in enumerate(chunks):
                        S.op("pe", lambda e, j=j, c=c, h=h, ob=ob, oc=oc, sbuf_i=sbuf_i, nmm=nmm: e.matmul(
                            ob[:, oc:oc + 65], Pb[sbuf_i][:, j * 128:(j + 1) * 128], vaug[:, c, h, :],
                            start=(j == 0), stop=False),
                            reads=[("Pw", sbuf_i)], writes=[("psO", h)])
                    for cc_ in range(2):
                        S.op("pe", lambda e, cc_=cc_, h=h, ob=ob, oc=oc, sbuf_i=sbuf_i: e.matmul(
                            ob[:, oc:oc + 65], Pb[sbuf_i][:, 640 + cc_ * 128: 768 + cc_ * 128], vaug[:, NT + cc_, h, :],
                            start=False, stop=(cc_ == 1)),
                            reads=[("Pc", sbuf_i)], writes=[("psO", h)])
                    S.op("dve", lambda e, h=h, ob=ob, oc=oc: e.reciprocal(out=rden[:, h:h + 1], in_=ob[:, oc + 64:oc + 65]),
                         reads=[("psO", h)], writes=[("rden", h)])
                    S.op("dve", lambda e, h=h, ob=ob, oc=oc, nb=nb: e.tensor_scalar(
                        out=natok[nb][:, h * 64:(h + 1) * 64], in0=ob[:, oc:oc + 64], scalar1=rden[:, h:h + 1],
                        scalar2=None, op0=ALU.mult),
                        reads=[("psO", h), ("rden", h)], writes=[("natok", nb)])
                    it += 1
                for j in range(4):
                    S.op("pe", lambda e, j=j, nb=nb: e.transpose(pT[nb][:, j * 128:(j + 1) * 128],
                                                                natok[nb][:, j * 128:(j + 1) * 128], identb[:]),
                         reads=[("natok", nb)], writes=[("pT", nb)])
                S.op("act", lambda e, i=i, nb=nb: e.activation(
                    out=mixT[:, 0:4, i * 128:(i + 1) * 128],
                    in_=pT[nb][:, 0:512].rearrange("p (j t) -> p j t", t=128), func=AF.Copy),
                    reads=[("pT", nb)], writes=[("mixna", i)])
            phase_end("p2")

        with contextlib.ExitStack() as p3:
            oacc = sb("oacc", [128, NT, 512], stack=p3)
            with contextlib.ExitStack() as p3a:
                wv = sb("wv", [128, 8, 512], BF16, stack=p3a)
                wk = load_w(wv, w_in, 3 * 512 + 3 * 512, 512, "wv")
                for T in range(NTT):
                    pb = T % 4
                    for k in range(8):
                        S.op("pe", lambda e, k=k, T=T, pb=pb: e.matmul(
                            ps[pb][:, :], hT[:, k, T * 128:(T + 1) * 128], wv[:, k, :],
                            start=(k == 0), stop=(k == 7)), reads=wk, writes=[("ps", pb)])
                    if T % 2 == 0:
                        S.op("act", lambda e, T=T, pb=pb: e.activation(out=vtok[:, T, :], in_=ps[pb][:, :], func=AF.Copy),
                             reads=[("ps", pb)], writes=[("vtok", T)])
                    else:
                        S.op("dve", lambda e, T=T, pb=pb: e.tensor_copy(out=vtok[:, T, :], in_=ps[pb][:, :]),
                             reads=[("ps", pb)], writes=[("vtok", T)])
                phase_end("p3a")
            with contextlib.ExitStack() as p3b:
                rmask = sb("rmask_sb", [128, TOK], stack=p3b)
                A_ = sb("hgA", [128, TOK], stack=p3b)
                B_ = sb("hgB", [128, TOK], stack=p3b)
                C_ = sb("hgC", [128, TOK], stack=p3b)
                qsb = sb("hgqs", [128, 512], stack=p3b)
                Qt = sb("hgQt", [128, SEQ], BF16, stack=p3b)
                Qs = sb("hgQs", [128, SEQ], BF16, stack=p3b)
                Ks = sb("hgKs", [128, TOK], BF16, stack=p3b)
                Kf = sb("hgKf", [128, TOK], BF16, stack=p3b)
                Ktok = sb("hgKtok", [128, NTT, 128], BF16, stack=p3b)
                wqf = sb("hgwqf", [128, 8, 256], BF16, stack=p3b)
                sc_ = sb("hgsc", [128, 5, NCH], stack=p3b)
                Sst = [sb(f"hgS{j}", [128, 128], stack=p3b) for j in range(2)]
                Usc = [sb(f"hgUsc{j}", [128, 128], stack=p3b) for j in range(4)]
                Smb = [sb(f"hgSmb{j}", [128, 128], BF16, stack=p3b) for j in range(2)]
                Qe = sb("hgQe", [128, SEQ], BF16, stack=p3b)
                Qo = sb("hgQo", [128, SEQ], BF16, stack=p3b)
                ATs = [sb(f"hgATs{j}", [128, 128], BF16, stack=p3b) for j in range(2)]

                S.dma("sp", lambda e: e.dma_start(out=rmask[:], in_=rmask_d), writes=["rmask"])

                def v3(t, n=TOK):
                    return t[:, 0:n].rearrange("p (t s) -> p t s", s=64)

                for dr in range(2):
                    for hh in range(4):
                        dh = dr * 4 + hh
                        S.dma("pool", lambda e, hh=hh: e.dma_start(
                            out=wqf[:, :, 0:128],
                            in_=w_in[:, 1536 + hh * 128:1536 + (hh + 1) * 128].rearrange("(k p) n -> p k n", p=128)),
                            writes=["wq_h"])
                        S.dma("pool", lambda e, hh=hh, dr=dr: e.dma_start(
                            out=wqf[:, :, 128:256],
                            in_=w_in[:, 2048 + dr * 512 + hh * 128:2048 + dr * 512 + (hh + 1) * 128].rearrange(
                                "(k p) n -> p k n", p=128)), writes=["wf_h"])
                        for ci, t0 in enumerate(range(0, TOK, 512)):
                            tw = min(512, TOK - t0)
                            pb = ci % 4
                            for k in range(8):
                                S.op("pe", lambda e, k=k, t0=t0, tw=tw, pb=pb: e.matmul(
                                    ps[pb][:, 0:tw], wqf[:, k, 128:256], hT[:, k, t0:t0 + tw],
                                    start=(k == 0), stop=(k == 7)), reads=["wf_h"], writes=[("ps", pb)])
                            S.op("act", lambda e, t0=t0, tw=tw, pb=pb: e.activation(
                                out=A_[:, t0:t0 + tw], in_=ps[pb][:, 0:tw], func=AF.Sigmoid),
                                reads=[("ps", pb)], writes=["A"])
                        for ci, t0 in enumerate(range(0, SEQ, 512)):
                            pb = ci % 4
                            for k in range(8):
                                S.op("pe", lambda e, k=k, t0=t0, pb=pb: e.matmul(
                                    ps[pb][:, :], wqf[:, k, 0:128], hT[:, k, t0:t0 + 512],
                                    start=(k == 0), stop=(k == 7)), reads=["wq_h"], writes=[("ps", pb)])
                        S.op("dve", lambda e, dh=dh: e.tensor_scalar(out=A_[:], in0=A_[:], scalar1=oml[:, dh:dh + 1],
                                                                     scalar2=lb[:, dh:dh + 1], op0=ALU.mult, op1=ALU.add),
                             reads=["A"], writes=["A"])
                        S.op("act", lambda e: e.activation(out=B_[:], in_=A_[:], func=AF.Ln), reads=["A"], writes=["B"])
                        S.op("dve", lambda e: e.tensor_scalar(out=A_[:], in0=A_[:], scalar1=-1.0, scalar2=1.0,
                                                              op0=ALU.mult, op1=ALU.add), reads=["A", "B"], writes=["A"])
                        S.op("dve", lambda e: e.tensor_tensor_scan(out=C_[:], data0=rmask[:], data1=B_[:], initial=0.0,
                                                                   op0=ALU.mult, op1=ALU.add),
                             reads=["B", "rmask"], writes=["C"])
                        if dr == 0:
                            gbuf, gkey = C_, "C"
                            refpos = 31
                        else:
                            S.op("dve", lambda e: e.tensor_tensor(out=B_[:], in0=B_[:], in1=C_[:], op=ALU.subtract),
                                 reads=["B", "C"], writes=["B"])
                            S.op("dve", lambda e: e.tensor_tensor(
                                out=v3(B_), in0=v3(B_), in1=v3(C_)[:, :, 63:64].to_broadcast([128, NCH, 64]), op=ALU.add),
                                reads=["B", "C"], writes=["B"])
                            gbuf, gkey = B_, "B"
                            refpos = 32
                        endpos = 63 if dr == 0 else 0
                        S.op("dve", lambda e, gbuf=gbuf, refpos=refpos: e.tensor_copy(
                            out=sc_[:, 0, :].unsqueeze(2), in_=v3(gbuf)[:, :, refpos:refpos + 1]), reads=[gkey], writes=["sc0"])
                        S.op("dve", lambda e, gbuf=gbuf, endpos=endpos: e.tensor_copy(
                            out=sc_[:, 1, :].unsqueeze(2), in_=v3(gbuf)[:, :, endpos:endpos + 1]), reads=[gkey], writes=["sc1"])
                        S.op("act", lambda e: e.activation(out=sc_[:, 2:4, :], in_=sc_[:, 0:2, :], func=AF.Exp),
                             reads=["sc0", "sc1"], writes=["sc23"])
                        S.op("dve", lambda e: e.tensor_tensor(out=sc_[:, 4, :], in0=sc_[:, 1, :], in1=sc_[:, 0, :],
                                                              op=ALU.subtract), reads=["sc0", "sc1"], writes=["sc4"])
                        S.op("act", lambda e: e.activation(out=sc_[:, 4, :], in_=sc_[:, 4, :], func=AF.Exp),
                             reads=["sc4"], writes=["sc4"])
                        obuf, okey = (B_, "B") if dr == 0 else (C_, "C")
                        S.op("dve", lambda e, gbuf=gbuf: e.tensor_tensor(
                            out=v3(gbuf), in0=v3(gbuf), in1=sc_[:, 0, :].unsqueeze(2).to_broadcast([128, NCH, 64]),
                            op=ALU.subtract), reads=[gkey, "sc0", "sc1"], writes=[gkey])
                        S.op("act", lambda e, gbuf=gbuf, obuf=obuf: e.activation(out=obuf[:, 0:SEQ], in_=gbuf[:, 0:SEQ], func=AF.Exp),
                             reads=[gkey, okey], writes=[okey])
                        S.op("act", lambda e, gbuf=gbuf: e.activation(out=gbuf[:], in_=gbuf[:], func=AF.Exp, scale=-1.0),
                             reads=[gkey, okey], writes=[gkey])
                        S.op("dve", lambda e, gbuf=gbuf: e.tensor_tensor(out=Kf[:], in0=A_[:], in1=gbuf[:], op=ALU.mult),
                             reads=["A", gkey], writes=["Kf"])
                        hk = 1 if dr == 0 else 0
                        hq = 1 - hk
                        def h32(t):
                            return t[:].rearrange("p (t two s) -> p t two s", two=2, s=32)

                        def h64(t):
                            return t[:].rearrange("p (t two s) -> p t two s", two=2, s=64)
                        if hh == 0:
                            S.op("pool", lambda e: e.memset(Ks[:], 0.0), reads=["Ks"], writes=["Ks"])
                            S.op("pool", lambda e: e.memset(Qs[:], 0.0), reads=["Qs"], writes=["Qs"])
                            if dr == 0:
                                S.op("pool", lambda e: e.memset(Qe[:], 0.0), reads=["Qe"], writes=["Qe"])
                                S.op("pool", lambda e: e.memset(Qo[:], 0.0), reads=["Qo"], writes=["Qo"])
                        S.op("dve", lambda e, hk=hk: e.tensor_copy(out=h32(Ks)[:, :, 1 - hk, :], in_=h32(Kf)[:, :, 1 - hk, :]),
                             reads=["Kf", "Ks"], writes=["Ks"])
                        for ci, t0 in enumerate(range(0, SEQ, 512)):
                            pb = ci % 4
                            S.op("act", lambda e, pb=pb: e.activation(out=qsb[:], in_=ps[pb][:, :], func=AF.Silu),
                                 reads=[("ps", pb)], writes=["qsb"])
                            S.op("dve", lambda e, t0=t0, obuf=obuf: e.tensor_tensor(out=Qt[:, t0:t0 + 512], in0=qsb[:],
                                                                                    in1=obuf[:, t0:t0 + 512], op=ALU.mult),
                                 reads=["qsb", okey], writes=["Qt"])
                        S.op("dve", lambda e, hq=hq: e.tensor_copy(out=h32(Qs)[:, :, 1 - hq, :], in_=h32(Qt)[:, :, 1 - hq, :]),
                             reads=["Qt", "Qs"], writes=["Qs"])
                        S.op("act", lambda e: e.activation(out=h64(Qe)[:, :, 0, :], in_=h64(Qt)[:, :, 0, :], func=AF.Copy),
                             reads=["Qt", "Qe"], writes=["Qe"])
                        S.op("act", lambda e: e.activation(out=h64(Qo)[:, :, 1, :], in_=h64(Qt)[:, :, 1, :], func=AF.Copy),
                             reads=["Qt", "Qo"], writes=["Qo"])
                        for g0 in range(0, NTT, 8):
                            nb = (g0 // 8) % 2
                            gn = min(8, NTT - g0)
                            for j in range(gn):
                                S.op("pe", lambda e, T=g0 + j, j=j, nb=nb: e.transpose(
                                    pT[nb][:, j * 128:(j + 1) * 128], Kf[:, T * 128:(T + 1) * 128], identb[:]),
                                    reads=["Kf"], writes=[("pT", nb)])
                            S.op("act", lambda e, g0=g0, gn=gn, nb=nb: e.activation(
                                out=Ktok[:, g0:g0 + gn, :],
                                in_=pT[nb][:, 0:gn * 128].rearrange("p (j t) -> p j t", t=128), func=AF.Copy),
                                reads=[("pT", nb)], writes=[("Ktok", T) for T in range(g0, g0 + gn)])
                        S.op("pool", lambda e, hk=hk: e.memset(
                            Kf[:].rearrange("p (t two s) -> p t two s", two=2, s=32)[:, :, 1 - hk, :], 0.0),
                            reads=["Kf"], writes=["Kf"])
                        order = [16, 17] + list(range(NT)) if dr == 0 else [17, 16] + list(range(NT - 1, -1, -1))
                        tri = TRIF if dr == 0 else TRIB
                        kcnt = [0]
                        S.op("dve", lambda e: e.memset(Sst[0][:], 0.0), reads=[("S", 0)], writes=[("S", 0)])

                        def chunks_of(T):
                            return [2 * T, 2 * T + 1] if dr == 0 else [2 * T + 1, 2 * T]

                        def stage_a(n):
                            T = order[n]
                            q = n % 2
                            for ci, c in enumerate(chunks_of(T)):
                                par = c % 2
                                S.op("pe", lambda e, T=T, par=par, ci=ci, hh=hh: e.matmul(
                                    ps[2 + ci][:, 0:128], Ktok[par * 64:(par + 1) * 64, T, :],
                                    vtok[par * 64:(par + 1) * 64, T, hh * 128:(hh + 1) * 128], start=True, stop=True),
                                    reads=[("Ktok", T)], writes=[("ps", 2 + ci)])
                                S.op("dve", lambda e, c=c, ci=ci, q=q: e.tensor_scalar(
                                    out=Usc[2 * q + ci][:], in0=ps[2 + ci][:, 0:128], scalar1=sc_[:, 4, c:c + 1], scalar2=None,
                                    op0=ALU.mult), reads=[("ps", 2 + ci), "sc4"], writes=[("Usc", 2 * q + ci)])
                            if T < NT:
                                S.op("pe", lambda e, T=T: e.matmul(ps[4][:, 0:128], Ks[:, T * 128:(T + 1) * 128],
                                                                   Qt[:, T * 128:(T + 1) * 128], start=True, stop=False),
                                     reads=["Ks", "Qt"], writes=[("ps", 4)])
                                S.op("pe", lambda e, T=T: e.matmul(ps[4][:, 0:128], Kf[:, T * 128:(T + 1) * 128],
                                                                   Qs[:, T * 128:(T + 1) * 128], start=False, stop=True),
                                     reads=["Kf", "Qs"], writes=[("ps", 4)])

                        def stage_a2(n):
                            T = order[n]
                            q = n % 2
                            if T < NT:
                                S.op("dve", lambda e, q=q, tri=tri: e.tensor_tensor(out=ATs[q][:], in0=ps[4][:, 0:128], in1=tri, op=ALU.mult),
                                     reads=[("ps", 4), "cA"], writes=[("ATs", q)])
                                ob = ps[5] if q == 0 else ps[1]
                                S.op("pe", lambda e, T=T, q=q, ob=ob, hh=hh: e.matmul(ob[:, 0:128], ATs[q][:], vtok[:, T, hh * 128:(hh + 1) * 128],
                                                                               start=True, stop=False),
                                     reads=[("ATs", q)], writes=[("ps", 5 if q == 0 else 1)])

                        def stage_b(n):
                            T = order[n]
                            q = n % 2
                            lat = T < NT
                            ob = ps[5] if q == 0 else ps[1]
                            for ci, c in enumerate(chunks_of(T)):
                                par = c % 2
                                k = kcnt[0]
                                kcnt[0] += 1
                                si, so = k % 2, (k + 1) % 2
                                if lat:
                                    S.op("act", lambda e, c=c, ci=ci, si=si: e.activation(
                                        out=Smb[ci][:], in_=Sst[si][:], func=AF.Copy, scale=sc_[:, 2, c:c + 1]),
                                        reads=[("S", si), "sc23"], writes=[("Smb", ci)])
                                    Qz, qzk = (Qe, "Qe") if par == 0 else (Qo, "Qo")
                                    S.op("pe", lambda e, T=T, ci=ci, Qz=Qz, ob=ob: e.matmul(
                                        ob[:, 0:128], Qz[:, T * 128:(T + 1) * 128], Smb[ci][:], start=False, stop=(ci == 1)),
                                        reads=[("Smb", ci), qzk], writes=[("ps", 5 if q == 0 else 1)])
                                S.op("dve", lambda e, c=c, ci=ci, q=q, si=si, so=so: e.scalar_tensor_tensor(
                                    out=Sst[so][:], in0=Sst[si][:], scalar=sc_[:, 3, c:c + 1], in1=Usc[2 * q + ci][:],
                                    op0=ALU.mult, op1=ALU.add), reads=[("Usc", 2 * q + ci), ("S", si), "sc23"], writes=[("S", so)])
                            if lat:
                                if dr == 0:
                                    S.op("act", lambda e, T=T, ob=ob, hh=hh: e.activation(
                                        out=oacc[:, T, hh * 128:(hh + 1) * 128], in_=ob[:, 0:128], func=AF.Copy),
                                        reads=[("ps", 5 if q == 0 else 1)], writes=[("oacc", T, hh)])
                                else:
                                    S.op("dve", lambda e, T=T, ob=ob, hh=hh: e.tensor_tensor(
                                        out=oacc[:, T, hh * 128:(hh + 1) * 128], in0=ob[:, 0:128],
                                        in1=oacc[:, T, hh * 128:(hh + 1) * 128], op=ALU.add),
                                        reads=[("ps", 5 if q == 0 else 1), ("oacc", T, hh)], writes=[("oacc", T, hh)])

                        stage_a(0)
                        stage_a2(0)
                        for n in range(len(order)):
                            if n + 1 < len(order):
                                stage_a(n + 1)
                            stage_b(n)
                            if n + 1 < len(order):
                                stage_a2(n + 1)
                        if kcnt[0] % 2 == 1:
                            pass
                phase_end("p3b")
            with contextlib.ExitStack() as p3c:
                wg = sb("wg", [128, 8, 512], BF16, stack=p3c)
                hgnb = sb("hgnb", [128, 512], stack=p3c)
                sg = [sb(f"sg{j}", [128, 512], stack=p3c) for j in range(2)]
                yb = [sb(f"yb{j}", [128, 512], stack=p3c) for j in range(2)]
                yt = [sb(f"yt{j}", [128, 512], BF16, stack=p3c) for j in range(2)]
                jk = sb("jk3", [128, 128], BF16, stack=p3c)
                st3 = sb("st3", [128, NT, 8], stack=p3c)
                wk = load_w(wg, w_in, 1536 + 4 * 512, 512, "wg")
                S.dma("sp", lambda e: e.dma_start(out=hgnb[:], in_=hgn), writes=["hgnb"])
                for T in range(NT):
                    for hh in range(4):
                        S.op("act", lambda e, T=T, hh=hh: e.activation(
                            out=jk[:], in_=oacc[:, T, hh * 128:(hh + 1) * 128], func=AF.Square,
                            accum_out=st3[:, T, hh:hh + 1]), reads=[], writes=["jk3", ("ss3", T)])
                ss_all = [("ss3", T) for T in range(NT)]
                S.op("dve", lambda e: e.tensor_scalar(out=st3[:, :, 4:8], in0=st3[:, :, 0:4], scalar1=1.0 / 128,
                                                      scalar2=EPS, op0=ALU.mult, op1=ALU.add),
                     reads=ss_all, writes=["ms3"])
                S.op("act", lambda e: e.activation(out=st3[:, :, 4:8], in_=st3[:, :, 4:8], func=AF.Sqrt),
                     reads=["ms3"], writes=["ms3"])
                S.op("dve", lambda e: e.reciprocal(out=st3[:, :, 0:4], in_=st3[:, :, 4:8]),
                     reads=["ms3"] + ss_all, writes=["rs3"])
                for T in range(NT):
                    b = T % 2
                    for k in range(8):
                        S.op("pe", lambda e, k=k, T=T, b=b: e.matmul(
                            ps[b][:, :], hT[:, k, T * 128:(T + 1) * 128], wg[:, k, :],
                            start=(k == 0), stop=(k == 7)), reads=wk, writes=[("ps", b)])
                    S.op("act", lambda e, b=b: e.activation(out=sg[b][:], in_=ps[b][:, :], func=AF.Silu),
                         reads=[("ps", b)], writes=[("sg", b)])
                    S.op("dve", lambda e, T=T, b=b: e.tensor_tensor(
                        out=yb[b][:].rearrange("p (h d) -> p h d", d=128),
                        in0=oacc[:, T, :].rearrange("p (h d) -> p h d", d=128),
                        in1=st3[:, T, 0:4].unsqueeze(2).to_broadcast([128, 4, 128]), op=ALU.mult),
                        reads=["rs3"], writes=[("yb", b)])
                    S.op("dve", lambda e, b=b: e.tensor_tensor(out=yb[b][:], in0=yb[b][:], in1=hgnb[:], op=ALU.mult),
                         reads=[("yb", b), "hgnb"], writes=[("yb", b)])
                    S.op("dve", lambda e, b=b: e.tensor_tensor(out=yt[b][:], in0=yb[b][:], in1=sg[b][:], op=ALU.mult),
                         reads=[("yb", b), ("sg", b)], writes=[("yt", b)])
                    for j in range(4):
                        S.op("pe", lambda e, j=j, b=b: e.transpose(pT[b][:, j * 128:(j + 1) * 128],
                                                                   yt[b][:, j * 128:(j + 1) * 128], identb[:]),
                             reads=[("yt", b)], writes=[("pT", b)])
                    S.op("act", lambda e, T=T, b=b: e.activation(
                        out=mixT[:, 4:8, T * 128:(T + 1) * 128],
                        in_=pT[b][:, 0:512].rearrange("p (j t) -> p j t", t=128), func=AF.Copy),
                        reads=[("pT", b)], writes=[("mixhg", T)])
                if debug:
                    S.dma("sp", lambda e: e.dma_start(out=dbg["mixT"], in_=mixT[:].rearrange("p k t -> p (k t)")),
                          reads=[("mixhg", T) for T in range(NT)])
                phase_end("p3")

        with contextlib.ExitStack() as p4:
            wo32 = sb("wo32", [128, 8, D], stack=p4)
            wob = sb("wob", [128, 8, D], BF16, stack=p4)
            g1b = sb("g1b", [128, D], stack=p4)
            S.dma("sp", lambda e: e.dma_start(out=g1b[:], in_=scr_bc[0]), writes=["g1b"])
            for k in range(8):
                S.dma("sp" if k % 2 == 0 else "act", lambda e, k=k: e.dma_start(
                    out=wo32[:, k, :], in_=w_out[k * 128:(k + 1) * 128, :]), writes=[("wo32", k)])
                S.op("dve" if k % 2 == 0 else "pool", lambda e, k=k: e.tensor_tensor(
                    out=wob[:, k, :], in0=wo32[:, k, :], in1=g1b[:], op=ALU.mult),
                    reads=[("wo32", k), "g1b"], writes=[("wob", k)])
            wob_all = [("wob", k) for k in range(8)]
            for T in range(NT):
                S.dma("sp" if T % 2 == 0 else "act", lambda e, T=T: e.dma_start(
                    out=x1[:, T, :], in_=x[T * 128:(T + 1) * 128, :]), writes=[("x1", T)])
                for hf in range(2):
                    pb = (2 * T + hf) % 4
                    for k in range(8):
                        S.op("pe", lambda e, k=k, T=T, hf=hf, pb=pb: e.matmul(
                            ps[pb][:, :], mixT[:, k, T * 128:(T + 1) * 128], wob[:, k, hf * 512:(hf + 1) * 512],
                            start=(k == 0), stop=(k == 7)), reads=wob_all, writes=[("ps", pb)])
                    S.op("dve", lambda e, T=T, hf=hf, pb=pb: e.tensor_tensor(
                        out=x1[:, T, hf * 512:(hf + 1) * 512], in0=ps[pb][:, :], in1=x1[:, T, hf * 512:(hf + 1) * 512],
                        op=ALU.add), reads=[("ps", pb), ("x1", T)], writes=[("x1", T)])
            if debug:
                S.dma("sp", lambda e: e.dma_start(out=dbg["x1"], in_=R[:]),
                      reads=[("x1", T) for T in range(NT)])
            phase_end("p4")

    eidx = sb("eidx", [128, NT, 128], I32)
    gw = sb("gw", [128, NT, 128])
    rs2 = sb("rs2", [128, NT])
    with contextlib.ExitStack() as p5:
        wqb = sb("wqb", [128, 8, 2048], BF16, stack=p5)
        kTb = sb("kTb", [128, 16, 128], BF16, stack=p5)
        junk = sb("junk5", [128, D], BF16, stack=p5)
        xs = [sb(f"xs5{j}", [128, D], BF16, stack=p5) for j in range(2)]
        h2T = [sb(f"h2T{j}", [128, 8, 128], BF16, stack=p5) for j in range(2)]
        qTp = [sb(f"qTp{j}", [128, 16, 128], BF16, stack=p5) for j in range(2)]
        ssb = sb("ssb", [128, 16, 128], stack=p5)
        s2 = sb("s2", [128, 16, 128], stack=p5)
        top = sb("top", [128, 16, 16], stack=p5)
        itop = sb("itop", [128, 16, 16], I32, stack=p5)
        ic = sb("ic", [128, 388], I32, stack=p5)
        itf = sb("itf", [128, 16, 16], stack=p5)
        cand = sb("cand", [128, 8, 256], stack=p5)
        cand2 = sb("cand2", [128, 8, 256], stack=p5)
        ctop = sb("ctop", [128, 8, 16], stack=p5)
        cpos = sb("cpos", [128, 8, 16], I32, stack=p5)
        paf = sb("paf", [128, 128], stack=p5)
        pai = sb("pai", [128, 128], I32, stack=p5)
        pbf = sb("pbf", [128, 128], stack=p5)
        oh = sb("oh", [128, 128, 16], stack=p5)
        selA = sb("selA", [128, 128], stack=p5)
        selB = sb("selB", [128, 128], stack=p5)
        ef = sb("ef", [128, 128], stack=p5)
        ee = sb("ee", [128, 8, 16], stack=p5)
        zz = sb("zz", [128, 16], stack=p5)
        st5 = sb("st5", [128, 2 * NT], stack=p5)

        stg = [sb(f"stg{j}", [128, 4, D], BF16, stack=p5) for j in range(2)]
        conv_steps = [(tab, c) for tab in range(2) for c in range(32)]

        def emit_conv(si):
            tab, c = conv_steps[si]
            src = (u_t, v_t)[tab].rearrange("(p j c) d -> c p j d", p=128, j=4, c=32)[c]
            dst = uv_bf.rearrange("(p j c) d -> c p j d", p=128, j=4, c=32)[c][:, :, tab * D:(tab + 1) * D]
            b = si % 2
            S.dma("pool", lambda e: e.dma_start(out=stg[b][:], in_=src), writes=[("stg", b)])
            S.dma("sp", lambda e: e.dma_start(out=dst, in_=stg[b][:]), reads=[("stg", b)], writes=[("tab", tab, c)])

        wkq = load_w(wqb[:, :, 0:1024], wq, 0, 1024, "wqa") + load_w(wqb[:, :, 1024:2048], wq, 1024, 1024, "wqb")
        S.dma("pool", lambda e: e.dma_start(out=kTb[:].rearrange("p c k -> p (c k)"), in_=keysT), writes=["kTb"])
        S.dma("sp", lambda e: e.dma_start(out=ic[:], in_=icst), writes=["ic"])
        for T in range(NT):
            S.op("act", lambda e, T=T: e.activation(out=junk[:], in_=x1[:, T, :], func=AF.Square,
                                                    accum_out=st5[:, T:T + 1]), reads=[], writes=["junk5", ("ssq5", T)])
        S.op("dve", lambda e: e.tensor_scalar(out=st5[:, NT:2 * NT], in0=st5[:, 0:NT],
                                              scalar1=1.0 / D, scalar2=EPS, op0=ALU.mult, op1=ALU.add),
             reads=[("ssq5", T) for T in range(NT)], writes=["ms5"])
        S.op("act", lambda e: e.activation(out=st5[:, NT:2 * NT], in_=st5[:, NT:2 * NT], func=AF.Sqrt),
             reads=["ms5"], writes=["ms5"])
        S.op("dve", lambda e: e.reciprocal(out=rs2[:, :], in_=st5[:, NT:2 * NT]), reads=["ms5"], writes=["rs2"])
        for T in range(NT):
            b = T % 2
            if not _NOCONV:
                for q_ in range(4):
                    emit_conv(4 * T + q_)
            S.op("act", lambda e, b=b, T=T: e.activation(out=xs[b][:], in_=x1[:, T, :], func=AF.Copy,
                                                         scale=rs2[:, T:T + 1]), reads=["rs2"], writes=[("xs5", b)])
            for k in range(8):
                S.op("pe", lambda e, b=b, k=k: e.transpose(pT[b][:, k * 128:(k + 1) * 128],
                                                           xs[b][:, k * 128:(k + 1) * 128], identb[:]),
                     reads=[("xs5", b)], writes=[("pT", b)])
            for k in range(8):
                S.op("dve" if k % 2 == 0 else "act", (lambda e, b=b, k=k: e.tensor_scalar(
                    out=h2T[b][:, k, :], in0=pT[b][:, k * 128:(k + 1) * 128],
                    scalar1=mc[:, 32 + k:33 + k], scalar2=mc[:, 40 + k:41 + k], op0=ALU.mult, op1=ALU.add))
                    if k % 2 == 0 else (lambda e, b=b, k=k: e.activation(
                        out=h2T[b][:, k, :], in_=pT[b][:, k * 128:(k + 1) * 128], func=AF.Identity,
                        scale=mc[:, 32 + k:33 + k], bias=mc[:, 40 + k:41 + k])),
                    reads=[("pT", b)], writes=[("h2T", b)])
            for g4 in range(4):
                pb = g4
                for j in range(4):
                    pc = g4 * 4 + j
                    for k in range(8):
                        S.op("pe", lambda e, b=b, k=k, pc=pc, j=j, pb=pb: e.matmul(
                            ps[pb][:, j * 128:(j + 1) * 128], wqb[:, k, pc * 128:(pc + 1) * 128], h2T[b][:, k, :],
                            start=(k == 0), stop=(k == 7)), reads=wkq + [("h2T", b)], writes=[("ps", pb)])
                if g4 % 2 == 0:
                    S.op("act", lambda e, b=b, g4=g4, pb=pb: e.activation(
                        out=qTp[b][:, g4 * 4:(g4 + 1) * 4, :], in_=ps[pb][:, :].rearrange("p (j t) -> p j t", t=128),
                        func=AF.Copy), reads=[("ps", pb)], writes=[("qTp", b, g4)])
                else:
                    S.op("dve", lambda e, b=b, g4=g4, pb=pb: e.tensor_copy(
                        out=qTp[b][:, g4 * 4:(g4 + 1) * 4, :], in_=ps[pb][:, :].rearrange("p (j t) -> p j t", t=128)),
                        reads=[("ps", pb)], writes=[("qTp", b, g4)])
            for g4 in range(4):
                pb = 4 + (g4 % 2)
                for j in range(4):
                    pc = g4 * 4 + j
                    S.op("pe", lambda e, b=b, pc=pc, j=j, pb=pb: e.matmul(
                        ps[pb][:, j * 128:(j + 1) * 128], qTp[b][:, pc, :], kTb[:, pc, :], start=True, stop=True),
                        reads=[("qTp", b, g4), "kTb"], writes=[("ps", pb)])
                S.op("act", lambda e, g4=g4, pb=pb: e.activation(
                    out=ssb[:, g4 * 4:(g4 + 1) * 4, :], in_=ps[pb][:, :].rearrange("p (j t) -> p j t", t=128),
                    func=AF.Copy), reads=[("ps", pb)], writes=[("ssb", g4)])
            ssb_keys = [("ssb", g) for g in range(4)]
            ssbi = ssb[:].bitcast(I32)
            S.op("dve", lambda e: e.scalar_tensor_tensor(
                out=ssbi, in0=ssbi, scalar=ic[:, 384:385], in1=ic[:, 0:128].unsqueeze(1).to_broadcast([128, 16, 128]),
                op0=ALU.bitwise_and, op1=ALU.bitwise_or), reads=ssb_keys + ["ic"], writes=ssb_keys)
            for pc in range(16):
                S.op("dve", lambda e, pc=pc: e.max(out=top[:, pc, 0:8], in_=ssb[:, pc, :]),
                     reads=[("ssb", pc // 4)], writes=[("top", pc, 0)])
            for pc in range(16):
                S.op("dve", lambda e, pc=pc: e.match_replace(out=s2[:, pc, :], in_to_replace=top[:, pc, 0:8],
                                                             in_values=ssb[:, pc, :], imm_value=NEG),
                     reads=[("ssb", pc // 4), ("top", pc, 0)], writes=[("s2", pc)])
            for pc in range(16):
                S.op("dve", lambda e, pc=pc: e.max(out=top[:, pc, 8:16], in_=s2[:, pc, :]),
                     reads=[("s2", pc)], writes=[("top", pc, 1)])
            tops = [("top", pc, j) for pc in range(16) for j in range(2)]
            S.op("dve", lambda e: e.tensor_tensor(out=itop[:], in0=top[:].bitcast(I32),
                                                  in1=ic[:, 386:387].unsqueeze(2).to_broadcast([128, 16, 16]),
                                                  op=ALU.bitwise_and), reads=tops + ["ic"], writes=["itop"])
            S.op("dve", lambda e: e.tensor_copy(out=itf[:], in_=itop[:]), reads=["itop"], writes=["itf"])
            topv = top[:].rearrange("p (h c) a -> p h c a", c=2)
            S.op("dve", lambda e: e.tensor_tensor(
                out=cand[:].rearrange("p h (a b) -> p h a b", b=16),
                in0=topv[:, :, 0, :].unsqueeze(3).to_broadcast([128, 8, 16, 16]),
                in1=topv[:, :, 1, :].unsqueeze(2).to_broadcast([128, 8, 16, 16]), op=ALU.add),
                reads=tops, writes=["cand"])
            candi = cand[:].bitcast(I32)
            S.op("dve", lambda e: e.scalar_tensor_tensor(
                out=candi, in0=candi, scalar=ic[:, 385:386], in1=ic[:, 128:384].unsqueeze(1).to_broadcast([128, 8, 256]),
                op0=ALU.bitwise_and, op1=ALU.bitwise_or), reads=["cand", "ic"], writes=["cand"])
            for p in range(8):
                S.op("dve", lambda e, p=p: e.max(out=ctop[:, p, 0:8], in_=cand[:, p, :]), reads=["cand"], writes=[("ctop", p, 0)])
            for p in range(8):
                S.op("dve", lambda e, p=p: e.match_replace(out=cand2[:, p, :], in_to_replace=ctop[:, p, 0:8],
                                                           in_values=cand[:, p, :], imm_value=NEG),
                     reads=["cand", ("ctop", p, 0)], writes=[("cand2", p)])
            for p in range(8):
                S.op("dve", lambda e, p=p: e.max(out=ctop[:, p, 8:16], in_=cand2[:, p, :]), reads=[("cand2", p)], writes=[("ctop", p, 1)])
            ctops = [("ctop", p, j) for p in range(8) for j in range(2)]
            S.op("dve", lambda e: e.tensor_tensor(out=cpos[:], in0=ctop[:].bitcast(I32),
                                                  in1=ic[:, 387:388].unsqueeze(2).to_broadcast([128, 8, 16]),
                                                  op=ALU.bitwise_and), reads=ctops + ["ic"], writes=["cpos"])
            cposs = ["cpos"]
            cposf = cpos[:].rearrange("p h j -> p (h j)")
            S.op("dve", lambda e: e.tensor_copy(out=selA[:], in_=cposf), reads=cposs + [("sel", 0)], writes=["posf"])
            S.op("dve", lambda e: e.tensor_scalar(out=pbf[:], in0=selA[:], scalar1=0.0625, scalar2=None, op0=ALU.mult),
                 reads=["posf"], writes=["pbf"])
            S.op("dve", lambda e: e.tensor_copy(out=pai[:], in_=pbf[:]), reads=["pbf"], writes=["pai"])
            S.op("dve", lambda e: e.tensor_copy(out=paf[:], in_=pai[:]), reads=["pai"], writes=["paf"])
            S.op("dve", lambda e: e.scalar_tensor_tensor(out=pbf[:], in0=paf[:], scalar=16.0, in1=selA[:],
                                                         op0=ALU.mult, op1=ALU.is_gt), reads=["paf", "posf", "pai"], writes=["pbf"])
            S.op("dve", lambda e: e.tensor_tensor(out=paf[:], in0=paf[:], in1=pbf[:], op=ALU.subtract),
                 reads=["paf", "pbf"], writes=["paf"])
            S.op("dve", lambda e: e.scalar_tensor_tensor(out=pbf[:], in0=paf[:], scalar=-16.0, in1=selA[:],
                                                         op0=ALU.mult, op1=ALU.add), reads=["paf", "posf"], writes=["pbf"])
            itv = itf[:].rearrange("p (h c) a -> p h c a", c=2)
            for which, pf, sel in ((0, paf, selA), (1, pbf, selB)):
                S.op("dve", lambda e, pf=pf: e.tensor_tensor(
                    out=oh[:], in0=pf[:].unsqueeze(2).to_broadcast([128, 128, 16]),
                    in1=IOTA16.unsqueeze(1).to_broadcast([128, 128, 16]), op=ALU.is_equal),
                    reads=["paf", "pbf", "cA", "oh"], writes=["oh"])
                S.op("dve", lambda e, which=which: e.tensor_tensor(
                    out=oh[:].rearrange("p (h j) a -> p h j a", j=16),
                    in0=oh[:].rearrange("p (h j) a -> p h j a", j=16),
                    in1=itv[:, :, which, :].unsqueeze(2).to_broadcast([128, 8, 16, 16]), op=ALU.mult),
                    reads=["oh", "itf"], writes=["oh"])
                S.op("dve", lambda e, sel=sel: e.tensor_reduce(out=sel[:], in_=oh[:], axis=AX.X, op=ALU.add),
                     reads=["oh", "posf"], writes=[("sel", which)])
            S.op("dve", lambda e: e.scalar_tensor_tensor(out=ef[:], in0=selA[:], scalar=128.0, in1=selB[:],
                                                         op0=ALU.mult, op1=ALU.add),
                 reads=[("sel", 0), ("sel", 1)], writes=["ef"])
            S.op("dve", lambda e, T=T: e.tensor_copy(out=eidx[:, T, :], in_=ef[:]), reads=["ef"], writes=[("eidx", T)])
            S.op("dve", lambda e: e.tensor_tensor(out=ee[:], in0=ctop[:], in1=ctop[:, :, 0:1].to_broadcast([128, 8, 16]),
                                                  op=ALU.subtract), reads=ctops, writes=["ee"])
            S.op("act", lambda e: e.activation(out=ee[:], in_=ee[:], func=AF.Exp), reads=["ee"], writes=["ee"])
            S.op("dve", lambda e: e.tensor_reduce(out=zz[:, 0:8], in_=ee[:], axis=AX.X, op=ALU.add),
                 reads=["ee"], writes=["zz"])
            S.op("dve", lambda e: e.reciprocal(out=zz[:, 8:16], in_=zz[:, 0:8]), reads=["zz"], writes=["zz2"])
            S.op("dve", lambda e, T=T: e.tensor_tensor(
                out=gw[:, T, :].rearrange("p (h j) -> p h j", j=16), in0=ee[:],
                in1=zz[:, 8:16].unsqueeze(2).to_broadcast([128, 8, 16]), op=ALU.mult),
                reads=["ee", "zz2"], writes=[("gw", T)])
        if debug:
            S.dma("sp", lambda e: e.dma_start(out=dbg["eidx"], in_=eidx[:].rearrange("p t s -> p (t s)")),
                  reads=[("eidx", T) for T in range(NT)])
            S.dma("sp", lambda e: e.dma_start(out=dbg["gw"], in_=gw[:].rearrange("p t s -> p (t s)")),
                  reads=[("gw", T) for T in range(NT)])
        phase_end("p5a")

    with contextlib.ExitStack() as p6:
        NB = 12
        ring = [sb(f"ring{j}", [128, 2 * D], BF16, stack=p6) for j in range(NB)]
        NDG = 6
        dg = [sb(f"dg{j}", [128, 128], BF16, stack=p6) for j in range(NDG)]
        bc = sb("bc5", [128, 4, D], stack=p6)
        h2 = [sb(f"h2_{j}", [128, D], stack=p6) for j in range(2)]
        junk = sb("junk6", [128, D], BF16, stack=p6)
        accs = sb("accs", [128, D], stack=p6)
        aa = [sb(f"aa{j}", [128, 128], stack=p6) for j in range(2)]
        gl = [sb(f"gl{j}", [128, 128], stack=p6) for j in range(2)]
        ww = [sb(f"ww{j}", [128, 128], stack=p6) for j in range(2)]
        st6 = sb("st6", [128, 2 * NT], stack=p6)
        S.dma("sp", lambda e: e.dma_start(out=bc[:, 0, :], in_=scr_bc[1]), writes=[("bc", 0)])
        S.dma("sp", lambda e: e.dma_start(out=bc[:, 1, :], in_=scr_bc[2]), writes=[("bc", 1)])
        S.dma("sp", lambda e: e.dma_start(out=bc[:, 2, :], in_=scr_bc[3]), writes=[("bc", 2)])
        S.dma("sp", lambda e: e.dma_start(out=bc[:, 3, :], in_=nfb), writes=[("bc", 3)])
        gi = gd = 0
        for T in range(NT):
            pu = T % 2
            S.op("dve", lambda e, T=T, pu=pu: e.scalar_tensor_tensor(out=h2[pu][:], in0=x1[:, T, :], scalar=rs2[:, T:T + 1],
                                                                     in1=bc[:, 1, :], op0=ALU.mult, op1=ALU.mult),
                 reads=[("bc", 1)], writes=[("h2", pu)])
            S.op("dve", lambda e, pu=pu: e.tensor_tensor(out=h2[pu][:], in0=h2[pu][:], in1=bc[:, 2, :], op=ALU.add),
                 reads=[("h2", pu), ("bc", 2)], writes=[("h2", pu)])
            S.op("dve", lambda e, pu=pu: e.memset(aa[pu][:], 0.0), writes=[("aa", pu)])
            for s_ in range(128):
                r = gi % NB
                gi += 1
                S.dma("pool", lambda e, T=T, s_=s_, r=r: e.indirect_dma_start(
                    out=ring[r][:], out_offset=None, in_=uv_bf,
                    in_offset=bass.IndirectOffsetOnAxis(ap=eidx[:, T, s_:s_ + 1], axis=0)),
                    reads=[], writes=[("ring", r)])
                S.op("dve", lambda e, s_=s_, r=r, pu=pu: e.scalar_tensor_tensor(
                    out=junk[:], in0=ring[r][:, 0:D], scalar=1.0, in1=h2[pu][:], op0=ALU.mult, op1=ALU.mult,
                    accum_out=aa[pu][:, s_:s_ + 1]), reads=[("ring", r), ("h2", pu), ("aa", pu)],
                    writes=["junk6", ("aas", pu, s_)])
                S.op("act", lambda e, s_=s_, pu=pu: e.activation(out=gl[pu][:, s_:s_ + 1], in_=aa[pu][:, s_:s_ + 1], func=AF.Gelu),
                     reads=[("aas", pu, s_)], writes=[("gl", pu, s_)])
                S.op("act", lambda e, T=T, s_=s_, pu=pu: e.activation(out=ww[pu][:, s_:s_ + 1], in_=gl[pu][:, s_:s_ + 1],
                                                                     func=AF.Copy, scale=gw[:, T, s_:s_ + 1]),
                     reads=[("gl", pu, s_)], writes=[("ww", pu, s_)])
                dj = gd % NDG
                gd += 1
                S.op("act", lambda e, s_=s_, dj=dj, pu=pu: e.activation(
                    out=dg[dj][:], in_=identb[:], func=AF.Copy, scale=ww[pu][:, s_:s_ + 1]),
                    reads=[("ww", pu, s_)], writes=[("dg", dj)])
                for hf in range(2):
                    S.op("pe", lambda e, s_=s_, dj=dj, r=r, pu=pu, hf=hf: e.matmul(
                        ps[2 * pu + hf][:, :], dg[dj][:], ring[r][:, D + hf * 512: D + (hf + 1) * 512],
                        start=(s_ == 0), stop=(s_ == 127)),
                        reads=[("dg", dj), ("ring", r)], writes=[("accP", pu, hf)])
            for hf in range(2):
                S.op("dve", lambda e, pu=pu, hf=hf: e.tensor_tensor(
                    out=accs[:, hf * 512:(hf + 1) * 512], in0=ps[2 * pu + hf][:, :], in1=bc[:, 0, hf * 512:(hf + 1) * 512],
                    op=ALU.mult), reads=[("accP", pu, hf), ("bc", 0)], writes=[("accs", hf)])
            S.op("dve", lambda e, T=T: e.tensor_tensor(out=accs[:], in0=accs[:], in1=x1[:, T, :], op=ALU.add),
                 reads=[("accs", 0), ("accs", 1)], writes=["accsum"])
            S.op("act", lambda e, T=T: e.activation(out=junk[:], in_=accs[:], func=AF.Square, accum_out=st6[:, T:T + 1]),
                 reads=["accsum"], writes=["junk6", ("ssq6", T)])
            S.op("dve", lambda e, T=T: e.tensor_scalar(out=st6[:, NT + T:NT + T + 1], in0=st6[:, T:T + 1],
                                                       scalar1=1.0 / D, scalar2=EPS, op0=ALU.mult, op1=ALU.add),
                 reads=[("ssq6", T)], writes=[("ms6", T)])
            S.op("act", lambda e, T=T: e.activation(out=st6[:, NT + T:NT + T + 1], in_=st6[:, NT + T:NT + T + 1],
                                                    func=AF.Sqrt), reads=[("ms6", T)], writes=[("ms6", T)])
            S.op("dve", lambda e, T=T: e.reciprocal(out=st6[:, T:T + 1], in_=st6[:, NT + T:NT + T + 1]),
                 reads=[("ms6", T)], writes=[("rs6", T)])
            S.op("dve", lambda e, T=T: e.scalar_tensor_tensor(out=x1[:, T, :], in0=accs[:], scalar=st6[:, T:T + 1],
                                                              in1=bc[:, 3, :], op0=ALU.mult, op1=ALU.mult),
                 reads=["accsum", ("rs6", T), ("bc", 3)], writes=[("xo", T), ("accs", 0), ("accs", 1)])
            S.dma("sp", lambda e, T=T: e.dma_start(out=out[T * 128:(T + 1) * 128, :], in_=x1[:, T, :]),
                  reads=[("xo", T)], writes=[("out", T)])
        phase_end("p5b")
    S.finish()
    es.close()
    return nc


def _col(v):
    return np.ascontiguousarray(np.asarray(v, np.float32).reshape(8, 128).T)


def make_inputs(inp):
    global _BIAS_IDX
    f = lambda a: np.ascontiguousarray(np.asarray(a, dtype=np.float32))
    if _BIAS_IDX is None:
        _BIAS_IDX = build_bias_index()
    rpb = f(inp["na_rpb"])[0]
    ext = np.concatenate([rpb.reshape(8, -1), np.full((8, 1), MASKV, np.float32)], axis=1)
    biasT = np.stack([ext[h][_BIAS_IDX] for h in range(8)], axis=1)
    cstm = np.zeros((128, 528), np.float32)
    cstm[:, 0:128] = np.eye(128, dtype=np.float32)
    sidx = np.arange(128)
    blk = (sidx[:, None] // 64) == (sidx[None, :] // 64)
    cstm[:, 128:256] = (blk & (sidx[:, None] <= sidx[None, :])).astype(np.float32)
    cstm[:, 256:384] = (blk & (sidx[:, None] >= sidx[None, :])).astype(np.float32)
    cstm[:, 384:512] = 1.0
    cstm[:, 512:528] = np.arange(16, dtype=np.float32)[None, :]
    icm = np.zeros((128, 388), np.int32)
    icm[:, 0:128] = np.arange(128, dtype=np.int32)[None, :]
    icm[:, 128:384] = np.arange(256, dtype=np.int32)[None, :]
    icm[:, 384] = -128
    icm[:, 385] = -256
    icm[:, 386] = 127
    icm[:, 387] = 255
    rmask = np.ones((128, TOK), np.float32)
    rmask[:, ::64] = 0.0
    c_ctx = f(inp["c_ctx"])
    hg_lb = f(inp["hg_lb"])
    lbraw = np.ascontiguousarray(hg_lb.reshape(2, 2, 4, 128).transpose(3, 0, 1, 2).reshape(128, 16))
    keys = f(inp["peer_keys"])[0]
    keysT = np.ascontiguousarray(keys.transpose(3, 0, 1, 2).reshape(128, 16 * 128))
    shared = dict(
        w_mod=f(inp["w_mod"])[0], b_mod=f(inp["b_mod"])[0].reshape(1, -1),
        n1c=_col(f(inp["norm1"])[0]), n2c=_col(f(inp["norm2"])[0]),
        n2b=np.ascontiguousarray(np.broadcast_to(f(inp["norm2"])[0][None, :], (128, D))),
        nfb=np.ascontiguousarray(np.broadcast_to(f(inp["norm_f"])[None, :], (128, D))),
        w_in=f(inp["w_in"])[0], w_out=f(inp["w_out"])[0], wq=f(inp["peer_wq"])[0],
        keysT=keysT, u=f(inp["peer_u"])[0], v=f(inp["peer_v"])[0], lbraw=lbraw,
        hgn=np.ascontiguousarray(np.broadcast_to(f(inp["hg_norm"])[0][None, :], (128, 512))),
        biasT=np.ascontiguousarray(biasT.reshape(128, -1)), cst=cstm, rmask=rmask, icst=icm,
    )
    xs = f(inp["x"]); cs = f(inp["c"]); ctxs = f(inp["ctx"])
    maps = []
    for b in range(xs.shape[0]):
        cc = np.stack([_col(cs[b]), _col(c_ctx)], axis=2).reshape(128, 16)
        m = dict(shared)
        m.update(x=xs[b], ctx=ctxs[b], ccol=np.ascontiguousarray(cc))
        maps.append(m)
    return maps


def kernel(**inputs):
    maps = make_inputs(inputs)
    nc = build()
    res = run_bass_kernel_spmd(nc, maps, core_ids=list(range(len(maps))))
    return np.stack([np.asarray(r["out"], dtype=np.float32) for r in res.results], axis=0)
```

```python
import contextlib
import numpy as np
import concourse.bass as bass
import concourse.mybir as mybir
from concourse.bass_utils import run_bass_kernel_spmd

F32 = mybir.dt.float32
BF16 = mybir.dt.bfloat16
I32 = mybir.dt.int32
U32 = mybir.dt.uint32
ALU = mybir.AluOpType
AF = mybir.ActivationFunctionType
AX = mybir.AxisListType

D = 1024
SEQ = 2048
CTX = 256
NT = 16
NTT = 18
TOK = SEQ + CTX
NCH = TOK // 64
EPS = 1e-6
MASKV = -30000.0
NEG = -1.0e30


class Sched:
    COMPUTE = ("pe", "dve", "act", "pool")

    def __init__(self, nc, n_dsem=None):
        self.nc = nc
        self.engs = {"pe": nc.tensor, "dve": nc.vector, "act": nc.scalar,
                     "pool": nc.gpsimd, "sp": nc.sync}
        self.n_dsem = n_dsem or {"sp": 8, "act": 4, "pool": 16}
        self.es = contextlib.ExitStack()
        self.csem = {e: self.es.enter_context(nc.semaphore("cs_" + e)) for e in self.COMPUTE}
        self.dsem = {q: [self.es.enter_context(nc.semaphore(f"ds_{q}{j}")) for j in range(n)]
                     for q, n in self.n_dsem.items()}
        self.ccount = {e: 0 for e in self.COMPUTE}
        self.dcount = {q: 0 for q in self.n_dsem}
        self.clock = {e: {} for e in self.engs}
        self.bar_sig = 0
        self.bar_clock = {}
        self.bar_tile = None
        self.ops = []
        self.last_writer = {}
        self.readers = {}
        self.total_ops = 0

    def op(self, eng, fn, reads=(), writes=(), dma=False):
        deps = set()
        for r in reads:
            w = self.last_writer.get(r)
            if w is not None:
                deps.add(w)
        for w_ in writes:
            w = self.last_writer.get(w_)
            if w is not None:
                deps.add(w)
            for rd in self.readers.get(w_, ()):
                deps.add(rd)
        i = len(self.ops)
        deps.discard(i)
        self.ops.append(dict(eng=eng, fn=fn, deps=deps, dma=dma))
        for r in reads:
            self.readers.setdefault(r, []).append(i)
        for w_ in writes:
            self.last_writer[w_] = i
            self.readers[w_] = []
        return i

    def dma(self, q, fn, reads=(), writes=()):
        return self.op(q, fn, reads, writes, dma=True)

    def _wait(self, E, key, sem, val):
        ck = self.clock[E]
        if ck.get(key, 0) < val:
            self.engs[E].wait_ge(sem, val)
            ck[key] = val

    def _merge(self, E, clk):
        ck = self.clock[E]
        for k, v in clk.items():
            if ck.get(k, 0) < v:
                ck[k] = v

    def flush(self, barrier=True):
        ops = self.ops
        need_sig = [False] * len(ops)
        for i, o in enumerate(ops):
            for d in o["deps"]:
                od = ops[d]
                if od["dma"]:
                    continue
                if od["eng"] == "pe" and o["eng"] == "pe" and not o["dma"]:
                    continue
                need_sig[d] = True
        if barrier:
            last = {}
            for i, o in enumerate(ops):
                if not o["dma"]:
                    last[o["eng"]] = i
            for e, i in last.items():
                need_sig[i] = True
        for i, o in enumerate(ops):
            E = o["eng"]
            eng = self.engs[E]
            if self.bar_sig:
                self._wait(E, ("c", "dve"), self.csem["dve"], self.bar_sig)
                self._merge(E, self.bar_clock)
            for d in sorted(o["deps"]):
                od = ops[d]
                if od["dma"]:
                    q = od["eng"]
                    self._wait(E, ("d", q, od["dsem_idx"]), self.dsem[q][od["dsem_idx"]], od["dval"])
                else:
                    F = od["eng"]
                    if F == "pe" and E == "pe" and not o["dma"]:
                        continue
                    self._wait(E, ("c", F), self.csem[F], od["sig"])
                self._merge(E, od["clk"])
            if o["dma"]:
                n = self.n_dsem[E]
                j = self.dcount[E] % n
                prev = self.dcount[E] // n
                if prev > 0:
                    self._wait(E, ("d", E, j), self.dsem[E][j], 16 * prev)
                ins = o["fn"](eng)
                ins.then_inc(self.dsem[E][j], 16)
                o["dsem_idx"] = j
                o["dval"] = 16 * (prev + 1)
                self.dcount[E] += 1
                o["clk"] = dict(self.clock[E])
            else:
                ins = o["fn"](eng)
                if need_sig[i]:
                    self.ccount[E] += 1
                    ins.then_inc(self.csem[E], 1)
                    o["sig"] = self.ccount[E]
                else:
                    o["sig"] = None
                o["clk"] = dict(self.clock[E])
            o["fn"] = None
        self.total_ops += len(ops)
        if barrier:
            self._barrier()
        self.ops = []
        self.last_writer = {}
        self.readers = {}

    def _wait_all(self, E):
        for q, n in self.n_dsem.items():
            for j in range(n):
                uses = (self.dcount[q] - j + n - 1) // n if self.dcount[q] > j else 0
                if uses > 0:
                    self._wait(E, ("d", q, j), self.dsem[q][j], 16 * uses)
        for e in self.COMPUTE:
            if self.ccount[e] > 0:
                self._wait(E, ("c", e), self.csem[e], self.ccount[e])

    def _barrier(self):
        self._wait_all("dve")
        ins = self.engs["dve"].memset(self.bar_tile, 0.0)
        self.ccount["dve"] += 1
        ins.then_inc(self.csem["dve"], 1)
        self.bar_sig = self.ccount["dve"]
        self.bar_clock = dict(self.clock["dve"])

    def finish(self, eng="sp"):
        self.flush(barrier=True)
        self._wait_all(eng)
        self.es.close()


NA_VARIANTS = [(-2, True), (-1, False), (0, False), (1, False), (2, True),
               (-3, False), (-2, False), (2, False), (3, False)]


def na_chunks(i):
    if i in (0, 1, 14, 15):
        cs = range(0, 4) if i < 2 else range(12, 16)
        out = []
        for c in cs:
            d = c - i
            t = {(-3): 5, (-2): 6, (-1): 1, 0: 2, 1: 3, 2: 7, 3: 8}[d]
            out.append((c, t))
        return out
    return [(i + d, d + 2) for d in range(-2, 3)]


def build_bias_index():
    idx = np.full((128, 9, 128), 15 * 31, dtype=np.int64)
    for t, (d, partial) in enumerate(NA_VARIANTS):
        for j in range(2):
            for jq in range(2):
                dr = 2 * d + j - jq
                if abs(dr) > 7:
                    continue
                if partial:
                    if d == -2 and not (j >= jq):
                        continue
                    if d == 2 and not (j == 0 and jq == 1):
                        continue
                for cq in range(64):
                    cstart = min(max(cq - 8, 0), 48)
                    for ck in range(cstart, cstart + 16):
                        idx[j * 64 + ck, t, jq * 64 + cq] = (dr + 7) * 31 + (ck - cq + 15)
    return idx


_BIAS_IDX = None


import os as _os
_NOCONV = bool(_os.environ.get('NOCONV'))


class _Stop(Exception):
    pass


def build(debug=False, stop=None):
    nc = bass.Bass("TRN2", target_bir_lowering=False)
    try:
        return _build(nc, debug, stop)
    except _Stop:
        return nc


def _build(nc, debug, stop):

    def din(name, shape, dt=F32):
        return nc.dram_tensor(name, shape, dt, kind="ExternalInput").ap()

    x = din("x", [SEQ, D])
    ctx = din("ctx", [CTX, D])
    ccol = din("ccol", [128, 16])
    w_mod = din("w_mod", [D, 6 * D])
    b_mod = din("b_mod", [1, 6 * D])
    n1c = din("n1c", [128, 8])
    n2c = din("n2c", [128, 8])
    n2b = din("n2b", [128, D])
    nfb = din("nfb", [128, D])
    w_in = din("w_in", [D, 4096])
    w_out = din("w_out", [D, D])
    wq = din("wq", [D, 2048])
    keysT = din("keysT", [128, 2048])
    u_t = din("u", [16384, D])
    v_t = din("v", [16384, D])
    lbraw = din("lbraw", [128, 16])
    hgn = din("hgn", [128, 512])
    biasT = din("biasT", [128, 8 * 9 * 128])
    cst = din("cst", [128, 528])
    rmask_d = din("rmask", [128, TOK])
    icst = din("icst", [128, 388], I32)
    out = nc.dram_tensor("out", [SEQ, D], F32, kind="ExternalOutput").ap()
    scr_bc = nc.dram_tensor("scr_bc", [4, 128, D], F32, kind="Internal").ap()
    uv_bf = nc.dram_tensor("uv_bf", [16384, 2 * D], BF16, kind="Internal").ap()
    dbg = {}
    if debug:
        dbg["hT"] = nc.dram_tensor("d_hT", [128, 8 * TOK], BF16, kind="ExternalOutput").ap()
        dbg["mixT"] = nc.dram_tensor("d_mixT", [128, 8 * SEQ], BF16, kind="ExternalOutput").ap()
        dbg["x1"] = nc.dram_tensor("d_x1", [128, NT * D], F32, kind="ExternalOutput").ap()
        dbg["mc"] = nc.dram_tensor("d_mc", [128, 48], F32, kind="ExternalOutput").ap()
        dbg["eidx"] = nc.dram_tensor("d_eidx", [128, NT * 128], I32, kind="ExternalOutput").ap()
        dbg["gw"] = nc.dram_tensor("d_gw", [128, NT * 128], F32, kind="ExternalOutput").ap()

    es = contextlib.ExitStack()

    def sb(name, shape, dt=F32, stack=None):
        return (stack or es).enter_context(nc.sbuf_tensor(name, shape, dt))

    bar = sb("bar", [128, 1])
    cA = sb("cA", [128, 528])
    identb = sb("identb", [128, 128], BF16)
    mc = sb("mc", [128, 48])
    lb = sb("lb", [128, 8])
    oml = sb("oml", [128, 8])
    ps = [es.enter_context(nc.psum_tensor(f"ps{j}", [128, 512], F32)) for j in range(6)]
    pT = [es.enter_context(nc.psum_tensor(f"pT{j}", [128, 1024], BF16)) for j in range(2)]

    S = Sched(nc)
    S.bar_tile = bar[:]

    def phase_end(name):
        S.flush()
        if stop == name:
            S.finish()
            raise _Stop()

    IDENT = cA[:, 0:128]
    TRIF = cA[:, 128:256]
    TRIB = cA[:, 256:384]
    ONES = cA[:, 384:512]
    IOTA16 = cA[:, 512:528]

    with contextlib.ExitStack() as p0:
        cc = sb("cc", [128, 16], stack=p0)
        scl = sb("scl", [128, 8, 33], BF16, stack=p0)
        wm = [sb(f"wm{j}", [128, 8, 512], stack=p0) for j in range(3)]
        wmb = [sb(f"wmb{j}", [128, 8, 512], BF16, stack=p0) for j in range(2)]
        bm = sb("bm", [33, 6 * D], stack=p0)
        modrow = sb("modrow", [33, 6 * D], stack=p0)
        mcol = sb("mcol", [128, 48], stack=p0)
        n1 = sb("n1", [128, 8], stack=p0)
        n2 = sb("n2", [128, 8], stack=p0)
        n2bt = sb("n2bt", [128, D], stack=p0)
        bct = [sb(f"bct{j}", [128, D], stack=p0) for j in range(2)]
        lbr = sb("lbr", [128, 16], stack=p0)

        S.dma("sp", lambda e: e.dma_start(out=cA[:], in_=cst), writes=["cA"])
        S.dma("sp", lambda e: e.dma_start(out=cc[:], in_=ccol), writes=["cc"])
        S.dma("sp", lambda e: e.dma_start(out=n1[:], in_=n1c), writes=["n1"])
        S.dma("sp", lambda e: e.dma_start(out=n2[:], in_=n2c), writes=["n2"])
        S.dma("sp", lambda e: e.dma_start(out=lbr[:], in_=lbraw), writes=["lbr"])
        S.dma("sp", lambda e: e.dma_start(out=n2bt[:], in_=n2b), writes=["n2bt"])
        S.op("dve", lambda e: e.memset(bm[:], 0.0), writes=["bm"])
        S.dma("sp", lambda e: e.dma_start(out=bm[0:1, :], in_=b_mod), reads=["bm"], writes=["bm0"])
        S.dma("sp", lambda e: e.dma_start(out=bm[32:33, :], in_=b_mod), reads=["bm"], writes=["bm32"])
        S.op("dve", lambda e: e.tensor_copy(out=identb[:], in_=IDENT), reads=["cA"], writes=["identb"])
        S.op("dve", lambda e: e.memset(scl[:], 0.0), writes=["scl"])
        ccv = cc[:].rearrange("p (k t) -> p k t", t=2)
        S.op("act", lambda e: e.activation(out=scl[:, :, 0:1], in_=ccv[:, :, 0:1], func=AF.Silu),
             reads=["cc", "scl"], writes=["scl"])
        S.op("act", lambda e: e.activation(out=scl[:, :, 32:33], in_=ccv[:, :, 1:2], func=AF.Silu),
             reads=["cc", "scl"], writes=["scl"])
        S.op("dve", lambda e: e.tensor_tensor(out=lb[:], in0=lbr[:, 0:8], in1=lbr[:, 8:16], op=ALU.subtract),
             reads=["lbr"], writes=["lb"])
        S.op("act", lambda e: e.activation(out=lb[:], in_=lb[:], func=AF.Sigmoid), reads=["lb"], writes=["lb"])
        S.op("dve", lambda e: e.tensor_scalar(out=oml[:], in0=lb[:], scalar1=-1.0, scalar2=1.0,
                                              op0=ALU.mult, op1=ALU.add), reads=["lb"], writes=["oml"])
        for n in range(12):
            wb = wm[n % 3]
            S.dma("sp" if n % 2 == 0 else "act",
                  lambda e, n=n, wb=wb: e.dma_start(
                      out=wb[:], in_=w_mod[:, n * 512:(n + 1) * 512].rearrange("(k p) n -> p k n", p=128)),
                  writes=[("wm", n % 3)])
            wbb = wmb[n % 2]
            if n % 2 == 0:
                S.op("dve", lambda e, wb=wb, wbb=wbb: e.tensor_copy(out=wbb[:], in_=wb[:]),
                     reads=[("wm", n % 3)], writes=[("wmb", n % 2)])
            else:
                S.op("act", lambda e, wb=wb, wbb=wbb: e.activation(out=wbb[:], in_=wb[:], func=AF.Copy),
                     reads=[("wm", n % 3)], writes=[("wmb", n % 2)])
            pb = ps[n % 2]
            for k in range(8):
                S.op("pe", lambda e, k=k, wbb=wbb, pb=pb: e.matmul(pb[0:33, :], scl[:, k, :], wbb[:, k, :],
                                                                  start=(k == 0), stop=(k == 7)),
                     reads=["scl", ("wmb", n % 2)], writes=[("ps", n % 2)])
            S.op("dve", lambda e, n=n, pb=pb: e.tensor_tensor(out=modrow[:, n * 512:(n + 1) * 512], in0=pb[0:33, :],
                                                             in1=bm[:, n * 512:(n + 1) * 512], op=ALU.add),
                 reads=[("ps", n % 2), "bm", "bm0", "bm32"], writes=[("modrow", n)])
        mr_all = [("modrow", n) for n in range(12)]
        col_specs = [(0, 0), (0, 1), (0, 3), (0, 4), (32, 0), (32, 1)]
        for si, (r, vi) in enumerate(col_specs):
            for k in range(8):
                c0 = 2 * (si * 8 + k)
                S.op("pe", lambda e, r=r, vi=vi, k=k, c0=c0: e.matmul(
                    ps[2][:, c0:c0 + 2], modrow[r:r + 1, vi * D + k * 128: vi * D + (k + 1) * 128],
                    cA[r:r + 1, 384:386], start=True, stop=True),
                    reads=mr_all + ["cA"], writes=[("ps", 2)])
        S.op("dve", lambda e: e.tensor_copy(out=mcol[:].unsqueeze(2), in_=ps[2][:, 0:96].rearrange("p (c two) -> p c two", two=2)[:, :, 0:1]), reads=[("ps", 2)], writes=["mcol"])
        S.op("dve", lambda e: e.scalar_tensor_tensor(out=mc[:, 0:8], in0=mcol[:, 8:16], scalar=1.0, in1=n1[:],
                                                     op0=ALU.add, op1=ALU.mult), reads=["mcol", "n1"], writes=["mc0"])
        S.op("dve", lambda e: e.tensor_copy(out=mc[:, 8:16], in_=mcol[:, 0:8]), reads=["mcol"], writes=["mc1"])
        S.op("dve", lambda e: e.scalar_tensor_tensor(out=mc[:, 16:24], in0=mcol[:, 40:48], scalar=1.0, in1=n1[:],
                                                     op0=ALU.add, op1=ALU.mult), reads=["mcol", "n1"], writes=["mc2"])
        S.op("dve", lambda e: e.tensor_copy(out=mc[:, 24:32], in_=mcol[:, 32:40]), reads=["mcol"], writes=["mc3"])
        S.op("dve", lambda e: e.scalar_tensor_tensor(out=mc[:, 32:40], in0=mcol[:, 24:32], scalar=1.0, in1=n2[:],
                                                     op0=ALU.add, op1=ALU.mult), reads=["mcol", "n2"], writes=["mc4"])
        S.op("dve", lambda e: e.tensor_copy(out=mc[:, 40:48], in_=mcol[:, 16:24]), reads=["mcol"], writes=["mc5"])
        for j, (vi, kind) in enumerate([(2, "copy"), (5, "copy"), (4, "g2"), (3, "copy")]):
            bt_ = bct[j % 2]
            for hf in range(2):
                pb = ps[3 + hf]
                S.op("pe", lambda e, vi=vi, hf=hf, pb=pb: e.matmul(
                    pb[:, :], cA[0:1, 384:512], modrow[0:1, vi * D + hf * 512: vi * D + (hf + 1) * 512],
                    start=True, stop=True), reads=mr_all + ["cA"], writes=[("ps", 3 + hf)])
                if kind == "copy":
                    S.op("dve", lambda e, bt_=bt_, hf=hf, pb=pb: e.tensor_copy(out=bt_[:, hf * 512:(hf + 1) * 512], in_=pb[:, :]),
                         reads=[("ps", 3 + hf)], writes=[("bct", j % 2, hf)])
                else:
                    S.op("dve", lambda e, bt_=bt_, hf=hf, pb=pb: e.scalar_tensor_tensor(
                        out=bt_[:, hf * 512:(hf + 1) * 512], in0=pb[:, :], scalar=1.0,
                        in1=n2bt[:, hf * 512:(hf + 1) * 512], op0=ALU.add, op1=ALU.mult),
                        reads=[("ps", 3 + hf), "n2bt"], writes=[("bct", j % 2, hf)])
            S.dma("sp", lambda e, j=j, bt_=bt_: e.dma_start(out=scr_bc[j], in_=bt_[:]),
                  reads=[("bct", j % 2, 0), ("bct", j % 2, 1)], writes=[("scr", j)])
        if debug:
            S.dma("sp", lambda e: e.dma_start(out=dbg["mc"], in_=mc[:]), reads=[f"mc{j}" for j in range(6)])
        phase_end("p0")

    R = sb("R", [128, NT * D])
    x1 = R[:].rearrange("p (t d) -> p t d", d=D)
    hT = R[:, 0:9216].bitcast(BF16).rearrange("p (k t) -> p k t", k=8)
    vtok = R[:, 9216:13824].bitcast(BF16).rearrange("p (t d) -> p t d", d=512)
    with contextlib.ExitStack() as pm:
        mixT = sb("mixT", [128, 8, SEQ], BF16, stack=pm)

        with contextlib.ExitStack() as p1:
            xt = [sb(f"xt{j}", [128, D], stack=p1) for j in range(2)]
            xs = [sb(f"xs{j}", [128, D], BF16, stack=p1) for j in range(2)]
            junk = sb("junk1", [128, D], BF16, stack=p1)
            st = sb("st1", [128, 3 * NTT], stack=p1)
            for T in range(NTT):
                b = T % 2
                src = x[T * 128:(T + 1) * 128, :] if T < NT else ctx[(T - NT) * 128:(T - NT + 1) * 128, :]
                S.dma("sp" if b == 0 else "act", lambda e, b=b, src=src: e.dma_start(out=xt[b][:], in_=src),
                      writes=[("xt", b)])
                S.op("act", lambda e, b=b, T=T: e.activation(out=junk[:], in_=xt[b][:], func=AF.Square,
                                                             accum_out=st[:, T:T + 1]),
                     reads=[("xt", b)], writes=["junk", ("ssq", T)])
                S.op("dve", lambda e, T=T: e.tensor_scalar(out=st[:, NTT + T:NTT + T + 1], in0=st[:, T:T + 1],
                                                           scalar1=1.0 / D, scalar2=EPS, op0=ALU.mult, op1=ALU.add),
                     reads=[("ssq", T)], writes=[("ms", T)])
                S.op("act", lambda e, T=T: e.activation(out=st[:, NTT + T:NTT + T + 1], in_=st[:, NTT + T:NTT + T + 1],
                                                        func=AF.Sqrt), reads=[("ms", T)], writes=[("ms", T)])
                S.op("dve", lambda e, T=T: e.reciprocal(out=st[:, 2 * NTT + T:2 * NTT + T + 1],
                                                        in_=st[:, NTT + T:NTT + T + 1]),
                     reads=[("ms", T)], writes=[("rstd", T)])
                S.op("act", lambda e, b=b, T=T: e.activation(out=xs[b][:], in_=xt[b][:], func=AF.Copy,
                                                             scale=st[:, 2 * NTT + T:2 * NTT + T + 1]),
                     reads=[("xt", b), ("rstd", T)], writes=[("xs", b)])
                for k in range(8):
                    S.op("pe", lambda e, b=b, k=k: e.transpose(pT[b][:, k * 128:(k + 1) * 128],
                                                               xs[b][:, k * 128:(k + 1) * 128], identb[:]),
                         reads=[("xs", b), "identb"], writes=[("pT", b)])
                go, so = (0, 8) if T < NT else (16, 24)
                for k in range(8):
                    S.op("dve", lambda e, b=b, k=k, T=T, go=go, so=so: e.tensor_scalar(
                        out=hT[:, k, T * 128:(T + 1) * 128], in0=pT[b][:, k * 128:(k + 1) * 128],
                        scalar1=mc[:, go + k:go + k + 1], scalar2=mc[:, so + k:so + k + 1],
                        op0=ALU.mult, op1=ALU.add),
                        reads=[("pT", b)], writes=[("hT", T)])
            if debug:
                S.dma("sp", lambda e: e.dma_start(out=dbg["hT"], in_=R[:, 0:9216].bitcast(BF16)),
                      reads=[("hT", T) for T in range(NTT)])
            phase_end("p1")
        hT_all = [("hT", T) for T in range(NTT)]

        def load_w(tile_ap, dram_w, col0, ncols, key):
            for k0 in range(0, 8, 4):
                S.dma("pool", lambda e, k0=k0: e.dma_start(
                    out=tile_ap[:, k0:k0 + 4, :],
                    in_=dram_w[k0 * 128:(k0 + 4) * 128, col0:col0 + ncols].rearrange("(k p) n -> p k n", p=128)),
                    writes=[(key, k0)])
            return [(key, 0), (key, 4)]

        with contextlib.ExitStack() as p2:
            qT = sb("qT", [128, 4, SEQ], BF16, stack=p2)
            kT = sb("kT", [128, 4, TOK], BF16, stack=p2)
            vaug = sb("vaug", [128, NTT, 8, 65], BF16, stack=p2)
            p2a = contextlib.ExitStack()
            wna = sb("wna", [128, 8, 1536], BF16, stack=p2a)
            wk = load_w(wna, w_in, 0, 1536, "wna")
            S.op("pool", lambda e: e.memset(vaug[:, :, :, 64:65], 1.0), writes=["vones"])
            cnt = 0
            for which, dst, ntok, cbase in (("q", qT, SEQ, 0), ("k", kT, TOK, 512)):
                for hp in range(4):
                    for t0 in range(0, ntok, 512):
                        tw = min(512, ntok - t0)
                        pb = cnt % 4
                        for k in range(8):
                            S.op("pe", lambda e, k=k, hp=hp, t0=t0, tw=tw, pb=pb, cbase=cbase: e.matmul(
                                ps[pb][:, 0:tw], wna[:, k, cbase + hp * 128: cbase + (hp + 1) * 128],
                                hT[:, k, t0:t0 + tw], start=(k == 0), stop=(k == 7)),
                                reads=wk + hT_all, writes=[("ps", pb)])
                        eng = "act" if cnt % 2 == 0 else "dve"
                        if eng == "act":
                            S.op("act", lambda e, dst=dst, hp=hp, t0=t0, tw=tw, pb=pb: e.activation(
                                out=dst[:, hp, t0:t0 + tw], in_=ps[pb][:, 0:tw], func=AF.Copy),
                                reads=[("ps", pb)], writes=[(which, hp, t0)])
                        else:
                            S.op("dve", lambda e, dst=dst, hp=hp, t0=t0, tw=tw, pb=pb: e.tensor_copy(
                                out=dst[:, hp, t0:t0 + tw], in_=ps[pb][:, 0:tw]),
                                reads=[("ps", pb)], writes=[(which, hp, t0)])
                        cnt += 1
            for T in range(NTT):
                pb = cnt % 4
                for k in range(8):
                    S.op("pe", lambda e, k=k, T=T, pb=pb: e.matmul(
                        ps[pb][:, :], hT[:, k, T * 128:(T + 1) * 128], wna[:, k, 1024:1536],
                        start=(k == 0), stop=(k == 7)), reads=wk + hT_all, writes=[("ps", pb)])
                if cnt % 2 == 0:
                    S.op("act", lambda e, T=T, pb=pb: e.activation(
                        out=vaug[:, T, :, 0:64], in_=ps[pb][:, :].rearrange("p (h d) -> p h d", d=64), func=AF.Copy),
                        reads=[("ps", pb)], writes=[("v", T)])
                else:
                    S.op("dve", lambda e, T=T, pb=pb: e.tensor_copy(
                        out=vaug[:, T, :, 0:64], in_=ps[pb][:, :].rearrange("p (h d) -> p h d", d=64)),
                        reads=[("ps", pb)], writes=[("v", T)])
                cnt += 1
            phase_end("p2a")
            p2a.close()
            bt = sb("bt", [128, 8, 9, 128], stack=p2)
            Ssb = [sb(f"Ssb{j}", [128, 640], stack=p2) for j in range(2)]
            Pb = [sb(f"Pb{j}", [128, 896], BF16, stack=p2) for j in range(2)]
            rden = sb("rden", [128, 16], stack=p2)
            natok = [sb(f"natok{j}", [128, 512], BF16, stack=p2) for j in range(2)]
            S.dma("sp", lambda e: e.dma_start(out=bt[:].rearrange("p h t q -> p (h t q)"), in_=biasT), writes=["bt"])

            items = [(i, h) for i in range(NT) for h in range(8)]

            def na_stage_a(it):
                i, h = items[it]
                chunks = na_chunks(i)
                nw = len(chunks)
                hp, po = h // 2, (h % 2) * 64
                sbuf_i = it % 2
                b0, b1 = ps[2 * sbuf_i], ps[2 * sbuf_i + 1]

                def sloc(j):
                    return (b0, j * 128) if j < 4 else (b1, (j - 4) * 128)
                for j, (c, t) in enumerate(chunks):
                    bk, co = sloc(j)
                    S.op("pe", lambda e, bk=bk, co=co, c=c, hp=hp, po=po, i=i: e.matmul(
                        bk[:, co:co + 128], kT[po:po + 64, hp, c * 128:(c + 1) * 128],
                        qT[po:po + 64, hp, i * 128:(i + 1) * 128], start=True, stop=True),
                        reads=[], writes=[("psS", sbuf_i, j // 4)])
                for cc_ in range(2):
                    S.op("pe", lambda e, cc_=cc_, hp=hp, po=po, i=i, b1=b1: e.matmul(
                        b1[:, 128 + cc_ * 128: 256 + cc_ * 128],
                        kT[po:po + 64, hp, SEQ + cc_ * 128: SEQ + (cc_ + 1) * 128],
                        qT[po:po + 64, hp, i * 128:(i + 1) * 128], start=True, stop=True),
                        reads=[], writes=[("psS", sbuf_i, 1)])
                for j, (c, t) in enumerate(chunks):
                    bk, co = sloc(j)
                    S.op("dve", lambda e, bk=bk, co=co, j=j, t=t, h=h, sbuf_i=sbuf_i: e.scalar_tensor_tensor(
                        out=Ssb[sbuf_i][:, j * 128:(j + 1) * 128], in0=bk[:, co:co + 128], scalar=0.125,
                        in1=bt[:, h, t, :], op0=ALU.mult, op1=ALU.add),
                        reads=[("psS", sbuf_i, j // 4), "bt"], writes=[("Ssb", sbuf_i)])
                S.op("act", lambda e, nw=nw, sbuf_i=sbuf_i: e.activation(
                    out=Pb[sbuf_i][:, 0:nw * 128], in_=Ssb[sbuf_i][:, 0:nw * 128], func=AF.Exp),
                    reads=[("Ssb", sbuf_i)], writes=[("Pw", sbuf_i)])
                S.op("act", lambda e, sbuf_i=sbuf_i, b1=b1: e.activation(
                    out=Pb[sbuf_i][:, 640:896], in_=b1[:, 128:384], func=AF.Exp, scale=0.125),
                    reads=[("psS", sbuf_i, 1)], writes=[("Pc", sbuf_i)])

            def na_stage_b(it):
                i, h = items[it]
                chunks = na_chunks(i)
                nb = i % 2
                sbuf_i = it % 2
                ob = ps[4 + h // 4]
                oc = (h % 4) * 128
                for j, (c, t) in enumerate(chunks):
                    S.op("pe", lambda e, j=j, c=c, h=h, ob=ob, oc=oc, sbuf_i=sbuf_i: e.matmul(
                        ob[:, oc:oc + 65], Pb[sbuf_i][:, j * 128:(j + 1) * 128], vaug[:, c, h, :],
                        start=(j == 0), stop=False),
                        reads=[("Pw", sbuf_i)], writes=[("psO", h)])
                for cc_ in range(2):
                    S.op("pe", lambda e, cc_=cc_, h=h, ob=ob, oc=oc, sbuf_i=sbuf_i: e.matmul(
                        ob[:, oc:oc + 65], Pb[sbuf_i][:, 640 + cc_ * 128: 768 + cc_ * 128], vaug[:, NT + cc_, h, :],
                        start=False, stop=(cc_ == 1)),
                        reads=[("Pc", sbuf_i)], writes=[("psO", h)])
                S.op("dve", lambda e, h=h, ob=ob, oc=oc: e.reciprocal(out=rden[:, h:h + 1], in_=ob[:, oc + 64:oc + 65]),
                     reads=[("psO", h)], writes=[("rden", h)])
                S.op("dve", lambda e, h=h, ob=ob, oc=oc, nb=nb: e.tensor_scalar(
                    out=natok[nb][:, h * 64:(h + 1) * 64], in0=ob[:, oc:oc + 64], scalar1=rden[:, h:h + 1],
                    scalar2=None, op0=ALU.mult),
                    reads=[("psO", h), ("rden", h)], writes=[("natok", nb)])
                if h == 7:
                    for j in range(4):
                        S.op("pe", lambda e, j=j, nb=nb: e.transpose(pT[nb][:, j * 128:(j + 1) * 128],
                                                                    natok[nb][:, j * 128:(j + 1) * 128], identb[:]),
                             reads=[("natok", nb)], writes=[("pT", nb)])
                    S.op("act", lambda e, i=i, nb=nb: e.activation(
                        out=mixT[:, 0:4, i * 128:(i + 1) * 128],
                        in_=pT[nb][:, 0:512].rearrange("p (j t) -> p j t", t=128), func=AF.Copy),
                        reads=[("pT", nb)], writes=[("mixna", i)])

            na_stage_a(0)
            for it in range(len(items)):
                if it + 1 < len(items):
                    na_stage_a(it + 1)
                na_stage_b(it)
            phase_end("p2")

        with contextlib.ExitStack() as p3:
            oacc = sb("oacc", [128, NT, 512], stack=p3)
            with contextlib.ExitStack() as p3a:
                wv = sb("wv", [128, 8, 512], BF16, stack=p3a)
                wk = load_w(wv, w_in, 3 * 512 + 3 * 512, 512, "wv")
                for T in range(NTT):
                    pb = T % 4
                    for k in range(8):
                        S.op("pe", lambda e, k=k, T=T, pb=pb: e.matmul(
                            ps[pb][:, :], hT[:, k, T * 128:(T + 1) * 128], wv[:, k, :],
                            start=(k == 0), stop=(k == 7)), reads=wk, writes=[("ps", pb)])
                    if T % 2 == 0:
                        S.op("act", lambda e, T=T, pb=pb: e.activation(out=vtok[:, T, :], in_=ps[pb][:, :], func=AF.Copy),
                             reads=[("ps", pb)], writes=[("vtok", T)])
                    else:
                        S.op("dve", lambda e, T=T, pb=pb: e.tensor_copy(out=vtok[:, T, :], in_=ps[pb][:, :]),
                             reads=[("ps", pb)], writes=[("vtok", T)])
                phase_end("p3a")
            with contextlib.ExitStack() as p3b:
                rmask = sb("rmask_sb", [128, TOK], stack=p3b)
                A_ = sb("hgA", [128, TOK], stack=p3b)
                B_ = sb("hgB", [128, TOK], stack=p3b)
                C_ = sb("hgC", [128, TOK], stack=p3b)
                qsb = sb("hgqs", [128, 512], stack=p3b)
                Qt = sb("hgQt", [128, SEQ], BF16, stack=p3b)
                Qs = sb("hgQs", [128, SEQ], BF16, stack=p3b)
                Ks = sb("hgKs", [128, TOK], BF16, stack=p3b)
                Kf = sb("hgKf", [128, TOK], BF16, stack=p3b)
                Ktok = sb("hgKtok", [128, NTT, 128], BF16, stack=p3b)
                wqf = sb("hgwqf", [128, 8, 256], BF16, stack=p3b)
                sc_ = sb("hgsc", [128, 5, NCH], stack=p3b)
                Sst = [sb(f"hgS{j}", [128, 128], stack=p3b) for j in range(2)]
                Usc = [sb(f"hgUsc{j}", [128, 128], stack=p3b) for j in range(4)]
                Smb = [sb(f"hgSmb{j}", [128, 128], BF16, stack=p3b) for j in range(2)]
                Qe = sb("hgQe", [128, SEQ], BF16, stack=p3b)
                Qo = sb("hgQo", [128, SEQ], BF16, stack=p3b)
                ATs = [sb(f"hgATs{j}", [128, 128], BF16, stack=p3b) for j in range(2)]

                S.dma("sp", lambda e: e.dma_start(out=rmask[:], in_=rmask_d), writes=["rmask"])

                def v3(t, n=TOK):
                    return t[:, 0:n].rearrange("p (t s) -> p t s", s=64)

                for dr in range(2):
                    for hh in range(4):
                        dh = dr * 4 + hh
                        S.dma("pool", lambda e, hh=hh: e.dma_start(
                            out=wqf[:, :, 0:128],
                            in_=w_in[:, 1536 + hh * 128:1536 + (hh + 1) * 128].rearrange("(k p) n -> p k n", p=128)),
                            writes=["wq_h"])
                        S.dma("pool", lambda e, hh=hh, dr=dr: e.dma_start(
                            out=wqf[:, :, 128:256],
                            in_=w_in[:, 2048 + dr * 512 + hh * 128:2048 + dr * 512 + (hh + 1) * 128].rearrange(
                                "(k p) n -> p k n", p=128)), writes=["wf_h"])
                        for ci, t0 in enumerate(range(0, TOK, 512)):
                            tw = min(512, TOK - t0)
                            pb = ci % 4
                            for k in range(8):
                                S.op("pe", lambda e, k=k, t0=t0, tw=tw, pb=pb: e.matmul(
                                    ps[pb][:, 0:tw], wqf[:, k, 128:256], hT[:, k, t0:t0 + tw],
                                    start=(k == 0), stop=(k == 7)), reads=["wf_h"], writes=[("ps", pb)])
                            S.op("act", lambda e, t0=t0, tw=tw, pb=pb: e.activation(
                                out=A_[:, t0:t0 + tw], in_=ps[pb][:, 0:tw], func=AF.Sigmoid),
                                reads=[("ps", pb)], writes=["A"])
                        for ci, t0 in enumerate(range(0, SEQ, 512)):
                            pb = ci % 4
                            for k in range(8):
                                S.op("pe", lambda e, k=k, t0=t0, pb=pb: e.matmul(
                                    ps[pb][:, :], wqf[:, k, 0:128], hT[:, k, t0:t0 + 512],
                                    start=(k == 0), stop=(k == 7)), reads=["wq_h"], writes=[("ps", pb)])
                        S.op("dve", lambda e, dh=dh: e.tensor_scalar(out=A_[:], in0=A_[:], scalar1=oml[:, dh:dh + 1],
                                                                     scalar2=lb[:, dh:dh + 1], op0=ALU.mult, op1=ALU.add),
                             reads=["A"], writes=["A"])
                        S.op("act", lambda e: e.activation(out=B_[:], in_=A_[:], func=AF.Ln), reads=["A"], writes=["B"])
                        S.op("dve", lambda e: e.tensor_scalar(out=A_[:], in0=A_[:], scalar1=-1.0, scalar2=1.0,
                                                              op0=ALU.mult, op1=ALU.add), reads=["A", "B"], writes=["A"])
                        S.op("dve", lambda e: e.tensor_tensor_scan(out=C_[:], data0=rmask[:], data1=B_[:], initial=0.0,
                                                                   op0=ALU.mult, op1=ALU.add),
                             reads=["B", "rmask"], writes=["C"])
                        if dr == 0:
                            gbuf, gkey = C_, "C"
                            refpos = 31
                        else:
                            S.op("dve", lambda e: e.tensor_tensor(out=B_[:], in0=B_[:], in1=C_[:], op=ALU.subtract),
                                 reads=["B", "C"], writes=["B"])
                            S.op("dve", lambda e: e.tensor_tensor(
                                out=v3(B_), in0=v3(B_), in1=v3(C_)[:, :, 63:64].to_broadcast([128, NCH, 64]), op=ALU.add),
                                reads=["B", "C"], writes=["B"])
                            gbuf, gkey = B_, "B"
                            refpos = 32
                        endpos = 63 if dr == 0 else 0
                        S.op("dve", lambda e, gbuf=gbuf, refpos=refpos: e.tensor_copy(
                            out=sc_[:, 0, :].unsqueeze(2), in_=v3(gbuf)[:, :, refpos:refpos + 1]), reads=[gkey], writes=["sc0"])
                        S.op("dve", lambda e, gbuf=gbuf, endpos=endpos: e.tensor_copy(
                            out=sc_[:, 1, :].unsqueeze(2), in_=v3(gbuf)[:, :, endpos:endpos + 1]), reads=[gkey], writes=["sc1"])
                        S.op("act", lambda e: e.activation(out=sc_[:, 2:4, :], in_=sc_[:, 0:2, :], func=AF.Exp),
                             reads=["sc0", "sc1"], writes=["sc23"])
                        S.op("dve", lambda e: e.tensor_tensor(out=sc_[:, 4, :], in0=sc_[:, 1, :], in1=sc_[:, 0, :],
                                                              op=ALU.subtract), reads=["sc0", "sc1"], writes=["sc4"])
                        S.op("act", lambda e: e.activation(out=sc_[:, 4, :], in_=sc_[:, 4, :], func=AF.Exp),
                             reads=["sc4"], writes=["sc4"])
                        obuf, okey = (B_, "B") if dr == 0 else (C_, "C")
                        S.op("dve", lambda e, gbuf=gbuf: e.tensor_tensor(
                            out=v3(gbuf), in0=v3(gbuf), in1=sc_[:, 0, :].unsqueeze(2).to_broadcast([128, NCH, 64]),
                            op=ALU.subtract), reads=[gkey, "sc0", "sc1"], writes=[gkey])
                        S.op("act", lambda e, gbuf=gbuf, obuf=obuf: e.activation(out=obuf[:, 0:SEQ], in_=gbuf[:, 0:SEQ], func=AF.Exp),
                             reads=[gkey, okey], writes=[okey])
                        S.op("act", lambda e, gbuf=gbuf: e.activation(out=gbuf[:], in_=gbuf[:], func=AF.Exp, scale=-1.0),
                             reads=[gkey, okey], writes=[gkey])
                        S.op("dve", lambda e, gbuf=gbuf: e.tensor_tensor(out=Kf[:], in0=A_[:], in1=gbuf[:], op=ALU.mult),
                             reads=["A", gkey], writes=["Kf"])
                        hk = 1 if dr == 0 else 0
                        hq = 1 - hk
                        def h32(t):
                            return t[:].rearrange("p (t two s) -> p t two s", two=2, s=32)

                        def h64(t):
                            return t[:].rearrange("p (t two s) -> p t two s", two=2, s=64)
                        if hh == 0:
                            S.op("pool", lambda e: e.memset(Ks[:], 0.0), reads=["Ks"], writes=["Ks"])
                            S.op("pool", lambda e: e.memset(Qs[:], 0.0), reads=["Qs"], writes=["Qs"])
                            if dr == 0:
                                S.op("pool", lambda e: e.memset(Qe[:], 0.0), reads=["Qe"], writes=["Qe"])
                                S.op("pool", lambda e: e.memset(Qo[:], 0.0), reads=["Qo"], writes=["Qo"])
                        S.op("dve", lambda e, hk=hk: e.tensor_copy(out=h32(Ks)[:, :, 1 - hk, :], in_=h32(Kf)[:, :, 1 - hk, :]),
                             reads=["Kf", "Ks"], writes=["Ks"])
                        for ci, t0 in enumerate(range(0, SEQ, 512)):
                            pb = ci % 4
                            S.op("act", lambda e, pb=pb: e.activation(out=qsb[:], in_=ps[pb][:, :], func=AF.Silu),
                                 reads=[("ps", pb)], writes=["qsb"])
                            S.op("dve", lambda e, t0=t0, obuf=obuf: e.tensor_tensor(out=Qt[:, t0:t0 + 512], in0=qsb[:],
                                                                                    in1=obuf[:, t0:t0 + 512], op=ALU.mult),
                                 reads=["qsb", okey], writes=["Qt"])
                        S.op("dve", lambda e, hq=hq: e.tensor_copy(out=h32(Qs)[:, :, 1 - hq, :], in_=h32(Qt)[:, :, 1 - hq, :]),
                             reads=["Qt", "Qs"], writes=["Qs"])
                        S.op("act", lambda e: e.activation(out=h64(Qe)[:, :, 0, :], in_=h64(Qt)[:, :, 0, :], func=AF.Copy),
                             reads=["Qt", "Qe"], writes=["Qe"])
                        S.op("act", lambda e: e.activation(out=h64(Qo)[:, :, 1, :], in_=h64(Qt)[:, :, 1, :], func=AF.Copy),
                             reads=["Qt", "Qo"], writes=["Qo"])
                        for g0 in range(0, NTT, 8):
                            nb = (g0 // 8) % 2
                            gn = min(8, NTT - g0)
                            for j in range(gn):
                                S.op("pe", lambda e, T=g0 + j, j=j, nb=nb: e.transpose(
                                    pT[nb][:, j * 128:(j + 1) * 128], Kf[:, T * 128:(T + 1) * 128], identb[:]),
                                    reads=["Kf"], writes=[("pT", nb)])
                            S.op("act", lambda e, g0=g0, gn=gn, nb=nb: e.activation(
                                out=Ktok[:, g0:g0 + gn, :],
                                in_=pT[nb][:, 0:gn * 128].rearrange("p (j t) -> p j t", t=128), func=AF.Copy),
                                reads=[("pT", nb)], writes=[("Ktok", T) for T in range(g0, g0 + gn)])
                        S.op("pool", lambda e, hk=hk: e.memset(
                            Kf[:].rearrange("p (t two s) -> p t two s", two=2, s=32)[:, :, 1 - hk, :], 0.0),
                            reads=["Kf"], writes=["Kf"])
                        order = [16, 17] + list(range(NT)) if dr == 0 else [17, 16] + list(range(NT - 1, -1, -1))
                        tri = TRIF if dr == 0 else TRIB
                        kcnt = [0]
                        S.op("dve", lambda e: e.memset(Sst[0][:], 0.0), reads=[("S", 0)], writes=[("S", 0)])

                        def chunks_of(T):
                            return [2 * T, 2 * T + 1] if dr == 0 else [2 * T + 1, 2 * T]

                        def stage_a(n):
                            T = order[n]
                            q = n % 2
                            for ci, c in enumerate(chunks_of(T)):
                                par = c % 2
                                S.op("pe", lambda e, T=T, par=par, ci=ci, hh=hh: e.matmul(
                                    ps[2 + ci][:, 0:128], Ktok[par * 64:(par + 1) * 64, T, :],
                                    vtok[par * 64:(par + 1) * 64, T, hh * 128:(hh + 1) * 128], start=True, stop=True),
                                    reads=[("Ktok", T)], writes=[("ps", 2 + ci)])
                                S.op("dve", lambda e, c=c, ci=ci, q=q: e.tensor_scalar(
                                    out=Usc[2 * q + ci][:], in0=ps[2 + ci][:, 0:128], scalar1=sc_[:, 4, c:c + 1], scalar2=None,
                                    op0=ALU.mult), reads=[("ps", 2 + ci), "sc4"], writes=[("Usc", 2 * q + ci)])
                            if T < NT:
                                S.op("pe", lambda e, T=T: e.matmul(ps[4][:, 0:128], Ks[:, T * 128:(T + 1) * 128],
                                                                   Qt[:, T * 128:(T + 1) * 128], start=True, stop=False),
                                     reads=["Ks", "Qt"], writes=[("ps", 4)])
                                S.op("pe", lambda e, T=T: e.matmul(ps[4][:, 0:128], Kf[:, T * 128:(T + 1) * 128],
                                                                   Qs[:, T * 128:(T + 1) * 128], start=False, stop=True),
                                     reads=["Kf", "Qs"], writes=[("ps", 4)])

                        def stage_a2(n):
                            T = order[n]
                            q = n % 2
                            if T < NT:
                                S.op("dve", lambda e, q=q, tri=tri: e.tensor_tensor(out=ATs[q][:], in0=ps[4][:, 0:128], in1=tri, op=ALU.mult),
                                     reads=[("ps", 4), "cA"], writes=[("ATs", q)])
                                ob = ps[5] if q == 0 else ps[1]
                                S.op("pe", lambda e, T=T, q=q, ob=ob, hh=hh: e.matmul(ob[:, 0:128], ATs[q][:], vtok[:, T, hh * 128:(hh + 1) * 128],
                                                                               start=True, stop=False),
                                     reads=[("ATs", q)], writes=[("ps", 5 if q == 0 else 1)])

                        def stage_b(n):
                            T = order[n]
                            q = n % 2
                            lat = T < NT
                            ob = ps[5] if q == 0 else ps[1]
                            for ci, c in enumerate(chunks_of(T)):
                                par = c % 2
                                k = kcnt[0]
                                kcnt[0] += 1
                                si, so = k % 2, (k + 1) % 2
                                if lat:
                                    S.op("act", lambda e, c=c, ci=ci, si=si: e.activation(
                                        out=Smb[ci][:], in_=Sst[si][:], func=AF.Copy, scale=sc_[:, 2, c:c + 1]),
                                        reads=[("S", si), "sc23"], writes=[("Smb", ci)])
                                    Qz, qzk = (Qe, "Qe") if par == 0 else (Qo, "Qo")
                                    S.op("pe", lambda e, T=T, ci=ci, Qz=Qz, ob=ob: e.matmul(
                                        ob[:, 0:128], Qz[:, T * 128:(T + 1) * 128], Smb[ci][:], start=False, stop=(ci == 1)),
                                        reads=[("Smb", ci), qzk], writes=[("ps", 5 if q == 0 else 1)])
                                S.op("dve", lambda e, c=c, ci=ci, q=q, si=si, so=so: e.scalar_tensor_tensor(
                                    out=Sst[so][:], in0=Sst[si][:], scalar=sc_[:, 3, c:c + 1], in1=Usc[2 * q + ci][:],
                                    op0=ALU.mult, op1=ALU.add), reads=[("Usc", 2 * q + ci), ("S", si), "sc23"], writes=[("S", so)])
                            if lat:
                                if dr == 0:
                                    S.op("act", lambda e, T=T, ob=ob, hh=hh: e.activation(
                                        out=oacc[:, T, hh * 128:(hh + 1) * 128], in_=ob[:, 0:128], func=AF.Copy),
                                        reads=[("ps", 5 if q == 0 else 1)], writes=[("oacc", T, hh)])
                                else:
                                    S.op("dve", lambda e, T=T, ob=ob, hh=hh: e.tensor_tensor(
                                        out=oacc[:, T, hh * 128:(hh + 1) * 128], in0=ob[:, 0:128],
                                        in1=oacc[:, T, hh * 128:(hh + 1) * 128], op=ALU.add),
                                        reads=[("ps", 5 if q == 0 else 1), ("oacc", T, hh)], writes=[("oacc", T, hh)])

                        stage_a(0)
                        stage_a2(0)
                        for n in range(len(order)):
                            if n + 1 < len(order):
                                stage_a(n + 1)
                            stage_b(n)
                            if n + 1 < len(order):
                                stage_a2(n + 1)
                        if kcnt[0] % 2 == 1:
                            pass
                phase_end("p3b")
            with contextlib.ExitStack() as p3c:
                wg = sb("wg", [128, 8, 512], BF16, stack=p3c)
                hgnb = sb("hgnb", [128, 512], stack=p3c)
                sg = [sb(f"sg{j}", [128, 512], stack=p3c) for j in range(2)]
                yb = [sb(f"yb{j}", [128, 512], stack=p3c) for j in range(2)]
                yt = [sb(f"yt{j}", [128, 512], BF16, stack=p3c) for j in range(2)]
                jk = sb("jk3", [128, 128], BF16, stack=p3c)
                st3 = sb("st3", [128, NT, 8], stack=p3c)
                wk = load_w(wg, w_in, 1536 + 4 * 512, 512, "wg")
                S.dma("sp", lambda e: e.dma_start(out=hgnb[:], in_=hgn), writes=["hgnb"])
                for T in range(NT):
                    for hh in range(4):
                        S.op("act", lambda e, T=T, hh=hh: e.activation(
                            out=jk[:], in_=oacc[:, T, hh * 128:(hh + 1) * 128], func=AF.Square,
                            accum_out=st3[:, T, hh:hh + 1]), reads=[], writes=["jk3", ("ss3", T)])
                ss_all = [("ss3", T) for T in range(NT)]
                S.op("dve", lambda e: e.tensor_scalar(out=st3[:, :, 4:8], in0=st3[:, :, 0:4], scalar1=1.0 / 128,
                                                      scalar2=EPS, op0=ALU.mult, op1=ALU.add),
                     reads=ss_all, writes=["ms3"])
                S.op("act", lambda e: e.activation(out=st3[:, :, 4:8], in_=st3[:, :, 4:8], func=AF.Sqrt),
                     reads=["ms3"], writes=["ms3"])
                S.op("dve", lambda e: e.reciprocal(out=st3[:, :, 0:4], in_=st3[:, :, 4:8]),
                     reads=["ms3"] + ss_all, writes=["rs3"])
                for T in range(NT):
                    b = T % 2
                    for k in range(8):
                        S.op("pe", lambda e, k=k, T=T, b=b: e.matmul(
                            ps[b][:, :], hT[:, k, T * 128:(T + 1) * 128], wg[:, k, :],
                            start=(k == 0), stop=(k == 7)), reads=wk, writes=[("ps", b)])
                    S.op("act", lambda e, b=b: e.activation(out=sg[b][:], in_=ps[b][:, :], func=AF.Silu),
                         reads=[("ps", b)], writes=[("sg", b)])
                    S.op("dve", lambda e, T=T, b=b: e.tensor_tensor(
                        out=yb[b][:].rearrange("p (h d) -> p h d", d=128),
                        in0=oacc[:, T, :].rearrange("p (h d) -> p h d", d=128),
                        in1=st3[:, T, 0:4].unsqueeze(2).to_broadcast([128, 4, 128]), op=ALU.mult),
                        reads=["rs3"], writes=[("yb", b)])
                    S.op("dve", lambda e, b=b: e.tensor_tensor(out=yb[b][:], in0=yb[b][:], in1=hgnb[:], op=ALU.mult),
                         reads=[("yb", b), "hgnb"], writes=[("yb", b)])
                    S.op("dve", lambda e, b=b: e.tensor_tensor(out=yt[b][:], in0=yb[b][:], in1=sg[b][:], op=ALU.mult),
                         reads=[("yb", b), ("sg", b)], writes=[("yt", b)])
                    for j in range(4):
                        S.op("pe", lambda e, j=j, b=b: e.transpose(pT[b][:, j * 128:(j + 1) * 128],
                                                                   yt[b][:, j * 128:(j + 1) * 128], identb[:]),
                             reads=[("yt", b)], writes=[("pT", b)])
                    S.op("act", lambda e, T=T, b=b: e.activation(
                        out=mixT[:, 4:8, T * 128:(T + 1) * 128],
                        in_=pT[b][:, 0:512].rearrange("p (j t) -> p j t", t=128), func=AF.Copy),
                        reads=[("pT", b)], writes=[("mixhg", T)])
                if debug:
                    S.dma("sp", lambda e: e.dma_start(out=dbg["mixT"], in_=mixT[:].rearrange("p k t -> p (k t)")),
                          reads=[("mixhg", T) for T in range(NT)])
                phase_end("p3")

        with contextlib.ExitStack() as p4:
            wo32 = sb("wo32", [128, 8, D], stack=p4)
            wob = sb("wob", [128, 8, D], BF16, stack=p4)
            g1b = sb("g1b", [128, D], stack=p4)
            S.dma("sp", lambda e: e.dma_start(out=g1b[:], in_=scr_bc[0]), writes=["g1b"])
            for k in range(8):
                S.dma("sp" if k % 2 == 0 else "act", lambda e, k=k: e.dma_start(
                    out=wo32[:, k, :], in_=w_out[k * 128:(k + 1) * 128, :]), writes=[("wo32", k)])
                S.op("dve" if k % 2 == 0 else "pool", lambda e, k=k: e.tensor_tensor(
                    out=wob[:, k, :], in0=wo32[:, k, :], in1=g1b[:], op=ALU.mult),
                    reads=[("wo32", k), "g1b"], writes=[("wob", k)])
            wob_all = [("wob", k) for k in range(8)]
            for T in range(NT):
                S.dma("sp" if T % 2 == 0 else "act", lambda e, T=T: e.dma_start(
                    out=x1[:, T, :], in_=x[T * 128:(T + 1) * 128, :]), writes=[("x1", T)])
                for hf in range(2):
                    pb = (2 * T + hf) % 4
                    for k in range(8):
                        S.op("pe", lambda e, k=k, T=T, hf=hf, pb=pb: e.matmul(
                            ps[pb][:, :], mixT[:, k, T * 128:(T + 1) * 128], wob[:, k, hf * 512:(hf + 1) * 512],
                            start=(k == 0), stop=(k == 7)), reads=wob_all, writes=[("ps", pb)])
                    S.op("dve", lambda e, T=T, hf=hf, pb=pb: e.tensor_tensor(
                        out=x1[:, T, hf * 512:(hf + 1) * 512], in0=ps[pb][:, :], in1=x1[:, T, hf * 512:(hf + 1) * 512],
                        op=ALU.add), reads=[("ps", pb), ("x1", T)], writes=[("x1", T)])
            if debug:
                S.dma("sp", lambda e: e.dma_start(out=dbg["x1"], in_=R[:]),
                      reads=[("x1", T) for T in range(NT)])
            phase_end("p4")

    eidx = sb("eidx", [128, NT, 128], I32)
    gw = sb("gw", [128, NT, 128])
    rs2 = sb("rs2", [128, NT])
    with contextlib.ExitStack() as p5:
        wqb = sb("wqb", [128, 8, 2048], BF16, stack=p5)
        kTb = sb("kTb", [128, 16, 128], BF16, stack=p5)
        junk = sb("junk5", [128, D], BF16, stack=p5)
        xs = [sb(f"xs5{j}", [128, D], BF16, stack=p5) for j in range(2)]
        h2T = [sb(f"h2T{j}", [128, 8, 128], BF16, stack=p5) for j in range(2)]
        qTp = [sb(f"qTp{j}", [128, 16, 128], BF16, stack=p5) for j in range(2)]
        ssb = sb("ssb", [128, 16, 128], stack=p5)
        s2 = sb("s2", [128, 16, 128], stack=p5)
        top = sb("top", [128, 16, 16], stack=p5)
        itop = sb("itop", [128, 16, 16], I32, stack=p5)
        ic = sb("ic", [128, 388], I32, stack=p5)
        itf = sb("itf", [128, 16, 16], stack=p5)
        cand = sb("cand", [128, 8, 256], stack=p5)
        cand2 = sb("cand2", [128, 8, 256], stack=p5)
        ctop = sb("ctop", [128, 8, 16], stack=p5)
        cpos = sb("cpos", [128, 8, 16], I32, stack=p5)
        paf = sb("paf", [128, 128], stack=p5)
        pai = sb("pai", [128, 128], I32, stack=p5)
        pbf = sb("pbf", [128, 128], stack=p5)
        oh = sb("oh", [128, 128, 16], stack=p5)
        selA = sb("selA", [128, 128], stack=p5)
        selB = sb("selB", [128, 128], stack=p5)
        ef = sb("ef", [128, 128], stack=p5)
        ee = sb("ee", [128, 8, 16], stack=p5)
        zz = sb("zz", [128, 16], stack=p5)
        st5 = sb("st5", [128, 2 * NT], stack=p5)

        stg = [sb(f"stg{j}", [128, 4, D], BF16, stack=p5) for j in range(2)]
        conv_steps = [(tab, c) for tab in range(2) for c in range(32)]

        def emit_conv(si):
            tab, c = conv_steps[si]
            src = (u_t, v_t)[tab].rearrange("(p j c) d -> c p j d", p=128, j=4, c=32)[c]
            dst = uv_bf.rearrange("(p j c) d -> c p j d", p=128, j=4, c=32)[c][:, :, tab * D:(tab + 1) * D]
            b = si % 2
            S.dma("pool", lambda e: e.dma_start(out=stg[b][:], in_=src), writes=[("stg", b)])
            S.dma("sp", lambda e: e.dma_start(out=dst, in_=stg[b][:]), reads=[("stg", b)], writes=[("tab", tab, c)])

        wkq = load_w(wqb[:, :, 0:1024], wq, 0, 1024, "wqa") + load_w(wqb[:, :, 1024:2048], wq, 1024, 1024, "wqb")
        S.dma("pool", lambda e: e.dma_start(out=kTb[:].rearrange("p c k -> p (c k)"), in_=keysT), writes=["kTb"])
        S.dma("sp", lambda e: e.dma_start(out=ic[:], in_=icst), writes=["ic"])
        for T in range(NT):
            S.op("act", lambda e, T=T: e.activation(out=junk[:], in_=x1[:, T, :], func=AF.Square,
                                                    accum_out=st5[:, T:T + 1]), reads=[], writes=["junk5", ("ssq5", T)])
        S.op("dve", lambda e: e.tensor_scalar(out=st5[:, NT:2 * NT], in0=st5[:, 0:NT],
                                              scalar1=1.0 / D, scalar2=EPS, op0=ALU.mult, op1=ALU.add),
             reads=[("ssq5", T) for T in range(NT)], writes=["ms5"])
        S.op("act", lambda e: e.activation(out=st5[:, NT:2 * NT], in_=st5[:, NT:2 * NT], func=AF.Sqrt),
             reads=["ms5"], writes=["ms5"])
        S.op("dve", lambda e: e.reciprocal(out=rs2[:, :], in_=st5[:, NT:2 * NT]), reads=["ms5"], writes=["rs2"])
        for T in range(NT):
            b = T % 2
            if not _NOCONV:
                for q_ in range(4):
                    emit_conv(4 * T + q_)
            S.op("act", lambda e, b=b, T=T: e.activation(out=xs[b][:], in_=x1[:, T, :], func=AF.Copy,
                                                         scale=rs2[:, T:T + 1]), reads=["rs2"], writes=[("xs5", b)])
            for k in range(8):
                S.op("pe", lambda e, b=b, k=k: e.transpose(pT[b][:, k * 128:(k + 1) * 128],
                                                           xs[b][:, k * 128:(k + 1) * 128], identb[:]),
                     reads=[("xs5", b)], writes=[("pT", b)])
            for k in range(8):
                S.op("dve" if k % 2 == 0 else "act", (lambda e, b=b, k=k: e.tensor_scalar(
                    out=h2T[b][:, k, :], in0=pT[b][:, k * 128:(k + 1) * 128],
                    scalar1=mc[:, 32 + k:33 + k], scalar2=mc[:, 40 + k:41 + k], op0=ALU.mult, op1=ALU.add))
                    if k % 2 == 0 else (lambda e, b=b, k=k: e.activation(
                        out=h2T[b][:, k, :], in_=pT[b][:, k * 128:(k + 1) * 128], func=AF.Identity,
                        scale=mc[:, 32 + k:33 + k], bias=mc[:, 40 + k:41 + k])),
                    reads=[("pT", b)], writes=[("h2T", b)])
            for g4 in range(4):
                pb = g4
                for j in range(4):
                    pc = g4 * 4 + j
                    for k in range(8):
                        S.op("pe", lambda e, b=b, k=k, pc=pc, j=j, pb=pb: e.matmul(
                            ps[pb][:, j * 128:(j + 1) * 128], wqb[:, k, pc * 128:(pc + 1) * 128], h2T[b][:, k, :],
                            start=(k == 0), stop=(k == 7)), reads=wkq + [("h2T", b)], writes=[("ps", pb)])
                if g4 % 2 == 0:
                    S.op("act", lambda e, b=b, g4=g4, pb=pb: e.activation(
                        out=qTp[b][:, g4 * 4:(g4 + 1) * 4, :], in_=ps[pb][:, :].rearrange("p (j t) -> p j t", t=128),
                        func=AF.Copy), reads=[("ps", pb)], writes=[("qTp", b, g4)])
                else:
                    S.op("dve", lambda e, b=b, g4=g4, pb=pb: e.tensor_copy(
                        out=qTp[b][:, g4 * 4:(g4 + 1) * 4, :], in_=ps[pb][:, :].rearrange("p (j t) -> p j t", t=128)),
                        reads=[("ps", pb)], writes=[("qTp", b, g4)])
            for g4 in range(4):
                pb = 4 + (g4 % 2)
                for j in range(4):
                    pc = g4 * 4 + j
                    S.op("pe", lambda e, b=b, pc=pc, j=j, pb=pb: e.matmul(
                        ps[pb][:, j * 128:(j + 1) * 128], qTp[b][:, pc, :], kTb[:, pc, :], start=True, stop=True),
                        reads=[("qTp", b, g4), "kTb"], writes=[("ps", pb)])
                S.op("act", lambda e, g4=g4, pb=pb: e.activation(
                    out=ssb[:, g4 * 4:(g4 + 1) * 4, :], in_=ps[pb][:, :].rearrange("p (j t) -> p j t", t=128),
                    func=AF.Copy), reads=[("ps", pb)], writes=[("ssb", g4)])
            ssb_keys = [("ssb", g) for g in range(4)]
            ssbi = ssb[:].bitcast(I32)
            S.op("dve", lambda e: e.scalar_tensor_tensor(
                out=ssbi, in0=ssbi, scalar=ic[:, 384:385], in1=ic[:, 0:128].unsqueeze(1).to_broadcast([128, 16, 128]),
                op0=ALU.bitwise_and, op1=ALU.bitwise_or), reads=ssb_keys + ["ic"], writes=ssb_keys)
            for pc in range(16):
                S.op("dve", lambda e, pc=pc: e.max(out=top[:, pc, 0:8], in_=ssb[:, pc, :]),
                     reads=[("ssb", pc // 4)], writes=[("top", pc, 0)])
            for pc in range(16):
                S.op("dve", lambda e, pc=pc: e.match_replace(out=s2[:, pc, :], in_to_replace=top[:, pc, 0:8],
                                                             in_values=ssb[:, pc, :], imm_value=NEG),
                     reads=[("ssb", pc // 4), ("top", pc, 0)], writes=[("s2", pc)])
            for pc in range(16):
                S.op("dve", lambda e, pc=pc: e.max(out=top[:, pc, 8:16], in_=s2[:, pc, :]),
                     reads=[("s2", pc)], writes=[("top", pc, 1)])
            tops = [("top", pc, j) for pc in range(16) for j in range(2)]
            S.op("dve", lambda e: e.tensor_tensor(out=itop[:], in0=top[:].bitcast(I32),
                                                  in1=ic[:, 386:387].unsqueeze(2).to_broadcast([128, 16, 16]),
                                                  op=ALU.bitwise_and), reads=tops + ["ic"], writes=["itop"])
            S.op("dve", lambda e: e.tensor_copy(out=itf[:], in_=itop[:]), reads=["itop"], writes=["itf"])
            topv = top[:].rearrange("p (h c) a -> p h c a", c=2)
            S.op("dve", lambda e: e.tensor_tensor(
                out=cand[:].rearrange("p h (a b) -> p h a b", b=16),
                in0=topv[:, :, 0, :].unsqueeze(3).to_broadcast([128, 8, 16, 16]),
                in1=topv[:, :, 1, :].unsqueeze(2).to_broadcast([128, 8, 16, 16]), op=ALU.add),
                reads=tops, writes=["cand"])
            candi = cand[:].bitcast(I32)
            S.op("dve", lambda e: e.scalar_tensor_tensor(
                out=candi, in0=candi, scalar=ic[:, 385:386], in1=ic[:, 128:384].unsqueeze(1).to_broadcast([128, 8, 256]),
                op0=ALU.bitwise_and, op1=ALU.bitwise_or), reads=["cand", "ic"], writes=["cand"])
            for p in range(8):
                S.op("dve", lambda e, p=p: e.max(out=ctop[:, p, 0:8], in_=cand[:, p, :]), reads=["cand"], writes=[("ctop", p, 0)])
            for p in range(8):
                S.op("dve", lambda e, p=p: e.match_replace(out=cand2[:, p, :], in_to_replace=ctop[:, p, 0:8],
                                                           in_values=cand[:, p, :], imm_value=NEG),
                     reads=["cand", ("ctop", p, 0)], writes=[("cand2", p)])
            for p in range(8):
                S.op("dve", lambda e, p=p: e.max(out=ctop[:, p, 8:16], in_=cand2[:, p, :]), reads=[("cand2", p)], writes=[("ctop", p, 1)])
            ctops = [("ctop", p, j) for p in range(8) for j in range(2)]
            S.op("dve", lambda e: e.tensor_tensor(out=cpos[:], in0=ctop[:].bitcast(I32),
                                                  in1=ic[:, 387:388].unsqueeze(2).to_broadcast([128, 8, 16]),
                                                  op=ALU.bitwise_and), reads=ctops + ["ic"], writes=["cpos"])
            cposs = ["cpos"]
            cposf = cpos[:].rearrange("p h j -> p (h j)")
            S.op("dve", lambda e: e.tensor_copy(out=selA[:], in_=cposf), reads=cposs + [("sel", 0)], writes=["posf"])
            S.op("dve", lambda e: e.tensor_scalar(out=pbf[:], in0=selA[:], scalar1=0.0625, scalar2=None, op0=ALU.mult),
                 reads=["posf"], writes=["pbf"])
            S.op("dve", lambda e: e.tensor_copy(out=pai[:], in_=pbf[:]), reads=["pbf"], writes=["pai"])
            S.op("dve", lambda e: e.tensor_copy(out=paf[:], in_=pai[:]), reads=["pai"], writes=["paf"])
            S.op("dve", lambda e: e.scalar_tensor_tensor(out=pbf[:], in0=paf[:], scalar=16.0, in1=selA[:],
                                                         op0=ALU.mult, op1=ALU.is_gt), reads=["paf", "posf", "pai"], writes=["pbf"])
            S.op("dve", lambda e: e.tensor_tensor(out=paf[:], in0=paf[:], in1=pbf[:], op=ALU.subtract),
                 reads=["paf", "pbf"], writes=["paf"])
            S.op("dve", lambda e: e.scalar_tensor_tensor(out=pbf[:], in0=paf[:], scalar=-16.0, in1=selA[:],
                                                         op0=ALU.mult, op1=ALU.add), reads=["paf", "posf"], writes=["pbf"])
            itv = itf[:].rearrange("p (h c) a -> p h c a", c=2)
            for which, pf, sel in ((0, paf, selA), (1, pbf, selB)):
                S.op("dve", lambda e, pf=pf: e.tensor_tensor(
                    out=oh[:], in0=pf[:].unsqueeze(2).to_broadcast([128, 128, 16]),
                    in1=IOTA16.unsqueeze(1).to_broadcast([128, 128, 16]), op=ALU.is_equal),
                    reads=["paf", "pbf", "cA", "oh"], writes=["oh"])
                S.op("dve", lambda e, which=which: e.tensor_tensor(
                    out=oh[:].rearrange("p (h j) a -> p h j a", j=16),
                    in0=oh[:].rearrange("p (h j) a -> p h j a", j=16),
                    in1=itv[:, :, which, :].unsqueeze(2).to_broadcast([128, 8, 16, 16]), op=ALU.mult),
                    reads=["oh", "itf"], writes=["oh"])
                S.op("dve", lambda e, sel=sel: e.tensor_reduce(out=sel[:], in_=oh[:], axis=AX.X, op=ALU.add),
                     reads=["oh", "posf"], writes=[("sel", which)])
            S.op("dve", lambda e: e.scalar_tensor_tensor(out=ef[:], in0=selA[:], scalar=128.0, in1=selB[:],
                                                         op0=ALU.mult, op1=ALU.add),
                 reads=[("sel", 0), ("sel", 1)], writes=["ef"])
            S.op("dve", lambda e, T=T: e.tensor_copy(out=eidx[:, T, :], in_=ef[:]), reads=["ef"], writes=[("eidx", T)])
            S.op("dve", lambda e: e.tensor_tensor(out=ee[:], in0=ctop[:], in1=ctop[:, :, 0:1].to_broadcast([128, 8, 16]),
                                                  op=ALU.subtract), reads=ctops, writes=["ee"])
            S.op("act", lambda e: e.activation(out=ee[:], in_=ee[:], func=AF.Exp), reads=["ee"], writes=["ee"])
            S.op("dve", lambda e: e.tensor_reduce(out=zz[:, 0:8], in_=ee[:], axis=AX.X, op=ALU.add),
                 reads=["ee"], writes=["zz"])
            S.op("dve", lambda e: e.reciprocal(out=zz[:, 8:16], in_=zz[:, 0:8]), reads=["zz"], writes=["zz2"])
            S.op("dve", lambda e, T=T: e.tensor_tensor(
                out=gw[:, T, :].rearrange("p (h j) -> p h j", j=16), in0=ee[:],
                in1=zz[:, 8:16].unsqueeze(2).to_broadcast([128, 8, 16]), op=ALU.mult),
                reads=["ee", "zz2"], writes=[("gw", T)])
        if debug:
            S.dma("sp", lambda e: e.dma_start(out=dbg["eidx"], in_=eidx[:].rearrange("p t s -> p (t s)")),
                  reads=[("eidx", T) for T in range(NT)])
            S.dma("sp", lambda e: e.dma_start(out=dbg["gw"], in_=gw[:].rearrange("p t s -> p (t s)")),
                  reads=[("gw", T) for T in range(NT)])
        phase_end("p5a")

    with contextlib.ExitStack() as p6:
        NB = 12
        ring = [sb(f"ring{j}", [128, 2 * D], BF16, stack=p6) for j in range(NB)]
        NDG = 6
        dg = [sb(f"dg{j}", [128, 128], BF16, stack=p6) for j in range(NDG)]
        bc = sb("bc5", [128, 4, D], stack=p6)
        h2 = [sb(f"h2_{j}", [128, D], stack=p6) for j in range(2)]
        junk = sb("junk6", [128, D], BF16, stack=p6)
        accs = sb("accs", [128, D], stack=p6)
        aa = [sb(f"aa{j}", [128, 128], stack=p6) for j in range(2)]
        gl = [sb(f"gl{j}", [128, 128], stack=p6) for j in range(2)]
        ww = [sb(f"ww{j}", [128, 128], stack=p6) for j in range(2)]
        st6 = sb("st6", [128, 2 * NT], stack=p6)
        S.dma("sp", lambda e: e.dma_start(out=bc[:, 0, :], in_=scr_bc[1]), writes=[("bc", 0)])
        S.dma("sp", lambda e: e.dma_start(out=bc[:, 1, :], in_=scr_bc[2]), writes=[("bc", 1)])
        S.dma("sp", lambda e: e.dma_start(out=bc[:, 2, :], in_=scr_bc[3]), writes=[("bc", 2)])
        S.dma("sp", lambda e: e.dma_start(out=bc[:, 3, :], in_=nfb), writes=[("bc", 3)])
        gi = gd = 0
        for T in range(NT):
            pu = T % 2
            S.op("dve", lambda e, T=T, pu=pu: e.scalar_tensor_tensor(out=h2[pu][:], in0=x1[:, T, :], scalar=rs2[:, T:T + 1],
                                                                     in1=bc[:, 1, :], op0=ALU.mult, op1=ALU.mult),
                 reads=[("bc", 1)], writes=[("h2", pu)])
            S.op("dve", lambda e, pu=pu: e.tensor_tensor(out=h2[pu][:], in0=h2[pu][:], in1=bc[:, 2, :], op=ALU.add),
                 reads=[("h2", pu), ("bc", 2)], writes=[("h2", pu)])
            S.op("dve", lambda e, pu=pu: e.memset(aa[pu][:], 0.0), writes=[("aa", pu)])
            for s_ in range(128):
                r = gi % NB
                gi += 1
                S.dma("pool", lambda e, T=T, s_=s_, r=r: e.indirect_dma_start(
                    out=ring[r][:], out_offset=None, in_=uv_bf,
                    in_offset=bass.IndirectOffsetOnAxis(ap=eidx[:, T, s_:s_ + 1], axis=0)),
                    reads=[], writes=[("ring", r)])
                S.op("dve", lambda e, s_=s_, r=r, pu=pu: e.scalar_tensor_tensor(
                    out=junk[:], in0=ring[r][:, 0:D], scalar=1.0, in1=h2[pu][:], op0=ALU.mult, op1=ALU.mult,
                    accum_out=aa[pu][:, s_:s_ + 1]), reads=[("ring", r), ("h2", pu), ("aa", pu)],
                    writes=["junk6", ("aas", pu, s_)])
                S.op("act", lambda e, s_=s_, pu=pu: e.activation(out=gl[pu][:, s_:s_ + 1], in_=aa[pu][:, s_:s_ + 1], func=AF.Gelu),
                     reads=[("aas", pu, s_)], writes=[("gl", pu, s_)])
                S.op("act", lambda e, T=T, s_=s_, pu=pu: e.activation(out=ww[pu][:, s_:s_ + 1], in_=gl[pu][:, s_:s_ + 1],
                                                                     func=AF.Copy, scale=gw[:, T, s_:s_ + 1]),
                     reads=[("gl", pu, s_)], writes=[("ww", pu, s_)])
                dj = gd % NDG
                gd += 1
                S.op("act", lambda e, s_=s_, dj=dj, pu=pu: e.activation(
                    out=dg[dj][:], in_=identb[:], func=AF.Copy, scale=ww[pu][:, s_:s_ + 1]),
                    reads=[("ww", pu, s_)], writes=[("dg", dj)])
                for hf in range(2):
                    S.op("pe", lambda e, s_=s_, dj=dj, r=r, pu=pu, hf=hf: e.matmul(
                        ps[2 * pu + hf][:, :], dg[dj][:], ring[r][:, D + hf * 512: D + (hf + 1) * 512],
                        start=(s_ == 0), stop=(s_ == 127)),
                        reads=[("dg", dj), ("ring", r)], writes=[("accP", pu, hf)])
            for hf in range(2):
                S.op("dve", lambda e, pu=pu, hf=hf: e.tensor_tensor(
                    out=accs[:, hf * 512:(hf + 1) * 512], in0=ps[2 * pu + hf][:, :], in1=bc[:, 0, hf * 512:(hf + 1) * 512],
                    op=ALU.mult), reads=[("accP", pu, hf), ("bc", 0)], writes=[("accs", hf)])
            S.op("dve", lambda e, T=T: e.tensor_tensor(out=accs[:], in0=accs[:], in1=x1[:, T, :], op=ALU.add),
                 reads=[("accs", 0), ("accs", 1)], writes=["accsum"])
            S.op("act", lambda e, T=T: e.activation(out=junk[:], in_=accs[:], func=AF.Square, accum_out=st6[:, T:T + 1]),
                 reads=["accsum"], writes=["junk6", ("ssq6", T)])
            S.op("dve", lambda e, T=T: e.tensor_scalar(out=st6[:, NT + T:NT + T + 1], in0=st6[:, T:T + 1],
                                                       scalar1=1.0 / D, scalar2=EPS, op0=ALU.mult, op1=ALU.add),
                 reads=[("ssq6", T)], writes=[("ms6", T)])
            S.op("act", lambda e, T=T: e.activation(out=st6[:, NT + T:NT + T + 1], in_=st6[:, NT + T:NT + T + 1],
                                                    func=AF.Sqrt), reads=[("ms6", T)], writes=[("ms6", T)])
            S.op("dve", lambda e, T=T: e.reciprocal(out=st6[:, T:T + 1], in_=st6[:, NT + T:NT + T + 1]),
                 reads=[("ms6", T)], writes=[("rs6", T)])
            S.op("dve", lambda e, T=T: e.scalar_tensor_tensor(out=x1[:, T, :], in0=accs[:], scalar=st6[:, T:T + 1],
                                                              in1=bc[:, 3, :], op0=ALU.mult, op1=ALU.mult),
                 reads=["accsum", ("rs6", T), ("bc", 3)], writes=[("xo", T), ("accs", 0), ("accs", 1)])
            S.dma("sp", lambda e, T=T: e.dma_start(out=out[T * 128:(T + 1) * 128, :], in_=x1[:, T, :]),
                  reads=[("xo", T)], writes=[("out", T)])
        phase_end("p5b")
    S.finish()
    es.close()
    return nc


def _col(v):
    return np.ascontiguousarray(np.asarray(v, np.float32).reshape(8, 128).T)


def make_inputs(inp):
    global _BIAS_IDX
    f = lambda a: np.ascontiguousarray(np.asarray(a, dtype=np.float32))
    if _BIAS_IDX is None:
        _BIAS_IDX = build_bias_index()
    rpb = f(inp["na_rpb"])[0]
    ext = np.concatenate([rpb.reshape(8, -1), np.full((8, 1), MASKV, np.float32)], axis=1)
    biasT = np.stack([ext[h][_BIAS_IDX] for h in range(8)], axis=1)
    cstm = np.zeros((128, 528), np.float32)
    cstm[:, 0:128] = np.eye(128, dtype=np.float32)
    sidx = np.arange(128)
    blk = (sidx[:, None] // 64) == (sidx[None, :] // 64)
    cstm[:, 128:256] = (blk & (sidx[:, None] <= sidx[None, :])).astype(np.float32)
    cstm[:, 256:384] = (blk & (sidx[:, None] >= sidx[None, :])).astype(np.float32)
    cstm[:, 384:512] = 1.0
    cstm[:, 512:528] = np.arange(16, dtype=np.float32)[None, :]
    icm = np.zeros((128, 388), np.int32)
    icm[:, 0:128] = np.arange(128, dtype=np.int32)[None, :]
    icm[:, 128:384] = np.arange(256, dtype=np.int32)[None, :]
    icm[:, 384] = -128
    icm[:, 385] = -256
    icm[:, 386] = 127
    icm[:, 387] = 255
    rmask = np.ones((128, TOK), np.float32)
    rmask[:, ::64] = 0.0
    c_ctx = f(inp["c_ctx"])
    hg_lb = f(inp["hg_lb"])
    lbraw = np.ascontiguousarray(hg_lb.reshape(2, 2, 4, 128).transpose(3, 0, 1, 2).reshape(128, 16))
    keys = f(inp["peer_keys"])[0]
    keysT = np.ascontiguousarray(keys.transpose(3, 0, 1, 2).reshape(128, 16 * 128))
    shared = dict(
        w_mod=f(inp["w_mod"])[0], b_mod=f(inp["b_mod"])[0].reshape(1, -1),
        n1c=_col(f(inp["norm1"])[0]), n2c=_col(f(inp["norm2"])[0]),
        n2b=np.ascontiguousarray(np.broadcast_to(f(inp["norm2"])[0][None, :], (128, D))),
        nfb=np.ascontiguousarray(np.broadcast_to(f(inp["norm_f"])[None, :], (128, D))),
        w_in=f(inp["w_in"])[0], w_out=f(inp["w_out"])[0], wq=f(inp["peer_wq"])[0],
        keysT=keysT, u=f(inp["peer_u"])[0], v=f(inp["peer_v"])[0], lbraw=lbraw,
        hgn=np.ascontiguousarray(np.broadcast_to(f(inp["hg_norm"])[0][None, :], (128, 512))),
        biasT=np.ascontiguousarray(biasT.reshape(128, -1)), cst=cstm, rmask=rmask, icst=icm,
    )
    xs = f(inp["x"]); cs = f(inp["c"]); ctxs = f(inp["ctx"])
    maps = []
    for b in range(xs.shape[0]):
        cc = np.stack([_col(cs[b]), _col(c_ctx)], axis=2).reshape(128, 16)
        m = dict(shared)
        m.update(x=xs[b], ctx=ctxs[b], ccol=np.ascontiguousarray(cc))
        maps.append(m)
    return maps


def kernel(**inputs):
    maps = make_inputs(inputs)
    nc = build()
    res = run_bass_kernel_spmd(nc, maps, core_ids=list(range(len(maps))))
    return np.stack([np.asarray(r["out"], dtype=np.float32) for r in res.results], axis=0)
```
